# Optimizing a Trainium2 kernel written in Bass

```python
import math
import jax, jax.numpy as jnp
from jax import lax
import numpy as np

D_MODEL = 1024
BATCH = 4
SEQ = 4096
DEPTH = 4

CHUNK = 64
HEAD_DIM = 64
N_HEADS_ATTN = 8
N_IDX_HEADS = 4
IDX_DIM = 64
TOPK_MAX = 256
Q_BLOCK = 64
N_HEADS_RET = 4
N_HEADS_MLSTM = 4
CONV_WIDTH = 4
ROPE_THETA = 10000.0
N_EXPERTS = 16
N_GROUPS = 4
EXPERTS_PER_GROUP = N_EXPERTS // N_GROUPS
TOP_K_EXPERTS = 2
D_FF_EXPERT = 256
LN_EPS = 1e-5

MIX_ATTN = N_HEADS_ATTN * HEAD_DIM
MIX_RET = N_HEADS_RET * HEAD_DIM
MIX_MLSTM = N_HEADS_MLSTM * HEAD_DIM
D_MIX = MIX_ATTN + MIX_RET + MIX_MLSTM

DEEPNORM_ALPHA = (2.0 * DEPTH) ** 0.25
DEEPNORM_BETA = (8.0 * DEPTH) ** -0.25

SPLIT_SIZES = (
    MIX_ATTN, MIX_ATTN, MIX_ATTN,
    N_IDX_HEADS * IDX_DIM, IDX_DIM, N_IDX_HEADS,
    MIX_RET, MIX_RET, MIX_RET, MIX_RET,
    MIX_MLSTM, MIX_MLSTM, MIX_MLSTM, MIX_MLSTM,
    N_HEADS_MLSTM, N_HEADS_MLSTM,
)
IN_PROJ_WIDTH = sum(SPLIT_SIZES)
SPLIT_OFFSETS = tuple(int(v) for v in np.cumsum((0,) + SPLIT_SIZES)[:-1])
SPLIT_POINTS = SPLIT_OFFSETS[1:]

kernel_name = 'hybrid_dsa_retention_mlstm_moe_trunk'


def layer_norm(x, g, b):
    xf = x.astype(jnp.float32)
    mu = jnp.mean(xf, axis=-1, keepdims=True)
    var = jnp.mean(jnp.square(xf - mu), axis=-1, keepdims=True)
    return ((xf - mu) * lax.rsqrt(var + LN_EPS) * g + b).astype(x.dtype)


def head_norm(x):
    xf = x.astype(jnp.float32)
    mu = jnp.mean(xf, axis=-1, keepdims=True)
    var = jnp.mean(jnp.square(xf - mu), axis=-1, keepdims=True)
    return ((xf - mu) * lax.rsqrt(var + LN_EPS)).astype(x.dtype)


def rope_tables(positions):
    half = HEAD_DIM // 2
    inv_freq = ROPE_THETA ** (-jnp.arange(half, dtype=jnp.float32) / half)
    ang = positions.astype(jnp.float32)[..., None] * inv_freq
    return jnp.cos(ang)[:, :, None, :], jnp.sin(ang)[:, :, None, :]


def apply_rope(x, cos, sin):
    half = x.shape[-1] // 2
    x1 = x[..., :half].astype(jnp.float32)
    x2 = x[..., half:].astype(jnp.float32)
    return jnp.concatenate([x1 * cos - x2 * sin, x2 * cos + x1 * sin], axis=-1).astype(x.dtype)


def causal_conv(x, w, b):
    seq = x.shape[1]
    xp = jnp.pad(x, ((0, 0), (CONV_WIDTH - 1, 0), (0, 0)))
    out = b
    for j in range(CONV_WIDTH):
        out = out + xp[:, j:j + seq] * w[j]
    return out


def dsa_attention(q, k, v, iq, ik, iw):
    bsz, seq, n_heads, hd = q.shape
    topk = min(TOPK_MAX, seq // 4)
    n_blocks = seq // Q_BLOCK
    key_pos = jnp.arange(seq)
    iw = iw.astype(jnp.float32) * (N_IDX_HEADS ** -0.5) * (IDX_DIM ** -0.5)
    gather = jax.vmap(lambda arr, ids: arr[ids])

    def one_block(blk):
        start = blk * Q_BLOCK
        qb = lax.dynamic_slice_in_dim(q, start, Q_BLOCK, axis=1)
        iqb = lax.dynamic_slice_in_dim(iq, start, Q_BLOCK, axis=1)
        iwb = lax.dynamic_slice_in_dim(iw, start, Q_BLOCK, axis=1)
        q_pos = start + jnp.arange(Q_BLOCK)
        limit = (q_pos // CHUNK + 1) * CHUNK
        visible = key_pos[None, :] < limit[:, None]
        rel = jax.nn.relu(jnp.einsum('bqhd,bsd->bqhs', iqb, ik).astype(jnp.float32))
        score = jnp.einsum('bqhs,bqh->bqs', rel, iwb)
        score = jnp.where(visible[None], score, -jnp.inf)
        _, idx = lax.top_k(score, topk)
        valid = idx < limit[None, :, None]
        k_sel = gather(k, idx)
        v_sel = gather(v, idx)
        s = jnp.einsum('bqhd,bqkhd->bqhk', qb, k_sel).astype(jnp.float32) * (hd ** -0.5)
        s = jnp.where(valid[:, :, None, :], s, -jnp.inf)
        p = jax.nn.softmax(s, axis=-1).astype(v.dtype)
        return jnp.einsum('bqhk,bqkhd->bqhd', p, v_sel)

    out = lax.map(one_block, jnp.arange(n_blocks))
    return out.transpose(1, 0, 2, 3, 4).reshape(bsz, seq, n_heads, hd)


def retention_chunkwise(q, k, v):
    bsz, seq, nh, hd = q.shape
    dv = v.shape[-1]
    n_chunks = seq // CHUNK
    f32 = jnp.float32
    log_gamma = jnp.log1p(-(2.0 ** (-5.0 - jnp.arange(nh, dtype=f32))))

    def to_chunks(a):
        return a.astype(f32).reshape(bsz, n_chunks, CHUNK, nh, -1)

    qc = to_chunks(q)
    kc = to_chunks(k) * (hd ** -0.5)
    vc = to_chunks(v)
    pos = jnp.arange(CHUNK, dtype=f32)
    diff = pos[:, None] - pos[None, :]
    decay_in = jnp.where(diff >= 0, jnp.exp(diff[None] * log_gamma[:, None, None]), 0.0)
    scores = jnp.einsum('bnihd,bnjhd->bnhij', qc, kc) * decay_in
    inner = jnp.einsum('bnhij,bnjhe->bnihe', scores, vc)
    k_decay = jnp.exp((CHUNK - 1.0 - pos)[None, :] * log_gamma[:, None])
    chunk_kv = jnp.einsum('bnjhd,hj,bnjhe->nbhde', kc, k_decay, vc)
    chunk_decay = jnp.exp(CHUNK * log_gamma)[None, :, None, None]

    def step(state, kv):
        return state * chunk_decay + kv, state

    _, prev = lax.scan(step, jnp.zeros((bsz, nh, hd, dv), f32), chunk_kv)
    q_decay = jnp.exp((pos + 1.0)[None, :] * log_gamma[:, None])
    cross = jnp.einsum('bnihd,hi,nbhde->bnihe', qc, q_decay, prev)
    return (inner + cross).reshape(bsz, seq, nh, dv)


def mlstm_chunkwise(q, k, v, i_pre, f_pre):
    bsz, seq, nh, hd = q.shape
    n_chunks = seq // CHUNK
    f32 = jnp.float32

    def to_chunks(a):
        return a.astype(f32).reshape(bsz, n_chunks, CHUNK, nh, -1).transpose(1, 0, 3, 2, 4)

    qc = to_chunks(q)
    kc = to_chunks(k) * (hd ** -0.5)
    vc = to_chunks(v)
    ic = to_chunks(i_pre[..., None])[..., 0]
    lfc = to_chunks(jax.nn.log_sigmoid(f_pre.astype(f32))[..., None])[..., 0]
    causal = jnp.tril(jnp.ones((CHUNK, CHUNK), dtype=bool))

    def step(carry, xs):
        c_mem, n_mem, m_mem = carry
        q_, k_, v_, i_, lf_ = xs
        b = jnp.cumsum(lf_, axis=-1)
        log_w = jnp.where(causal, b[..., :, None] - b[..., None, :] + i_[..., None, :], -jnp.inf)
        log_inter = b + m_mem[..., None]
        m_q = jnp.maximum(log_inter, jnp.max(log_w, axis=-1))
        w = jnp.exp(log_w - m_q[..., None])
        inter = jnp.exp(log_inter - m_q)
        s = jnp.einsum('bhjd,bhld->bhjl', q_, k_) * w
        num = jnp.einsum('bhjl,bhle->bhje', s, v_) + inter[..., None] * jnp.einsum('bhjd,bhde->bhje', q_, c_mem)
        den = jnp.sum(s, axis=-1) + inter * jnp.einsum('bhjd,bhd->bhj', q_, n_mem)
        h = num / jnp.maximum(jnp.abs(den), jnp.exp(-m_q))[..., None]
        b_last = b[..., -1]
        log_k = b_last[..., None] - b + i_
        m_new = jnp.maximum(b_last + m_mem, jnp.max(log_k, axis=-1))
        kw = jnp.exp(log_k - m_new[..., None])
        decay = jnp.exp(b_last + m_mem - m_new)
        c_new = decay[..., None, None] * c_mem + jnp.einsum('bhl,bhld,bhle->bhde', kw, k_, v_)
        n_new = decay[..., None] * n_mem + jnp.einsum('bhl,bhld->bhd', kw, k_)
        return (c_new, n_new, m_new), h

    init = (jnp.zeros((bsz, nh, hd, hd), f32), jnp.zeros((bsz, nh, hd), f32), jnp.zeros((bsz, nh), f32))
    _, hs = lax.scan(step, init, (qc, kc, vc, ic, lfc))
    return hs.transpose(1, 0, 3, 2, 4).reshape(bsz, seq, nh, hd)


def token_mixers(h, w_in, w_out, i_bias, f_bias, conv_w, conv_b, cos, sin):
    bsz, seq, _ = h.shape
    (aq, ak, av, iq, ik, iw, rq, rk, rv, rg,
     mq, mk, mv, mo, mi, mf) = jnp.split(h @ w_in, SPLIT_POINTS, axis=-1)

    def heads(a, n):
        return a.reshape(bsz, seq, n, -1)

    o_a = dsa_attention(apply_rope(heads(aq, N_HEADS_ATTN), cos, sin),
                        apply_rope(heads(ak, N_HEADS_ATTN), cos, sin),
                        heads(av, N_HEADS_ATTN),
                        apply_rope(heads(iq, N_IDX_HEADS), cos, sin),
                        apply_rope(heads(ik, 1), cos, sin)[:, :, 0],
                        iw).reshape(bsz, seq, MIX_ATTN)
    y_r = retention_chunkwise(apply_rope(heads(rq, N_HEADS_RET), cos, sin),
                              apply_rope(heads(rk, N_HEADS_RET), cos, sin),
                              heads(rv, N_HEADS_RET))
    o_b = (head_norm(y_r).reshape(bsz, seq, MIX_RET) * jax.nn.silu(rg)).astype(h.dtype)
    qk = jax.nn.silu(causal_conv(jnp.concatenate([mq, mk], axis=-1), conv_w, conv_b))
    mq_c, mk_c = jnp.split(qk, 2, axis=-1)
    h_tilde = mlstm_chunkwise(heads(mq_c, N_HEADS_MLSTM), heads(mk_c, N_HEADS_MLSTM),
                              heads(mv, N_HEADS_MLSTM), mi + i_bias, mf + f_bias)
    h_t = jax.nn.sigmoid(heads(mo, N_HEADS_MLSTM).astype(jnp.float32)) * h_tilde
    o_c = head_norm(h_t).reshape(bsz, seq, MIX_MLSTM).astype(h.dtype)
    return jnp.concatenate([o_a, o_b, o_c], axis=-1) @ w_out


def moe_ffn(h, w_router, b_router, w_gate, w_up, w_down):
    n_tok = h.shape[0]
    scores = jax.nn.sigmoid((h @ w_router).astype(jnp.float32))
    biased = scores + b_router.astype(jnp.float32)
    grouped = biased.reshape(n_tok, N_GROUPS, EXPERTS_PER_GROUP)
    group_score = jnp.sum(lax.top_k(grouped, TOP_K_EXPERTS)[0], axis=-1)
    best_group = jnp.argmax(group_score, axis=-1)
    in_group = jnp.take_along_axis(grouped, best_group[:, None, None], axis=1)[:, 0]
    _, local = lax.top_k(in_group, TOP_K_EXPERTS)
    expert_idx = best_group[:, None] * EXPERTS_PER_GROUP + local
    wts = jnp.take_along_axis(scores, expert_idx, axis=1)
    wts = wts / jnp.sum(wts, axis=-1, keepdims=True)
    gate = jnp.einsum('tk,tke->te', wts, jax.nn.one_hot(expert_idx, N_EXPERTS, dtype=jnp.float32))
    hid = jax.nn.silu(jnp.einsum('td,edf->tef', h, w_gate)) * jnp.einsum('td,edf->tef', h, w_up)
    hid = hid * gate[:, :, None].astype(hid.dtype)
    return jnp.einsum('tef,efd->td', hid, w_down)


def setup_inputs(seed: int = 0) -> dict:
    key = jax.random.key(seed)
    ks = jax.random.split(key, 20)
    f32 = jnp.float32

    def nrm(k, shape, scale):
        return jax.random.normal(k, shape, f32) * scale

    x = nrm(ks[0], (BATCH, SEQ, D_MODEL), 1.0)
    c = nrm(ks[1], (BATCH, D_MODEL), 1.0)
    offset = jax.random.randint(ks[2], (BATCH, 1), 0, 1024, dtype=jnp.int32)
    positions = offset + jnp.arange(SEQ, dtype=jnp.int32)[None, :]
    w_ada = nrm(ks[3], (DEPTH, D_MODEL, 6 * D_MODEL), 0.1 * D_MODEL ** -0.5)
    b_ada = nrm(ks[4], (DEPTH, 6 * D_MODEL), 0.02)
    col_scale = np.ones(IN_PROJ_WIDTH, np.float32)
    for part in (2, 8, 12):
        col_scale[SPLIT_OFFSETS[part]:SPLIT_OFFSETS[part] + SPLIT_SIZES[part]] = DEEPNORM_BETA
    w_in = nrm(ks[5], (DEPTH, D_MODEL, IN_PROJ_WIDTH), D_MODEL ** -0.5) * jnp.asarray(col_scale)
    i_bias = nrm(ks[6], (DEPTH, N_HEADS_MLSTM), 0.1)
    f_bias = jnp.linspace(3.0, 6.0, N_HEADS_MLSTM, dtype=f32)[None, :] + nrm(ks[7], (DEPTH, N_HEADS_MLSTM), 0.01)
    conv_w = nrm(ks[8], (DEPTH, CONV_WIDTH, 2 * MIX_MLSTM), CONV_WIDTH ** -0.5)
    conv_b = nrm(ks[9], (DEPTH, 2 * MIX_MLSTM), 0.01)
    w_out = nrm(ks[10], (DEPTH, D_MIX, D_MODEL), DEEPNORM_BETA * D_MIX ** -0.5)
    ln_mix_g = 1.0 + nrm(ks[11], (DEPTH, D_MODEL), 0.01)
    ln_mix_b = nrm(ks[12], (DEPTH, D_MODEL), 0.01)
    w_router = nrm(ks[13], (D_MODEL, N_EXPERTS), D_MODEL ** -0.5)
    b_router = nrm(ks[14], (N_EXPERTS,), 0.01)
    w_gate = nrm(ks[15], (DEPTH, N_EXPERTS, D_MODEL, D_FF_EXPERT), D_MODEL ** -0.5)
    w_up = nrm(ks[16], (DEPTH, N_EXPERTS, D_MODEL, D_FF_EXPERT), DEEPNORM_BETA * D_MODEL ** -0.5)
    w_down = nrm(ks[17], (DEPTH, N_EXPERTS, D_FF_EXPERT, D_MODEL), DEEPNORM_BETA * D_FF_EXPERT ** -0.5)
    ln_ffn_g = 1.0 + nrm(ks[18], (DEPTH, D_MODEL), 0.01)
    ln_ffn_b = nrm(ks[19], (DEPTH, D_MODEL), 0.01)
    return {'x': x, 'c': c, 'positions': positions, 'w_ada': w_ada, 'b_ada': b_ada,
            'w_in': w_in, 'i_bias': i_bias, 'f_bias': f_bias, 'conv_w': conv_w, 'conv_b': conv_b,
            'w_out': w_out, 'ln_mix_g': ln_mix_g, 'ln_mix_b': ln_mix_b,
            'w_router': w_router, 'b_router': b_router, 'w_gate': w_gate, 'w_up': w_up,
            'w_down': w_down, 'ln_ffn_g': ln_ffn_g, 'ln_ffn_b': ln_ffn_b}


def reference(x, c, positions, w_ada, b_ada, w_in, i_bias, f_bias, conv_w, conv_b,
              w_out, ln_mix_g, ln_mix_b, w_router, b_router, w_gate, w_up, w_down,
              ln_ffn_g, ln_ffn_b):
    bsz, seq, d = x.shape
    cos, sin = rope_tables(positions)
    c_act = jax.nn.silu(c)
    for l in range(DEPTH):
        mod = (c_act @ w_ada[l] + b_ada[l])[:, None, :]
        sh_m, sc_m, g_m, sh_f, sc_f, g_f = jnp.split(mod, 6, axis=-1)
        h = x * (1.0 + sc_m) + sh_m
        mix = token_mixers(h, w_in[l], w_out[l], i_bias[l], f_bias[l], conv_w[l], conv_b[l], cos, sin)
        x = layer_norm(DEEPNORM_ALPHA * x + (1.0 + g_m) * mix, ln_mix_g[l], ln_mix_b[l])
        h = x * (1.0 + sc_f) + sh_f
        y = moe_ffn(h.reshape(bsz * seq, d), w_router, b_router, w_gate[l], w_up[l], w_down[l])
        x = layer_norm(DEEPNORM_ALPHA * x + (1.0 + g_f) * y.reshape(bsz, seq, d), ln_ffn_g[l], ln_ffn_b[l])
    return x
```

```python
import numpy as np
from contextlib import ExitStack

import concourse.bass as bass
import concourse.mybir as mybir
from concourse.bass_utils import run_bass_kernel_spmd

F32 = mybir.dt.float32
BF16 = mybir.dt.bfloat16
ALU = mybir.AluOpType
AF = mybir.ActivationFunctionType
AX = mybir.AxisListType

D = 1024
SEQ = 4096
BATCH = 4
DEPTH = 4
NT = 16
TOK = NT * 128
INW = 3916
LN_EPS = 1e-5
ALPHA = (2.0 * DEPTH) ** 0.25
NEG = -1.0e30

ENGS = ("pe", "act", "dve", "pool", "sp")
NRING = 8


class Res:
    __slots__ = ("name", "w", "r")

    def __init__(self, name=""):
        self.name = name
        self.w = None
        self.r = []


class B:
    def __init__(self, nc, es):
        self.nc = nc
        self.es = es
        self.root_es = es
        self.epoch = 0
        self.q = {e: [] for e in ENGS}
        self.sem = {e: es.enter_context(nc.semaphore("s_" + e)) for e in ENGS}
        self.cnt = {e: 0 for e in ENGS}
        self.seen = {e: {} for e in ENGS}
        self.dq = ("sp", "pool")
        self.ring = {e: [es.enter_context(nc.semaphore("d_%s%d" % (e, i))) for i in range(NRING)]
                     for e in self.dq}
        self.rcnt = {e: [0] * NRING for e in self.dq}
        self.rnext = {e: 0 for e in self.dq}
        self.nres = 0

    def res(self, name=""):
        self.nres += 1
        return Res(name or ("r%d" % self.nres))

    def sb(self, name, shape, dt):
        self.nres += 1
        return self.es.enter_context(self.nc.sbuf_tensor("%s_%d" % (name, self.nres), list(shape), dt))

    def ps(self, name, shape, dt=F32):
        self.nres += 1
        return self.es.enter_context(self.nc.psum_tensor("%s_%d" % (name, self.nres), list(shape), dt))

    def _deps(self, eng, reads, writes):
        need = {}

        def add(ev):
            if ev is None:
                return
            s, v = ev
            k = id(s)
            if k not in need or need[k][1] < v:
                need[k] = (s, v)

        for r in reads:
            add(r.w)
        for w in writes:
            add(w.w)
            for ev in w.r:
                add(ev)
        out = []
        seen = self.seen[eng]
        own = id(self.sem[eng])
        for k, (s, v) in need.items():
            if eng == "pe" and k == own:
                continue
            if seen.get(k, 0) >= v:
                continue
            seen[k] = v
            out.append((s, v))
        return out

    def op(self, eng, fn, reads=(), writes=(), inc=True):
        waits = self._deps(eng, reads, writes)
        if inc:
            self.cnt[eng] += 1
            ev = (self.sem[eng], self.cnt[eng])
        else:
            ev = (self.sem[eng], self.cnt[eng] + 1)
        for r in reads:
            r.r.append(ev)
            if len(r.r) > 64:
                r.r = r.r[-64:] if False else self._compact(r.r)
        for w in writes:
            w.w = ev
            w.r = []
        self.q[eng].append((waits, fn, self.sem[eng] if inc else None, 1))

    @staticmethod
    def _compact(evs):
        best = {}
        for s, v in evs:
            k = id(s)
            if k not in best or best[k][1] < v:
                best[k] = (s, v)
        return list(best.values())

    def dma(self, q, out_ap, in_ap, reads=(), writes=()):
        waits = self._deps(q, reads, writes)
        i = self.rnext[q]
        self.rnext[q] = (i + 1) % NRING
        s = self.ring[q][i]
        if self.rcnt[q][i] > 0:
            v = 16 * self.rcnt[q][i]
            if self.seen[q].get(id(s), 0) < v:
                self.seen[q][id(s)] = v
                waits.append((s, v))
        self.rcnt[q][i] += 1
        ev = (s, 16 * self.rcnt[q][i])
        for r in reads:
            r.r.append(ev)
        for w in writes:
            w.w = ev
            w.r = []

        def fn(e, out_ap=out_ap, in_ap=in_ap):
            return e.dma_start(out=out_ap, in_=in_ap)

        self.q[q].append((waits, fn, s, 16))

    def coll(self, kind, groups, in_ap, out_ap, reads=(), writes=()):
        q = "pool"
        waits = self._deps(q, reads, writes)
        i = self.rnext[q]
        self.rnext[q] = (i + 1) % NRING
        s = self.ring[q][i]
        if self.rcnt[q][i] > 0:
            v = 16 * self.rcnt[q][i]
            if self.seen[q].get(id(s), 0) < v:
                self.seen[q][id(s)] = v
                waits.append((s, v))
        self.rcnt[q][i] += 1
        ev = (s, 16 * self.rcnt[q][i])
        for r in reads:
            r.r.append(ev)
        for w in writes:
            w.w = ev
            w.r = []

        def fn(e):
            return e.collective_compute(kind, ALU.bypass, replica_groups=groups, ins=[in_ap], outs=[out_ap])

        self.q[q].append((waits, fn, s, 16))

    def new_epoch(self):
        self.barrier()
        nc, es = self.nc, self.root_es
        self.epoch += 1
        k = self.epoch
        self.sem = {e: es.enter_context(nc.semaphore("s%d_%s" % (k, e))) for e in ENGS}
        self.cnt = {e: 0 for e in ENGS}
        self.seen = {e: {} for e in ENGS}
        self.ring = {e: [es.enter_context(nc.semaphore("d%d_%s%d" % (k, e, i))) for i in range(NRING)]
                     for e in self.dq}
        self.rcnt = {e: [0] * NRING for e in self.dq}
        self.rnext = {e: 0 for e in self.dq}

    def barrier(self, label=None):
        if not hasattr(self, "marks"):
            self.marks = []
        self.marks.append((self.epoch, dict(self.cnt)))
        evs = []
        for q in self.dq:
            for i in range(NRING):
                if self.rcnt[q][i] > 0:
                    evs.append((self.ring[q][i], 16 * self.rcnt[q][i]))
        for e in ENGS:
            if e != "sp" and self.cnt[e] > 0:
                evs.append((self.sem[e], self.cnt[e]))
        for e in ENGS:
            waits = []
            for s_, v in evs:
                if id(s_) == id(self.sem[e]) and e == "pe":
                    continue
                if self.seen[e].get(id(s_), 0) >= v:
                    continue
                self.seen[e][id(s_)] = v
                waits.append((s_, v))
            if waits:
                self.q[e].append((waits, None, None, 0))

    def finish(self):
        nc = self.nc
        fin = []
        for q in self.dq:
            for i in range(NRING):
                if self.rcnt[q][i] > 0:
                    fin.append((self.ring[q][i], 16 * self.rcnt[q][i]))
        for e in ENGS:
            if e != "sp" and self.cnt[e] > 0:
                fin.append((self.sem[e], self.cnt[e]))
        qs = self.q

        def run(e, lst, extra=()):
            for waits, fn, s, n in lst:
                for ws, wv in waits:
                    e.wait_ge(ws, wv)
                if fn is None:
                    continue
                ins = fn(e)
                if s is not None:
                    ins.then_inc(s, n)
            for ws, wv in extra:
                e.wait_ge(ws, wv)

        with nc.Block() as blk:
            @blk.sync
            def _(e):
                run(e, qs["sp"], fin)

            @blk.tensor
            def _(e):
                run(e, qs["pe"])

            @blk.scalar
            def _(e):
                run(e, qs["act"])

            @blk.vector
            def _(e):
                run(e, qs["dve"])

            @blk.gpsimd
            def _(e):
                run(e, qs["pool"])


def emit_mod(b, ccol_d, wada_d, bada_d, modrow_d, modcol_d):
    nc = b.nc
    with ExitStack() as es:
        old = b.es
        b.es = es
        ccol = b.sb("m_ccol", [128, 8], F32)
        cact = b.sb("m_cact", [128, 8], F32)
        one = b.sb("m_one", [1, 1], F32)
        wbuf = [b.sb("m_w%d" % i, [128, 8, 512], F32) for i in range(2)]
        brow = b.sb("m_brow", [1, 6144], F32)
        mrow = b.sb("m_mrow", [1, 6144], F32)
        mcol = b.sb("m_mcol", [128, DEPTH * 48], F32)
        pr = [b.ps("m_pr%d" % i, [1, 512]) for i in range(2)]
        pc = b.ps("m_pc", [128, 48])
        r_ccol, r_cact, r_one, r_brow, r_mrow, r_mcol, r_pc = (b.res() for _ in range(7))
        r_w = [b.res(), b.res()]
        r_pr = [b.res(), b.res()]
        r_out = b.res()

        b.dma("sp", ccol[:], ccol_d, writes=[r_ccol])
        b.op("act", lambda e: e.activation(out=cact[:], in_=ccol[:], func=AF.Silu),
             reads=[r_ccol], writes=[r_cact])
        b.op("dve", lambda e: e.memset(one[:], 1.0), writes=[r_one])
        it = 0
        for l in range(DEPTH):
            b.dma("sp", brow[:], bada_d[l:l + 1, :], writes=[r_brow])
            for nb in range(12):
                s = it % 2
                it += 1
                src = wada_d[l, :, nb * 512:(nb + 1) * 512].rearrange("(k p) n -> p k n", p=128)
                b.dma("sp", wbuf[s][:], src, writes=[r_w[s]])
                for k in range(8):
                    b.op("pe", lambda e, s=s, k=k: e.matmul(
                        pr[s][:], lhsT=cact[:, k:k + 1], rhs=wbuf[s][:, k, :],
                        start=(k == 0), stop=(k == 7)),
                        reads=[r_cact, r_w[s]], writes=[r_pr[s]], inc=(k == 7))
                b.op("dve", lambda e, s=s, nb=nb: e.tensor_tensor(
                    out=mrow[:, nb * 512:(nb + 1) * 512], in0=pr[s][:],
                    in1=brow[:, nb * 512:(nb + 1) * 512], op=ALU.add),
                    reads=[r_pr[s], r_brow], writes=[r_mrow])
            b.dma("pool", modrow_d[l:l + 1, :], mrow[:], reads=[r_mrow], writes=[r_out])
            for c in range(48):
                b.op("pe", lambda e, c=c: e.matmul(
                    pc[:, c:c + 1], lhsT=mrow[:, c * 128:(c + 1) * 128], rhs=one[:, :],
                    start=True, stop=True),
                    reads=[r_mrow, r_one], writes=[r_pc], inc=(c == 47))
            b.op("act", lambda e, l=l: e.copy(out=mcol[:, l * 48:(l + 1) * 48], in_=pc[:]),
                 reads=[r_pc], writes=[r_mcol])
        b.dma("pool", modcol_d, mcol[:], reads=[r_mcol], writes=[r_out])
        b.barrier()
        b.es = old


def build_mod():
    nc = bass.Bass("TRN2", target_bir_lowering=False)
    ccol_d = nc.dram_tensor("ccol", [128, 8], F32, kind="ExternalInput").ap()
    wada_d = nc.dram_tensor("w_ada", [DEPTH, D, 6 * D], F32, kind="ExternalInput").ap()
    bada_d = nc.dram_tensor("b_ada", [DEPTH, 6 * D], F32, kind="ExternalInput").ap()
    modrow_d = nc.dram_tensor("modrow", [DEPTH, 6 * D], F32, kind="ExternalOutput").ap()
    modcol_d = nc.dram_tensor("modcol", [128, DEPTH * 48], F32, kind="ExternalOutput").ap()
    with ExitStack() as es:
        b = B(nc, es)
        emit_mod(b, ccol_d, wada_d, bada_d, modrow_d, modcol_d)
        b.finish()
    return nc


def run_mod(c, w_ada, b_ada):
    nc = build_mod()
    in_maps = []
    for core in range(8):
        bi = core // 2
        in_maps.append({"ccol": np.ascontiguousarray(c[bi].reshape(8, 128).T),
                        "w_ada": w_ada, "b_ada": b_ada})
    res = run_bass_kernel_spmd(nc, in_maps, core_ids=list(range(8)))
    return [r["modrow"] for r in res.results], [r["modcol"] for r in res.results]


NFT = 19
P1_BLK = [(0, 512), (512, 512), (1024, 512), (1536, 332), (1868, 512), (2380, 512),
          (2892, 512), (3404, 512)]


def emit_rope_tables(b, pos_d, invf_d, cos, sin, r_tab, nt):
    with ExitStack() as es:
        old = b.es
        b.es = es
        posi = b.sb("rt_posi", [128, nt], mybir.dt.int32)
        posf = b.sb("rt_posf", [128, nt], F32)
        invf = b.sb("rt_invf", [128, 32], F32)
        ang = b.sb("rt_ang", [128, nt, 32], F32)
        u = b.sb("rt_u", [128, nt, 32], F32)
        r_pi, r_pf, r_if, r_ang, r_u = (b.res() for _ in range(5))
        b.dma("sp", posi[:], pos_d, writes=[r_pi])
        b.dma("sp", invf[:], invf_d, writes=[r_if])
        b.op("dve", lambda e: e.tensor_copy(out=posf[:], in_=posi[:]), reads=[r_pi], writes=[r_pf])
        for t in range(nt):
            b.op("dve", lambda e, t=t: e.tensor_scalar(
                out=ang[:, t, :], in0=invf[:], scalar1=posf[:, t:t + 1], scalar2=None,
                op0=ALU.mult), reads=[r_if, r_pf], writes=[r_ang])
        two_pi = float(np.float32(2.0 * np.pi))
        pi = float(np.float32(np.pi))
        ki = b.sb("rt_ki", [128, nt, 32], mybir.dt.int32)
        kf = b.sb("rt_kf", [128, nt, 32], F32)
        r_ki, r_kf = b.res(), b.res()

        def reduced_sin(dst, shift):
            b.op("dve", lambda e: e.tensor_scalar(
                out=u[:], in0=ang[:], scalar1=shift, scalar2=None, op0=ALU.add),
                reads=[r_ang], writes=[r_u])
            b.op("dve", lambda e: e.tensor_scalar(
                out=kf[:], in0=u[:], scalar1=float(1.0 / (2.0 * np.pi)), scalar2=None, op0=ALU.mult),
                reads=[r_u], writes=[r_kf])
            b.op("dve", lambda e: e.tensor_copy(out=ki[:], in_=kf[:]), reads=[r_kf], writes=[r_ki])
            b.op("dve", lambda e: e.tensor_copy(out=kf[:], in_=ki[:]), reads=[r_ki], writes=[r_kf])
            b.op("dve", lambda e: e.scalar_tensor_tensor(
                out=u[:], in0=kf[:], scalar=-two_pi, in1=u[:], op0=ALU.mult, op1=ALU.add),
                reads=[r_kf, r_u], writes=[r_u])
            b.op("dve", lambda e: e.tensor_scalar(
                out=kf[:], in0=u[:], scalar1=pi, scalar2=two_pi, op0=ALU.is_gt, op1=ALU.mult),
                reads=[r_u], writes=[r_kf])
            b.op("dve", lambda e: e.tensor_tensor(out=u[:], in0=u[:], in1=kf[:], op=ALU.subtract),
                 reads=[r_u, r_kf], writes=[r_u])
            b.op("dve", lambda e: e.tensor_scalar(
                out=kf[:], in0=u[:], scalar1=-pi, scalar2=two_pi, op0=ALU.is_lt, op1=ALU.mult),
                reads=[r_u], writes=[r_kf])
            b.op("dve", lambda e: e.tensor_tensor(out=u[:], in0=u[:], in1=kf[:], op=ALU.add),
                 reads=[r_u, r_kf], writes=[r_u])
            b.op("dve", lambda e: e.tensor_scalar(
                out=u[:], in0=u[:], scalar1=pi, scalar2=-pi, op0=ALU.min, op1=ALU.max),
                reads=[r_u], writes=[r_u])
            b.op("act", lambda e: e.activation(out=dst[:], in_=u[:], func=AF.Sin),
                 reads=[r_u], writes=[r_tab])

        reduced_sin(sin, 0.0)
        reduced_sin(cos, float(np.float32(np.pi / 2)))
        b.barrier()
        b.es = old


def emit_p1(b, l, nt, x_d, win_d, modcol_d, ibias_d, fbias_d, identf_d,
            cos, sin, r_tab, featT_d, tokA_d, tokF_d, gT_d, r_xd, r_out):
    nc = b.nc
    with ExitStack() as es:
        old = b.es
        b.es = es
        wsb = b.sb("p1_w", [128, 8, INW], BF16)
        wst = [b.sb("p1_wst%d" % i, [128, 8, 512], F32) for i in range(2)]
        identf = b.sb("p1_idf", [128, 128], F32)
        identb = b.sb("p1_idb", [128, 128], BF16)
        mcol = b.sb("p1_mcol", [128, 48], F32)
        sc1 = b.sb("p1_sc1", [128, 8], F32)
        bias8 = b.sb("p1_bias8", [128, 8], F32)
        xt = [b.sb("p1_x%d" % i, [128, D], F32) for i in range(2)]
        hT = [b.sb("p1_hT%d" % i, [128, 8, 128], BF16) for i in range(2)]
        rin = [b.sb("p1_rin%d" % i, [128, 512], BF16) for i in range(5)]
        tmpa = b.sb("p1_tmpa", [128, 256], F32)
        tmpb = b.sb("p1_tmpb", [128, 256], F32)
        stA = [b.sb("p1_stA%d" % i, [128, 1792], BF16) for i in range(2)]
        stF = [b.sb("p1_stF%d" % i, [128, 12], F32) for i in range(2)]
        stT = [b.sb("p1_stT%d" % i, [128, NFT, 128], BF16) for i in range(2)]
        stG = [b.sb("p1_stG%d" % i, [8, 128], F32) for i in range(2)]
        r_stG = [b.res(), b.res()]
        ptr = [b.ps("p1_ptr%d" % i, [128, 512]) for i in range(2)]
        pin = [b.ps("p1_pin%d" % i, [128, 512]) for i in range(4)]
        pto = [b.ps("p1_pto%d" % i, [128, 1024], BF16) for i in range(2)]

        r_w, r_idf, r_idb, r_mcol, r_sc1, r_b8, r_ta, r_tb = (b.res() for _ in range(8))
        r_wst = [b.res(), b.res()]
        r_x = [b.res(), b.res()]
        r_hT = [b.res(), b.res()]
        r_rin = [b.res() for _ in range(5)]
        r_stA = [b.res(), b.res()]
        r_stF = [b.res(), b.res()]
        r_stT = [b.res(), b.res()]
        r_ptr = [b.res(), b.res()]
        r_pin = [b.res() for _ in range(4)]
        r_pto = [b.res(), b.res()]

        b.dma("sp", identf[:], identf_d, writes=[r_idf])
        b.op("dve", lambda e: e.tensor_copy(out=identb[:], in_=identf[:]), reads=[r_idf], writes=[r_idb])
        b.dma("sp", mcol[:], modcol_d[:, l * 48:(l + 1) * 48], writes=[r_mcol])
        b.op("dve", lambda e: e.tensor_scalar(out=sc1[:], in0=mcol[:, 8:16], scalar1=1.0, scalar2=None,
                                              op0=ALU.add), reads=[r_mcol], writes=[r_sc1])
        b.dma("sp", bias8[:, 0:4], ibias_d[l:l + 1, :].to_broadcast([128, 4]), writes=[r_b8])
        b.dma("sp", bias8[:, 4:8], fbias_d[l:l + 1, :].to_broadcast([128, 4]), writes=[r_b8])
        pieces = []
        for (s0, d0, n) in ((0, 0, 1860), (3908, 1860, 8), (1860, 1868, 2048)):
            o = 0
            while o < n:
                m = min(512, n - o)
                pieces.append((s0 + o, d0 + o, m))
                o += m
        for i, (s0, d0, m) in enumerate(pieces):
            s = i % 2
            src = win_d[l, :, s0:s0 + m].rearrange("(k p) n -> p k n", p=128)
            b.dma("sp", wst[s][:, :, 0:m], src, writes=[r_wst[s]])
            eng = "pool" if i % 2 == 0 else "act"
            if eng == "pool":
                b.op("pool", lambda e, s=s, d0=d0, m=m: e.tensor_copy(
                    out=wsb[:, :, d0:d0 + m], in_=wst[s][:, :, 0:m]), reads=[r_wst[s]], writes=[r_w])
            else:
                b.op("act", lambda e, s=s, d0=d0, m=m: e.copy(
                    out=wsb[:, :, d0:d0 + m], in_=wst[s][:, :, 0:m]), reads=[r_wst[s]], writes=[r_w])

        def rope(t, src, H, dst, r_src, r_dst, col0=0):
            sv = src.rearrange("p (h two d) -> p h two d", two=2, d=32)
            dv = dst.rearrange("p (h two d) -> p h two d", two=2, d=32)
            cb = cos[:, t:t + 1, :].to_broadcast([128, H, 32])
            sb_ = sin[:, t:t + 1, :].to_broadcast([128, H, 32])
            ta = tmpa[:, 0:H * 32].rearrange("p (h d) -> p h d", d=32)
            tb = tmpb[:, 0:H * 32].rearrange("p (h d) -> p h d", d=32)
            b.op("dve", lambda e: e.tensor_tensor(out=ta, in0=sv[:, :, 0, :], in1=cb, op=ALU.mult),
                 reads=[r_src, r_tab], writes=[r_ta])
            b.op("dve", lambda e: e.tensor_tensor(out=tb, in0=sv[:, :, 1, :], in1=sb_, op=ALU.mult),
                 reads=[r_src, r_tab], writes=[r_tb])
            b.op("dve", lambda e: e.tensor_tensor(out=dv[:, :, 0, :], in0=ta, in1=tb, op=ALU.subtract),
                 reads=[r_ta, r_tb], writes=[r_dst])
            b.op("dve", lambda e: e.tensor_tensor(out=ta, in0=sv[:, :, 1, :], in1=cb, op=ALU.mult),
                 reads=[r_src, r_tab], writes=[r_ta])
            b.op("dve", lambda e: e.tensor_tensor(out=tb, in0=sv[:, :, 0, :], in1=sb_, op=ALU.mult),
                 reads=[r_src, r_tab], writes=[r_tb])
            b.op("dve", lambda e: e.tensor_tensor(out=dv[:, :, 1, :], in0=ta, in1=tb, op=ALU.add),
                 reads=[r_ta, r_tb], writes=[r_dst])

        pin_i = 0
        pto_i = 0
        pending = []
        pgt = pto[1][:, 0:256].bitcast(F32)
        r_pgt = r_pto[1]
        for t in range(nt):
            s = t % 2
            b.dma("sp", xt[s][:], x_d[t * 128:(t + 1) * 128, :], reads=[r_xd], writes=[r_x[s]])
            for c in range(8):
                b.op("pe", lambda e, s=s, c=c: e.transpose(
                    out=ptr[c // 4][:, (c % 4) * 128:(c % 4 + 1) * 128],
                    in_=xt[s][:, c * 128:(c + 1) * 128], identity=identf[:]),
                    reads=[r_x[s], r_idf], writes=[r_ptr[c // 4]], inc=(c % 4 == 3))
            for c in range(8):
                b.op("act", lambda e, s=s, c=c: e.activation(
                    out=hT[s][:, c, :], in_=ptr[c // 4][:, (c % 4) * 128:(c % 4 + 1) * 128],
                    func=AF.Identity, scale=sc1[:, c:c + 1], bias=mcol[:, c:c + 1]),
                    reads=[r_ptr[c // 4], r_sc1, r_mcol], writes=[r_hT[s]])
            A = stA[s]
            Fs = stF[s]
            T = stT[s]
            for bi, (c0, n) in enumerate(P1_BLK):
                pi = pin_i % 4
                pin_i += 1
                P = pin[pi]
                for k in range(8):
                    b.op("pe", lambda e, s=s, k=k, c0=c0, n=n, P=P: e.matmul(
                        P[:, 0:n], lhsT=hT[s][:, k, :], rhs=wsb[:, k, c0:c0 + n],
                        start=(k == 0), stop=(k == 7)),
                        reads=[r_hT[s], r_w], writes=[r_pin[pi]], inc=(k == 7))
                rp = r_pin[pi]
                if bi == 0 or bi == 1:
                    rope(t, P[:, 0:512], 8, rin[bi][:, 0:512], rp, r_rin[bi])
                elif bi == 2:
                    b.op("act", lambda e, P=P, A=A: e.copy(out=A[:, 0:512], in_=P[:, 0:512]),
                         reads=[rp], writes=[r_stA[s]])
                elif bi == 3:
                    rope(t, P[:, 0:320], 5, rin[2][:, 0:320], rp, r_rin[2])
                    b.op("dve", lambda e: e.tensor_copy(out=rin[2][:, 320:384], in_=rin[2][:, 256:320]),
                         reads=[r_rin[2]], writes=[r_rin[2]])
                    b.op("dve", lambda e, P=P, Fs=Fs: e.tensor_copy(out=Fs[:, 0:4], in_=P[:, 320:324]),
                         reads=[rp], writes=[r_stF[s]])
                    b.op("dve", lambda e, P=P, Fs=Fs: e.tensor_tensor(
                        out=Fs[:, 4:12], in0=P[:, 324:332], in1=bias8[:], op=ALU.add),
                        reads=[rp, r_b8], writes=[r_stF[s]])
                elif bi == 4:
                    rope(t, P[:, 0:512], 8, rin[3][:, 0:512], rp, r_rin[3])
                    b.op("pool", lambda e, A=A: e.tensor_copy(out=A[:, 512:768], in_=rin[3][:, 256:512]),
                         reads=[r_rin[3]], writes=[r_stA[s]])
                elif bi == 5:
                    b.op("act", lambda e, P=P, A=A: e.copy(out=A[:, 768:1024], in_=P[:, 0:256]),
                         reads=[rp], writes=[r_stA[s]])
                    b.op("act", lambda e, P=P, A=A: e.activation(out=A[:, 1024:1280], in_=P[:, 256:512],
                                                                 func=AF.Silu),
                         reads=[rp], writes=[r_stA[s]])
                elif bi == 6:
                    b.op("act", lambda e, P=P: e.copy(out=rin[4][:, 0:512], in_=P[:, 0:512]),
                         reads=[rp], writes=[r_rin[4]])
                else:
                    b.op("act", lambda e, P=P, A=A: e.copy(out=A[:, 1280:1536], in_=P[:, 0:256]),
                         reads=[rp], writes=[r_stA[s]])
                    b.op("act", lambda e, P=P, A=A: e.activation(out=A[:, 1536:1792], in_=P[:, 256:512],
                                                                 func=AF.Sigmoid),
                         reads=[rp], writes=[r_stA[s]])
                if bi == 0:
                    srcs = [(rin[0], r_rin[0], i * 128, i) for i in range(4)]
                elif bi == 1:
                    srcs = [(rin[1], r_rin[1], i * 128, 4 + i) for i in range(4)]
                elif bi == 3:
                    srcs = [(rin[2], r_rin[2], 0, 8), (rin[2], r_rin[2], 128, 9), (rin[2], r_rin[2], 256, 10)]
                elif bi == 4:
                    srcs = [(rin[3], r_rin[3], i * 128, 11 + i) for i in range(4)]
                elif bi == 6:
                    srcs = [(rin[4], r_rin[4], i * 128, 15 + i) for i in range(4)]
                else:
                    srcs = []
                if srcs:
                    def tr_job(srcs=srcs, T=T, s=s):
                        nonlocal pto_i
                        po = pto_i % 2
                        pto_i += 1
                        for i, (buf, rb, c, slot) in enumerate(srcs):
                            b.op("pe", lambda e, buf=buf, c=c, i=i, po=po: e.transpose(
                                out=pto[po][:, i * 128:(i + 1) * 128], in_=buf[:, c:c + 128], identity=identb[:]),
                                reads=[rb, r_idb], writes=[r_pto[po]], inc=(i == len(srcs) - 1))
                        s0 = srcs[0][3]
                        n_ = len(srcs)
                        b.op("act", lambda e, po=po, s0=s0, n_=n_, T=T: e.copy(
                            out=T[:, s0:s0 + n_, :], in_=pto[po][:, 0:n_ * 128].rearrange("p (n k) -> p n k", k=128)),
                            reads=[r_pto[po]], writes=[r_stT[s]])
                    pending.append(tr_job)
                while len(pending) > 2:
                    pending.pop(0)()
            def out_job(t=t, s=s, A=A, Fs=Fs, T=T):
                b.dma("pool", tokA_d[t * 128:(t + 1) * 128, :], A[:], reads=[r_stA[s]], writes=[r_out])
                b.dma("pool", tokF_d[t * 128:(t + 1) * 128, :], Fs[:], reads=[r_stF[s]], writes=[r_out])
                b.op("pe", lambda e, Fs=Fs: e.transpose(out=pgt[0:8, 0:128], in_=Fs[:, 4:12], identity=identf[:]),
                     reads=[r_stF[s], r_idf], writes=[r_pgt])
                b.op("act", lambda e, s=s: e.copy(out=stG[s][:], in_=pgt[0:8, 0:128]),
                     reads=[r_pgt], writes=[r_stG[s]])
                b.dma("pool", gT_d[t], stG[s][:], reads=[r_stG[s]], writes=[r_out])
                b.dma("pool", featT_d[t], T[:], reads=[r_stT[s]], writes=[r_out])
            pending.append(out_job)
        while pending:
            pending.pop(0)()
        b.barrier()
        b.es = old


def inv_freq_table():
    half = 32
    inv = (np.float32(10000.0) ** (-(np.arange(half, dtype=np.float32) / np.float32(half)))).astype(np.float32)
    return np.ascontiguousarray(np.broadcast_to(inv[None, :], (128, half))).astype(np.float32)


def build_p1(l, nt):
    nc = bass.Bass("TRN2", target_bir_lowering=False)
    x_d = nc.dram_tensor("x", [nt * 128, D], F32, kind="ExternalInput").ap()
    pos_d = nc.dram_tensor("pos", [128, nt], mybir.dt.int32, kind="ExternalInput").ap()
    invf_d = nc.dram_tensor("invf", [128, 32], F32, kind="ExternalInput").ap()
    identf_d = nc.dram_tensor("identf", [128, 128], F32, kind="ExternalInput").ap()
    win_d = nc.dram_tensor("w_in", [DEPTH, D, INW], F32, kind="ExternalInput").ap()
    modcol_d = nc.dram_tensor("modcol", [128, DEPTH * 48], F32, kind="ExternalInput").ap()
    ib_d = nc.dram_tensor("i_bias", [DEPTH, 4], F32, kind="ExternalInput").ap()
    fb_d = nc.dram_tensor("f_bias", [DEPTH, 4], F32, kind="ExternalInput").ap()
    featT_d = nc.dram_tensor("featT", [nt, 128, NFT, 128], BF16, kind="ExternalOutput").ap()
    tokA_d = nc.dram_tensor("tokA", [nt * 128, 1792], BF16, kind="ExternalOutput").ap()
    tokF_d = nc.dram_tensor("tokF", [nt * 128, 12], F32, kind="ExternalOutput").ap()
    gT_d = nc.dram_tensor("gT", [nt, 8, 128], F32, kind="ExternalOutput").ap()
    with ExitStack() as es, nc.allow_low_precision("bf16 matmul operands, fp32 accumulation"):
        b = B(nc, es)
        cos = b.sb("cos", [128, nt, 32], F32)
        sin = b.sb("sin", [128, nt, 32], F32)
        r_tab = b.res()
        emit_rope_tables(b, pos_d, invf_d, cos, sin, r_tab, nt)
        emit_p1(b, l, nt, x_d, win_d, modcol_d, ib_d, fb_d, identf_d, cos, sin, r_tab,
                featT_d, tokA_d, tokF_d, gT_d, b.res(), b.res())
        b.finish()
    return nc


NIT = 18
TOPK = 256


def gidx(g):
    return (g % 2) * NT + g // 2


def emit_p2a(b, nslots, featT_all, tokA_all, featT_own, tokF_own, vis_d, pw_d, identf_d, o_d, r_in, r_out):
    nc = b.nc
    NACT = 0
    with ExitStack() as es:
        old = b.es
        b.es = es
        kT = b.sb("a_kT", [128, 4, SEQ], BF16)
        ikT = b.sb("a_ikT", [128, SEQ], BF16)
        Va = b.sb("a_Va", [128, 32, 8, 65], BF16)
        iw = b.sb("a_iw", [128, NT, 4], F32)
        vis = b.sb("a_vis", [128, 256], F32)
        pw = b.sb("a_pw", [128, NIT + 1], F32)
        identf = b.sb("a_idf", [128, 128], F32)
        identb = b.sb("a_idb", [128, 128], BF16)
        Ib = [b.sb("a_I%d" % i, [128, SEQ], F32) for i in range(3)]
        rl = [b.sb("a_rl%d" % i, [128, 512], F32) for i in range(2)]
        Mbs = [b.sb("a_Mb%d" % i, [128, SEQ], BF16) for i in range(3)]
        MT = [b.sb("a_MT%d" % i, [128, 32, 128], BF16) for i in range(3)]
        E = [b.sb("a_E%d" % i, [128, 512], BF16) for i in range(3)]
        qpad = [b.sb("a_qpad%d" % i, [128, 8, 128], BF16) for i in range(2)]
        iqpad = [b.sb("a_iqpad%d" % i, [128, 4, 128], BF16) for i in range(3)]
        r_qpad = [b.res(), b.res()]
        r_iqpad = [b.res(), b.res(), b.res()]
        sms = [b.sb("a_sm%d" % i, [128, 16], F32) for i in range(3)]
        rcp = b.sb("a_rcp", [128, 8], F32)
        deltas = [b.sb("a_delta%d" % i, [128, NIT + 1], F32) for i in range(3)]
        ndeltas = [b.sb("a_ndelta%d" % i, [128, NIT + 1], F32) for i in range(3)]
        osb = [b.sb("a_o%d" % i, [128, 512], BF16) for i in range(2)]
        pI = [b.ps("a_pI%d" % i, [128, 512]) for i in range(2)]
        pS = [b.ps("a_pS%d" % i, [128, 512]) for i in range(3)]
        pO = [b.ps("a_pO%d" % i, [128, 512]) for i in range(2)]
        pMT = b.ps("a_pMT", [128, 1024], BF16)

        (r_kT, r_ikT, r_Va, r_qT, r_iqT, r_iw, r_vis, r_pw, r_idf, r_idb, r_pMT, r_vst, r_rcp) = (
            b.res() for _ in range(13))
        r_I = [b.res(), b.res(), b.res()]
        r_rl = [b.res(), b.res()]
        r_Mb = [b.res(), b.res(), b.res()]
        r_MT = [b.res(), b.res(), b.res()]
        r_E = [b.res() for _ in range(3)]
        r_sm = [b.res(), b.res(), b.res()]
        r_delta = [b.res(), b.res(), b.res()]
        r_osb = [b.res(), b.res()]
        r_pI = [b.res(), b.res()]
        r_pS = [b.res() for _ in range(3)]
        r_pO = [b.res(), b.res()]

        b.dma("sp", identf[:], identf_d, writes=[r_idf])
        b.op("dve", lambda e: e.tensor_copy(out=identb[:], in_=identf[:]), reads=[r_idf], writes=[r_idb])
        b.dma("sp", vis[:], vis_d, writes=[r_vis])
        b.dma("sp", pw[:], pw_d, writes=[r_pw])
        b.dma("sp", iw[:], tokF_own[:, 0:4].rearrange("(t p) c -> p t c", p=128), reads=[r_in], writes=[r_iw])
        ngl = 2 * nslots
        for g in range(ngl):
            gi = gidx(g)
            b.dma("sp", kT[:, :, g * 128:(g + 1) * 128], featT_all[gi, :, 4:8, :], reads=[r_in], writes=[r_kT])
            b.dma("sp", ikT[:, g * 128:(g + 1) * 128], featT_all[gi, :, 10, :], reads=[r_in], writes=[r_ikT])
        b.op("pool", lambda e: e.memset(Va[:, :, :, 64:65], 1.0), writes=[r_Va])
        vst = b.sb("a_vst", [128, 8, 512], BF16)
        for g0 in range(0, ngl, 8):
            n = min(8, ngl - g0)
            for i in range(n):
                gi = gidx(g0 + i)
                b.dma("sp", vst[:, i, :], tokA_all[gi * 128:(gi + 1) * 128, 0:512], reads=[r_in], writes=[r_vst])
            b.op("pool", lambda e, g0=g0, n=n: e.tensor_copy(
                out=Va[:, g0:g0 + n, :, 0:64],
                in_=vst[:, 0:n, :].rearrange("p g (h d) -> p g h d", d=64)),
                reads=[r_vst], writes=[r_Va])

        for i in range(2):
            b.op("pool", lambda e, i=i: e.memset(qpad[i][:], 0.0), writes=[r_qpad[i]])
        for i in range(3):
            b.op("pool", lambda e, i=i: e.memset(iqpad[i][:], 0.0), writes=[r_iqpad[i]])
        LO, HI, RNG, CAND, CNT, VV, TT, NCD, SS, SG = range(10)
        MBIG = 30000.0
        cnts = {"i": 0, "s": 0}

        def sel_gen(m):
            L = (2 * m + 2) * 128
            nkb = 2 * m + 2
            s = m % 3
            I = Ib[s]
            sm = sms[s]
            delta = deltas[s]
            ndelta = ndeltas[s]
            Mb = Mbs[s]
            rsm = r_sm[s]
            rdl = r_delta[s]
            for half in range(2):
                hsl = slice(half * 64, half * 64 + 64)
                b.dma("sp", iqpad[s][hsl, half::2, :], featT_own[m, hsl, 8:10, :], reads=[r_in],
                      writes=[r_iqpad[s]])
            for kg in range((L + 511) // 512):
                w = min(512, L - kg * 512)
                for h in range(4):
                    pi = cnts["i"] % 2
                    cnts["i"] += 1
                    hs = slice((h % 2) * 64, (h % 2) * 64 + 64)
                    b.op("pe", lambda e, pi=pi, h=h, kg=kg, w=w: e.matmul(
                        pI[pi][:, 0:w], lhsT=iqpad[s][:, h, :],
                        rhs=ikT[:, kg * 512:kg * 512 + w], start=True, stop=True),
                        reads=[r_iqpad[s], r_ikT], writes=[r_pI[pi]])
                    b.op("act", lambda e, pi=pi, w=w: e.activation(out=rl[pi][:, 0:w], in_=pI[pi][:, 0:w],
                                                                    func=AF.Relu),
                         reads=[r_pI[pi]], writes=[r_rl[pi]])
                    dst = I[:, kg * 512:kg * 512 + w]
                    if h == 0:
                        b.op("dve", lambda e, pi=pi, w=w, dst=dst: e.tensor_scalar(
                            out=dst, in0=rl[pi][:, 0:w], scalar1=iw[:, m, 0:1], scalar2=None, op0=ALU.mult),
                            reads=[r_rl[pi], r_iw], writes=[r_I[s]])
                    else:
                        b.op("dve", lambda e, pi=pi, w=w, dst=dst, h=h: e.scalar_tensor_tensor(
                            out=dst, in0=rl[pi][:, 0:w], scalar=iw[:, m, h:h + 1], in1=dst,
                            op0=ALU.mult, op1=ALU.add),
                            reads=[r_rl[pi], r_iw, r_I[s]], writes=[r_I[s]])
                    yield
            if m == 0:
                b.op("dve", lambda e: e.tensor_tensor(out=I[:, L - 256:L], in0=I[:, L - 256:L],
                                                      in1=vis[:], op=ALU.add),
                     reads=[r_I[s], r_vis], writes=[r_I[s]])
                b.op("dve", lambda e: e.memset(sm[:, TT:TT + 1], 0.5 * NEG), writes=[rsm])
            else:
                b.op("dve", lambda e: e.tensor_reduce(out=sm[:, LO:LO + 1], in_=I[:, 0:L - 256],
                                                      axis=AX.X, op=ALU.min),
                     reads=[r_I[s]], writes=[rsm])
                b.op("dve", lambda e: e.tensor_tensor(out=I[:, L - 256:L], in0=I[:, L - 256:L],
                                                      in1=vis[:], op=ALU.add),
                     reads=[r_I[s], r_vis], writes=[r_I[s]])
                b.op("dve", lambda e: e.tensor_reduce(out=sm[:, HI:HI + 1], in_=I[:, 0:L],
                                                      axis=AX.X, op=ALU.max),
                     reads=[r_I[s]], writes=[rsm])
                yield
                b.op("dve", lambda e: e.tensor_tensor(out=sm[:, RNG:RNG + 1], in0=sm[:, HI:HI + 1],
                                                      in1=sm[:, LO:LO + 1], op=ALU.subtract),
                     reads=[rsm], writes=[rsm])
                b.op("dve", lambda e: e.tensor_scalar(out=delta[:], in0=pw[:], scalar1=sm[:, RNG:RNG + 1],
                                                      scalar2=None, op0=ALU.mult),
                     reads=[rsm, r_pw], writes=[rdl])
                b.op("dve", lambda e: e.tensor_scalar(out=ndelta[:], in0=delta[:], scalar1=-1.0,
                                                      scalar2=None, op0=ALU.mult),
                     reads=[rdl], writes=[rdl])
                b.op("dve", lambda e: e.tensor_tensor(out=sm[:, NCD:NCD + 1], in0=ndelta[:, 0:1],
                                                      in1=sm[:, LO:LO + 1], op=ALU.subtract),
                     reads=[rsm, rdl], writes=[rsm])
                yield
                for n in range(NACT):
                    b.op("act", lambda e: e.activation(
                        out=Mb[:, 0:L], in_=I[:, 0:L], func=AF.Sign, bias=sm[:, NCD:NCD + 1],
                        accum_out=sm[:, SS:SS + 1]),
                        reads=[r_I[s], rsm], writes=[r_Mb[s], rsm])
                    b.op("act", lambda e: e.activation(
                        out=sm[:, SG:SG + 1], in_=sm[:, SS:SS + 1], func=AF.Sign, bias=float(L - (2 * TOPK - 1))),
                        reads=[rsm], writes=[rsm])
                    b.op("act", lambda e, n=n: e.activation(
                        out=sm[:, NCD:NCD + 1], in_=sm[:, SG:SG + 1], func=AF.Identity,
                        scale=ndelta[:, n + 1:n + 2], bias=sm[:, NCD:NCD + 1]),
                        reads=[rsm, rdl], writes=[rsm])
                    yield
                b.op("dve", lambda e: e.tensor_scalar(out=sm[:, CAND:CAND + 1], in0=sm[:, NCD:NCD + 1],
                                                      scalar1=-1.0, scalar2=None, op0=ALU.mult),
                     reads=[rsm], writes=[rsm])
                for n in range(NACT, NIT):
                    b.op("dve", lambda e: e.tensor_scalar(
                        out=Mb[:, 0:L], in0=I[:, 0:L], scalar1=sm[:, CAND:CAND + 1], scalar2=0.0,
                        op0=ALU.is_ge, op1=ALU.add, accum_out=sm[:, CNT:CNT + 1]),
                        reads=[r_I[s], rsm], writes=[r_Mb[s], rsm])
                    b.op("dve", lambda e: e.tensor_scalar(
                        out=sm[:, VV:VV + 1], in0=sm[:, CNT:CNT + 1], scalar1=TOPK - 0.5, scalar2=0.5,
                        op0=ALU.is_ge, op1=ALU.subtract),
                        reads=[rsm], writes=[rsm])
                    b.op("dve", lambda e, n=n: e.scalar_tensor_tensor(
                        out=sm[:, CAND:CAND + 1], in0=sm[:, VV:VV + 1], scalar=delta[:, n:n + 1],
                        in1=sm[:, CAND:CAND + 1], op0=ALU.mult, op1=ALU.add),
                        reads=[rsm, rdl], writes=[rsm])
                    yield
                b.op("dve", lambda e: e.tensor_tensor(out=sm[:, TT:TT + 1], in0=sm[:, CAND:CAND + 1],
                                                      in1=delta[:, NIT:NIT + 1], op=ALU.subtract),
                     reads=[rsm, rdl], writes=[rsm])
            b.op("dve", lambda e: e.tensor_scalar(
                out=Mb[:, 0:L], in0=I[:, 0:L], scalar1=sm[:, TT:TT + 1], scalar2=None, op0=ALU.is_ge),
                reads=[r_I[s], rsm], writes=[r_Mb[s]])
            yield
            MTs = MT[s]
            for k0 in range(0, nkb, 8):
                n = min(8, nkb - k0)
                for i in range(n):
                    b.op("pe", lambda e, i=i, k0=k0: e.transpose(
                        out=pMT[:, i * 128:(i + 1) * 128], in_=Mb[:, (k0 + i) * 128:(k0 + i + 1) * 128],
                        identity=identb[:]),
                        reads=[r_Mb[s], r_idb], writes=[r_pMT], inc=(i == n - 1))
                b.op("act", lambda e, k0=k0, n=n: e.activation(
                    out=MTs[:, k0:k0 + n, :], in_=pMT[:, 0:n * 128].rearrange("p (n k) -> p n k", k=128),
                    func=AF.Identity, scale=MBIG, bias=-MBIG),
                    reads=[r_pMT], writes=[r_MT[s]])
                yield

        def att_gen(m):
            nkb = 2 * m + 2
            s = m % 2
            s3 = m % 3
            MTs = MT[s3]
            O = osb[s]
            for half in range(2):
                hsl = slice(half * 64, half * 64 + 64)
                b.dma("sp", qpad[s][hsl, half::2, :], featT_own[m, hsl, 0:4, :], reads=[r_in],
                      writes=[r_qpad[s]])
            units = [(h, k0, min(4, nkb - k0)) for h in range(8) for k0 in range(0, nkb, 4)]

            def emit_pv(u, si):
                h, k0, n = u
                p = h // 2
                po = pO[h // 4]
                r_po = r_pO[h // 4]
                oc = (h % 4) * 65
                for i in range(n):
                    kb = k0 + i
                    b.op("pe", lambda e, si=si, i=i, kb=kb, h=h, po=po, oc=oc: e.matmul(
                        po[:, oc:oc + 65], lhsT=E[si][:, i * 128:(i + 1) * 128], rhs=Va[:, kb, h, :],
                        start=(kb == 0), stop=(kb == nkb - 1)),
                        reads=[r_E[si], r_Va], writes=[r_po], inc=(i == n - 1))
                if k0 + n == nkb:
                    b.op("dve", lambda e, po=po, oc=oc, h=h: e.reciprocal(out=rcp[:, h:h + 1],
                                                                           in_=po[:, oc + 64:oc + 65]),
                         reads=[r_po], writes=[r_rcp])
                    b.op("dve", lambda e, po=po, oc=oc, h=h: e.tensor_scalar(
                        out=O[:, h * 64:(h + 1) * 64], in0=po[:, oc:oc + 64], scalar1=rcp[:, h:h + 1],
                        scalar2=None, op0=ALU.mult),
                        reads=[r_po, r_rcp], writes=[r_osb[s]])

            prev = None
            for u in units:
                h, k0, n = u
                p = h // 2
                si = cnts["s"] % 3
                cnts["s"] += 1
                b.op("pe", lambda e, si=si, k0=k0, n=n: e.matmul(
                    pS[si][:, 0:n * 128], lhsT=identb[:, :],
                    rhs=MTs[:, k0:k0 + n, :].rearrange("p n k -> p (n k)"), start=True, stop=False),
                    reads=[r_idb, r_MT[s3]], writes=[r_pS[si]], inc=False)
                for i in range(n):
                    kb = k0 + i
                    b.op("pe", lambda e, si=si, i=i, kb=kb, p=p, h=h, n=n: e.matmul(
                        pS[si][:, i * 128:(i + 1) * 128], lhsT=kT[:, p, kb * 128:(kb + 1) * 128],
                        rhs=qpad[s][:, h, :], start=False, stop=(i == n - 1)),
                        reads=[r_kT, r_qpad[s]], writes=[r_pS[si]], inc=(i == n - 1))
                b.op("act", lambda e, si=si, n=n: e.activation(
                    out=E[si][:, 0:n * 128], in_=pS[si][:, 0:n * 128], func=AF.Exp, scale=0.125),
                    reads=[r_pS[si]], writes=[r_E[si]])
                if prev is not None:
                    emit_pv(*prev)
                prev = (u, si)
                yield
            emit_pv(*prev)
            b.dma("pool", o_d[m * 128:(m + 1) * 128, 0:512], O[:], reads=[r_osb[s]], writes=[r_out])

        def step(gen):
            try:
                next(gen)
                return True
            except StopIteration:
                return False

        for _ in sel_gen(0):
            pass
        cur = sel_gen(1) if nslots > 1 else None
        for m in range(nslots):
            A = att_gen(m)
            nxt = sel_gen(m + 2) if m + 2 < nslots else None
            a_live = True
            while a_live:
                a_live = step(A)
                if cur is not None and not step(cur):
                    cur = None
                if nxt is not None and cur is not None:
                    if not step(nxt):
                        nxt = None
            while cur is not None:
                if not step(cur):
                    cur = None
            cur = nxt
        b.barrier()
        b.es = old


def vis_table(j):
    v = np.zeros((128, 256), np.float32)
    diag = np.zeros((128, 128), np.float32)
    diag[0:64, 64:128] = NEG
    if j == 0:
        v[:, 0:128] = diag
        v[:, 128:256] = NEG
    else:
        v[:, 128:256] = diag
    return v


def pw_table():
    return np.ascontiguousarray(np.broadcast_to(
        (0.5 ** np.arange(1, NIT + 2, dtype=np.float64)).astype(np.float32)[None, :], (128, NIT + 1)))


def build_p2a(nslots):
    nc = bass.Bass("TRN2", target_bir_lowering=False)
    featT_all = nc.dram_tensor("featT_all", [2 * NT, 128, NFT, 128], BF16, kind="ExternalInput").ap()
    tokA_all = nc.dram_tensor("tokA_all", [2 * TOK, 1792], BF16, kind="ExternalInput").ap()
    featT_own = nc.dram_tensor("featT_own", [NT, 128, NFT, 128], BF16, kind="ExternalInput").ap()
    tokF_own = nc.dram_tensor("tokF_own", [TOK, 12], F32, kind="ExternalInput").ap()
    vis_d = nc.dram_tensor("vis", [128, 256], F32, kind="ExternalInput").ap()
    pw_d = nc.dram_tensor("pw", [128, NIT + 1], F32, kind="ExternalInput").ap()
    identf_d = nc.dram_tensor("identf", [128, 128], F32, kind="ExternalInput").ap()
    o_d = nc.dram_tensor("o", [TOK, D], BF16, kind="ExternalOutput").ap()
    with ExitStack() as es, nc.allow_low_precision("bf16 matmul operands, fp32 accumulation"):
        b = B(nc, es)
        emit_p2a(b, nslots, featT_all, tokA_all, featT_own, tokF_own, vis_d, pw_d, identf_d, o_d,
                 b.res(), b.res())
        b.finish()
    return nc


def emit_head_norm(b, src, r_src, gate, r_gate, dst, r_dst, tmp, st, r_tmp, r_st):
    b.op("dve", lambda e: e.tensor_reduce(out=st[:, 0:4], in_=src[:], axis=AX.X, op=ALU.add),
         reads=[r_src], writes=[r_st])
    b.op("dve", lambda e: e.tensor_scalar(out=st[:, 0:4], in0=st[:, 0:4], scalar1=1.0 / 64, scalar2=None,
                                          op0=ALU.mult), reads=[r_st], writes=[r_st])
    b.op("dve", lambda e: e.tensor_tensor(out=src[:], in0=src[:],
                                          in1=st[:, 0:4].unsqueeze(2).to_broadcast([128, 4, 64]),
                                          op=ALU.subtract), reads=[r_src, r_st], writes=[r_src])
    b.op("dve", lambda e: e.tensor_tensor(out=tmp[:], in0=src[:], in1=src[:], op=ALU.mult),
         reads=[r_src], writes=[r_tmp])
    b.op("dve", lambda e: e.tensor_reduce(out=st[:, 4:8], in_=tmp[:], axis=AX.X, op=ALU.add),
         reads=[r_tmp], writes=[r_st])
    b.op("dve", lambda e: e.tensor_scalar(out=st[:, 4:8], in0=st[:, 4:8], scalar1=1.0 / 64, scalar2=LN_EPS,
                                          op0=ALU.mult, op1=ALU.add), reads=[r_st], writes=[r_st])
    b.op("act", lambda e: e.activation(out=st[:, 8:12], in_=st[:, 4:8], func=AF.Sqrt),
         reads=[r_st], writes=[r_st])
    b.op("dve", lambda e: e.reciprocal(out=st[:, 12:16], in_=st[:, 8:12]), reads=[r_st], writes=[r_st])
    if gate is None:
        b.op("dve", lambda e: e.tensor_tensor(
            out=dst.rearrange("p (h d) -> p h d", d=64), in0=src[:],
            in1=st[:, 12:16].unsqueeze(2).to_broadcast([128, 4, 64]), op=ALU.mult),
            reads=[r_src, r_st], writes=[r_dst])
    else:
        b.op("dve", lambda e: e.tensor_tensor(
            out=tmp[:], in0=src[:], in1=st[:, 12:16].unsqueeze(2).to_broadcast([128, 4, 64]), op=ALU.mult),
            reads=[r_src, r_st], writes=[r_tmp])
        b.op("dve", lambda e: e.tensor_tensor(
            out=dst.rearrange("p (h d) -> p h d", d=64), in0=tmp[:],
            in1=gate.rearrange("p (h d) -> p h d", d=64), op=ALU.mult),
            reads=[r_tmp, r_gate], writes=[r_dst])


GAMMAS = [1.0 - 2.0 ** (-5.0 - h) for h in range(4)]


def ret_tables():
    i = np.arange(128)
    decT = np.zeros((128, 4, 128), np.float32)
    qdecT = np.zeros((128, 2, 128), np.float32)
    kdec = np.zeros((128, 4), np.float32)
    for h, g in enumerate(GAMMAS):
        diff = i[None, :] - i[:, None]
        decT[:, h, :] = np.where(diff >= 0, g ** np.maximum(diff, 0), 0.0) / 8.0
        qdecT[(h % 2) * 64:(h % 2) * 64 + 64, h // 2, :] = (g ** (i + 1.0))[None, :]
        kdec[:, h] = g ** (127.0 - i) / 8.0
    return decT, qdecT, kdec


def emit_p2b(b, nslots, tokA_all, featT_own, tokA_own, decT_d, qdecT_d, kdec_d, jf_d, o_d, r_in, r_out,
             whole=False):
    with ExitStack() as es:
        old = b.es
        b.es = es
        decT = b.sb("b_decT", [128, 4, 128], F32)
        qdecT = b.sb("b_qdecT", [128, 2, 128], F32)
        kdec = b.sb("b_kdec", [128, 4], F32)
        jf = b.sb("b_jf", [128, 1], F32)
        S = b.sb("b_S", [128, 2, 64], F32)
        SA = b.sb("b_SA", [128, 2, 64], F32)
        Sd = b.sb("b_Sd", [128, 2, 64], F32)
        Sbf = b.sb("b_Sbf", [128, 2, 64], BF16)
        kvin = [b.sb("b_kvin%d" % i, [128, 512], BF16) for i in range(2)]
        vdec = [b.sb("b_vdec%d" % i, [128, 256], BF16) for i in range(2)]
        qk = [b.sb("b_qk%d" % i, [128, 4, 128], BF16) for i in range(2)]
        qd = [b.sb("b_qd%d" % i, [128, 2, 128], BF16) for i in range(2)]
        own = [b.sb("b_own%d" % i, [128, 512], BF16) for i in range(2)]
        scT = [b.sb("b_scT%d" % i, [128, 128], BF16) for i in range(2)]
        ysb = b.sb("b_ysb", [128, 4, 64], F32)
        tmp = b.sb("b_tmp", [128, 4, 64], F32)
        st = b.sb("b_st", [128, 16], F32)
        osb = [b.sb("b_o%d" % i, [128, 256], BF16) for i in range(2)]
        pKV = [b.ps("b_pKV%d" % i, [128, 128]) for i in range(2)]
        pSC = [b.ps("b_pSC%d" % i, [128, 128]) for i in range(2)]
        pY = b.ps("b_pY", [128, 256])
        (r_c, r_S, r_SA, r_Sd, r_Sbf, r_ysb, r_tmp, r_st, r_pY) = (b.res() for _ in range(9))
        r_kvin = [b.res(), b.res()]
        r_vdec = [b.res(), b.res()]
        r_qk = [b.res(), b.res()]
        r_qd = [b.res(), b.res()]
        r_own = [b.res(), b.res()]
        r_scT = [b.res(), b.res()]
        r_osb = [b.res(), b.res()]
        r_pKV = [b.res(), b.res()]
        r_pSC = [b.res(), b.res()]

        b.dma("sp", decT[:], decT_d, writes=[r_c])
        b.dma("sp", qdecT[:], qdecT_d, writes=[r_c])
        b.dma("sp", kdec[:], kdec_d, writes=[r_c])
        b.dma("sp", jf[:], jf_d, writes=[r_c])
        b.op("dve", lambda e: e.memset(S[:], 0.0), writes=[r_S])
        sc_i = 0
        for g in range(2 * nslots):
            m, r = g // 2, g % 2
            s = g % 2
            gi = gidx(g)
            b.dma("sp", kvin[s][:], tokA_all[gi * 128:(gi + 1) * 128, 512:1024], reads=[r_in], writes=[r_kvin[s]])
            if whole:
                so = g % 2
                orow = gi * 128
                b.dma("sp", qk[so][:], featT_own[gi, :, 11:15, :], reads=[r_in], writes=[r_qk[so]])
                b.dma("sp", own[so][:], tokA_all[gi * 128:(gi + 1) * 128, 768:1280], reads=[r_in], writes=[r_own[so]])
                b.op("dve", lambda e: e.tensor_copy(out=Sbf[:], in_=S[:]), reads=[r_S], writes=[r_Sbf])
            elif r == 0:
                b.op("dve", lambda e: e.tensor_copy(out=SA[:], in_=S[:]), reads=[r_S], writes=[r_SA])
                so = m % 2
                b.dma("sp", qk[so][:], featT_own[m, :, 11:15, :], reads=[r_in], writes=[r_qk[so]])
                b.dma("sp", own[so][:], tokA_own[m * 128:(m + 1) * 128, 768:1280], reads=[r_in], writes=[r_own[so]])
            else:
                so = m % 2
                orow = m * 128
                b.op("dve", lambda e: e.tensor_tensor(out=Sd[:], in0=S[:], in1=SA[:], op=ALU.subtract),
                     reads=[r_S, r_SA], writes=[r_Sd])
                b.op("dve", lambda e: e.scalar_tensor_tensor(
                    out=Sbf[:].rearrange("p a e -> p (a e)"), in0=Sd[:].rearrange("p a e -> p (a e)"),
                    scalar=jf[:, 0:1], in1=SA[:].rearrange("p a e -> p (a e)"), op0=ALU.mult, op1=ALU.add),
                    reads=[r_Sd, r_SA, r_c], writes=[r_Sbf])
            if whole or r == 1:
                b.op("dve", lambda e, so=so: e.tensor_tensor(
                    out=qd[so][:], in0=qk[so][:, 0:2, :], in1=qdecT[:], op=ALU.mult),
                    reads=[r_qk[so], r_c], writes=[r_qd[so]])
                for h in range(4):
                    hs = slice((h % 2) * 64, (h % 2) * 64 + 64)
                    p = h // 2
                    si = sc_i % 2
                    sc_i += 1
                    b.op("pe", lambda e, so=so, hs=hs, p=p, si=si: e.matmul(
                        pSC[si][:, :], lhsT=qk[so][hs, 2 + p, :], rhs=qk[so][hs, p, :], start=True, stop=True),
                        reads=[r_qk[so]], writes=[r_pSC[si]])
                    b.op("dve", lambda e, si=si, h=h: e.tensor_tensor(
                        out=scT[si][:], in0=pSC[si][:], in1=decT[:, h, :], op=ALU.mult),
                        reads=[r_pSC[si], r_c], writes=[r_scT[si]])
                    b.op("pe", lambda e, si=si, h=h, so=so: e.matmul(
                        pY[:, h * 64:(h + 1) * 64], lhsT=scT[si][:], rhs=own[so][:, h * 64:(h + 1) * 64],
                        start=True, stop=False),
                        reads=[r_scT[si], r_own[so]], writes=[r_pY], inc=False)
                    b.op("pe", lambda e, h=h, so=so, hs=hs, p=p: e.matmul(
                        pY[:, h * 64:(h + 1) * 64], lhsT=qd[so][hs, p, :], rhs=Sbf[hs, p, :],
                        start=False, stop=True),
                        reads=[r_qd[so], r_Sbf], writes=[r_pY])
                b.op("act", lambda e: e.copy(out=ysb[:].rearrange("p h d -> p (h d)"), in_=pY[:]),
                     reads=[r_pY], writes=[r_ysb])
                emit_head_norm(b, ysb, r_ysb, own[so][:, 256:512], r_own[so], osb[so][:], r_osb[so],
                               tmp, st, r_tmp, r_st)
                b.dma("pool", o_d[orow:orow + 128, 512:768], osb[so][:], reads=[r_osb[so]], writes=[r_out])
            if g == 2 * nslots - 1:
                break
            b.op("dve", lambda e, s=s: e.tensor_tensor(
                out=vdec[s][:].rearrange("p (h d) -> p h d", d=64),
                in0=kvin[s][:, 256:512].rearrange("p (h d) -> p h d", d=64),
                in1=kdec[:].unsqueeze(2).to_broadcast([128, 4, 64]), op=ALU.mult),
                reads=[r_kvin[s], r_c], writes=[r_vdec[s]])
            for p in range(2):
                b.op("pe", lambda e, s=s, p=p: e.matmul(
                    pKV[p][:, :], lhsT=kvin[s][:, p * 128:(p + 1) * 128], rhs=vdec[s][:, p * 128:(p + 1) * 128],
                    start=True, stop=True), reads=[r_kvin[s], r_vdec[s]], writes=[r_pKV[p]])
                for half in range(2):
                    h = 2 * p + half
                    hs = slice(half * 64, half * 64 + 64)
                    b.op("dve", lambda e, p=p, hs=hs, half=half, h=h: e.scalar_tensor_tensor(
                        out=S[hs, p, :], in0=S[hs, p, :], scalar=float(GAMMAS[h] ** 128),
                        in1=pKV[p][hs, half * 64:(half + 1) * 64], op0=ALU.mult, op1=ALU.add),
                        reads=[r_S, r_pKV[p]], writes=[r_S])
        b.barrier()
        b.es = old


def emit_p2c(b, l, nslots, featT_all, tokA_all, gT_all, tokA_own, convw_d, convb_d, caus_d, sel4_d, eye4_d,
             jf_d, identf_d, o_d, r_in, r_out, whole=False):
    NG = 2 * nslots
    NTK = NG * 128
    NOWN = NG if whole else nslots
    with ExitStack() as es:
        old = b.es
        b.es = es
        jf = b.sb("c_jf", [128, 1], F32)
        caus = b.sb("c_caus", [128, 128], F32)
        sel4 = b.sb("c_sel4", [4, 4, 128], F32)
        eye4 = b.sb("c_eye4", [4, 4], F32)
        ones4 = b.sb("c_ones4", [4, 128], F32)
        identf = b.sb("c_idf", [128, 128], F32)
        identb = b.sb("c_idb", [128, 128], BF16)
        qkall = b.sb("c_qkall", [128, 4, NTK], BF16)
        qkown = qkall if whole else b.sb("c_qkown", [128, 4, nslots * 128], BF16)
        GTo = b.sb("c_GTo", [4, NOWN, 128], F32)
        acol = b.sb("c_acol", [128, NG, 4], F32)
        ecol = b.sb("c_ecol", [128, NG, 4], F32)
        acolo = b.sb("c_acolo", [128, NOWN, 4], F32)
        ecolo = b.sb("c_ecolo", [128, NOWN, 4], F32)
        GR = b.sb("c_GR", [128, NG + 1, 4], F32)
        GRo = b.sb("c_GRo", [128, NOWN, 4], F32)
        dec = b.sb("c_dec", [128, NG, 4], F32)
        kw = b.sb("c_kw", [128, NG, 4], F32)
        r_c, r_qkall, r_qkown, r_GTo, r_cols, r_GR = (b.res() for _ in range(6))
        if whole:
            r_qkown = r_qkall

        b.dma("sp", jf[:], jf_d, writes=[r_c])
        b.dma("sp", caus[:], caus_d, writes=[r_c])
        b.dma("sp", sel4[:].rearrange("k h n -> k (h n)"), sel4_d, writes=[r_c])
        b.dma("sp", eye4[:], eye4_d, writes=[r_c])
        b.dma("sp", identf[:], identf_d, writes=[r_c])
        b.op("dve", lambda e: e.tensor_copy(out=identb[:], in_=identf[:]), reads=[r_c], writes=[r_c])
        b.op("dve", lambda e: e.memset(ones4[:], 1.0), writes=[r_c])

        with ExitStack() as es1:
            b.es = es1
            gi_ = b.sb("c1_gi", [4, NTK], F32)
            gf_ = b.sb("c1_gf", [4, NTK], F32)
            t1 = b.sb("c1_t1", [4, NTK], F32)
            Bn = b.sb("c1_Bn", [4, NTK], F32)
            aT = b.sb("c1_aT", [4, NTK], F32)
            GT = b.sb("c1_GT", [4, NTK], F32)
            eT = b.sb("c1_eT", [4, NTK], F32)
            onesr = b.sb("c1_ones", [4, NTK], F32)
            gend = b.sb("c1_gend", [4, NG, 4], F32)
            pT = b.ps("c1_pT", [128, 2 * NG * 4])
            pR = b.ps("c1_pR", [128, NG * 4])
            r_g, r_t1, r_Bn, r_aT, r_GT, r_eT, r_on, r_ge, r_pT, r_pR = (b.res() for _ in range(10))
            for g in range(NG):
                gi = gidx(g)
                b.dma("sp", gi_[:, g * 128:(g + 1) * 128], gT_all[gi, 0:4, :], reads=[r_in], writes=[r_g])
                b.dma("sp", gf_[:, g * 128:(g + 1) * 128], gT_all[gi, 4:8, :], reads=[r_in], writes=[r_g])
            b.op("dve", lambda e: e.memset(onesr[:], 1.0), writes=[r_on])
            b.op("act", lambda e: e.activation(out=t1[:], in_=gf_[:], func=AF.Exp, scale=-1.0),
                 reads=[r_g], writes=[r_t1])
            b.op("act", lambda e: e.activation(out=t1[:], in_=t1[:], func=AF.Ln, bias=1.0),
                 reads=[r_t1], writes=[r_t1])
            b.op("dve", lambda e: e.tensor_tensor_scan(out=Bn[:], data0=onesr[:], data1=t1[:], initial=0.0,
                                                       op0=ALU.mult, op1=ALU.add),
                 reads=[r_on, r_t1], writes=[r_Bn])
            b.op("dve", lambda e: e.tensor_tensor(out=aT[:], in0=gi_[:], in1=Bn[:], op=ALU.add),
                 reads=[r_g, r_Bn], writes=[r_aT])
            b.op("dve", lambda e: e.tensor_tensor_scan(out=GT[:], data0=onesr[:], data1=aT[:], initial=0.0,
                                                       op0=ALU.mult, op1=ALU.max),
                 reads=[r_on, r_aT], writes=[r_GT])
            b.op("dve", lambda e: e.tensor_tensor(out=eT[:], in0=Bn[:], in1=GT[:], op=ALU.subtract),
                 reads=[r_Bn, r_GT], writes=[r_eT])
            b.op("act", lambda e: e.activation(out=eT[:], in_=eT[:], func=AF.Exp), reads=[r_eT], writes=[r_eT])
            if whole:
                b.op("dve", lambda e: e.tensor_copy(out=GTo[:].rearrange("k g n -> k (g n)"), in_=GT[:]),
                     reads=[r_GT], writes=[r_GTo])
            else:
                GTv = GT[:].rearrange("k (m r n) -> k m r n", r=2, n=128)
                b.op("dve", lambda e: e.tensor_tensor(out=t1[:, 0:nslots * 128].rearrange("k (m n) -> k m n", n=128),
                                                      in0=GTv[:, :, 1, :], in1=GTv[:, :, 0, :], op=ALU.subtract),
                     reads=[r_GT, r_t1], writes=[r_t1])
                b.op("dve", lambda e: e.scalar_tensor_tensor(
                    out=GTo[:], in0=t1[:, 0:nslots * 128].rearrange("k (m n) -> k m n", n=128), scalar=jf[0:4, 0:1],
                    in1=GTv[:, :, 0, :], op0=ALU.mult, op1=ALU.add),
                    reads=[r_t1, r_GT, r_c], writes=[r_GTo])
            for g in range(NG):
                b.op("pe", lambda e, g=g: e.transpose(out=pT[:, g * 4:(g + 1) * 4], in_=aT[:, g * 128:(g + 1) * 128],
                                                      identity=identf[0:4, 0:4]),
                     reads=[r_aT, r_c], writes=[r_pT], inc=False)
                b.op("pe", lambda e, g=g: e.transpose(out=pT[:, (NG + g) * 4:(NG + g + 1) * 4],
                                                      in_=eT[:, g * 128:(g + 1) * 128], identity=identf[0:4, 0:4]),
                     reads=[r_eT, r_c], writes=[r_pT], inc=(g == NG - 1))
            b.op("act", lambda e: e.copy(out=acol[:].rearrange("p g h -> p (g h)"), in_=pT[:, 0:NG * 4]),
                 reads=[r_pT], writes=[r_cols])
            b.op("act", lambda e: e.copy(out=ecol[:].rearrange("p g h -> p (g h)"), in_=pT[:, NG * 4:2 * NG * 4]),
                 reads=[r_pT], writes=[r_cols])
            b.op("dve", lambda e: e.tensor_tensor(
                out=gend[:], in0=GT[:].rearrange("k (g n) -> k g n", n=128)[:, :, 127:128].to_broadcast([4, NG, 4]),
                in1=eye4[:].unsqueeze(1).to_broadcast([4, NG, 4]), op=ALU.mult),
                reads=[r_GT, r_c], writes=[r_ge])
            b.op("pe", lambda e: e.matmul(pR[:, :], lhsT=ones4[:, :], rhs=gend[:].rearrange("k g h -> k (g h)"),
                                          start=True, stop=True), reads=[r_ge, r_c], writes=[r_pR])
            b.op("dve", lambda e: e.memset(GR[:, 0, :], 0.0), writes=[r_GR])
            b.op("act", lambda e: e.copy(out=GR[:, 1:NG + 1, :].rearrange("p g h -> p (g h)"), in_=pR[:]),
                 reads=[r_pR], writes=[r_GR])
            b.op("dve", lambda e: e.tensor_tensor(out=dec[:], in0=GR[:, 0:NG, :], in1=GR[:, 1:NG + 1, :],
                                                  op=ALU.subtract), reads=[r_GR], writes=[r_cols])
            b.op("act", lambda e: e.activation(out=dec[:], in_=dec[:], func=AF.Exp), reads=[r_cols], writes=[r_cols])
            b.op("dve", lambda e: e.tensor_tensor(out=kw[:], in0=acol[:], in1=GR[:, 1:NG + 1, :], op=ALU.subtract),
                 reads=[r_cols, r_GR], writes=[r_cols])
            b.op("act", lambda e: e.activation(out=kw[:], in_=kw[:], func=AF.Exp), reads=[r_cols], writes=[r_cols])
            b.op("dve", lambda e: e.tensor_scalar(out=kw[:], in0=kw[:], scalar1=0.125, scalar2=None, op0=ALU.mult),
                 reads=[r_cols], writes=[r_cols])

            def blend(dst, src, ncol):
                sv = src.rearrange("p (m r) h -> p m r h", r=2)
                b.op("dve", lambda e: e.tensor_tensor(out=dst, in0=sv[:, :, 1, :], in1=sv[:, :, 0, :],
                                                      op=ALU.subtract), reads=[r_cols, r_GR], writes=[r_cols])
                b.op("dve", lambda e: e.scalar_tensor_tensor(
                    out=dst, in0=dst, scalar=jf[:, 0:1], in1=sv[:, :, 0, :], op0=ALU.mult, op1=ALU.add),
                    reads=[r_cols, r_GR, r_c], writes=[r_cols])
            if whole:
                for dst_, src_ in ((acolo, acol[:]), (ecolo, ecol[:]), (GRo, GR[:, 0:NG, :])):
                    b.op("dve", lambda e, dst_=dst_, src_=src_: e.tensor_copy(out=dst_[:], in_=src_),
                         reads=[r_cols, r_GR], writes=[r_cols])
            else:
                blend(acolo[:], acol[:], 4)
                blend(ecolo[:], ecol[:], 4)
                blend(GRo[:], GR[:, 0:NG, :], 4)
            b.barrier()
            b.es = es

        with ExitStack() as es2:
            b.es = es2
            pre = b.sb("c2_pre", [128, 4, NTK + 3], BF16)
            acc = [b.sb("c2_acc%d" % i, [128, NTK], F32) for i in range(2)]
            cw = b.sb("c2_cw", [128, 4, 4], F32)
            cb = b.sb("c2_cb", [128, 4], F32)
            r_pre, r_cw = b.res(), b.res()
            r_acc = [b.res(), b.res()]
            b.dma("sp", cw[:], convw_d[l], writes=[r_cw])
            b.dma("sp", cb[:], convb_d[l], writes=[r_cw])
            b.op("pool", lambda e: e.memset(pre[:, :, 0:3], 0.0), writes=[r_pre])
            for g in range(NG):
                gi = gidx(g)
                b.dma("sp", pre[:, :, 3 + g * 128:3 + (g + 1) * 128], featT_all[gi, :, 15:19, :],
                      reads=[r_in], writes=[r_pre])
            for c in range(4):
                a = acc[c % 2]
                ra = r_acc[c % 2]
                b.op("dve", lambda e, c=c, a=a: e.tensor_scalar(
                    out=a[:], in0=pre[:, c, 0:NTK], scalar1=cw[:, c, 0:1], scalar2=cb[:, c:c + 1],
                    op0=ALU.mult, op1=ALU.add), reads=[r_pre, r_cw], writes=[ra])
                for j in range(1, 4):
                    b.op("dve", lambda e, c=c, a=a, j=j: e.scalar_tensor_tensor(
                        out=a[:], in0=pre[:, c, j:j + NTK], scalar=cw[:, c, j:j + 1], in1=a[:],
                        op0=ALU.mult, op1=ALU.add), reads=[r_pre, r_cw, ra], writes=[ra])
                b.op("act", lambda e, c=c, a=a: e.activation(out=qkall[:, c, :], in_=a[:], func=AF.Silu),
                     reads=[ra], writes=[r_qkall])
            if not whole:
                for c in range(4):
                    a = acc[c % 2]
                    ra = r_acc[c % 2]
                    v = qkall[:, c, :].rearrange("p (m r n) -> p m r n", r=2, n=128)
                    av = a[:, 0:nslots * 128].rearrange("p (m n) -> p m n", n=128)
                    b.op("dve", lambda e, v=v, av=av: e.tensor_tensor(out=av, in0=v[:, :, 1, :], in1=v[:, :, 0, :],
                                                                      op=ALU.subtract),
                         reads=[r_qkall, ra], writes=[ra])
                    b.op("dve", lambda e, v=v, av=av, c=c: e.scalar_tensor_tensor(
                        out=qkown[:, c, :].rearrange("p (m n) -> p m n", n=128), in0=av, scalar=jf[:, 0:1],
                        in1=v[:, :, 0, :], op0=ALU.mult, op1=ALU.add),
                        reads=[ra, r_qkall, r_c], writes=[r_qkown])
            b.barrier()
            b.es = es

        Cst = b.sb("c_Cst", [128, 2, 65], F32)
        CA = b.sb("c_CA", [128, 2, 65], F32)
        Cd = b.sb("c_Cd", [128, 2, 65], F32)
        Cbf = b.sb("c_Cbf", [128, 2, 65], BF16)
        vin = [b.sb("c_vin%d" % i, [128, 256], BF16) for i in range(2)]
        vk = [b.sb("c_vk%d" % i, [128, 4, 65], BF16) for i in range(2)]
        ktok = [b.sb("c_ktok%d" % i, [128, 256], BF16) for i in range(2)]
        ownv = [b.sb("c_ownv%d" % i, [128, 512], BF16) for i in range(2)]
        vaug = [b.sb("c_vaug%d" % i, [128, 4, 65], BF16) for i in range(2)]
        ngc = [b.sb("c_ngc%d" % i, [128, 128], F32) for i in range(2)]
        WT = [b.sb("c_WT%d" % i, [128, 128], F32) for i in range(2)]
        DT = [b.sb("c_DT%d" % i, [128, 128], BF16) for i in range(2)]
        igq = [b.sb("c_igq%d" % i, [128, 128], F32) for i in range(2)]
        qs = [b.sb("c_qs%d" % i, [128, 128], BF16) for i in range(2)]
        nd = b.sb("c_nd", [128, 4, 65], F32)
        hsb = b.sb("c_hsb", [128, 4, 64], F32)
        tmp = b.sb("c_tmp", [128, 4, 64], F32)
        st = b.sb("c_st", [128, 16], F32)
        dn = b.sb("c_dn", [128, 8], F32)
        osb = [b.sb("c_o%d" % i, [128, 256], BF16) for i in range(2)]
        pKt = b.ps("c_pKt", [128, 256], BF16)
        pKV = [b.ps("c_pKV%d" % i, [128, 130]) for i in range(2)]
        pG = [b.ps("c_pG%d" % i, [128, 128]) for i in range(2)]
        pQK = [b.ps("c_pQK%d" % i, [128, 128]) for i in range(2)]
        pN = b.ps("c_pN", [128, 260])
        (r_Cst, r_CA, r_Cd, r_Cbf, r_nd, r_hsb, r_tmp, r_st, r_dn, r_pKt, r_pN) = (b.res() for _ in range(11))
        r_vin = [b.res(), b.res()]
        r_vk = [b.res(), b.res()]
        r_ktok = [b.res(), b.res()]
        r_ownv = [b.res(), b.res()]
        r_vaug = [b.res(), b.res()]
        r_ngc = [b.res(), b.res()]
        r_WT = [b.res(), b.res()]
        r_DT = [b.res(), b.res()]
        r_igq = [b.res(), b.res()]
        r_qs = [b.res(), b.res()]
        r_osb = [b.res(), b.res()]
        r_pKV = [b.res(), b.res()]
        r_pG = [b.res(), b.res()]
        r_pQK = [b.res(), b.res()]

        b.op("dve", lambda e: e.memset(Cst[:], 0.0), writes=[r_Cst])
        hi = 0
        for g in range(NG):
            m, r = g // 2, g % 2
            s = g % 2
            gi = gidx(g)
            b.dma("sp", vin[s][:], tokA_all[gi * 128:(gi + 1) * 128, 1280:1536], reads=[r_in], writes=[r_vin[s]])
            if whole:
                so = g % 2
                mm = g
                orow = gi * 128
                b.dma("sp", ownv[so][:], tokA_all[gi * 128:(gi + 1) * 128, 1280:1792], reads=[r_in],
                      writes=[r_ownv[so]])
                b.op("pool", lambda e, so=so: e.memset(vaug[so][:, :, 64:65], 1.0), writes=[r_vaug[so]])
                b.op("pool", lambda e, so=so: e.tensor_copy(
                    out=vaug[so][:, :, 0:64], in_=ownv[so][:, 0:256].rearrange("p (h d) -> p h d", d=64)),
                    reads=[r_ownv[so]], writes=[r_vaug[so]])
                b.op("dve", lambda e: e.tensor_copy(out=Cbf[:], in_=Cst[:]), reads=[r_Cst], writes=[r_Cbf])
            elif r == 0:
                so = m % 2
                b.op("dve", lambda e: e.tensor_copy(out=CA[:], in_=Cst[:]), reads=[r_Cst], writes=[r_CA])
                b.dma("sp", ownv[so][:], tokA_own[m * 128:(m + 1) * 128, 1280:1792], reads=[r_in],
                      writes=[r_ownv[so]])
                b.op("pool", lambda e, so=so: e.memset(vaug[so][:, :, 64:65], 1.0), writes=[r_vaug[so]])
                b.op("pool", lambda e, so=so: e.tensor_copy(
                    out=vaug[so][:, :, 0:64], in_=ownv[so][:, 0:256].rearrange("p (h d) -> p h d", d=64)),
                    reads=[r_ownv[so]], writes=[r_vaug[so]])
            else:
                so = m % 2
                mm = m
                orow = m * 128
                b.op("dve", lambda e: e.tensor_tensor(out=Cd[:], in0=Cst[:], in1=CA[:], op=ALU.subtract),
                     reads=[r_Cst, r_CA], writes=[r_Cd])
                b.op("dve", lambda e: e.scalar_tensor_tensor(
                    out=Cbf[:].rearrange("p a e -> p (a e)"), in0=Cd[:].rearrange("p a e -> p (a e)"),
                    scalar=jf[:, 0:1], in1=CA[:].rearrange("p a e -> p (a e)"), op0=ALU.mult, op1=ALU.add),
                    reads=[r_Cd, r_CA, r_c], writes=[r_Cbf])
            if whole or r == 1:
                m = mm
                for h in range(4):
                    hs = slice((h % 2) * 64, (h % 2) * 64 + 64)
                    p = h // 2
                    x = hi % 2
                    hi += 1
                    b.op("pe", lambda e, x=x, h=h, m=m: e.matmul(
                        pG[x][:, :], lhsT=sel4[:, h, :], rhs=GTo[:, m, :], start=True, stop=True),
                        reads=[r_c, r_GTo], writes=[r_pG[x]])
                    b.op("dve", lambda e, x=x: e.tensor_tensor(out=ngc[x][:], in0=caus[:], in1=pG[x][:],
                                                               op=ALU.subtract),
                         reads=[r_c, r_pG[x]], writes=[r_ngc[x]])
                    b.op("act", lambda e, x=x, m=m, h=h: e.activation(
                        out=WT[x][:], in_=ngc[x][:], func=AF.Exp, bias=acolo[:, m, h:h + 1]),
                        reads=[r_ngc[x], r_cols], writes=[r_WT[x]])
                    b.op("pe", lambda e, x=x, hs=hs, p=p, m=m: e.matmul(
                        pQK[x][:, :], lhsT=qkown[hs, 2 + p, m * 128:(m + 1) * 128],
                        rhs=qkown[hs, p, m * 128:(m + 1) * 128], start=True, stop=True),
                        reads=[r_qkown], writes=[r_pQK[x]])
                    b.op("dve", lambda e, x=x: e.scalar_tensor_tensor(
                        out=DT[x][:], in0=pQK[x][:], scalar=0.125, in1=WT[x][:], op0=ALU.mult, op1=ALU.mult),
                        reads=[r_pQK[x], r_WT[x]], writes=[r_DT[x]])
                    b.op("act", lambda e, x=x, hs=hs, m=m, h=h: e.activation(
                        out=igq[x][hs, :], in_=pG[x][hs, :], func=AF.Exp, scale=-1.0, bias=GRo[hs, m, h:h + 1]),
                        reads=[r_pG[x], r_cols], writes=[r_igq[x]])
                    b.op("dve", lambda e, x=x, hs=hs, p=p, m=m: e.tensor_tensor(
                        out=qs[x][hs, :], in0=qkown[hs, p, m * 128:(m + 1) * 128], in1=igq[x][hs, :], op=ALU.mult),
                        reads=[r_qkown, r_igq[x]], writes=[r_qs[x]])
                    b.op("pe", lambda e, x=x, h=h, so=so: e.matmul(
                        pN[:, h * 65:(h + 1) * 65], lhsT=DT[x][:], rhs=vaug[so][:, h, :], start=True, stop=False),
                        reads=[r_DT[x], r_vaug[so]], writes=[r_pN], inc=False)
                    b.op("pe", lambda e, x=x, h=h, hs=hs, p=p: e.matmul(
                        pN[:, h * 65:(h + 1) * 65], lhsT=qs[x][hs, :], rhs=Cbf[hs, p, :], start=False, stop=True),
                        reads=[r_qs[x], r_Cbf], writes=[r_pN])
                b.op("act", lambda e: e.copy(out=nd[:].rearrange("p h d -> p (h d)"), in_=pN[:]),
                     reads=[r_pN], writes=[r_nd])
                b.op("dve", lambda e: e.tensor_scalar(
                    out=dn[:, 0:4].unsqueeze(2), in0=nd[:, :, 64:65], scalar1=-1.0, scalar2=None, op0=ALU.mult),
                    reads=[r_nd], writes=[r_dn])
                b.op("dve", lambda e: e.tensor_tensor(
                    out=dn[:, 0:4].unsqueeze(2), in0=dn[:, 0:4].unsqueeze(2), in1=nd[:, :, 64:65], op=ALU.max),
                    reads=[r_nd, r_dn], writes=[r_dn])
                b.op("dve", lambda e, m=m: e.tensor_tensor(out=dn[:, 0:4], in0=dn[:, 0:4], in1=ecolo[:, m, :],
                                                           op=ALU.max),
                     reads=[r_dn, r_cols], writes=[r_dn])
                b.op("dve", lambda e: e.reciprocal(out=dn[:, 4:8], in_=dn[:, 0:4]), reads=[r_dn], writes=[r_dn])
                b.op("dve", lambda e: e.tensor_tensor(
                    out=tmp[:], in0=nd[:, :, 0:64], in1=dn[:, 4:8].unsqueeze(2).to_broadcast([128, 4, 64]),
                    op=ALU.mult), reads=[r_nd, r_dn], writes=[r_tmp])
                b.op("dve", lambda e, so=so: e.tensor_tensor(
                    out=hsb[:], in0=tmp[:], in1=ownv[so][:, 256:512].rearrange("p (h d) -> p h d", d=64),
                    op=ALU.mult), reads=[r_tmp, r_ownv[so]], writes=[r_hsb])
                emit_head_norm(b, hsb, r_hsb, None, None, osb[so][:], r_osb[so], tmp, st, r_tmp, r_st)
                b.dma("pool", o_d[orow:orow + 128, 768:1024], osb[so][:], reads=[r_osb[so]], writes=[r_out])
            if g == NG - 1:
                break
            b.op("dve", lambda e, s=s, g=g: e.tensor_tensor(
                out=vk[s][:, :, 0:64], in0=vin[s][:].rearrange("p (h d) -> p h d", d=64),
                in1=kw[:, g, :].unsqueeze(2).to_broadcast([128, 4, 64]), op=ALU.mult),
                reads=[r_vin[s], r_cols], writes=[r_vk[s]])
            b.op("dve", lambda e, s=s, g=g: e.tensor_copy(out=vk[s][:, :, 64:65], in_=kw[:, g, :].unsqueeze(2)),
                 reads=[r_cols], writes=[r_vk[s]])
            for p in range(2):
                b.op("pe", lambda e, p=p, g=g: e.transpose(
                    out=pKt[:, p * 128:(p + 1) * 128], in_=qkall[:, 2 + p, g * 128:(g + 1) * 128],
                    identity=identb[:]), reads=[r_qkall, r_c], writes=[r_pKt], inc=(p == 1))
            b.op("act", lambda e, s=s: e.copy(out=ktok[s][:], in_=pKt[:]), reads=[r_pKt], writes=[r_ktok[s]])
            for p in range(2):
                b.op("pe", lambda e, s=s, p=p: e.matmul(
                    pKV[p][:, :], lhsT=ktok[s][:, p * 128:(p + 1) * 128],
                    rhs=vk[s][:, 2 * p:2 * p + 2, :].rearrange("p a e -> p (a e)"), start=True, stop=True),
                    reads=[r_ktok[s], r_vk[s]], writes=[r_pKV[p]])
                for half in range(2):
                    h = 2 * p + half
                    hs = slice(half * 64, half * 64 + 64)
                    b.op("dve", lambda e, p=p, hs=hs, half=half, h=h, g=g: e.scalar_tensor_tensor(
                        out=Cst[hs, p, :], in0=Cst[hs, p, :], scalar=dec[hs, g, h:h + 1],
                        in1=pKV[p][hs, half * 65:(half + 1) * 65], op0=ALU.mult, op1=ALU.add),
                        reads=[r_Cst, r_pKV[p], r_cols], writes=[r_Cst])
        b.barrier()
        b.es = old


def mlstm_consts():
    i = np.arange(128)
    caus = np.where(i[:, None] <= i[None, :], 0.0, NEG).astype(np.float32)
    sel4 = np.zeros((4, 4, 128), np.float32)
    for h in range(4):
        sel4[h, h, :] = 1.0
    return caus, sel4.reshape(4, 512), np.eye(4, dtype=np.float32)


def build_p2bc(l, nslots):
    nc = bass.Bass("TRN2", target_bir_lowering=False)
    featT_all = nc.dram_tensor("featT_all", [2 * NT, 128, NFT, 128], BF16, kind="ExternalInput").ap()
    tokA_all = nc.dram_tensor("tokA_all", [2 * TOK, 1792], BF16, kind="ExternalInput").ap()
    gT_all = nc.dram_tensor("gT_all", [2 * NT, 8, 128], F32, kind="ExternalInput").ap()
    featT_own = nc.dram_tensor("featT_own", [NT, 128, NFT, 128], BF16, kind="ExternalInput").ap()
    tokA_own = nc.dram_tensor("tokA_own", [TOK, 1792], BF16, kind="ExternalInput").ap()
    decT_d = nc.dram_tensor("decT", [128, 4, 128], F32, kind="ExternalInput").ap()
    qdecT_d = nc.dram_tensor("qdecT", [128, 2, 128], F32, kind="ExternalInput").ap()
    kdec_d = nc.dram_tensor("kdec", [128, 4], F32, kind="ExternalInput").ap()
    jf_d = nc.dram_tensor("jf", [128, 1], F32, kind="ExternalInput").ap()
    convw_d = nc.dram_tensor("convw", [DEPTH, 128, 4, 4], F32, kind="ExternalInput").ap()
    convb_d = nc.dram_tensor("convb", [DEPTH, 128, 4], F32, kind="ExternalInput").ap()
    caus_d = nc.dram_tensor("caus", [128, 128], F32, kind="ExternalInput").ap()
    sel4_d = nc.dram_tensor("sel4", [4, 512], F32, kind="ExternalInput").ap()
    eye4_d = nc.dram_tensor("eye4", [4, 4], F32, kind="ExternalInput").ap()
    identf_d = nc.dram_tensor("identf", [128, 128], F32, kind="ExternalInput").ap()
    o_d = nc.dram_tensor("o", [TOK, D], BF16, kind="ExternalOutput").ap()
    with ExitStack() as es, nc.allow_low_precision("bf16 matmul operands, fp32 accumulation"):
        b = B(nc, es)
        r_in, r_out = b.res(), b.res()
        emit_p2b(b, nslots, tokA_all, featT_own, tokA_own, decT_d, qdecT_d, kdec_d, jf_d, o_d, r_in, r_out)
        emit_p2c(b, l, nslots, featT_all, tokA_all, gT_all, tokA_own, convw_d, convb_d, caus_d, sel4_d, eye4_d,
                 jf_d, identf_d, o_d, r_in, r_out)
        b.finish()
    return nc


def build_p2bc_whole(l, nslots):
    nc = bass.Bass("TRN2", target_bir_lowering=False)
    featT_all = nc.dram_tensor("featT_all", [2 * NT, 128, NFT, 128], BF16, kind="ExternalInput").ap()
    tokA_all = nc.dram_tensor("tokA_all", [2 * TOK, 1792], BF16, kind="ExternalInput").ap()
    gT_all = nc.dram_tensor("gT_all", [2 * NT, 8, 128], F32, kind="ExternalInput").ap()
    decT_d = nc.dram_tensor("decT", [128, 4, 128], F32, kind="ExternalInput").ap()
    qdecT_d = nc.dram_tensor("qdecT", [128, 2, 128], F32, kind="ExternalInput").ap()
    kdec_d = nc.dram_tensor("kdec", [128, 4], F32, kind="ExternalInput").ap()
    jf_d = nc.dram_tensor("jf", [128, 1], F32, kind="ExternalInput").ap()
    convw_d = nc.dram_tensor("convw", [DEPTH, 128, 4, 4], F32, kind="ExternalInput").ap()
    convb_d = nc.dram_tensor("convb", [DEPTH, 128, 4], F32, kind="ExternalInput").ap()
    caus_d = nc.dram_tensor("caus", [128, 128], F32, kind="ExternalInput").ap()
    sel4_d = nc.dram_tensor("sel4", [4, 512], F32, kind="ExternalInput").ap()
    eye4_d = nc.dram_tensor("eye4", [4, 4], F32, kind="ExternalInput").ap()
    identf_d = nc.dram_tensor("identf", [128, 128], F32, kind="ExternalInput").ap()
    o_d = nc.dram_tensor("o", [2 * TOK, D], BF16, kind="ExternalOutput").ap()
    with ExitStack() as es, nc.allow_low_precision("bf16 matmul operands, fp32 accumulation"):
        b = B(nc, es)
        r_in, r_out = b.res(), b.res()
        emit_p2b(b, nslots, tokA_all, featT_all, None, decT_d, qdecT_d, kdec_d, jf_d, o_d, r_in, r_out, whole=True)
        emit_p2c(b, l, nslots, featT_all, tokA_all, gT_all, None, convw_d, convb_d, caus_d, sel4_d, eye4_d,
                 jf_d, identf_d, o_d, r_in, r_out, whole=True)
        b.finish()
    return nc


def conv_layouts(conv_w, conv_b):
    cw = np.ascontiguousarray(conv_w.reshape(DEPTH, 4, 4, 128).transpose(0, 3, 2, 1))
    cb = np.ascontiguousarray(conv_b.reshape(DEPTH, 4, 128).transpose(0, 2, 1))
    return cw, cb


def emit_ln_stats(b, z, r_z, junk, r_junk, st, r_st):
    b.op("act", lambda e: e.activation(out=junk[:], in_=z, func=AF.Identity, accum_out=st[:, 2:3]),
         reads=[r_z], writes=[r_junk, r_st])
    b.op("act", lambda e: e.activation(out=junk[:], in_=z, func=AF.Square, accum_out=st[:, 3:4]),
         reads=[r_z], writes=[r_junk, r_st])
    b.op("dve", lambda e: e.tensor_scalar(out=st[:, 0:1], in0=st[:, 2:3], scalar1=1.0 / D, scalar2=None,
                                          op0=ALU.mult), reads=[r_st], writes=[r_st])
    b.op("dve", lambda e: e.tensor_tensor(out=st[:, 4:5], in0=st[:, 0:1], in1=st[:, 0:1], op=ALU.mult),
         reads=[r_st], writes=[r_st])
    b.op("dve", lambda e: e.scalar_tensor_tensor(out=st[:, 5:6], in0=st[:, 3:4], scalar=1.0 / D, in1=st[:, 4:5],
                                                 op0=ALU.mult, op1=ALU.subtract), reads=[r_st], writes=[r_st])
    b.op("dve", lambda e: e.tensor_scalar(out=st[:, 5:6], in0=st[:, 5:6], scalar1=LN_EPS, scalar2=None,
                                          op0=ALU.add), reads=[r_st], writes=[r_st])
    b.op("act", lambda e: e.activation(out=st[:, 6:7], in_=st[:, 5:6], func=AF.Sqrt), reads=[r_st], writes=[r_st])
    b.op("dve", lambda e: e.reciprocal(out=st[:, 1:2], in_=st[:, 6:7]), reads=[r_st], writes=[r_st])


def emit_p3(b, l, nt, x_d, o_d, wout_d, modrow_d, modcol_d, lnmg_d, lnmb_d, wr_d, br_d, wg_d, wu_d, wd_d,
            lnfg_d, lnfb_d, identf_d, xout_d, r_in, r_out, n_exp=16):
    ntok = nt * 128
    ngrp = nt // 4
    with ExitStack() as es:
        old = b.es
        b.es = es
        xacc = b.sb("p3_xacc", [128, nt, D], F32)
        h2T = b.sb("p3_h2T", [128, 8, ntok], BF16)
        gate = b.sb("p3_gate", [128, nt, 16], F32)
        lng = b.sb("p3_lng", [128, D], F32)
        lnb = b.sb("p3_lnb", [128, D], F32)
        gbc = b.sb("p3_gbc", [128, D], F32)
        junk = b.sb("p3_junk", [128, D], F32)
        st = b.sb("p3_st", [128, 8], F32)
        identf = b.sb("p3_idf", [128, 128], F32)
        identb = b.sb("p3_idb", [128, 128], BF16)
        r_xacc, r_h2T, r_gate, r_ln, r_gbc, r_junk, r_st, r_id = (b.res() for _ in range(8))
        b.dma("sp", identf[:], identf_d, writes=[r_id])
        b.op("dve", lambda e: e.tensor_copy(out=identb[:], in_=identf[:]), reads=[r_id], writes=[r_id])

        with ExitStack() as esa:
            b.es = esa
            wob = b.sb("p3a_wob", [128, 8, D], BF16)
            wst = [b.sb("p3a_wst%d" % i, [128, 8, 256], F32) for i in range(2)]
            mcol = b.sb("p3a_mcol", [128, 48], F32)
            sc1 = b.sb("p3a_sc1", [128, 8], F32)
            wr = b.sb("p3a_wr", [128, 8, 16], F32)
            brow = b.sb("p3a_brow", [128, 16], F32)
            ot = [b.sb("p3a_ot%d" % i, [128, D], BF16) for i in range(2)]
            oT = [b.sb("p3a_oT%d" % i, [128, 8, 128], BF16) for i in range(2)]
            xt = [b.sb("p3a_xt%d" % i, [128, D], F32) for i in range(2)]
            z = b.sb("p3a_z", [128, D], F32)
            x1 = b.sb("p3a_x1", [128, D], F32)
            h2f = b.sb("p3a_h2f", [128, 8, 128], F32)
            rt = b.sb("p3a_rt", [128, 160], F32)
            pOT = b.ps("p3a_pOT", [128, 1024], BF16)
            pM = [b.ps("p3a_pM%d" % i, [128, 512]) for i in range(2)]
            pX = [b.ps("p3a_pX%d" % i, [128, 512]) for i in range(2)]
            pR = b.ps("p3a_pR", [128, 16])
            r_wob, r_mc, r_wr, r_z, r_x1, r_h2f, r_rt, r_pOT, r_pR = (b.res() for _ in range(9))
            r_wst = [b.res(), b.res()]
            r_ot = [b.res(), b.res()]
            r_oT = [b.res(), b.res()]
            r_xt = [b.res(), b.res()]
            r_pM = [b.res(), b.res()]
            r_pX = [b.res(), b.res()]

            b.dma("sp", mcol[:], modcol_d[:, l * 48:(l + 1) * 48], writes=[r_mc])
            b.op("dve", lambda e: e.tensor_scalar(out=sc1[:], in0=mcol[:, 32:40], scalar1=1.0, scalar2=None,
                                                  op0=ALU.add), reads=[r_mc], writes=[r_mc])
            b.dma("sp", wr[:], wr_d.rearrange("(k p) n -> p k n", p=128), writes=[r_wr])
            b.dma("sp", brow[:], br_d.to_broadcast([128, 16]), writes=[r_wr])
            b.dma("sp", lng[:], lnmg_d[l:l + 1, :].to_broadcast([128, D]), writes=[r_ln])
            b.dma("sp", lnb[:], lnmb_d[l:l + 1, :].to_broadcast([128, D]), writes=[r_ln])
            b.dma("sp", gbc[:], modrow_d[l:l + 1, 2048:3072].to_broadcast([128, D]), writes=[r_gbc])
            b.op("dve", lambda e: e.tensor_scalar(out=gbc[:], in0=gbc[:], scalar1=1.0, scalar2=None, op0=ALU.add),
                 reads=[r_gbc], writes=[r_gbc])
            for i4 in range(4):
                i = i4 % 2
                b.dma("sp", wst[i][:], wout_d[l, :, i4 * 256:(i4 + 1) * 256].rearrange("(k p) n -> p k n", p=128),
                      writes=[r_wst[i]])
                b.op("pool", lambda e, i=i, i4=i4: e.tensor_tensor(
                    out=wob[:, :, i4 * 256:(i4 + 1) * 256], in0=wst[i][:],
                    in1=gbc[:, i4 * 256:(i4 + 1) * 256].unsqueeze(1).to_broadcast([128, 8, 256]), op=ALU.mult),
                    reads=[r_wst[i], r_gbc], writes=[r_wob])
            zs = [z, b.sb("p3a_z2", [128, D], F32)]
            x1s = [x1, b.sb("p3a_x12", [128, D], F32)]
            h2fs = [h2f, b.sb("p3a_h2f2", [128, 8, 128], F32)]
            rts = [rt, b.sb("p3a_rt2", [128, 160], F32)]
            sts = [st, b.sb("p3a_st2", [128, 8], F32)]
            junks = [junk, junk]
            r_zs = [r_z, b.res()]
            r_x1s = [r_x1, b.res()]
            r_h2fs = [r_h2f, b.res()]
            r_rts = [r_rt, b.res()]
            r_sts = [r_st, b.res()]
            r_junks = [r_junk, r_junk]

            def stage_A(t):
                s = t % 2
                z_, r_z_ = zs[s], r_zs[s]
                b.dma("sp", ot[s][:], o_d[t * 128:(t + 1) * 128, :], reads=[r_in], writes=[r_ot[s]])
                b.dma("sp", xt[s][:], x_d[t * 128:(t + 1) * 128, :], reads=[r_in], writes=[r_xt[s]])
                for c in range(8):
                    b.op("pe", lambda e, s=s, c=c: e.transpose(
                        out=pOT[:, c * 128:(c + 1) * 128], in_=ot[s][:, c * 128:(c + 1) * 128], identity=identb[:]),
                        reads=[r_ot[s], r_id], writes=[r_pOT], inc=(c == 7))
                b.op("act", lambda e, s=s: e.copy(out=oT[s][:].rearrange("p k n -> p (k n)"), in_=pOT[:]),
                     reads=[r_pOT], writes=[r_oT[s]])
                for nb in range(2):
                    for k in range(8):
                        b.op("pe", lambda e, s=s, k=k, nb=nb: e.matmul(
                            pM[nb][:, :], lhsT=oT[s][:, k, :], rhs=wob[:, k, nb * 512:(nb + 1) * 512],
                            start=(k == 0), stop=(k == 7)),
                            reads=[r_oT[s], r_wob], writes=[r_pM[nb]], inc=(k == 7))
                    b.op("dve", lambda e, s=s, nb=nb: e.scalar_tensor_tensor(
                        out=z_[:, nb * 512:(nb + 1) * 512], in0=xt[s][:, nb * 512:(nb + 1) * 512], scalar=ALPHA,
                        in1=pM[nb][:, :], op0=ALU.mult, op1=ALU.add),
                        reads=[r_xt[s], r_pM[nb]], writes=[r_z_])
                yield

            def stage_B(t):
                s = t % 2
                z_, r_z_ = zs[s], r_zs[s]
                x1_, r_x1_ = x1s[s], r_x1s[s]
                h2f_, r_h2f_ = h2fs[s], r_h2fs[s]
                rt_, r_rt_ = rts[s], r_rts[s]
                st_, r_st_ = sts[s], r_sts[s]
                junk_, r_junk_ = junks[s], r_junks[s]
                emit_ln_stats(b, z_[:], r_z_, junk_, r_junk_, st_, r_st_)
                b.op("dve", lambda e: e.tensor_scalar(out=x1_[:], in0=z_[:], scalar1=st_[:, 0:1], scalar2=st_[:, 1:2],
                                                      op0=ALU.subtract, op1=ALU.mult),
                     reads=[r_z_, r_st_], writes=[r_x1_])
                b.op("pool", lambda e: e.tensor_tensor(out=x1_[:], in0=x1_[:], in1=lng[:], op=ALU.mult),
                     reads=[r_x1_, r_ln], writes=[r_x1_])
                b.op("pool", lambda e: e.tensor_tensor(out=x1_[:], in0=x1_[:], in1=lnb[:], op=ALU.add),
                     reads=[r_x1_, r_ln], writes=[r_x1_])
                for c in range(8):
                    b.op("pe", lambda e, c=c: e.transpose(
                        out=pX[c // 4][:, (c % 4) * 128:(c % 4 + 1) * 128], in_=x1_[:, c * 128:(c + 1) * 128],
                        identity=identf[:]), reads=[r_x1_, r_id], writes=[r_pX[c // 4]], inc=(c % 4 == 3))
                for c in range(8):
                    b.op("act", lambda e, c=c: e.activation(
                        out=h2f_[:, c, :], in_=pX[c // 4][:, (c % 4) * 128:(c % 4 + 1) * 128], func=AF.Identity,
                        scale=sc1[:, c:c + 1], bias=mcol[:, 24 + c:25 + c]),
                        reads=[r_pX[c // 4], r_mc], writes=[r_h2f_])
                b.op("pool", lambda e, t=t: e.tensor_copy(out=h2T[:, :, t * 128:(t + 1) * 128], in_=h2f_[:]),
                     reads=[r_h2f_], writes=[r_h2T])
                b.op("act", lambda e, t=t: e.mul(out=xacc[:, t, :], in_=x1_[:], mul=ALPHA),
                     reads=[r_x1_], writes=[r_xacc])
                yield

            def stage_B2(t):
                s = t % 2
                h2f_, r_h2f_ = h2fs[s], r_h2fs[s]
                rt_, r_rt_ = rts[s], r_rts[s]
                for k in range(8):
                    b.op("pe", lambda e, k=k: e.matmul(pR[:, :], lhsT=h2f_[:, k, :], rhs=wr[:, k, :],
                                                       start=(k == 0), stop=(k == 7)),
                         reads=[r_h2f_, r_wr], writes=[r_pR], inc=(k == 7))
                S_, BS, EQ1, MSK, EQ2, WT_ = 0, 16, 32, 48, 64, 80
                M1, M2, GS, GM, GSEL, TOT, RT = 96, 100, 104, 108, 112, 116, 117

                def v3(o):
                    return rt_[:, o:o + 16].rearrange("p (g e) -> p g e", e=4)

                def bc(o):
                    return rt_[:, o:o + 4].unsqueeze(2).to_broadcast([128, 4, 4])

                def R(fn):
                    b.op("dve", fn, reads=[r_rt_, r_wr], writes=[r_rt_])

                b.op("act", lambda e: e.activation(out=rt_[:, S_:S_ + 16], in_=pR[:, :], func=AF.Sigmoid),
                     reads=[r_pR], writes=[r_rt_])
                R(lambda e: e.tensor_tensor(out=rt_[:, BS:BS + 16], in0=rt_[:, S_:S_ + 16], in1=brow[:], op=ALU.add))
                R(lambda e: e.tensor_reduce(out=rt_[:, M1:M1 + 4], in_=v3(BS), axis=AX.X, op=ALU.max))
                R(lambda e: e.tensor_tensor(out=v3(EQ1), in0=v3(BS), in1=bc(M1), op=ALU.is_equal))
                R(lambda e: e.scalar_tensor_tensor(out=rt_[:, MSK:MSK + 16], in0=rt_[:, EQ1:EQ1 + 16], scalar=-1.0e9,
                                                   in1=rt_[:, BS:BS + 16], op0=ALU.mult, op1=ALU.add))
                R(lambda e: e.tensor_reduce(out=rt_[:, M2:M2 + 4], in_=v3(MSK), axis=AX.X, op=ALU.max))
                R(lambda e: e.tensor_tensor(out=rt_[:, GS:GS + 4], in0=rt_[:, M1:M1 + 4], in1=rt_[:, M2:M2 + 4],
                                            op=ALU.add))
                R(lambda e: e.tensor_reduce(out=rt_[:, GM:GM + 1], in_=rt_[:, GS:GS + 4], axis=AX.X, op=ALU.max))
                R(lambda e: e.tensor_scalar(out=rt_[:, GSEL:GSEL + 4], in0=rt_[:, GS:GS + 4], scalar1=rt_[:, GM:GM + 1],
                                            scalar2=None, op0=ALU.is_equal))
                R(lambda e: e.tensor_tensor(out=v3(EQ2), in0=v3(MSK), in1=bc(M2), op=ALU.is_equal))
                R(lambda e: e.tensor_tensor(out=rt_[:, EQ2:EQ2 + 16], in0=rt_[:, EQ2:EQ2 + 16], in1=rt_[:, EQ1:EQ1 + 16],
                                            op=ALU.add))
                R(lambda e: e.tensor_tensor(out=v3(EQ2), in0=v3(EQ2), in1=bc(GSEL), op=ALU.mult))
                R(lambda e: e.tensor_tensor(out=rt_[:, WT_:WT_ + 16], in0=rt_[:, EQ2:EQ2 + 16], in1=rt_[:, S_:S_ + 16],
                                            op=ALU.mult))
                R(lambda e: e.tensor_reduce(out=rt_[:, TOT:TOT + 1], in_=rt_[:, WT_:WT_ + 16], axis=AX.X, op=ALU.add))
                R(lambda e: e.reciprocal(out=rt_[:, RT:RT + 1], in_=rt_[:, TOT:TOT + 1]))
                b.op("dve", lambda e, t=t: e.tensor_scalar(out=gate[:, t, :], in0=rt_[:, WT_:WT_ + 16],
                                                           scalar1=rt_[:, RT:RT + 1], scalar2=None, op0=ALU.mult),
                     reads=[r_rt_], writes=[r_gate])
                yield

            for _ in stage_A(0):
                pass
            for t in range(nt + 1):
                if t + 1 < nt:
                    for _ in stage_A(t + 1):
                        pass
                if t < nt:
                    for _ in stage_B(t):
                        pass
                if t >= 1:
                    for _ in stage_B2(t - 1):
                        pass
            b.barrier()
            b.es = es

        with ExitStack() as esb:
            b.es = esb
            b.dma("sp", gbc[:], modrow_d[l:l + 1, 5120:6144].to_broadcast([128, D]), writes=[r_gbc])
            b.op("dve", lambda e: e.tensor_scalar(out=gbc[:], in0=gbc[:], scalar1=1.0, scalar2=None, op0=ALU.add),
                 reads=[r_gbc], writes=[r_gbc])
            b.dma("sp", lng[:], lnfg_d[l:l + 1, :].to_broadcast([128, D]), writes=[r_ln])
            b.dma("sp", lnb[:], lnfb_d[l:l + 1, :].to_broadcast([128, D]), writes=[r_ln])
            sgu = [b.sb("p3b_sgu%d" % i, [128, 8, 256], F32) for i in range(2)]
            sd = [b.sb("p3b_sd%d" % i, [128, 2, D], F32) for i in range(2)]
            wg = [b.sb("p3b_wg%d" % i, [128, 8, 256], BF16) for i in range(2)]
            wu = [b.sb("p3b_wu%d" % i, [128, 8, 256], BF16) for i in range(2)]
            wd = [b.sb("p3b_wd%d" % i, [128, 2, D], BF16) for i in range(2)]
            sg = [b.sb("p3b_sg%d" % i, [128, 512], BF16) for i in range(2)]
            hid = [b.sb("p3b_hid%d" % i, [128, 512], BF16) for i in range(4)]
            pg = [b.ps("p3b_pg%d" % i, [128, 512]) for i in range(2)]
            pu = [b.ps("p3b_pu%d" % i, [128, 512]) for i in range(2)]
            py = [b.ps("p3b_py%d" % i, [128, 512]) for i in range(4)]
            r_sgu = [b.res(), b.res()]
            r_sd = [b.res(), b.res()]
            r_wg = [b.res(), b.res()]
            r_wu = [b.res(), b.res()]
            r_wd = [b.res(), b.res()]
            r_sg = [b.res(), b.res()]
            r_hid = [b.res() for _ in range(4)]
            r_pg = [b.res(), b.res()]
            r_pu = [b.res(), b.res()]
            r_py = [b.res() for _ in range(4)]
            sti = 0
            hc = 0
            yc = 0
            pending_down = None
            for ex in range(n_exp):
                w = ex % 2
                si = sti % 2
                sti += 1
                b.dma("sp", sgu[si][:], wg_d[l, ex].rearrange("(k p) f -> p k f", p=128), writes=[r_sgu[si]])
                b.op("pool", lambda e, si=si, w=w: e.tensor_copy(out=wg[w][:], in_=sgu[si][:]),
                     reads=[r_sgu[si]], writes=[r_wg[w]])
                si = sti % 2
                sti += 1
                b.dma("sp", sgu[si][:], wu_d[l, ex].rearrange("(k p) f -> p k f", p=128), writes=[r_sgu[si]])
                b.op("pool", lambda e, si=si, w=w: e.tensor_copy(out=wu[w][:], in_=sgu[si][:]),
                     reads=[r_sgu[si]], writes=[r_wu[w]])
                b.dma("sp", sd[w][:], wd_d[l, ex].rearrange("(k p) n -> p k n", p=128), writes=[r_sd[w]])
                b.op("pool", lambda e, w=w: e.tensor_tensor(
                    out=wd[w][:], in0=sd[w][:], in1=gbc[:].unsqueeze(1).to_broadcast([128, 2, D]), op=ALU.mult),
                    reads=[r_sd[w], r_gbc], writes=[r_wd[w]])
                for tg in range(ngrp):
                    hids = []
                    for fc in range(2):
                        x = fc
                        for k in range(8):
                            b.op("pe", lambda e, w=w, k=k, fc=fc, tg=tg, x=x: e.matmul(
                                pg[x][:, :], lhsT=wg[w][:, k, fc * 128:(fc + 1) * 128],
                                rhs=h2T[:, k, tg * 512:(tg + 1) * 512], start=(k == 0), stop=(k == 7)),
                                reads=[r_wg[w], r_h2T], writes=[r_pg[x]], inc=(k == 7))
                        for k in range(8):
                            b.op("pe", lambda e, w=w, k=k, fc=fc, tg=tg, x=x: e.matmul(
                                pu[x][:, :], lhsT=wu[w][:, k, fc * 128:(fc + 1) * 128],
                                rhs=h2T[:, k, tg * 512:(tg + 1) * 512], start=(k == 0), stop=(k == 7)),
                                reads=[r_wu[w], r_h2T], writes=[r_pu[x]], inc=(k == 7))
                        b.op("act", lambda e, x=x: e.activation(out=sg[x][:], in_=pg[x][:, :], func=AF.Silu),
                             reads=[r_pg[x]], writes=[r_sg[x]])
                        hx = hc % 4
                        hc += 1
                        b.op("dve", lambda e, x=x, hx=hx: e.tensor_tensor(out=hid[hx][:], in0=pu[x][:, :],
                                                                          in1=sg[x][:], op=ALU.mult),
                             reads=[r_pu[x], r_sg[x]], writes=[r_hid[hx]])
                        hids.append(hx)
                    def down_job(tg=tg, w=w, ex=ex, hids=tuple(hids)):
                        nonlocal yc
                        for tt in range(4):
                            t = tg * 4 + tt
                            for nb in range(2):
                                yx = yc % 4
                                yc += 1
                                for fc in range(2):
                                    hx = hids[fc]
                                    b.op("pe", lambda e, hx=hx, tt=tt, w=w, fc=fc, nb=nb, yx=yx: e.matmul(
                                        py[yx][:, :], lhsT=hid[hx][:, tt * 128:(tt + 1) * 128],
                                        rhs=wd[w][:, fc, nb * 512:(nb + 1) * 512], start=(fc == 0), stop=(fc == 1)),
                                        reads=[r_hid[hx], r_wd[w]], writes=[r_py[yx]], inc=(fc == 1))
                                b.op("dve", lambda e, yx=yx, t=t, nb=nb, ex=ex: e.scalar_tensor_tensor(
                                    out=xacc[:, t, nb * 512:(nb + 1) * 512], in0=py[yx][:, :],
                                    scalar=gate[:, t, ex:ex + 1], in1=xacc[:, t, nb * 512:(nb + 1) * 512],
                                    op0=ALU.mult, op1=ALU.add),
                                    reads=[r_py[yx], r_gate, r_xacc], writes=[r_xacc])
                    if pending_down is not None:
                        pending_down()
                    pending_down = down_job
            if pending_down is not None:
                pending_down()
            b.barrier()
            b.es = es

        with ExitStack() as esc:
            b.es = esc
            xo = [b.sb("p3c_xo%d" % i, [128, D], F32) for i in range(2)]
            r_xo = [b.res(), b.res()]
            for t in range(nt):
                s = t % 2
                emit_ln_stats(b, xacc[:, t, :], r_xacc, junk, r_junk, st, r_st)
                b.op("dve", lambda e, t=t, s=s: e.tensor_scalar(
                    out=xo[s][:], in0=xacc[:, t, :], scalar1=st[:, 0:1], scalar2=st[:, 1:2],
                    op0=ALU.subtract, op1=ALU.mult), reads=[r_xacc, r_st], writes=[r_xo[s]])
                b.op("pool", lambda e, s=s: e.tensor_tensor(out=xo[s][:], in0=xo[s][:], in1=lng[:], op=ALU.mult),
                     reads=[r_xo[s], r_ln], writes=[r_xo[s]])
                b.op("pool", lambda e, s=s: e.tensor_tensor(out=xo[s][:], in0=xo[s][:], in1=lnb[:], op=ALU.add),
                     reads=[r_xo[s], r_ln], writes=[r_xo[s]])
                b.dma("pool", xout_d[t * 128:(t + 1) * 128, :], xo[s][:], reads=[r_xo[s]], writes=[r_out])
            b.barrier()
            b.es = es
        b.es = old


def build_p3(l, nt, n_exp=16):
    nc = bass.Bass("TRN2", target_bir_lowering=False)
    x_d = nc.dram_tensor("x", [nt * 128, D], F32, kind="ExternalInput").ap()
    o_d = nc.dram_tensor("o", [nt * 128, D], BF16, kind="ExternalInput").ap()
    wout_d = nc.dram_tensor("w_out", [DEPTH, D, D], F32, kind="ExternalInput").ap()
    modrow_d = nc.dram_tensor("modrow", [DEPTH, 6 * D], F32, kind="ExternalInput").ap()
    modcol_d = nc.dram_tensor("modcol", [128, DEPTH * 48], F32, kind="ExternalInput").ap()
    lnmg_d = nc.dram_tensor("ln_mix_g", [DEPTH, D], F32, kind="ExternalInput").ap()
    lnmb_d = nc.dram_tensor("ln_mix_b", [DEPTH, D], F32, kind="ExternalInput").ap()
    wr_d = nc.dram_tensor("w_router", [D, 16], F32, kind="ExternalInput").ap()
    br_d = nc.dram_tensor("b_router", [1, 16], F32, kind="ExternalInput").ap()
    wg_d = nc.dram_tensor("w_gate", [DEPTH, 16, D, 256], F32, kind="ExternalInput").ap()
    wu_d = nc.dram_tensor("w_up", [DEPTH, 16, D, 256], F32, kind="ExternalInput").ap()
    wd_d = nc.dram_tensor("w_down", [DEPTH, 16, 256, D], F32, kind="ExternalInput").ap()
    lnfg_d = nc.dram_tensor("ln_ffn_g", [DEPTH, D], F32, kind="ExternalInput").ap()
    lnfb_d = nc.dram_tensor("ln_ffn_b", [DEPTH, D], F32, kind="ExternalInput").ap()
    identf_d = nc.dram_tensor("identf", [128, 128], F32, kind="ExternalInput").ap()
    xout_d = nc.dram_tensor("xout", [nt * 128, D], F32, kind="ExternalOutput").ap()
    with ExitStack() as es, nc.allow_low_precision("bf16 matmul operands, fp32 accumulation"):
        b = B(nc, es)
        emit_p3(b, l, nt, x_d, o_d, wout_d, modrow_d, modcol_d, lnmg_d, lnmb_d, wr_d, br_d, wg_d, wu_d, wd_d,
                lnfg_d, lnfb_d, identf_d, xout_d, b.res(), b.res(), n_exp=n_exp)
        b.finish()
    return nc


I32 = mybir.dt.int32


def _decl_p1_inputs(nc):
    d = {}
    d["pos"] = nc.dram_tensor("pos", [128, NT], I32, kind="ExternalInput").ap()
    d["invf"] = nc.dram_tensor("invf", [128, 32], F32, kind="ExternalInput").ap()
    d["w_in"] = nc.dram_tensor("w_in", [1, D, INW], F32, kind="ExternalInput").ap()
    d["i_bias"] = nc.dram_tensor("i_bias", [1, 4], F32, kind="ExternalInput").ap()
    d["f_bias"] = nc.dram_tensor("f_bias", [1, 4], F32, kind="ExternalInput").ap()
    return d


def _decl_p1_outputs(nc):
    d = {}
    d["featT"] = nc.dram_tensor("featT", [NT, 128, NFT, 128], BF16, kind="ExternalOutput").ap()
    d["tokA"] = nc.dram_tensor("tokA", [TOK, 1792], BF16, kind="ExternalOutput").ap()
    d["tokF"] = nc.dram_tensor("tokF", [TOK, 12], F32, kind="ExternalOutput").ap()
    d["gT"] = nc.dram_tensor("gT", [NT, 8, 128], F32, kind="ExternalOutput").ap()
    return d


def _decl_p3_inputs(nc):
    d = {}
    d["o"] = nc.dram_tensor("o", [TOK, D], BF16, kind="ExternalInput").ap()
    d["w_out"] = nc.dram_tensor("w_out", [1, D, D], F32, kind="ExternalInput").ap()
    d["modrow3"] = nc.dram_tensor("modrow3", [1, 6 * D], F32, kind="ExternalInput").ap()
    d["modcol3"] = nc.dram_tensor("modcol3", [128, 48], F32, kind="ExternalInput").ap()
    d["ln_mix_g"] = nc.dram_tensor("ln_mix_g", [1, D], F32, kind="ExternalInput").ap()
    d["ln_mix_b"] = nc.dram_tensor("ln_mix_b", [1, D], F32, kind="ExternalInput").ap()
    d["w_router"] = nc.dram_tensor("w_router", [D, 16], F32, kind="ExternalInput").ap()
    d["b_router"] = nc.dram_tensor("b_router", [1, 16], F32, kind="ExternalInput").ap()
    d["w_gate"] = nc.dram_tensor("w_gate", [1, 16, D, 256], F32, kind="ExternalInput").ap()
    d["w_up"] = nc.dram_tensor("w_up", [1, 16, D, 256], F32, kind="ExternalInput").ap()
    d["w_down"] = nc.dram_tensor("w_down", [1, 16, 256, D], F32, kind="ExternalInput").ap()
    d["ln_ffn_g"] = nc.dram_tensor("ln_ffn_g", [1, D], F32, kind="ExternalInput").ap()
    d["ln_ffn_b"] = nc.dram_tensor("ln_ffn_b", [1, D], F32, kind="ExternalInput").ap()
    return d


def _emit_p3_from(b, i3, x_d, identf_d, xout_d, r_in, r_out):
    emit_p3(b, 0, NT, x_d, i3["o"], i3["w_out"], i3["modrow3"], i3["modcol3"], i3["ln_mix_g"], i3["ln_mix_b"],
            i3["w_router"], i3["b_router"], i3["w_gate"], i3["w_up"], i3["w_down"], i3["ln_ffn_g"],
            i3["ln_ffn_b"], identf_d, xout_d, r_in, r_out)


def build_A():
    nc = bass.Bass("TRN2", target_bir_lowering=False)
    x_d = nc.dram_tensor("x", [TOK, D], F32, kind="ExternalInput").ap()
    identf_d = nc.dram_tensor("identf", [128, 128], F32, kind="ExternalInput").ap()
    ccol_d = nc.dram_tensor("ccol", [128, 8], F32, kind="ExternalInput").ap()
    wada_d = nc.dram_tensor("w_ada", [DEPTH, D, 6 * D], F32, kind="ExternalInput").ap()
    bada_d = nc.dram_tensor("b_ada", [DEPTH, 6 * D], F32, kind="ExternalInput").ap()
    modrow_d = nc.dram_tensor("modrow", [DEPTH, 6 * D], F32, kind="ExternalOutput").ap()
    modcol_d = nc.dram_tensor("modcol", [128, DEPTH * 48], F32, kind="ExternalOutput").ap()
    i1 = _decl_p1_inputs(nc)
    o1 = _decl_p1_outputs(nc)
    with ExitStack() as es, nc.allow_low_precision("bf16 matmul operands, fp32 accumulation"):
        b = B(nc, es)
        cos = b.sb("cos", [128, NT, 32], F32)
        sin = b.sb("sin", [128, NT, 32], F32)
        r_tab, r_mod, r_out = b.res(), b.res(), b.res()
        emit_mod(b, ccol_d, wada_d, bada_d, modrow_d, modcol_d)
        emit_rope_tables(b, i1["pos"], i1["invf"], cos, sin, r_tab, NT)
        emit_p1(b, 0, NT, x_d, i1["w_in"], modcol_d, i1["i_bias"], i1["f_bias"], identf_d, cos, sin, r_tab,
                o1["featT"], o1["tokA"], o1["tokF"], o1["gT"], b.res(), r_out)
        b.finish()
    return nc


def build_B():
    nc = bass.Bass("TRN2", target_bir_lowering=False)
    featT_all = nc.dram_tensor("featT_all", [2 * NT, 128, NFT, 128], BF16, kind="ExternalInput").ap()
    tokA_all = nc.dram_tensor("tokA_all", [2 * TOK, 1792], BF16, kind="ExternalInput").ap()
    gT_all = nc.dram_tensor("gT_all", [2 * NT, 8, 128], F32, kind="ExternalInput").ap()
    featT_own = nc.dram_tensor("featT_own", [NT, 128, NFT, 128], BF16, kind="ExternalInput").ap()
    tokA_own = nc.dram_tensor("tokA_own", [TOK, 1792], BF16, kind="ExternalInput").ap()
    tokF_own = nc.dram_tensor("tokF_own", [TOK, 12], F32, kind="ExternalInput").ap()
    vis_d = nc.dram_tensor("vis", [128, 256], F32, kind="ExternalInput").ap()
    pw_d = nc.dram_tensor("pw", [128, NIT + 1], F32, kind="ExternalInput").ap()
    decT_d = nc.dram_tensor("decT", [128, 4, 128], F32, kind="ExternalInput").ap()
    qdecT_d = nc.dram_tensor("qdecT", [128, 2, 128], F32, kind="ExternalInput").ap()
    kdec_d = nc.dram_tensor("kdec", [128, 4], F32, kind="ExternalInput").ap()
    jf_d = nc.dram_tensor("jf", [128, 1], F32, kind="ExternalInput").ap()
    convw_d = nc.dram_tensor("convw", [1, 128, 4, 4], F32, kind="ExternalInput").ap()
    convb_d = nc.dram_tensor("convb", [1, 128, 4], F32, kind="ExternalInput").ap()
    caus_d = nc.dram_tensor("caus", [128, 128], F32, kind="ExternalInput").ap()
    sel4_d = nc.dram_tensor("sel4", [4, 512], F32, kind="ExternalInput").ap()
    eye4_d = nc.dram_tensor("eye4", [4, 4], F32, kind="ExternalInput").ap()
    identf_d = nc.dram_tensor("identf", [128, 128], F32, kind="ExternalInput").ap()
    o_d = nc.dram_tensor("o", [TOK, D], BF16, kind="ExternalOutput").ap()
    with ExitStack() as es, nc.allow_low_precision("bf16 matmul operands, fp32 accumulation"):
        b = B(nc, es)
        r_in, r_out = b.res(), b.res()
        emit_p2a(b, NT, featT_all, tokA_all, featT_own, tokF_own, vis_d, pw_d, identf_d, o_d, r_in, r_out)
        emit_p2b(b, NT, tokA_all, featT_own, tokA_own, decT_d, qdecT_d, kdec_d, jf_d, o_d, r_in, r_out)
        emit_p2c(b, 0, NT, featT_all, tokA_all, gT_all, tokA_own, convw_d, convb_d, caus_d, sel4_d, eye4_d,
                 jf_d, identf_d, o_d, r_in, r_out)
        b.finish()
    return nc


def build_C(with_p1):
    nc = bass.Bass("TRN2", target_bir_lowering=False)
    x_d = nc.dram_tensor("x", [TOK, D], F32, kind="ExternalInput").ap()
    identf_d = nc.dram_tensor("identf", [128, 128], F32, kind="ExternalInput").ap()
    i3 = _decl_p3_inputs(nc)
    xout_d = nc.dram_tensor("xout", [TOK, D], F32, kind="ExternalOutput").ap()
    if with_p1:
        i1 = _decl_p1_inputs(nc)
        modcol1_d = nc.dram_tensor("modcol1", [128, 48], F32, kind="ExternalInput").ap()
        o1 = _decl_p1_outputs(nc)
    with ExitStack() as es, nc.allow_low_precision("bf16 matmul operands, fp32 accumulation"):
        b = B(nc, es)
        r_in, r_x = b.res(), b.res()
        if with_p1:
            cos = b.sb("cos", [128, NT, 32], F32)
            sin = b.sb("sin", [128, NT, 32], F32)
            r_tab = b.res()
            emit_rope_tables(b, i1["pos"], i1["invf"], cos, sin, r_tab, NT)
        _emit_p3_from(b, i3, x_d, identf_d, xout_d, r_in, r_x)
        if with_p1:
            emit_p1(b, 0, NT, xout_d, i1["w_in"], modcol1_d, i1["i_bias"], i1["f_bias"], identf_d, cos, sin,
                    r_tab, o1["featT"], o1["tokA"], o1["tokF"], o1["gT"], r_x, b.res())
        b.finish()
    return nc


_PROGS = {}


def _prog(name, fn):
    if name not in _PROGS:
        _PROGS[name] = fn()
    return _PROGS[name]


def _own_tiles(a, j):
    sh = a.shape
    return np.ascontiguousarray(a.reshape((32, 128) + sh[1:])[j::2]).reshape((TOK,) + sh[1:])


def kernel(x, c, positions, w_ada, b_ada, w_in, i_bias, f_bias, conv_w, conv_b, w_out, ln_mix_g, ln_mix_b,
           w_router, b_router, w_gate, w_up, w_down, ln_ffn_g, ln_ffn_b):
    f32 = np.float32
    x = np.asarray(x, f32)
    c = np.asarray(c, f32)
    positions = np.asarray(positions, np.int32)
    w_ada, b_ada, w_in = np.asarray(w_ada, f32), np.asarray(b_ada, f32), np.asarray(w_in, f32)
    i_bias, f_bias = np.asarray(i_bias, f32), np.asarray(f_bias, f32)
    w_out = np.asarray(w_out, f32)
    ln_mix_g, ln_mix_b = np.asarray(ln_mix_g, f32), np.asarray(ln_mix_b, f32)
    w_router, b_router = np.asarray(w_router, f32), np.asarray(b_router, f32).reshape(1, 16)
    w_gate, w_up, w_down = np.asarray(w_gate, f32), np.asarray(w_up, f32), np.asarray(w_down, f32)
    ln_ffn_g, ln_ffn_b = np.asarray(ln_ffn_g, f32), np.asarray(ln_ffn_b, f32)
    cw, cb = conv_layouts(np.asarray(conv_w, f32), np.asarray(conv_b, f32))
    ident = np.eye(128, dtype=f32)
    invf = inv_freq_table()
    decT, qdecT, kdec = ret_tables()
    caus, sel4, eye4 = mlstm_consts()
    pw = pw_table()
    cores = list(range(8))

    def p1_in(core, l):
        bi, j = core // 2, core % 2
        pos = np.ascontiguousarray(positions[bi].reshape(32, 128)[j::2].T)
        return {"pos": pos, "invf": invf, "w_in": w_in[l:l + 1], "i_bias": i_bias[l:l + 1],
                "f_bias": f_bias[l:l + 1]}

    def p3_in(core, l, o, modrow, modcol):
        return {"o": o, "w_out": w_out[l:l + 1], "modrow3": np.ascontiguousarray(modrow[l:l + 1]),
                "modcol3": np.ascontiguousarray(modcol[:, l * 48:(l + 1) * 48]),
                "ln_mix_g": ln_mix_g[l:l + 1], "ln_mix_b": ln_mix_b[l:l + 1], "w_router": w_router,
                "b_router": b_router, "w_gate": w_gate[l:l + 1], "w_up": w_up[l:l + 1], "w_down": w_down[l:l + 1],
                "ln_ffn_g": ln_ffn_g[l:l + 1], "ln_ffn_b": ln_ffn_b[l:l + 1]}

    ims = []
    xs = []
    for core in cores:
        bi, j = core // 2, core % 2
        xo = _own_tiles(x[bi], j)
        xs.append(xo)
        im = {"x": xo, "identf": ident, "ccol": np.ascontiguousarray(c[bi].reshape(8, 128).T),
              "w_ada": w_ada, "b_ada": b_ada}
        im.update(p1_in(core, 0))
        ims.append(im)
    res = run_bass_kernel_spmd(_prog("A", build_A), ims, core_ids=cores).results
    modrow = [np.asarray(r["modrow"]) for r in res]
    modcol = [np.asarray(r["modcol"]) for r in res]
    p1o = res
    for l in range(DEPTH):
        ims = []
        for core in cores:
            bi, j = core // 2, core % 2
            a, bb = p1o[2 * bi], p1o[2 * bi + 1]
            ims.append({
                "featT_all": np.concatenate([np.asarray(a["featT"]), np.asarray(bb["featT"])], axis=0),
                "tokA_all": np.concatenate([np.asarray(a["tokA"]), np.asarray(bb["tokA"])], axis=0),
                "gT_all": np.concatenate([np.asarray(a["gT"]), np.asarray(bb["gT"])], axis=0),
                "featT_own": np.asarray(p1o[core]["featT"]), "tokA_own": np.asarray(p1o[core]["tokA"]),
                "tokF_own": np.asarray(p1o[core]["tokF"]),
                "vis": vis_table(j), "pw": pw, "decT": decT, "qdecT": qdecT, "kdec": kdec,
                "jf": np.full((128, 1), float(j), f32), "convw": cw[l:l + 1], "convb": cb[l:l + 1],
                "caus": caus, "sel4": sel4, "eye4": eye4, "identf": ident})
        ob = run_bass_kernel_spmd(_prog("B", build_B), ims, core_ids=cores).results
        last = (l == DEPTH - 1)
        ims = []
        for core in cores:
            im = {"x": xs[core], "identf": ident}
            im.update(p3_in(core, l, np.asarray(ob[core]["o"]), modrow[core], modcol[core]))
            if not last:
                im.update(p1_in(core, l + 1))
                im["modcol1"] = np.ascontiguousarray(modcol[core][:, (l + 1) * 48:(l + 2) * 48])
            ims.append(im)
        if last:
            res = run_bass_kernel_spmd(_prog("E", lambda: build_C(False)), ims, core_ids=cores).results
        else:
            res = run_bass_kernel_spmd(_prog("C", lambda: build_C(True)), ims, core_ids=cores).results
            p1o = res
        xs = [np.asarray(r["xout"]) for r in res]
    out = np.zeros((BATCH, SEQ, D), f32)
    for core in cores:
        bi, j = core // 2, core % 2
        out[bi].reshape(32, 128, D)[j::2] = xs[core].reshape(NT, 128, D)
    return out


def build_fused(depth=DEPTH):
    nc = bass.Bass("TRN2", target_bir_lowering=False)

    def inp(name, shape, dt=F32):
        return nc.dram_tensor(name, list(shape), dt, kind="ExternalInput").ap()

    def scr(name, shape, dt=F32):
        return nc.dram_tensor(name, list(shape), dt).ap()

    x_in = inp("x2", [2 * TOK, D])
    pos_d = inp("pos2", [2, 128, NT], I32)
    invf_d = inp("invf", [128, 32])
    identf_d = inp("identf", [128, 128])
    ccol_d = inp("ccol", [128, 8])
    wada_d = inp("w_ada", [DEPTH, D, 6 * D])
    bada_d = inp("b_ada", [DEPTH, 6 * D])
    win_d = inp("w_in", [DEPTH, D, INW])
    ib_d = inp("i_bias", [DEPTH, 4])
    fb_d = inp("f_bias", [DEPTH, 4])
    convw_d = inp("convw", [DEPTH, 128, 4, 4])
    convb_d = inp("convb", [DEPTH, 128, 4])
    wout_d = inp("w_out", [DEPTH, D, D])
    lnmg_d = inp("ln_mix_g", [DEPTH, D])
    lnmb_d = inp("ln_mix_b", [DEPTH, D])
    wr_d = inp("w_router", [D, 16])
    br_d = inp("b_router", [1, 16])
    wg_d = inp("w_gate", [DEPTH, 16, D, 256])
    wu_d = inp("w_up", [DEPTH, 16, D, 256])
    wd_d = inp("w_down", [DEPTH, 16, 256, D])
    lnfg_d = inp("ln_ffn_g", [DEPTH, D])
    lnfb_d = inp("ln_ffn_b", [DEPTH, D])
    vis_d = inp("vis2", [2, 128, 256])
    jf_d = inp("jf2", [2, 128, 1])
    pw_d = inp("pw", [128, NIT + 1])
    decT_d = inp("decT", [128, 4, 128])
    qdecT_d = inp("qdecT", [128, 2, 128])
    kdec_d = inp("kdec", [128, 4])
    caus_d = inp("caus", [128, 128])
    sel4_d = inp("sel4", [4, 512])
    eye4_d = inp("eye4", [4, 4])
    xfin = nc.dram_tensor("xfin", [2 * TOK, D], F32, kind="ExternalOutput").ap()

    modrow_s = scr("modrow_s", [DEPTH, 6 * D])
    modcol_s = scr("modcol_s", [128, DEPTH * 48])
    featT_s = scr("featT_s", [2 * NT, 128, NFT, 128], BF16)
    tokA_s = scr("tokA_s", [2 * TOK, 1792], BF16)
    tokF_s = scr("tokF_s", [2 * TOK, 12])
    gT_s = scr("gT_s", [2 * NT, 8, 128])
    o_s = scr("o_s", [2 * TOK, D], BF16)
    xs = [scr("xs0", [2 * TOK, D]), scr("xs1", [2 * TOK, D])]

    with ExitStack() as es, nc.allow_low_precision("bf16 matmul operands, fp32 accumulation"):
        b = B(nc, es)
        cos = [b.sb("cos%d" % j, [128, NT, 32], F32) for j in range(2)]
        sin = [b.sb("sin%d" % j, [128, NT, 32], F32) for j in range(2)]
        r_tab = [b.res(), b.res()]
        r_bun, r_o, r_x = b.res(), b.res(), b.res()
        emit_mod(b, ccol_d, wada_d, bada_d, modrow_s, modcol_s)
        for j in range(2):
            emit_rope_tables(b, pos_d[j], invf_d, cos[j], sin[j], r_tab[j], NT)
        for l in range(depth):
            x_l = x_in if l == 0 else xs[l % 2]
            x_n = xfin if l == depth - 1 else xs[(l + 1) % 2]
            for j in range(2):
                sl = slice(j * TOK, (j + 1) * TOK)
                emit_p1(b, l, NT, x_l[sl], win_d, modcol_s, ib_d, fb_d, identf_d, cos[j], sin[j], r_tab[j],
                        featT_s[j * NT:(j + 1) * NT], tokA_s[sl], tokF_s[sl], gT_s[j * NT:(j + 1) * NT],
                        r_x, r_bun)
            for j in range(2):
                sl = slice(j * TOK, (j + 1) * TOK)
                f_own = featT_s[j * NT:(j + 1) * NT]
                emit_p2a(b, NT, featT_s, tokA_s, f_own, tokF_s[sl], vis_d[j], pw_d, identf_d, o_s[sl],
                         r_bun, r_o)
            emit_p2b(b, NT, tokA_s, featT_s, None, decT_d, qdecT_d, kdec_d, jf_d[0], o_s, r_bun, r_o, whole=True)
            emit_p2c(b, l, NT, featT_s, tokA_s, gT_s, None, convw_d, convb_d, caus_d, sel4_d, eye4_d,
                     jf_d[0], identf_d, o_s, r_bun, r_o, whole=True)
            for j in range(2):
                sl = slice(j * TOK, (j + 1) * TOK)
                emit_p3(b, l, NT, x_l[sl], o_s[sl], wout_d, modrow_s, modcol_s, lnmg_d, lnmb_d, wr_d, br_d,
                        wg_d, wu_d, wd_d, lnfg_d, lnfb_d, identf_d, x_n[sl], r_o, r_x)
            if l != depth - 1:
                b.new_epoch()
        b.finish()
    return nc


def kernel_fused(x, c, positions, w_ada, b_ada, w_in, i_bias, f_bias, conv_w, conv_b, w_out, ln_mix_g, ln_mix_b,
                 w_router, b_router, w_gate, w_up, w_down, ln_ffn_g, ln_ffn_b):
    f32 = np.float32
    x = np.asarray(x, f32)
    c = np.asarray(c, f32)
    positions = np.asarray(positions, np.int32)
    cw, cb = conv_layouts(np.asarray(conv_w, f32), np.asarray(conv_b, f32))
    decT, qdecT, kdec = ret_tables()
    caus, sel4, eye4 = mlstm_consts()
    shared = {
        "invf": inv_freq_table(), "identf": np.eye(128, dtype=f32),
        "w_ada": np.asarray(w_ada, f32), "b_ada": np.asarray(b_ada, f32), "w_in": np.asarray(w_in, f32),
        "i_bias": np.asarray(i_bias, f32), "f_bias": np.asarray(f_bias, f32), "convw": cw, "convb": cb,
        "w_out": np.asarray(w_out, f32), "ln_mix_g": np.asarray(ln_mix_g, f32),
        "ln_mix_b": np.asarray(ln_mix_b, f32), "w_router": np.asarray(w_router, f32),
        "b_router": np.asarray(b_router, f32).reshape(1, 16), "w_gate": np.asarray(w_gate, f32),
        "w_up": np.asarray(w_up, f32), "w_down": np.asarray(w_down, f32),
        "ln_ffn_g": np.asarray(ln_ffn_g, f32), "ln_ffn_b": np.asarray(ln_ffn_b, f32),
        "vis2": np.stack([vis_table(0), vis_table(1)]),
        "jf2": np.stack([np.zeros((128, 1), f32), np.ones((128, 1), f32)]),
        "pw": pw_table(), "decT": decT, "qdecT": qdecT, "kdec": kdec, "caus": caus, "sel4": sel4, "eye4": eye4,
    }
    cores = list(range(8))
    ims = []
    for core in cores:
        bi = core % BATCH
        xb = x[bi].reshape(32, 128, D)
        pb = positions[bi].reshape(32, 128)
        im = dict(shared)
        im["x2"] = np.ascontiguousarray(np.concatenate([xb[0::2], xb[1::2]], axis=0)).reshape(2 * TOK, D)
        im["pos2"] = np.ascontiguousarray(np.stack([pb[0::2].T, pb[1::2].T]))
        im["ccol"] = np.ascontiguousarray(c[bi].reshape(8, 128).T)
        ims.append(im)
    res = run_bass_kernel_spmd(_prog("F", build_fused), ims, core_ids=cores).results
    out = np.zeros((BATCH, SEQ, D), f32)
    for bi in range(BATCH):
        xf = np.asarray(res[bi]["xfin"]).reshape(2, NT, 128, D)
        ob = out[bi].reshape(32, 128, D)
        ob[0::2] = xf[0]
        ob[1::2] = xf[1]
    return out


kernel_unfused = kernel
kernel = kernel_fused
```

```python
import numpy as np
from contextlib import ExitStack

import concourse.bass as bass
import concourse.mybir as mybir
from concourse.bass_utils import run_bass_kernel_spmd

F32 = mybir.dt.float32
BF16 = mybir.dt.bfloat16
ALU = mybir.AluOpType
AF = mybir.ActivationFunctionType
AX = mybir.AxisListType

D = 1024
SEQ = 4096
BATCH = 4
DEPTH = 4
NT = 16
TOK = NT * 128
INW = 3916
LN_EPS = 1e-5
ALPHA = (2.0 * DEPTH) ** 0.25
NEG = -1.0e30

ENGS = ("pe", "act", "dve", "pool", "sp")
NRING = 8


class Res:
    __slots__ = ("name", "w", "r")

    def __init__(self, name=""):
        self.name = name
        self.w = None
        self.r = []


class B:
    def __init__(self, nc, es):
        self.nc = nc
        self.es = es
        self.root_es = es
        self.epoch = 0
        self.q = {e: [] for e in ENGS}
        self.sem = {e: es.enter_context(nc.semaphore("s_" + e)) for e in ENGS}
        self.cnt = {e: 0 for e in ENGS}
        self.seen = {e: {} for e in ENGS}
        self.dq = ("sp", "pool")
        self.ring = {e: [es.enter_context(nc.semaphore("d_%s%d" % (e, i))) for i in range(NRING)]
                     for e in self.dq}
        self.rcnt = {e: [0] * NRING for e in self.dq}
        self.rnext = {e: 0 for e in self.dq}
        self.nres = 0

    def res(self, name=""):
        self.nres += 1
        return Res(name or ("r%d" % self.nres))

    def sb(self, name, shape, dt):
        self.nres += 1
        return self.es.enter_context(self.nc.sbuf_tensor("%s_%d" % (name, self.nres), list(shape), dt))

    def ps(self, name, shape, dt=F32):
        self.nres += 1
        return self.es.enter_context(self.nc.psum_tensor("%s_%d" % (name, self.nres), list(shape), dt))

    def _deps(self, eng, reads, writes):
        need = {}

        def add(ev):
            if ev is None:
                return
            s, v = ev
            k = id(s)
            if k not in need or need[k][1] < v:
                need[k] = (s, v)

        for r in reads:
            add(r.w)
        for w in writes:
            add(w.w)
            for ev in w.r:
                add(ev)
        out = []
        seen = self.seen[eng]
        own = id(self.sem[eng])
        for k, (s, v) in need.items():
            if eng == "pe" and k == own:
                continue
            if seen.get(k, 0) >= v:
                continue
            seen[k] = v
            out.append((s, v))
        return out

    def op(self, eng, fn, reads=(), writes=(), inc=True):
        waits = self._deps(eng, reads, writes)
        if inc:
            self.cnt[eng] += 1
            ev = (self.sem[eng], self.cnt[eng])
        else:
            ev = (self.sem[eng], self.cnt[eng] + 1)
        for r in reads:
            r.r.append(ev)
            if len(r.r) > 64:
                r.r = r.r[-64:] if False else self._compact(r.r)
        for w in writes:
            w.w = ev
            w.r = []
        self.q[eng].append((waits, fn, self.sem[eng] if inc else None, 1))

    @staticmethod
    def _compact(evs):
        best = {}
        for s, v in evs:
            k = id(s)
            if k not in best or best[k][1] < v:
                best[k] = (s, v)
        return list(best.values())

    def dma(self, q, out_ap, in_ap, reads=(), writes=()):
        waits = self._deps(q, reads, writes)
        i = self.rnext[q]
        self.rnext[q] = (i + 1) % NRING
        s = self.ring[q][i]
        if self.rcnt[q][i] > 0:
            v = 16 * self.rcnt[q][i]
            if self.seen[q].get(id(s), 0) < v:
                self.seen[q][id(s)] = v
                waits.append((s, v))
        self.rcnt[q][i] += 1
        ev = (s, 16 * self.rcnt[q][i])
        for r in reads:
            r.r.append(ev)
        for w in writes:
            w.w = ev
            w.r = []

        def fn(e, out_ap=out_ap, in_ap=in_ap):
            return e.dma_start(out=out_ap, in_=in_ap)

        self.q[q].append((waits, fn, s, 16))

    def coll(self, kind, groups, in_ap, out_ap, reads=(), writes=()):
        q = "pool"
        waits = self._deps(q, reads, writes)
        i = self.rnext[q]
        self.rnext[q] = (i + 1) % NRING
        s = self.ring[q][i]
        if self.rcnt[q][i] > 0:
            v = 16 * self.rcnt[q][i]
            if self.seen[q].get(id(s), 0) < v:
                self.seen[q][id(s)] = v
                waits.append((s, v))
        self.rcnt[q][i] += 1
        ev = (s, 16 * self.rcnt[q][i])
        for r in reads:
            r.r.append(ev)
        for w in writes:
            w.w = ev
            w.r = []

        def fn(e):
            return e.collective_compute(kind, ALU.bypass, replica_groups=groups, ins=[in_ap], outs=[out_ap])

        self.q[q].append((waits, fn, s, 16))

    def new_epoch(self):
        self.barrier()
        nc, es = self.nc, self.root_es
        self.epoch += 1
        k = self.epoch
        self.sem = {e: es.enter_context(nc.semaphore("s%d_%s" % (k, e))) for e in ENGS}
        self.cnt = {e: 0 for e in ENGS}
        self.seen = {e: {} for e in ENGS}
        self.ring = {e: [es.enter_context(nc.semaphore("d%d_%s%d" % (k, e, i))) for i in range(NRING)]
                     for e in self.dq}
        self.rcnt = {e: [0] * NRING for e in self.dq}
        self.rnext = {e: 0 for e in self.dq}

    def barrier(self, label=None):
        if not hasattr(self, "marks"):
            self.marks = []
        self.marks.append((self.epoch, dict(self.cnt)))
        evs = []
        for q in self.dq:
            for i in range(NRING):
                if self.rcnt[q][i] > 0:
                    evs.append((self.ring[q][i], 16 * self.rcnt[q][i]))
        for e in ENGS:
            if e != "sp" and self.cnt[e] > 0:
                evs.append((self.sem[e], self.cnt[e]))
        for e in ENGS:
            waits = []
            for s_, v in evs:
                if id(s_) == id(self.sem[e]) and e == "pe":
                    continue
                if self.seen[e].get(id(s_), 0) >= v:
                    continue
                self.seen[e][id(s_)] = v
                waits.append((s_, v))
            if waits:
                self.q[e].append((waits, None, None, 0))

    def finish(self):
        nc = self.nc
        fin = []
        for q in self.dq:
            for i in range(NRING):
                if self.rcnt[q][i] > 0:
                    fin.append((self.ring[q][i], 16 * self.rcnt[q][i]))
        for e in ENGS:
            if e != "sp" and self.cnt[e] > 0:
                fin.append((self.sem[e], self.cnt[e]))
        qs = self.q

        def run(e, lst, extra=()):
            for waits, fn, s, n in lst:
                for ws, wv in waits:
                    e.wait_ge(ws, wv)
                if fn is None:
                    continue
                ins = fn(e)
                if s is not None:
                    ins.then_inc(s, n)
            for ws, wv in extra:
                e.wait_ge(ws, wv)

        with nc.Block() as blk:
            @blk.sync
            def _(e):
                run(e, qs["sp"], fin)

            @blk.tensor
            def _(e):
                run(e, qs["pe"])

            @blk.scalar
            def _(e):
                run(e, qs["act"])

            @blk.vector
            def _(e):
                run(e, qs["dve"])

            @blk.gpsimd
            def _(e):
                run(e, qs["pool"])


def emit_mod(b, ccol_d, wada_d, bada_d, modrow_d, modcol_d):
    nc = b.nc
    with ExitStack() as es:
        old = b.es
        b.es = es
        ccol = b.sb("m_ccol", [128, 8], F32)
        cact = b.sb("m_cact", [128, 8], F32)
        one = b.sb("m_one", [1, 1], F32)
        wbuf = [b.sb("m_w%d" % i, [128, 8, 512], F32) for i in range(2)]
        brow = b.sb("m_brow", [1, 6144], F32)
        mrow = b.sb("m_mrow", [1, 6144], F32)
        mcol = b.sb("m_mcol", [128, DEPTH * 48], F32)
        pr = [b.ps("m_pr%d" % i, [1, 512]) for i in range(2)]
        pc = b.ps("m_pc", [128, 48])
        r_ccol, r_cact, r_one, r_brow, r_mrow, r_mcol, r_pc = (b.res() for _ in range(7))
        r_w = [b.res(), b.res()]
        r_pr = [b.res(), b.res()]
        r_out = b.res()

        b.dma("sp", ccol[:], ccol_d, writes=[r_ccol])
        b.op("act", lambda e: e.activation(out=cact[:], in_=ccol[:], func=AF.Silu),
             reads=[r_ccol], writes=[r_cact])
        b.op("dve", lambda e: e.memset(one[:], 1.0), writes=[r_one])
        it = 0
        for l in range(DEPTH):
            b.dma("sp", brow[:], bada_d[l:l + 1, :], writes=[r_brow])
            for nb in range(12):
                s = it % 2
                it += 1
                src = wada_d[l, :, nb * 512:(nb + 1) * 512].rearrange("(k p) n -> p k n", p=128)
                b.dma("sp", wbuf[s][:], src, writes=[r_w[s]])
                for k in range(8):
                    b.op("pe", lambda e, s=s, k=k: e.matmul(
                        pr[s][:], lhsT=cact[:, k:k + 1], rhs=wbuf[s][:, k, :],
                        start=(k == 0), stop=(k == 7)),
                        reads=[r_cact, r_w[s]], writes=[r_pr[s]], inc=(k == 7))
                b.op("dve", lambda e, s=s, nb=nb: e.tensor_tensor(
                    out=mrow[:, nb * 512:(nb + 1) * 512], in0=pr[s][:],
                    in1=brow[:, nb * 512:(nb + 1) * 512], op=ALU.add),
                    reads=[r_pr[s], r_brow], writes=[r_mrow])
            b.dma("pool", modrow_d[l:l + 1, :], mrow[:], reads=[r_mrow], writes=[r_out])
            for c in range(48):
                b.op("pe", lambda e, c=c: e.matmul(
                    pc[:, c:c + 1], lhsT=mrow[:, c * 128:(c + 1) * 128], rhs=one[:, :],
                    start=True, stop=True),
                    reads=[r_mrow, r_one], writes=[r_pc], inc=(c == 47))
            b.op("act", lambda e, l=l: e.copy(out=mcol[:, l * 48:(l + 1) * 48], in_=pc[:]),
                 reads=[r_pc], writes=[r_mcol])
        b.dma("pool", modcol_d, mcol[:], reads=[r_mcol], writes=[r_out])
        b.barrier()
        b.es = old


def build_mod():
    nc = bass.Bass("TRN2", target_bir_lowering=False)
    ccol_d = nc.dram_tensor("ccol", [128, 8], F32, kind="ExternalInput").ap()
    wada_d = nc.dram_tensor("w_ada", [DEPTH, D, 6 * D], F32, kind="ExternalInput").ap()
    bada_d = nc.dram_tensor("b_ada", [DEPTH, 6 * D], F32, kind="ExternalInput").ap()
    modrow_d = nc.dram_tensor("modrow", [DEPTH, 6 * D], F32, kind="ExternalOutput").ap()
    modcol_d = nc.dram_tensor("modcol", [128, DEPTH * 48], F32, kind="ExternalOutput").ap()
    with ExitStack() as es:
        b = B(nc, es)
        emit_mod(b, ccol_d, wada_d, bada_d, modrow_d, modcol_d)
        b.finish()
    return nc


def run_mod(c, w_ada, b_ada):
    nc = build_mod()
    in_maps = []
    for core in range(8):
        bi = core // 2
        in_maps.append({"ccol": np.ascontiguousarray(c[bi].reshape(8, 128).T),
                        "w_ada": w_ada, "b_ada": b_ada})
    res = run_bass_kernel_spmd(nc, in_maps, core_ids=list(range(8)))
    return [r["modrow"] for r in res.results], [r["modcol"] for r in res.results]


NFT = 19
P1_BLK = [(0, 512), (512, 512), (1024, 512), (1536, 332), (1868, 512), (2380, 512),
          (2892, 512), (3404, 512)]


def emit_rope_tables(b, pos_d, invf_d, cos, sin, r_tab, nt):
    with ExitStack() as es:
        old = b.es
        b.es = es
        posi = b.sb("rt_posi", [128, nt], mybir.dt.int32)
        posf = b.sb("rt_posf", [128, nt], F32)
        invf = b.sb("rt_invf", [128, 32], F32)
        ang = b.sb("rt_ang", [128, nt, 32], F32)
        u = b.sb("rt_u", [128, nt, 32], F32)
        r_pi, r_pf, r_if, r_ang, r_u = (b.res() for _ in range(5))
        b.dma("sp", posi[:], pos_d, writes=[r_pi])
        b.dma("sp", invf[:], invf_d, writes=[r_if])
        b.op("dve", lambda e: e.tensor_copy(out=posf[:], in_=posi[:]), reads=[r_pi], writes=[r_pf])
        for t in range(nt):
            b.op("dve", lambda e, t=t: e.tensor_scalar(
                out=ang[:, t, :], in0=invf[:], scalar1=posf[:, t:t + 1], scalar2=None,
                op0=ALU.mult), reads=[r_if, r_pf], writes=[r_ang])
        two_pi = float(np.float32(2.0 * np.pi))
        pi = float(np.float32(np.pi))
        ki = b.sb("rt_ki", [128, nt, 32], mybir.dt.int32)
        kf = b.sb("rt_kf", [128, nt, 32], F32)
        r_ki, r_kf = b.res(), b.res()

        def reduced_sin(dst, shift):
            b.op("dve", lambda e: e.tensor_scalar(
                out=u[:], in0=ang[:], scalar1=shift, scalar2=None, op0=ALU.add),
                reads=[r_ang], writes=[r_u])
            b.op("dve", lambda e: e.tensor_scalar(
                out=kf[:], in0=u[:], scalar1=float(1.0 / (2.0 * np.pi)), scalar2=None, op0=ALU.mult),
                reads=[r_u], writes=[r_kf])
            b.op("dve", lambda e: e.tensor_copy(out=ki[:], in_=kf[:]), reads=[r_kf], writes=[r_ki])
            b.op("dve", lambda e: e.tensor_copy(out=kf[:], in_=ki[:]), reads=[r_ki], writes=[r_kf])
            b.op("dve", lambda e: e.scalar_tensor_tensor(
                out=u[:], in0=kf[:], scalar=-two_pi, in1=u[:], op0=ALU.mult, op1=ALU.add),
                reads=[r_kf, r_u], writes=[r_u])
            b.op("dve", lambda e: e.tensor_scalar(
                out=kf[:], in0=u[:], scalar1=pi, scalar2=two_pi, op0=ALU.is_gt, op1=ALU.mult),
                reads=[r_u], writes=[r_kf])
            b.op("dve", lambda e: e.tensor_tensor(out=u[:], in0=u[:], in1=kf[:], op=ALU.subtract),
                 reads=[r_u, r_kf], writes=[r_u])
            b.op("dve", lambda e: e.tensor_scalar(
                out=kf[:], in0=u[:], scalar1=-pi, scalar2=two_pi, op0=ALU.is_lt, op1=ALU.mult),
                reads=[r_u], writes=[r_kf])
            b.op("dve", lambda e: e.tensor_tensor(out=u[:], in0=u[:], in1=kf[:], op=ALU.add),
                 reads=[r_u, r_kf], writes=[r_u])
            b.op("dve", lambda e: e.tensor_scalar(
                out=u[:], in0=u[:], scalar1=pi, scalar2=-pi, op0=ALU.min, op1=ALU.max),
                reads=[r_u], writes=[r_u])
            b.op("act", lambda e: e.activation(out=dst[:], in_=u[:], func=AF.Sin),
                 reads=[r_u], writes=[r_tab])

        reduced_sin(sin, 0.0)
        reduced_sin(cos, float(np.float32(np.pi / 2)))
        b.barrier()
        b.es = old


def emit_p1(b, l, nt, x_d, win_d, modcol_d, ibias_d, fbias_d, identf_d,
            cos, sin, r_tab, featT_d, tokA_d, tokF_d, gT_d, r_xd, r_out):
    nc = b.nc
    with ExitStack() as es:
        old = b.es
        b.es = es
        wsb = b.sb("p1_w", [128, 8, INW], BF16)
        wst = [b.sb("p1_wst%d" % i, [128, 8, 512], F32) for i in range(2)]
        identf = b.sb("p1_idf", [128, 128], F32)
        identb = b.sb("p1_idb", [128, 128], BF16)
        mcol = b.sb("p1_mcol", [128, 48], F32)
        sc1 = b.sb("p1_sc1", [128, 8], F32)
        bias8 = b.sb("p1_bias8", [128, 8], F32)
        xt = [b.sb("p1_x%d" % i, [128, D], F32) for i in range(2)]
        hT = [b.sb("p1_hT%d" % i, [128, 8, 128], BF16) for i in range(2)]
        rin = [b.sb("p1_rin%d" % i, [128, 512], BF16) for i in range(5)]
        tmpa = b.sb("p1_tmpa", [128, 256], F32)
        tmpb = b.sb("p1_tmpb", [128, 256], F32)
        stA = [b.sb("p1_stA%d" % i, [128, 1792], BF16) for i in range(2)]
        stF = [b.sb("p1_stF%d" % i, [128, 12], F32) for i in range(2)]
        stT = [b.sb("p1_stT%d" % i, [128, NFT, 128], BF16) for i in range(2)]
        stG = [b.sb("p1_stG%d" % i, [8, 128], F32) for i in range(2)]
        r_stG = [b.res(), b.res()]
        ptr = [b.ps("p1_ptr%d" % i, [128, 512]) for i in range(2)]
        pin = [b.ps("p1_pin%d" % i, [128, 512]) for i in range(4)]
        pto = [b.ps("p1_pto%d" % i, [128, 1024], BF16) for i in range(2)]

        r_w, r_idf, r_idb, r_mcol, r_sc1, r_b8, r_ta, r_tb = (b.res() for _ in range(8))
        r_wst = [b.res(), b.res()]
        r_x = [b.res(), b.res()]
        r_hT = [b.res(), b.res()]
        r_rin = [b.res() for _ in range(5)]
        r_stA = [b.res(), b.res()]
        r_stF = [b.res(), b.res()]
        r_stT = [b.res(), b.res()]
        r_ptr = [b.res(), b.res()]
        r_pin = [b.res() for _ in range(4)]
        r_pto = [b.res(), b.res()]

        b.dma("sp", identf[:], identf_d, writes=[r_idf])
        b.op("dve", lambda e: e.tensor_copy(out=identb[:], in_=identf[:]), reads=[r_idf], writes=[r_idb])
        b.dma("sp", mcol[:], modcol_d[:, l * 48:(l + 1) * 48], writes=[r_mcol])
        b.op("dve", lambda e: e.tensor_scalar(out=sc1[:], in0=mcol[:, 8:16], scalar1=1.0, scalar2=None,
                                              op0=ALU.add), reads=[r_mcol], writes=[r_sc1])
        b.dma("sp", bias8[:, 0:4], ibias_d[l:l + 1, :].to_broadcast([128, 4]), writes=[r_b8])
        b.dma("sp", bias8[:, 4:8], fbias_d[l:l + 1, :].to_broadcast([128, 4]), writes=[r_b8])
        pieces = []
        for (s0, d0, n) in ((0, 0, 1860), (3908, 1860, 8), (1860, 1868, 2048)):
            o = 0
            while o < n:
                m = min(512, n - o)
                pieces.append((s0 + o, d0 + o, m))
                o += m
        for i, (s0, d0, m) in enumerate(pieces):
            s = i % 2
            src = win_d[l, :, s0:s0 + m].rearrange("(k p) n -> p k n", p=128)
            b.dma("sp", wst[s][:, :, 0:m], src, writes=[r_wst[s]])
            eng = "pool" if i % 2 == 0 else "act"
            if eng == "pool":
                b.op("pool", lambda e, s=s, d0=d0, m=m: e.tensor_copy(
                    out=wsb[:, :, d0:d0 + m], in_=wst[s][:, :, 0:m]), reads=[r_wst[s]], writes=[r_w])
            else:
                b.op("act", lambda e, s=s, d0=d0, m=m: e.copy(
                    out=wsb[:, :, d0:d0 + m], in_=wst[s][:, :, 0:m]), reads=[r_wst[s]], writes=[r_w])

        def rope(t, src, H, dst, r_src, r_dst, col0=0):
            sv = src.rearrange("p (h two d) -> p h two d", two=2, d=32)
            dv = dst.rearrange("p (h two d) -> p h two d", two=2, d=32)
            cb = cos[:, t:t + 1, :].to_broadcast([128, H, 32])
            sb_ = sin[:, t:t + 1, :].to_broadcast([128, H, 32])
            ta = tmpa[:, 0:H * 32].rearrange("p (h d) -> p h d", d=32)
            tb = tmpb[:, 0:H * 32].rearrange("p (h d) -> p h d", d=32)
            b.op("dve", lambda e: e.tensor_tensor(out=ta, in0=sv[:, :, 0, :], in1=cb, op=ALU.mult),
                 reads=[r_src, r_tab], writes=[r_ta])
            b.op("dve", lambda e: e.tensor_tensor(out=tb, in0=sv[:, :, 1, :], in1=sb_, op=ALU.mult),
                 reads=[r_src, r_tab], writes=[r_tb])
            b.op("dve", lambda e: e.tensor_tensor(out=dv[:, :, 0, :], in0=ta, in1=tb, op=ALU.subtract),
                 reads=[r_ta, r_tb], writes=[r_dst])
            b.op("dve", lambda e: e.tensor_tensor(out=ta, in0=sv[:, :, 1, :], in1=cb, op=ALU.mult),
                 reads=[r_src, r_tab], writes=[r_ta])
            b.op("dve", lambda e: e.tensor_tensor(out=tb, in0=sv[:, :, 0, :], in1=sb_, op=ALU.mult),
                 reads=[r_src, r_tab], writes=[r_tb])
            b.op("dve", lambda e: e.tensor_tensor(out=dv[:, :, 1, :], in0=ta, in1=tb, op=ALU.add),
                 reads=[r_ta, r_tb], writes=[r_dst])

        pin_i = 0
        pto_i = 0
        pending = []
        pgt = pto[1][:, 0:256].bitcast(F32)
        r_pgt = r_pto[1]
        for t in range(nt):
            s = t % 2
            b.dma("sp", xt[s][:], x_d[t * 128:(t + 1) * 128, :], reads=[r_xd], writes=[r_x[s]])
            for c in range(8):
                b.op("pe", lambda e, s=s, c=c: e.transpose(
                    out=ptr[c // 4][:, (c % 4) * 128:(c % 4 + 1) * 128],
                    in_=xt[s][:, c * 128:(c + 1) * 128], identity=identf[:]),
                    reads=[r_x[s], r_idf], writes=[r_ptr[c // 4]], inc=(c % 4 == 3))
            for c in range(8):
                b.op("act", lambda e, s=s, c=c: e.activation(
                    out=hT[s][:, c, :], in_=ptr[c // 4][:, (c % 4) * 128:(c % 4 + 1) * 128],
                    func=AF.Identity, scale=sc1[:, c:c + 1], bias=mcol[:, c:c + 1]),
                    reads=[r_ptr[c // 4], r_sc1, r_mcol], writes=[r_hT[s]])
            A = stA[s]
            Fs = stF[s]
            T = stT[s]
            for bi, (c0, n) in enumerate(P1_BLK):
                pi = pin_i % 4
                pin_i += 1
                P = pin[pi]
                for k in range(8):
                    b.op("pe", lambda e, s=s, k=k, c0=c0, n=n, P=P: e.matmul(
                        P[:, 0:n], lhsT=hT[s][:, k, :], rhs=wsb[:, k, c0:c0 + n],
                        start=(k == 0), stop=(k == 7)),
                        reads=[r_hT[s], r_w], writes=[r_pin[pi]], inc=(k == 7))
                rp = r_pin[pi]
                if bi == 0 or bi == 1:
                    rope(t, P[:, 0:512], 8, rin[bi][:, 0:512], rp, r_rin[bi])
                elif bi == 2:
                    b.op("act", lambda e, P=P, A=A: e.copy(out=A[:, 0:512], in_=P[:, 0:512]),
                         reads=[rp], writes=[r_stA[s]])
                elif bi == 3:
                    rope(t, P[:, 0:320], 5, rin[2][:, 0:320], rp, r_rin[2])
                    b.op("dve", lambda e: e.tensor_copy(out=rin[2][:, 320:384], in_=rin[2][:, 256:320]),
                         reads=[r_rin[2]], writes=[r_rin[2]])
                    b.op("dve", lambda e, P=P, Fs=Fs: e.tensor_copy(out=Fs[:, 0:4], in_=P[:, 320:324]),
                         reads=[rp], writes=[r_stF[s]])
                    b.op("dve", lambda e, P=P, Fs=Fs: e.tensor_tensor(
                        out=Fs[:, 4:12], in0=P[:, 324:332], in1=bias8[:], op=ALU.add),
                        reads=[rp, r_b8], writes=[r_stF[s]])
                elif bi == 4:
                    rope(t, P[:, 0:512], 8, rin[3][:, 0:512], rp, r_rin[3])
                    b.op("pool", lambda e, A=A: e.tensor_copy(out=A[:, 512:768], in_=rin[3][:, 256:512]),
                         reads=[r_rin[3]], writes=[r_stA[s]])
                elif bi == 5:
                    b.op("act", lambda e, P=P, A=A: e.copy(out=A[:, 768:1024], in_=P[:, 0:256]),
                         reads=[rp], writes=[r_stA[s]])
                    b.op("act", lambda e, P=P, A=A: e.activation(out=A[:, 1024:1280], in_=P[:, 256:512],
                                                                 func=AF.Silu),
                         reads=[rp], writes=[r_stA[s]])
                elif bi == 6:
                    b.op("act", lambda e, P=P: e.copy(out=rin[4][:, 0:512], in_=P[:, 0:512]),
                         reads=[rp], writes=[r_rin[4]])
                else:
                    b.op("act", lambda e, P=P, A=A: e.copy(out=A[:, 1280:1536], in_=P[:, 0:256]),
                         reads=[rp], writes=[r_stA[s]])
                    b.op("act", lambda e, P=P, A=A: e.activation(out=A[:, 1536:1792], in_=P[:, 256:512],
                                                                 func=AF.Sigmoid),
                         reads=[rp], writes=[r_stA[s]])
                if bi == 0:
                    srcs = [(rin[0], r_rin[0], i * 128, i) for i in range(4)]
                elif bi == 1:
                    srcs = [(rin[1], r_rin[1], i * 128, 4 + i) for i in range(4)]
                elif bi == 3:
                    srcs = [(rin[2], r_rin[2], 0, 8), (rin[2], r_rin[2], 128, 9), (rin[2], r_rin[2], 256, 10)]
                elif bi == 4:
                    srcs = [(rin[3], r_rin[3], i * 128, 11 + i) for i in range(4)]
                elif bi == 6:
                    srcs = [(rin[4], r_rin[4], i * 128, 15 + i) for i in range(4)]
                else:
                    srcs = []
                if srcs:
                    def tr_job(srcs=srcs, T=T, s=s):
                        nonlocal pto_i
                        po = pto_i % 2
                        pto_i += 1
                        for i, (buf, rb, c, slot) in enumerate(srcs):
                            b.op("pe", lambda e, buf=buf, c=c, i=i, po=po: e.transpose(
                                out=pto[po][:, i * 128:(i + 1) * 128], in_=buf[:, c:c + 128], identity=identb[:]),
                                reads=[rb, r_idb], writes=[r_pto[po]], inc=(i == len(srcs) - 1))
                        s0 = srcs[0][3]
                        n_ = len(srcs)
                        b.op("act", lambda e, po=po, s0=s0, n_=n_, T=T: e.copy(
                            out=T[:, s0:s0 + n_, :], in_=pto[po][:, 0:n_ * 128].rearrange("p (n k) -> p n k", k=128)),
                            reads=[r_pto[po]], writes=[r_stT[s]])
                    pending.append(tr_job)
                while len(pending) > 2:
                    pending.pop(0)()
            def out_job(t=t, s=s, A=A, Fs=Fs, T=T):
                b.dma("pool", tokA_d[t * 128:(t + 1) * 128, :], A[:], reads=[r_stA[s]], writes=[r_out])
                b.dma("pool", tokF_d[t * 128:(t + 1) * 128, :], Fs[:], reads=[r_stF[s]], writes=[r_out])
                b.op("pe", lambda e, Fs=Fs: e.transpose(out=pgt[0:8, 0:128], in_=Fs[:, 4:12], identity=identf[:]),
                     reads=[r_stF[s], r_idf], writes=[r_pgt])
                b.op("act", lambda e, s=s: e.copy(out=stG[s][:], in_=pgt[0:8, 0:128]),
                     reads=[r_pgt], writes=[r_stG[s]])
                b.dma("pool", gT_d[t], stG[s][:], reads=[r_stG[s]], writes=[r_out])
                b.dma("pool", featT_d[t], T[:], reads=[r_stT[s]], writes=[r_out])
            pending.append(out_job)
        while pending:
            pending.pop(0)()
        b.barrier()
        b.es = old


def inv_freq_table():
    half = 32
    inv = (np.float32(10000.0) ** (-(np.arange(half, dtype=np.float32) / np.float32(half)))).astype(np.float32)
    return np.ascontiguousarray(np.broadcast_to(inv[None, :], (128, half))).astype(np.float32)


def build_p1(l, nt):
    nc = bass.Bass("TRN2", target_bir_lowering=False)
    x_d = nc.dram_tensor("x", [nt * 128, D], F32, kind="ExternalInput").ap()
    pos_d = nc.dram_tensor("pos", [128, nt], mybir.dt.int32, kind="ExternalInput").ap()
    invf_d = nc.dram_tensor("invf", [128, 32], F32, kind="ExternalInput").ap()
    identf_d = nc.dram_tensor("identf", [128, 128], F32, kind="ExternalInput").ap()
    win_d = nc.dram_tensor("w_in", [DEPTH, D, INW], F32, kind="ExternalInput").ap()
    modcol_d = nc.dram_tensor("modcol", [128, DEPTH * 48], F32, kind="ExternalInput").ap()
    ib_d = nc.dram_tensor("i_bias", [DEPTH, 4], F32, kind="ExternalInput").ap()
    fb_d = nc.dram_tensor("f_bias", [DEPTH, 4], F32, kind="ExternalInput").ap()
    featT_d = nc.dram_tensor("featT", [nt, 128, NFT, 128], BF16, kind="ExternalOutput").ap()
    tokA_d = nc.dram_tensor("tokA", [nt * 128, 1792], BF16, kind="ExternalOutput").ap()
    tokF_d = nc.dram_tensor("tokF", [nt * 128, 12], F32, kind="ExternalOutput").ap()
    gT_d = nc.dram_tensor("gT", [nt, 8, 128], F32, kind="ExternalOutput").ap()
    with ExitStack() as es, nc.allow_low_precision("bf16 matmul operands, fp32 accumulation"):
        b = B(nc, es)
        cos = b.sb("cos", [128, nt, 32], F32)
        sin = b.sb("sin", [128, nt, 32], F32)
        r_tab = b.res()
        emit_rope_tables(b, pos_d, invf_d, cos, sin, r_tab, nt)
        emit_p1(b, l, nt, x_d, win_d, modcol_d, ib_d, fb_d, identf_d, cos, sin, r_tab,
                featT_d, tokA_d, tokF_d, gT_d, b.res(), b.res())
        b.finish()
    return nc


NIT = 18
TOPK = 256


def gidx(g):
    return (g % 2) * NT + g // 2


def emit_p2a(b, nslots, featT_all, tokA_all, featT_own, tokF_own, vis_d, pw_d, identf_d, o_d, r_in, r_out):
    nc = b.nc
    NACT = 0
    with ExitStack() as es:
        old = b.es
        b.es = es
        kT = b.sb("a_kT", [128, 4, SEQ], BF16)
        ikT = b.sb("a_ikT", [128, SEQ], BF16)
        Va = b.sb("a_Va", [128, 32, 8, 65], BF16)
        iw = b.sb("a_iw", [128, NT, 4], F32)
        vis = b.sb("a_vis", [128, 256], F32)
        pw = b.sb("a_pw", [128, NIT + 1], F32)
        identf = b.sb("a_idf", [128, 128], F32)
        identb = b.sb("a_idb", [128, 128], BF16)
        Ib = [b.sb("a_I%d" % i, [128, SEQ], F32) for i in range(3)]
        rl = [b.sb("a_rl%d" % i, [128, 512], F32) for i in range(2)]
        Mbs = [b.sb("a_Mb%d" % i, [128, SEQ], BF16) for i in range(3)]
        MT = [b.sb("a_MT%d" % i, [128, 32, 128], BF16) for i in range(3)]
        E = [b.sb("a_E%d" % i, [128, 512], BF16) for i in range(3)]
        qpad = [b.sb("a_qpad%d" % i, [128, 8, 128], BF16) for i in range(2)]
        iqpad = [b.sb("a_iqpad%d" % i, [128, 4, 128], BF16) for i in range(3)]
        r_qpad = [b.res(), b.res()]
        r_iqpad = [b.res(), b.res(), b.res()]
        sms = [b.sb("a_sm%d" % i, [128, 16], F32) for i in range(3)]
        rcp = b.sb("a_rcp", [128, 8], F32)
        deltas = [b.sb("a_delta%d" % i, [128, NIT + 1], F32) for i in range(3)]
        ndeltas = [b.sb("a_ndelta%d" % i, [128, NIT + 1], F32) for i in range(3)]
        osb = [b.sb("a_o%d" % i, [128, 512], BF16) for i in range(2)]
        pI = [b.ps("a_pI%d" % i, [128, 512]) for i in range(2)]
        pS = [b.ps("a_pS%d" % i, [128, 512]) for i in range(3)]
        pO = [b.ps("a_pO%d" % i, [128, 512]) for i in range(2)]
        pMT = b.ps("a_pMT", [128, 1024], BF16)

        (r_kT, r_ikT, r_Va, r_qT, r_iqT, r_iw, r_vis, r_pw, r_idf, r_idb, r_pMT, r_vst, r_rcp) = (
            b.res() for _ in range(13))
        r_I = [b.res(), b.res(), b.res()]
        r_rl = [b.res(), b.res()]
        r_Mb = [b.res(), b.res(), b.res()]
        r_MT = [b.res(), b.res(), b.res()]
        r_E = [b.res() for _ in range(3)]
        r_sm = [b.res(), b.res(), b.res()]
        r_delta = [b.res(), b.res(), b.res()]
        r_osb = [b.res(), b.res()]
        r_pI = [b.res(), b.res()]
        r_pS = [b.res() for _ in range(3)]
        r_pO = [b.res(), b.res()]

        b.dma("sp", identf[:], identf_d, writes=[r_idf])
        b.op("dve", lambda e: e.tensor_copy(out=identb[:], in_=identf[:]), reads=[r_idf], writes=[r_idb])
        b.dma("sp", vis[:], vis_d, writes=[r_vis])
        b.dma("sp", pw[:], pw_d, writes=[r_pw])
        b.dma("sp", iw[:], tokF_own[:, 0:4].rearrange("(t p) c -> p t c", p=128), reads=[r_in], writes=[r_iw])
        ngl = 2 * nslots
        for g in range(ngl):
            gi = gidx(g)
            b.dma("sp", kT[:, :, g * 128:(g + 1) * 128], featT_all[gi, :, 4:8, :], reads=[r_in], writes=[r_kT])
            b.dma("sp", ikT[:, g * 128:(g + 1) * 128], featT_all[gi, :, 10, :], reads=[r_in], writes=[r_ikT])
        b.op("pool", lambda e: e.memset(Va[:, :, :, 64:65], 1.0), writes=[r_Va])
        vst = b.sb("a_vst", [128, 8, 512], BF16)
        for g0 in range(0, ngl, 8):
            n = min(8, ngl - g0)
            for i in range(n):
                gi = gidx(g0 + i)
                b.dma("sp", vst[:, i, :], tokA_all[gi * 128:(gi + 1) * 128, 0:512], reads=[r_in], writes=[r_vst])
            b.op("pool", lambda e, g0=g0, n=n: e.tensor_copy(
                out=Va[:, g0:g0 + n, :, 0:64],
                in_=vst[:, 0:n, :].rearrange("p g (h d) -> p g h d", d=64)),
                reads=[r_vst], writes=[r_Va])

        for i in range(2):
            b.op("pool", lambda e, i=i: e.memset(qpad[i][:], 0.0), writes=[r_qpad[i]])
        for i in range(3):
            b.op("pool", lambda e, i=i: e.memset(iqpad[i][:], 0.0), writes=[r_iqpad[i]])
        LO, HI, RNG, CAND, CNT, VV, TT, NCD, SS, SG = range(10)
        MBIG = 30000.0
        cnts = {"i": 0, "s": 0}

        def sel_gen(m):
            L = (2 * m + 2) * 128
            nkb = 2 * m + 2
            s = m % 3
            I = Ib[s]
            sm = sms[s]
            delta = deltas[s]
            ndelta = ndeltas[s]
            Mb = Mbs[s]
            rsm = r_sm[s]
            rdl = r_delta[s]
            for half in range(2):
                hsl = slice(half * 64, half * 64 + 64)
                b.dma("sp", iqpad[s][hsl, half::2, :], featT_own[m, hsl, 8:10, :], reads=[r_in],
                      writes=[r_iqpad[s]])
            for kg in range((L + 511) // 512):
                w = min(512, L - kg * 512)
                for h in range(4):
                    pi = cnts["i"] % 2
                    cnts["i"] += 1
                    hs = slice((h % 2) * 64, (h % 2) * 64 + 64)
                    b.op("pe", lambda e, pi=pi, h=h, kg=kg, w=w: e.matmul(
                        pI[pi][:, 0:w], lhsT=iqpad[s][:, h, :],
                        rhs=ikT[:, kg * 512:kg * 512 + w], start=True, stop=True),
                        reads=[r_iqpad[s], r_ikT], writes=[r_pI[pi]])
                    b.op("act", lambda e, pi=pi, w=w: e.activation(out=rl[pi][:, 0:w], in_=pI[pi][:, 0:w],
                                                                    func=AF.Relu),
                         reads=[r_pI[pi]], writes=[r_rl[pi]])
                    dst = I[:, kg * 512:kg * 512 + w]
                    if h == 0:
                        b.op("dve", lambda e, pi=pi, w=w, dst=dst: e.tensor_scalar(
                            out=dst, in0=rl[pi][:, 0:w], scalar1=iw[:, m, 0:1], scalar2=None, op0=ALU.mult),
                            reads=[r_rl[pi], r_iw], writes=[r_I[s]])
                    else:
                        b.op("dve", lambda e, pi=pi, w=w, dst=dst, h=h: e.scalar_tensor_tensor(
                            out=dst, in0=rl[pi][:, 0:w], scalar=iw[:, m, h:h + 1], in1=dst,
                            op0=ALU.mult, op1=ALU.add),
                            reads=[r_rl[pi], r_iw, r_I[s]], writes=[r_I[s]])
                    yield
            if m == 0:
                b.op("dve", lambda e: e.tensor_tensor(out=I[:, L - 256:L], in0=I[:, L - 256:L],
                                                      in1=vis[:], op=ALU.add),
                     reads=[r_I[s], r_vis], writes=[r_I[s]])
                b.op("dve", lambda e: e.memset(sm[:, TT:TT + 1], 0.5 * NEG), writes=[rsm])
            else:
                b.op("dve", lambda e: e.tensor_reduce(out=sm[:, LO:LO + 1], in_=I[:, 0:L - 256],
                                                      axis=AX.X, op=ALU.min),
                     reads=[r_I[s]], writes=[rsm])
                b.op("dve", lambda e: e.tensor_tensor(out=I[:, L - 256:L], in0=I[:, L - 256:L],
                                                      in1=vis[:], op=ALU.add),
                     reads=[r_I[s], r_vis], writes=[r_I[s]])
                b.op("dve", lambda e: e.tensor_reduce(out=sm[:, HI:HI + 1], in_=I[:, 0:L],
                                                      axis=AX.X, op=ALU.max),
                     reads=[r_I[s]], writes=[rsm])
                yield
                b.op("dve", lambda e: e.tensor_tensor(out=sm[:, RNG:RNG + 1], in0=sm[:, HI:HI + 1],
                                                      in1=sm[:, LO:LO + 1], op=ALU.subtract),
                     reads=[rsm], writes=[rsm])
                b.op("dve", lambda e: e.tensor_scalar(out=delta[:], in0=pw[:], scalar1=sm[:, RNG:RNG + 1],
                                                      scalar2=None, op0=ALU.mult),
                     reads=[rsm, r_pw], writes=[rdl])
                b.op("dve", lambda e: e.tensor_scalar(out=ndelta[:], in0=delta[:], scalar1=-1.0,
                                                      scalar2=None, op0=ALU.mult),
                     reads=[rdl], writes=[rdl])
                b.op("dve", lambda e: e.tensor_tensor(out=sm[:, NCD:NCD + 1], in0=ndelta[:, 0:1],
                                                      in1=sm[:, LO:LO + 1], op=ALU.subtract),
                     reads=[rsm, rdl], writes=[rsm])
                yield
                for n in range(NACT):
                    b.op("act", lambda e: e.activation(
                        out=Mb[:, 0:L], in_=I[:, 0:L], func=AF.Sign, bias=sm[:, NCD:NCD + 1],
                        accum_out=sm[:, SS:SS + 1]),
                        reads=[r_I[s], rsm], writes=[r_Mb[s], rsm])
                    b.op("act", lambda e: e.activation(
                        out=sm[:, SG:SG + 1], in_=sm[:, SS:SS + 1], func=AF.Sign, bias=float(L - (2 * TOPK - 1))),
                        reads=[rsm], writes=[rsm])
                    b.op("act", lambda e, n=n: e.activation(
                        out=sm[:, NCD:NCD + 1], in_=sm[:, SG:SG + 1], func=AF.Identity,
                        scale=ndelta[:, n + 1:n + 2], bias=sm[:, NCD:NCD + 1]),
                        reads=[rsm, rdl], writes=[rsm])
                    yield
                b.op("dve", lambda e: e.tensor_scalar(out=sm[:, CAND:CAND + 1], in0=sm[:, NCD:NCD + 1],
                                                      scalar1=-1.0, scalar2=None, op0=ALU.mult),
                     reads=[rsm], writes=[rsm])
                for n in range(NACT, NIT):
                    b.op("dve", lambda e: e.tensor_scalar(
                        out=Mb[:, 0:L], in0=I[:, 0:L], scalar1=sm[:, CAND:CAND + 1], scalar2=0.0,
                        op0=ALU.is_ge, op1=ALU.add, accum_out=sm[:, CNT:CNT + 1]),
                        reads=[r_I[s], rsm], writes=[r_Mb[s], rsm])
                    b.op("dve", lambda e: e.tensor_scalar(
                        out=sm[:, VV:VV + 1], in0=sm[:, CNT:CNT + 1], scalar1=TOPK - 0.5, scalar2=0.5,
                        op0=ALU.is_ge, op1=ALU.subtract),
                        reads=[rsm], writes=[rsm])
                    b.op("dve", lambda e, n=n: e.scalar_tensor_tensor(
                        out=sm[:, CAND:CAND + 1], in0=sm[:, VV:VV + 1], scalar=delta[:, n:n + 1],
                        in1=sm[:, CAND:CAND + 1], op0=ALU.mult, op1=ALU.add),
                        reads=[rsm, rdl], writes=[rsm])
                    yield
                b.op("dve", lambda e: e.tensor_tensor(out=sm[:, TT:TT + 1], in0=sm[:, CAND:CAND + 1],
                                                      in1=delta[:, NIT:NIT + 1], op=ALU.subtract),
                     reads=[rsm, rdl], writes=[rsm])
            b.op("dve", lambda e: e.tensor_scalar(
                out=Mb[:, 0:L], in0=I[:, 0:L], scalar1=sm[:, TT:TT + 1], scalar2=None, op0=ALU.is_ge),
                reads=[r_I[s], rsm], writes=[r_Mb[s]])
            yield
            MTs = MT[s]
            for k0 in range(0, nkb, 8):
                n = min(8, nkb - k0)
                for i in range(n):
                    b.op("pe", lambda e, i=i, k0=k0: e.transpose(
                        out=pMT[:, i * 128:(i + 1) * 128], in_=Mb[:, (k0 + i) * 128:(k0 + i + 1) * 128],
                        identity=identb[:]),
                        reads=[r_Mb[s], r_idb], writes=[r_pMT], inc=(i == n - 1))
                b.op("act", lambda e, k0=k0, n=n: e.activation(
                    out=MTs[:, k0:k0 + n, :], in_=pMT[:, 0:n * 128].rearrange("p (n k) -> p n k", k=128),
                    func=AF.Identity, scale=MBIG, bias=-MBIG),
                    reads=[r_pMT], writes=[r_MT[s]])
                yield

        def att_gen(m):
            nkb = 2 * m + 2
            s = m % 2
            s3 = m % 3
            MTs = MT[s3]
            O = osb[s]
            for half in range(2):
                hsl = slice(half * 64, half * 64 + 64)
                b.dma("sp", qpad[s][hsl, half::2, :], featT_own[m, hsl, 0:4, :], reads=[r_in],
                      writes=[r_qpad[s]])
            units = [(h, k0, min(4, nkb - k0)) for h in range(8) for k0 in range(0, nkb, 4)]

            def emit_pv(u, si):
                h, k0, n = u
                p = h // 2
                po = pO[h // 4]
                r_po = r_pO[h // 4]
                oc = (h % 4) * 65
                for i in range(n):
                    kb = k0 + i
                    b.op("pe", lambda e, si=si, i=i, kb=kb, h=h, po=po, oc=oc: e.matmul(
                        po[:, oc:oc + 65], lhsT=E[si][:, i * 128:(i + 1) * 128], rhs=Va[:, kb, h, :],
                        start=(kb == 0), stop=(kb == nkb - 1)),
                        reads=[r_E[si], r_Va], writes=[r_po], inc=(i == n - 1))
                if k0 + n == nkb:
                    b.op("dve", lambda e, po=po, oc=oc, h=h: e.reciprocal(out=rcp[:, h:h + 1],
                                                                           in_=po[:, oc + 64:oc + 65]),
                         reads=[r_po], writes=[r_rcp])
                    b.op("dve", lambda e, po=po, oc=oc, h=h: e.tensor_scalar(
                        out=O[:, h * 64:(h + 1) * 64], in0=po[:, oc:oc + 64], scalar1=rcp[:, h:h + 1],
                        scalar2=None, op0=ALU.mult),
                        reads=[r_po, r_rcp], writes=[r_osb[s]])

            prev = None
            for u in units:
                h, k0, n = u
                p = h // 2
                si = cnts["s"] % 3
                cnts["s"] += 1
                b.op("pe", lambda e, si=si, k0=k0, n=n: e.matmul(
                    pS[si][:, 0:n * 128], lhsT=identb[:, :],
                    rhs=MTs[:, k0:k0 + n, :].rearrange("p n k -> p (n k)"), start=True, stop=False),
                    reads=[r_idb, r_MT[s3]], writes=[r_pS[si]], inc=False)
                for i in range(n):
                    kb = k0 + i
                    b.op("pe", lambda e, si=si, i=i, kb=kb, p=p, h=h, n=n: e.matmul(
                        pS[si][:, i * 128:(i + 1) * 128], lhsT=kT[:, p, kb * 128:(kb + 1) * 128],
                        rhs=qpad[s][:, h, :], start=False, stop=(i == n - 1)),
                        reads=[r_kT, r_qpad[s]], writes=[r_pS[si]], inc=(i == n - 1))
                b.op("act", lambda e, si=si, n=n: e.activation(
                    out=E[si][:, 0:n * 128], in_=pS[si][:, 0:n * 128], func=AF.Exp, scale=0.125),
                    reads=[r_pS[si]], writes=[r_E[si]])
                if prev is not None:
                    emit_pv(*prev)
                prev = (u, si)
                yield
            emit_pv(*prev)
            b.dma("pool", o_d[m * 128:(m + 1) * 128, 0:512], O[:], reads=[r_osb[s]], writes=[r_out])

        def step(gen):
            try:
                next(gen)
                return True
            except StopIteration:
                return False

        for _ in sel_gen(0):
            pass
        cur = sel_gen(1) if nslots > 1 else None
        for m in range(nslots):
            A = att_gen(m)
            nxt = sel_gen(m + 2) if m + 2 < nslots else None
            a_live = True
            while a_live:
                a_live = step(A)
                if cur is not None and not step(cur):
                    cur = None
                if nxt is not None and cur is not None:
                    if not step(nxt):
                        nxt = None
            while cur is not None:
                if not step(cur):
                    cur = None
            cur = nxt
        b.barrier()
        b.es = old


def vis_table(j):
    v = np.zeros((128, 256), np.float32)
    diag = np.zeros((128, 128), np.float32)
    diag[0:64, 64:128] = NEG
    if j == 0:
        v[:, 0:128] = diag
        v[:, 128:256] = NEG
    else:
        v[:, 128:256] = diag
    return v


def pw_table():
    return np.ascontiguousarray(np.broadcast_to(
        (0.5 ** np.arange(1, NIT + 2, dtype=np.float64)).astype(np.float32)[None, :], (128, NIT + 1)))


def build_p2a(nslots):
    nc = bass.Bass("TRN2", target_bir_lowering=False)
    featT_all = nc.dram_tensor("featT_all", [2 * NT, 128, NFT, 128], BF16, kind="ExternalInput").ap()
    tokA_all = nc.dram_tensor("tokA_all", [2 * TOK, 1792], BF16, kind="ExternalInput").ap()
    featT_own = nc.dram_tensor("featT_own", [NT, 128, NFT, 128], BF16, kind="ExternalInput").ap()
    tokF_own = nc.dram_tensor("tokF_own", [TOK, 12], F32, kind="ExternalInput").ap()
    vis_d = nc.dram_tensor("vis", [128, 256], F32, kind="ExternalInput").ap()
    pw_d = nc.dram_tensor("pw", [128, NIT + 1], F32, kind="ExternalInput").ap()
    identf_d = nc.dram_tensor("identf", [128, 128], F32, kind="ExternalInput").ap()
    o_d = nc.dram_tensor("o", [TOK, D], BF16, kind="ExternalOutput").ap()
    with ExitStack() as es, nc.allow_low_precision("bf16 matmul operands, fp32 accumulation"):
        b = B(nc, es)
        emit_p2a(b, nslots, featT_all, tokA_all, featT_own, tokF_own, vis_d, pw_d, identf_d, o_d,
                 b.res(), b.res())
        b.finish()
    return nc


def gen_head_norm(b, src, r_src, gate, r_gate, dst, r_dst, tmp, st, r_tmp, r_st):
    b.op("dve", lambda e: e.tensor_reduce(out=st[:, 0:4], in_=src[:], axis=AX.X, op=ALU.add),
         reads=[r_src], writes=[r_st])
    yield
    b.op("dve", lambda e: e.tensor_scalar(out=st[:, 0:4], in0=st[:, 0:4], scalar1=1.0 / 64, scalar2=None,
                                          op0=ALU.mult), reads=[r_st], writes=[r_st])
    yield
    b.op("dve", lambda e: e.tensor_tensor(out=src[:], in0=src[:],
                                          in1=st[:, 0:4].unsqueeze(2).to_broadcast([128, 4, 64]),
                                          op=ALU.subtract), reads=[r_src, r_st], writes=[r_src])
    yield
    b.op("dve", lambda e: e.tensor_tensor(out=tmp[:], in0=src[:], in1=src[:], op=ALU.mult),
         reads=[r_src], writes=[r_tmp])
    yield
    b.op("dve", lambda e: e.tensor_reduce(out=st[:, 4:8], in_=tmp[:], axis=AX.X, op=ALU.add),
         reads=[r_tmp], writes=[r_st])
    yield
    b.op("dve", lambda e: e.tensor_scalar(out=st[:, 4:8], in0=st[:, 4:8], scalar1=1.0 / 64, scalar2=LN_EPS,
                                          op0=ALU.mult, op1=ALU.add), reads=[r_st], writes=[r_st])
    yield
    b.op("act", lambda e: e.activation(out=st[:, 8:12], in_=st[:, 4:8], func=AF.Sqrt),
         reads=[r_st], writes=[r_st])
    yield
    b.op("dve", lambda e: e.reciprocal(out=st[:, 12:16], in_=st[:, 8:12]), reads=[r_st], writes=[r_st])
    yield
    if gate is None:
        b.op("dve", lambda e: e.tensor_tensor(
            out=dst.rearrange("p (h d) -> p h d", d=64), in0=src[:],
            in1=st[:, 12:16].unsqueeze(2).to_broadcast([128, 4, 64]), op=ALU.mult),
            reads=[r_src, r_st], writes=[r_dst])
        yield
    else:
        b.op("dve", lambda e: e.tensor_tensor(
            out=tmp[:], in0=src[:], in1=st[:, 12:16].unsqueeze(2).to_broadcast([128, 4, 64]), op=ALU.mult),
            reads=[r_src, r_st], writes=[r_tmp])
        yield
        b.op("dve", lambda e: e.tensor_tensor(
            out=dst.rearrange("p (h d) -> p h d", d=64), in0=tmp[:],
            in1=gate.rearrange("p (h d) -> p h d", d=64), op=ALU.mult),
            reads=[r_tmp, r_gate], writes=[r_dst])
        yield


def emit_head_norm(b, src, r_src, gate, r_gate, dst, r_dst, tmp, st, r_tmp, r_st):
    for _ in gen_head_norm(b, src, r_src, gate, r_gate, dst, r_dst, tmp, st, r_tmp, r_st):
        pass


GAMMAS = [1.0 - 2.0 ** (-5.0 - h) for h in range(4)]


def ret_tables():
    i = np.arange(128)
    decT = np.zeros((128, 4, 128), np.float32)
    qdecT = np.zeros((128, 2, 128), np.float32)
    kdec = np.zeros((128, 4), np.float32)
    for h, g in enumerate(GAMMAS):
        diff = i[None, :] - i[:, None]
        decT[:, h, :] = np.where(diff >= 0, g ** np.maximum(diff, 0), 0.0) / 8.0
        qdecT[(h % 2) * 64:(h % 2) * 64 + 64, h // 2, :] = (g ** (i + 1.0))[None, :]
        kdec[:, h] = g ** (127.0 - i) / 8.0
    return decT, qdecT, kdec


def emit_p2b(b, nslots, tokA_all, featT_own, tokA_own, decT_d, qdecT_d, kdec_d, jf_d, o_d, r_in, r_out,
             whole=False):
    with ExitStack() as es:
        old = b.es
        b.es = es
        decT = b.sb("b_decT", [128, 4, 128], F32)
        qdecT = b.sb("b_qdecT", [128, 2, 128], F32)
        kdec = b.sb("b_kdec", [128, 4], F32)
        jf = b.sb("b_jf", [128, 1], F32)
        S = b.sb("b_S", [128, 2, 64], F32)
        SA = b.sb("b_SA", [128, 2, 64], F32)
        Sd = b.sb("b_Sd", [128, 2, 64], F32)
        Sbf = b.sb("b_Sbf", [128, 2, 64], BF16)
        kvin = [b.sb("b_kvin%d" % i, [128, 512], BF16) for i in range(2)]
        vdec = [b.sb("b_vdec%d" % i, [128, 256], BF16) for i in range(2)]
        qk = [b.sb("b_qk%d" % i, [128, 4, 128], BF16) for i in range(2)]
        qd = [b.sb("b_qd%d" % i, [128, 2, 128], BF16) for i in range(2)]
        own = [b.sb("b_own%d" % i, [128, 512], BF16) for i in range(2)]
        scT = [b.sb("b_scT%d" % i, [128, 128], BF16) for i in range(2)]
        ysbs = [b.sb("b_ysb%d" % i, [128, 4, 64], F32) for i in range(2)]
        tmps = [b.sb("b_tmp%d" % i, [128, 4, 64], F32) for i in range(2)]
        sts = [b.sb("b_st%d" % i, [128, 16], F32) for i in range(2)]
        r_ysbs, r_tmps, r_sts = ([b.res(), b.res()] for _ in range(3))
        pending_fin = None
        fin_i = 0
        osb = [b.sb("b_o%d" % i, [128, 256], BF16) for i in range(2)]
        pKV = [b.ps("b_pKV%d" % i, [128, 128]) for i in range(2)]
        pSC = [b.ps("b_pSC%d" % i, [128, 128]) for i in range(2)]
        pY = b.ps("b_pY", [128, 256])
        (r_c, r_S, r_SA, r_Sd, r_Sbf, r_ysb, r_tmp, r_st, r_pY) = (b.res() for _ in range(9))
        r_kvin = [b.res(), b.res()]
        r_vdec = [b.res(), b.res()]
        r_qk = [b.res(), b.res()]
        r_qd = [b.res(), b.res()]
        r_own = [b.res(), b.res()]
        r_scT = [b.res(), b.res()]
        r_osb = [b.res(), b.res()]
        r_pKV = [b.res(), b.res()]
        r_pSC = [b.res(), b.res()]

        b.dma("sp", decT[:], decT_d, writes=[r_c])
        b.dma("sp", qdecT[:], qdecT_d, writes=[r_c])
        b.dma("sp", kdec[:], kdec_d, writes=[r_c])
        b.dma("sp", jf[:], jf_d, writes=[r_c])
        b.op("dve", lambda e: e.memset(S[:], 0.0), writes=[r_S])
        sc_i = 0
        for g in range(2 * nslots):
            m, r = g // 2, g % 2
            s = g % 2
            gi = gidx(g)
            b.dma("sp", kvin[s][:], tokA_all[gi * 128:(gi + 1) * 128, 512:1024], reads=[r_in], writes=[r_kvin[s]])
            if whole:
                so = g % 2
                orow = gi * 128
                b.dma("sp", qk[so][:], featT_own[gi, :, 11:15, :], reads=[r_in], writes=[r_qk[so]])
                b.dma("sp", own[so][:], tokA_all[gi * 128:(gi + 1) * 128, 768:1280], reads=[r_in], writes=[r_own[so]])
                b.op("dve", lambda e: e.tensor_copy(out=Sbf[:], in_=S[:]), reads=[r_S], writes=[r_Sbf])
            elif r == 0:
                b.op("dve", lambda e: e.tensor_copy(out=SA[:], in_=S[:]), reads=[r_S], writes=[r_SA])
                so = m % 2
                b.dma("sp", qk[so][:], featT_own[m, :, 11:15, :], reads=[r_in], writes=[r_qk[so]])
                b.dma("sp", own[so][:], tokA_own[m * 128:(m + 1) * 128, 768:1280], reads=[r_in], writes=[r_own[so]])
            else:
                so = m % 2
                orow = m * 128
                b.op("dve", lambda e: e.tensor_tensor(out=Sd[:], in0=S[:], in1=SA[:], op=ALU.subtract),
                     reads=[r_S, r_SA], writes=[r_Sd])
                b.op("dve", lambda e: e.scalar_tensor_tensor(
                    out=Sbf[:].rearrange("p a e -> p (a e)"), in0=Sd[:].rearrange("p a e -> p (a e)"),
                    scalar=jf[:, 0:1], in1=SA[:].rearrange("p a e -> p (a e)"), op0=ALU.mult, op1=ALU.add),
                    reads=[r_Sd, r_SA, r_c], writes=[r_Sbf])
            if whole or r == 1:
                b.op("dve", lambda e, so=so: e.tensor_tensor(
                    out=qd[so][:], in0=qk[so][:, 0:2, :], in1=qdecT[:], op=ALU.mult),
                    reads=[r_qk[so], r_c], writes=[r_qd[so]])
                for h in range(4):
                    hs = slice((h % 2) * 64, (h % 2) * 64 + 64)
                    p = h // 2
                    si = sc_i % 2
                    sc_i += 1
                    b.op("pe", lambda e, so=so, hs=hs, p=p, si=si: e.matmul(
                        pSC[si][:, :], lhsT=qk[so][hs, 2 + p, :], rhs=qk[so][hs, p, :], start=True, stop=True),
                        reads=[r_qk[so]], writes=[r_pSC[si]])
                    b.op("dve", lambda e, si=si, h=h: e.tensor_tensor(
                        out=scT[si][:], in0=pSC[si][:], in1=decT[:, h, :], op=ALU.mult),
                        reads=[r_pSC[si], r_c], writes=[r_scT[si]])
                    b.op("pe", lambda e, si=si, h=h, so=so: e.matmul(
                        pY[:, h * 64:(h + 1) * 64], lhsT=scT[si][:], rhs=own[so][:, h * 64:(h + 1) * 64],
                        start=True, stop=False),
                        reads=[r_scT[si], r_own[so]], writes=[r_pY], inc=False)
                    b.op("pe", lambda e, h=h, so=so, hs=hs, p=p: e.matmul(
                        pY[:, h * 64:(h + 1) * 64], lhsT=qd[so][hs, p, :], rhs=Sbf[hs, p, :],
                        start=False, stop=True),
                        reads=[r_qd[so], r_Sbf], writes=[r_pY])
                    for _k in range(3):
                        if pending_fin is not None:
                            try:
                                next(pending_fin)
                            except StopIteration:
                                pending_fin = None
                fb = fin_i % 2
                fin_i += 1
                ysb_, tmp_, st_ = ysbs[fb], tmps[fb], sts[fb]
                r_ysb_, r_tmp_, r_st_ = r_ysbs[fb], r_tmps[fb], r_sts[fb]
                b.op("act", lambda e, ysb_=ysb_: e.copy(out=ysb_[:].rearrange("p h d -> p (h d)"), in_=pY[:]),
                     reads=[r_pY], writes=[r_ysb_])

                def fin_gen(ysb_=ysb_, tmp_=tmp_, st_=st_, r_ysb_=r_ysb_, r_tmp_=r_tmp_, r_st_=r_st_, so=so,
                            orow=orow):
                    for _ in gen_head_norm(b, ysb_, r_ysb_, own[so][:, 256:512], r_own[so], osb[so][:], r_osb[so],
                                           tmp_, st_, r_tmp_, r_st_):
                        yield
                    b.dma("pool", o_d[orow:orow + 128, 512:768], osb[so][:], reads=[r_osb[so]], writes=[r_out])

                while pending_fin is not None:
                    try:
                        next(pending_fin)
                    except StopIteration:
                        pending_fin = None
                pending_fin = fin_gen()
            if g == 2 * nslots - 1:
                break
            b.op("dve", lambda e, s=s: e.tensor_tensor(
                out=vdec[s][:].rearrange("p (h d) -> p h d", d=64),
                in0=kvin[s][:, 256:512].rearrange("p (h d) -> p h d", d=64),
                in1=kdec[:].unsqueeze(2).to_broadcast([128, 4, 64]), op=ALU.mult),
                reads=[r_kvin[s], r_c], writes=[r_vdec[s]])
            for p in range(2):
                b.op("pe", lambda e, s=s, p=p: e.matmul(
                    pKV[p][:, :], lhsT=kvin[s][:, p * 128:(p + 1) * 128], rhs=vdec[s][:, p * 128:(p + 1) * 128],
                    start=True, stop=True), reads=[r_kvin[s], r_vdec[s]], writes=[r_pKV[p]])
                for half in range(2):
                    h = 2 * p + half
                    hs = slice(half * 64, half * 64 + 64)
                    b.op("dve", lambda e, p=p, hs=hs, half=half, h=h: e.scalar_tensor_tensor(
                        out=S[hs, p, :], in0=S[hs, p, :], scalar=float(GAMMAS[h] ** 128),
                        in1=pKV[p][hs, half * 64:(half + 1) * 64], op0=ALU.mult, op1=ALU.add),
                        reads=[r_S, r_pKV[p]], writes=[r_S])
        while pending_fin is not None:
            try:
                next(pending_fin)
            except StopIteration:
                pending_fin = None
        b.barrier()
        b.es = old


def emit_p2c(b, l, nslots, featT_all, tokA_all, gT_all, tokA_own, convw_d, convb_d, caus_d, sel4_d, eye4_d,
             jf_d, identf_d, o_d, r_in, r_out, whole=False):
    NG = 2 * nslots
    NTK = NG * 128
    NOWN = NG if whole else nslots
    with ExitStack() as es:
        old = b.es
        b.es = es
        jf = b.sb("c_jf", [128, 1], F32)
        caus = b.sb("c_caus", [128, 128], F32)
        sel4 = b.sb("c_sel4", [4, 4, 128], F32)
        eye4 = b.sb("c_eye4", [4, 4], F32)
        ones4 = b.sb("c_ones4", [4, 128], F32)
        identf = b.sb("c_idf", [128, 128], F32)
        identb = b.sb("c_idb", [128, 128], BF16)
        qkall = b.sb("c_qkall", [128, 4, NTK], BF16)
        qkown = qkall if whole else b.sb("c_qkown", [128, 4, nslots * 128], BF16)
        GTo = b.sb("c_GTo", [4, NOWN, 128], F32)
        acol = b.sb("c_acol", [128, NG, 4], F32)
        ecol = b.sb("c_ecol", [128, NG, 4], F32)
        acolo = b.sb("c_acolo", [128, NOWN, 4], F32)
        ecolo = b.sb("c_ecolo", [128, NOWN, 4], F32)
        GR = b.sb("c_GR", [128, NG + 1, 4], F32)
        GRo = b.sb("c_GRo", [128, NOWN, 4], F32)
        dec = b.sb("c_dec", [128, NG, 4], F32)
        kw = b.sb("c_kw", [128, NG, 4], F32)
        r_c, r_qkall, r_qkown, r_GTo, r_cols, r_GR = (b.res() for _ in range(6))
        if whole:
            r_qkown = r_qkall

        b.dma("sp", jf[:], jf_d, writes=[r_c])
        b.dma("sp", caus[:], caus_d, writes=[r_c])
        b.dma("sp", sel4[:].rearrange("k h n -> k (h n)"), sel4_d, writes=[r_c])
        b.dma("sp", eye4[:], eye4_d, writes=[r_c])
        b.dma("sp", identf[:], identf_d, writes=[r_c])
        b.op("dve", lambda e: e.tensor_copy(out=identb[:], in_=identf[:]), reads=[r_c], writes=[r_c])
        b.op("dve", lambda e: e.memset(ones4[:], 1.0), writes=[r_c])

        with ExitStack() as es1:
            b.es = es1
            gi_ = b.sb("c1_gi", [4, NTK], F32)
            gf_ = b.sb("c1_gf", [4, NTK], F32)
            t1 = b.sb("c1_t1", [4, NTK], F32)
            Bn = b.sb("c1_Bn", [4, NTK], F32)
            aT = b.sb("c1_aT", [4, NTK], F32)
            GT = b.sb("c1_GT", [4, NTK], F32)
            eT = b.sb("c1_eT", [4, NTK], F32)
            onesr = b.sb("c1_ones", [4, NTK], F32)
            gend = b.sb("c1_gend", [4, NG, 4], F32)
            pT = b.ps("c1_pT", [128, 2 * NG * 4])
            pR = b.ps("c1_pR", [128, NG * 4])
            r_g, r_t1, r_Bn, r_aT, r_GT, r_eT, r_on, r_ge, r_pT, r_pR = (b.res() for _ in range(10))
            for g in range(NG):
                gi = gidx(g)
                b.dma("sp", gi_[:, g * 128:(g + 1) * 128], gT_all[gi, 0:4, :], reads=[r_in], writes=[r_g])
                b.dma("sp", gf_[:, g * 128:(g + 1) * 128], gT_all[gi, 4:8, :], reads=[r_in], writes=[r_g])
            b.op("dve", lambda e: e.memset(onesr[:], 1.0), writes=[r_on])
            b.op("act", lambda e: e.activation(out=t1[:], in_=gf_[:], func=AF.Exp, scale=-1.0),
                 reads=[r_g], writes=[r_t1])
            b.op("act", lambda e: e.activation(out=t1[:], in_=t1[:], func=AF.Ln, bias=1.0),
                 reads=[r_t1], writes=[r_t1])
            b.op("dve", lambda e: e.tensor_tensor_scan(out=Bn[:], data0=onesr[:], data1=t1[:], initial=0.0,
                                                       op0=ALU.mult, op1=ALU.add),
                 reads=[r_on, r_t1], writes=[r_Bn])
            b.op("dve", lambda e: e.tensor_tensor(out=aT[:], in0=gi_[:], in1=Bn[:], op=ALU.add),
                 reads=[r_g, r_Bn], writes=[r_aT])
            b.op("dve", lambda e: e.tensor_tensor_scan(out=GT[:], data0=onesr[:], data1=aT[:], initial=0.0,
                                                       op0=ALU.mult, op1=ALU.max),
                 reads=[r_on, r_aT], writes=[r_GT])
            b.op("dve", lambda e: e.tensor_tensor(out=eT[:], in0=Bn[:], in1=GT[:], op=ALU.subtract),
                 reads=[r_Bn, r_GT], writes=[r_eT])
            b.op("act", lambda e: e.activation(out=eT[:], in_=eT[:], func=AF.Exp), reads=[r_eT], writes=[r_eT])
            if whole:
                b.op("dve", lambda e: e.tensor_copy(out=GTo[:].rearrange("k g n -> k (g n)"), in_=GT[:]),
                     reads=[r_GT], writes=[r_GTo])
            else:
                GTv = GT[:].rearrange("k (m r n) -> k m r n", r=2, n=128)
                b.op("dve", lambda e: e.tensor_tensor(out=t1[:, 0:nslots * 128].rearrange("k (m n) -> k m n", n=128),
                                                      in0=GTv[:, :, 1, :], in1=GTv[:, :, 0, :], op=ALU.subtract),
                     reads=[r_GT, r_t1], writes=[r_t1])
                b.op("dve", lambda e: e.scalar_tensor_tensor(
                    out=GTo[:], in0=t1[:, 0:nslots * 128].rearrange("k (m n) -> k m n", n=128), scalar=jf[0:4, 0:1],
                    in1=GTv[:, :, 0, :], op0=ALU.mult, op1=ALU.add),
                    reads=[r_t1, r_GT, r_c], writes=[r_GTo])
            for g in range(NG):
                b.op("pe", lambda e, g=g: e.transpose(out=pT[:, g * 4:(g + 1) * 4], in_=aT[:, g * 128:(g + 1) * 128],
                                                      identity=identf[0:4, 0:4]),
                     reads=[r_aT, r_c], writes=[r_pT], inc=False)
                b.op("pe", lambda e, g=g: e.transpose(out=pT[:, (NG + g) * 4:(NG + g + 1) * 4],
                                                      in_=eT[:, g * 128:(g + 1) * 128], identity=identf[0:4, 0:4]),
                     reads=[r_eT, r_c], writes=[r_pT], inc=(g == NG - 1))
            b.op("act", lambda e: e.copy(out=acol[:].rearrange("p g h -> p (g h)"), in_=pT[:, 0:NG * 4]),
                 reads=[r_pT], writes=[r_cols])
            b.op("act", lambda e: e.copy(out=ecol[:].rearrange("p g h -> p (g h)"), in_=pT[:, NG * 4:2 * NG * 4]),
                 reads=[r_pT], writes=[r_cols])
            b.op("dve", lambda e: e.tensor_tensor(
                out=gend[:], in0=GT[:].rearrange("k (g n) -> k g n", n=128)[:, :, 127:128].to_broadcast([4, NG, 4]),
                in1=eye4[:].unsqueeze(1).to_broadcast([4, NG, 4]), op=ALU.mult),
                reads=[r_GT, r_c], writes=[r_ge])
            b.op("pe", lambda e: e.matmul(pR[:, :], lhsT=ones4[:, :], rhs=gend[:].rearrange("k g h -> k (g h)"),
                                          start=True, stop=True), reads=[r_ge, r_c], writes=[r_pR])
            b.op("dve", lambda e: e.memset(GR[:, 0, :], 0.0), writes=[r_GR])
            b.op("act", lambda e: e.copy(out=GR[:, 1:NG + 1, :].rearrange("p g h -> p (g h)"), in_=pR[:]),
                 reads=[r_pR], writes=[r_GR])
            b.op("dve", lambda e: e.tensor_tensor(out=dec[:], in0=GR[:, 0:NG, :], in1=GR[:, 1:NG + 1, :],
                                                  op=ALU.subtract), reads=[r_GR], writes=[r_cols])
            b.op("act", lambda e: e.activation(out=dec[:], in_=dec[:], func=AF.Exp), reads=[r_cols], writes=[r_cols])
            b.op("dve", lambda e: e.tensor_tensor(out=kw[:], in0=acol[:], in1=GR[:, 1:NG + 1, :], op=ALU.subtract),
                 reads=[r_cols, r_GR], writes=[r_cols])
            b.op("act", lambda e: e.activation(out=kw[:], in_=kw[:], func=AF.Exp), reads=[r_cols], writes=[r_cols])
            b.op("dve", lambda e: e.tensor_scalar(out=kw[:], in0=kw[:], scalar1=0.125, scalar2=None, op0=ALU.mult),
                 reads=[r_cols], writes=[r_cols])

            def blend(dst, src, ncol):
                sv = src.rearrange("p (m r) h -> p m r h", r=2)
                b.op("dve", lambda e: e.tensor_tensor(out=dst, in0=sv[:, :, 1, :], in1=sv[:, :, 0, :],
                                                      op=ALU.subtract), reads=[r_cols, r_GR], writes=[r_cols])
                b.op("dve", lambda e: e.scalar_tensor_tensor(
                    out=dst, in0=dst, scalar=jf[:, 0:1], in1=sv[:, :, 0, :], op0=ALU.mult, op1=ALU.add),
                    reads=[r_cols, r_GR, r_c], writes=[r_cols])
            if whole:
                for dst_, src_ in ((acolo, acol[:]), (ecolo, ecol[:]), (GRo, GR[:, 0:NG, :])):
                    b.op("dve", lambda e, dst_=dst_, src_=src_: e.tensor_copy(out=dst_[:], in_=src_),
                         reads=[r_cols, r_GR], writes=[r_cols])
            else:
                blend(acolo[:], acol[:], 4)
                blend(ecolo[:], ecol[:], 4)
                blend(GRo[:], GR[:, 0:NG, :], 4)
            b.barrier()
            b.es = es

        with ExitStack() as es2:
            b.es = es2
            pre = b.sb("c2_pre", [128, 4, NTK + 3], BF16)
            acc = [b.sb("c2_acc%d" % i, [128, NTK], F32) for i in range(2)]
            cw = b.sb("c2_cw", [128, 4, 4], F32)
            cb = b.sb("c2_cb", [128, 4], F32)
            r_pre, r_cw = b.res(), b.res()
            r_acc = [b.res(), b.res()]
            b.dma("sp", cw[:], convw_d[l], writes=[r_cw])
            b.dma("sp", cb[:], convb_d[l], writes=[r_cw])
            b.op("pool", lambda e: e.memset(pre[:, :, 0:3], 0.0), writes=[r_pre])
            for g in range(NG):
                gi = gidx(g)
                b.dma("sp", pre[:, :, 3 + g * 128:3 + (g + 1) * 128], featT_all[gi, :, 15:19, :],
                      reads=[r_in], writes=[r_pre])
            for c in range(4):
                a = acc[c % 2]
                ra = r_acc[c % 2]
                b.op("dve", lambda e, c=c, a=a: e.tensor_scalar(
                    out=a[:], in0=pre[:, c, 0:NTK], scalar1=cw[:, c, 0:1], scalar2=cb[:, c:c + 1],
                    op0=ALU.mult, op1=ALU.add), reads=[r_pre, r_cw], writes=[ra])
                for j in range(1, 4):
                    b.op("dve", lambda e, c=c, a=a, j=j: e.scalar_tensor_tensor(
                        out=a[:], in0=pre[:, c, j:j + NTK], scalar=cw[:, c, j:j + 1], in1=a[:],
                        op0=ALU.mult, op1=ALU.add), reads=[r_pre, r_cw, ra], writes=[ra])
                b.op("act", lambda e, c=c, a=a: e.activation(out=qkall[:, c, :], in_=a[:], func=AF.Silu),
                     reads=[ra], writes=[r_qkall])
            if not whole:
                for c in range(4):
                    a = acc[c % 2]
                    ra = r_acc[c % 2]
                    v = qkall[:, c, :].rearrange("p (m r n) -> p m r n", r=2, n=128)
                    av = a[:, 0:nslots * 128].rearrange("p (m n) -> p m n", n=128)
                    b.op("dve", lambda e, v=v, av=av: e.tensor_tensor(out=av, in0=v[:, :, 1, :], in1=v[:, :, 0, :],
                                                                      op=ALU.subtract),
                         reads=[r_qkall, ra], writes=[ra])
                    b.op("dve", lambda e, v=v, av=av, c=c: e.scalar_tensor_tensor(
                        out=qkown[:, c, :].rearrange("p (m n) -> p m n", n=128), in0=av, scalar=jf[:, 0:1],
                        in1=v[:, :, 0, :], op0=ALU.mult, op1=ALU.add),
                        reads=[ra, r_qkall, r_c], writes=[r_qkown])
            b.barrier()
            b.es = es

        Cst = b.sb("c_Cst", [128, 2, 65], F32)
        CA = b.sb("c_CA", [128, 2, 65], F32)
        Cd = b.sb("c_Cd", [128, 2, 65], F32)
        Cbf = b.sb("c_Cbf", [128, 2, 65], BF16)
        vin = [b.sb("c_vin%d" % i, [128, 256], BF16) for i in range(2)]
        vk = [b.sb("c_vk%d" % i, [128, 4, 65], BF16) for i in range(2)]
        ktok = [b.sb("c_ktok%d" % i, [128, 256], BF16) for i in range(2)]
        ownv = [b.sb("c_ownv%d" % i, [128, 512], BF16) for i in range(2)]
        vaug = [b.sb("c_vaug%d" % i, [128, 4, 65], BF16) for i in range(2)]
        ngc = [b.sb("c_ngc%d" % i, [128, 128], F32) for i in range(2)]
        WT = [b.sb("c_WT%d" % i, [128, 128], F32) for i in range(2)]
        DT = [b.sb("c_DT%d" % i, [128, 128], BF16) for i in range(2)]
        igq = [b.sb("c_igq%d" % i, [128, 128], F32) for i in range(2)]
        qs = [b.sb("c_qs%d" % i, [128, 128], BF16) for i in range(2)]
        nds = [b.sb("c_nd%d" % i, [128, 4, 65], F32) for i in range(2)]
        hsbs = [b.sb("c_hsb%d" % i, [128, 4, 64], F32) for i in range(2)]
        tmps = [b.sb("c_tmp%d" % i, [128, 4, 64], F32) for i in range(2)]
        sts = [b.sb("c_st%d" % i, [128, 16], F32) for i in range(2)]
        dns = [b.sb("c_dn%d" % i, [128, 8], F32) for i in range(2)]
        r_nds, r_hsbs, r_tmps, r_sts, r_dns = ([b.res(), b.res()] for _ in range(5))
        pending_fin = None
        fin_i = 0
        osb = [b.sb("c_o%d" % i, [128, 256], BF16) for i in range(2)]
        pKt = b.ps("c_pKt", [128, 256], BF16)
        pKV = [b.ps("c_pKV%d" % i, [128, 130]) for i in range(2)]
        pG = [b.ps("c_pG%d" % i, [128, 128]) for i in range(2)]
        pQK = [b.ps("c_pQK%d" % i, [128, 128]) for i in range(2)]
        pN = b.ps("c_pN", [128, 260])
        (r_Cst, r_CA, r_Cd, r_Cbf, r_nd, r_hsb, r_tmp, r_st, r_dn, r_pKt, r_pN) = (b.res() for _ in range(11))
        r_vin = [b.res(), b.res()]
        r_vk = [b.res(), b.res()]
        r_ktok = [b.res(), b.res()]
        r_ownv = [b.res(), b.res()]
        r_vaug = [b.res(), b.res()]
        r_ngc = [b.res(), b.res()]
        r_WT = [b.res(), b.res()]
        r_DT = [b.res(), b.res()]
        r_igq = [b.res(), b.res()]
        r_qs = [b.res(), b.res()]
        r_osb = [b.res(), b.res()]
        r_pKV = [b.res(), b.res()]
        r_pG = [b.res(), b.res()]
        r_pQK = [b.res(), b.res()]

        b.op("dve", lambda e: e.memset(Cst[:], 0.0), writes=[r_Cst])
        hi = 0
        for g in range(NG):
            m, r = g // 2, g % 2
            s = g % 2
            gi = gidx(g)
            b.dma("sp", vin[s][:], tokA_all[gi * 128:(gi + 1) * 128, 1280:1536], reads=[r_in], writes=[r_vin[s]])
            if whole:
                so = g % 2
                mm = g
                orow = gi * 128
                b.dma("sp", ownv[so][:], tokA_all[gi * 128:(gi + 1) * 128, 1280:1792], reads=[r_in],
                      writes=[r_ownv[so]])
                b.op("pool", lambda e, so=so: e.memset(vaug[so][:, :, 64:65], 1.0), writes=[r_vaug[so]])
                b.op("pool", lambda e, so=so: e.tensor_copy(
                    out=vaug[so][:, :, 0:64], in_=ownv[so][:, 0:256].rearrange("p (h d) -> p h d", d=64)),
                    reads=[r_ownv[so]], writes=[r_vaug[so]])
                b.op("dve", lambda e: e.tensor_copy(out=Cbf[:], in_=Cst[:]), reads=[r_Cst], writes=[r_Cbf])
            elif r == 0:
                so = m % 2
                b.op("dve", lambda e: e.tensor_copy(out=CA[:], in_=Cst[:]), reads=[r_Cst], writes=[r_CA])
                b.dma("sp", ownv[so][:], tokA_own[m * 128:(m + 1) * 128, 1280:1792], reads=[r_in],
                      writes=[r_ownv[so]])
                b.op("pool", lambda e, so=so: e.memset(vaug[so][:, :, 64:65], 1.0), writes=[r_vaug[so]])
                b.op("pool", lambda e, so=so: e.tensor_copy(
                    out=vaug[so][:, :, 0:64], in_=ownv[so][:, 0:256].rearrange("p (h d) -> p h d", d=64)),
                    reads=[r_ownv[so]], writes=[r_vaug[so]])
            else:
                so = m % 2
                mm = m
                orow = m * 128
                b.op("dve", lambda e: e.tensor_tensor(out=Cd[:], in0=Cst[:], in1=CA[:], op=ALU.subtract),
                     reads=[r_Cst, r_CA], writes=[r_Cd])
                b.op("dve", lambda e: e.scalar_tensor_tensor(
                    out=Cbf[:].rearrange("p a e -> p (a e)"), in0=Cd[:].rearrange("p a e -> p (a e)"),
                    scalar=jf[:, 0:1], in1=CA[:].rearrange("p a e -> p (a e)"), op0=ALU.mult, op1=ALU.add),
                    reads=[r_Cd, r_CA, r_c], writes=[r_Cbf])
            if whole or r == 1:
                m = mm
                for h in range(4):
                    hs = slice((h % 2) * 64, (h % 2) * 64 + 64)
                    p = h // 2
                    x = hi % 2
                    hi += 1
                    b.op("pe", lambda e, x=x, h=h, m=m: e.matmul(
                        pG[x][:, :], lhsT=sel4[:, h, :], rhs=GTo[:, m, :], start=True, stop=True),
                        reads=[r_c, r_GTo], writes=[r_pG[x]])
                    b.op("dve", lambda e, x=x: e.tensor_tensor(out=ngc[x][:], in0=caus[:], in1=pG[x][:],
                                                               op=ALU.subtract),
                         reads=[r_c, r_pG[x]], writes=[r_ngc[x]])
                    b.op("act", lambda e, x=x, m=m, h=h: e.activation(
                        out=WT[x][:], in_=ngc[x][:], func=AF.Exp, bias=acolo[:, m, h:h + 1]),
                        reads=[r_ngc[x], r_cols], writes=[r_WT[x]])
                    b.op("pe", lambda e, x=x, hs=hs, p=p, m=m: e.matmul(
                        pQK[x][:, :], lhsT=qkown[hs, 2 + p, m * 128:(m + 1) * 128],
                        rhs=qkown[hs, p, m * 128:(m + 1) * 128], start=True, stop=True),
                        reads=[r_qkown], writes=[r_pQK[x]])
                    b.op("dve", lambda e, x=x: e.scalar_tensor_tensor(
                        out=DT[x][:], in0=pQK[x][:], scalar=0.125, in1=WT[x][:], op0=ALU.mult, op1=ALU.mult),
                        reads=[r_pQK[x], r_WT[x]], writes=[r_DT[x]])
                    b.op("act", lambda e, x=x, hs=hs, m=m, h=h: e.activation(
                        out=igq[x][hs, :], in_=pG[x][hs, :], func=AF.Exp, scale=-1.0, bias=GRo[hs, m, h:h + 1]),
                        reads=[r_pG[x], r_cols], writes=[r_igq[x]])
                    b.op("dve", lambda e, x=x, hs=hs, p=p, m=m: e.tensor_tensor(
                        out=qs[x][hs, :], in0=qkown[hs, p, m * 128:(m + 1) * 128], in1=igq[x][hs, :], op=ALU.mult),
                        reads=[r_qkown, r_igq[x]], writes=[r_qs[x]])
                    b.op("pe", lambda e, x=x, h=h, so=so: e.matmul(
                        pN[:, h * 65:(h + 1) * 65], lhsT=DT[x][:], rhs=vaug[so][:, h, :], start=True, stop=False),
                        reads=[r_DT[x], r_vaug[so]], writes=[r_pN], inc=False)
                    b.op("pe", lambda e, x=x, h=h, hs=hs, p=p: e.matmul(
                        pN[:, h * 65:(h + 1) * 65], lhsT=qs[x][hs, :], rhs=Cbf[hs, p, :], start=False, stop=True),
                        reads=[r_qs[x], r_Cbf], writes=[r_pN])
                    for _k in range(5):
                        if pending_fin is not None:
                            try:
                                next(pending_fin)
                            except StopIteration:
                                pending_fin = None
                fb = fin_i % 2
                fin_i += 1
                nd_, hsb_, tmp_, st_, dn_ = nds[fb], hsbs[fb], tmps[fb], sts[fb], dns[fb]
                r_nd_, r_hsb_, r_tmp_, r_st_, r_dn_ = r_nds[fb], r_hsbs[fb], r_tmps[fb], r_sts[fb], r_dns[fb]
                b.op("act", lambda e, nd_=nd_: e.copy(out=nd_[:].rearrange("p h d -> p (h d)"), in_=pN[:]),
                     reads=[r_pN], writes=[r_nd_])

                def fin_gen(nd_=nd_, hsb_=hsb_, tmp_=tmp_, st_=st_, dn_=dn_, r_nd_=r_nd_, r_hsb_=r_hsb_,
                            r_tmp_=r_tmp_, r_st_=r_st_, r_dn_=r_dn_, so=so, m=m, orow=orow):
                    b.op("dve", lambda e: e.tensor_scalar(
                        out=dn_[:, 0:4].unsqueeze(2), in0=nd_[:, :, 64:65], scalar1=-1.0, scalar2=None, op0=ALU.mult),
                        reads=[r_nd_], writes=[r_dn_])
                    yield
                    b.op("dve", lambda e: e.tensor_tensor(
                        out=dn_[:, 0:4].unsqueeze(2), in0=dn_[:, 0:4].unsqueeze(2), in1=nd_[:, :, 64:65], op=ALU.max),
                        reads=[r_nd_, r_dn_], writes=[r_dn_])
                    yield
                    b.op("dve", lambda e: e.tensor_tensor(out=dn_[:, 0:4], in0=dn_[:, 0:4], in1=ecolo[:, m, :],
                                                          op=ALU.max),
                         reads=[r_dn_, r_cols], writes=[r_dn_])
                    yield
                    b.op("dve", lambda e: e.reciprocal(out=dn_[:, 4:8], in_=dn_[:, 0:4]), reads=[r_dn_], writes=[r_dn_])
                    yield
                    b.op("dve", lambda e: e.tensor_tensor(
                        out=tmp_[:], in0=nd_[:, :, 0:64], in1=dn_[:, 4:8].unsqueeze(2).to_broadcast([128, 4, 64]),
                        op=ALU.mult), reads=[r_nd_, r_dn_], writes=[r_tmp_])
                    yield
                    b.op("dve", lambda e: e.tensor_tensor(
                        out=hsb_[:], in0=tmp_[:], in1=ownv[so][:, 256:512].rearrange("p (h d) -> p h d", d=64),
                        op=ALU.mult), reads=[r_tmp_, r_ownv[so]], writes=[r_hsb_])
                    yield
                    for _ in gen_head_norm(b, hsb_, r_hsb_, None, None, osb[so][:], r_osb[so], tmp_, st_,
                                           r_tmp_, r_st_):
                        yield
                    b.dma("pool", o_d[orow:orow + 128, 768:1024], osb[so][:], reads=[r_osb[so]], writes=[r_out])

                while pending_fin is not None:
                    try:
                        next(pending_fin)
                    except StopIteration:
                        pending_fin = None
                pending_fin = fin_gen()
            if g == NG - 1:
                break
            b.op("dve", lambda e, s=s, g=g: e.tensor_tensor(
                out=vk[s][:, :, 0:64], in0=vin[s][:].rearrange("p (h d) -> p h d", d=64),
                in1=kw[:, g, :].unsqueeze(2).to_broadcast([128, 4, 64]), op=ALU.mult),
                reads=[r_vin[s], r_cols], writes=[r_vk[s]])
            b.op("dve", lambda e, s=s, g=g: e.tensor_copy(out=vk[s][:, :, 64:65], in_=kw[:, g, :].unsqueeze(2)),
                 reads=[r_cols], writes=[r_vk[s]])
            for p in range(2):
                b.op("pe", lambda e, p=p, g=g: e.transpose(
                    out=pKt[:, p * 128:(p + 1) * 128], in_=qkall[:, 2 + p, g * 128:(g + 1) * 128],
                    identity=identb[:]), reads=[r_qkall, r_c], writes=[r_pKt], inc=(p == 1))
            b.op("act", lambda e, s=s: e.copy(out=ktok[s][:], in_=pKt[:]), reads=[r_pKt], writes=[r_ktok[s]])
            for p in range(2):
                b.op("pe", lambda e, s=s, p=p: e.matmul(
                    pKV[p][:, :], lhsT=ktok[s][:, p * 128:(p + 1) * 128],
                    rhs=vk[s][:, 2 * p:2 * p + 2, :].rearrange("p a e -> p (a e)"), start=True, stop=True),
                    reads=[r_ktok[s], r_vk[s]], writes=[r_pKV[p]])
                for half in range(2):
                    h = 2 * p + half
                    hs = slice(half * 64, half * 64 + 64)
                    b.op("dve", lambda e, p=p, hs=hs, half=half, h=h, g=g: e.scalar_tensor_tensor(
                        out=Cst[hs, p, :], in0=Cst[hs, p, :], scalar=dec[hs, g, h:h + 1],
                        in1=pKV[p][hs, half * 65:(half + 1) * 65], op0=ALU.mult, op1=ALU.add),
                        reads=[r_Cst, r_pKV[p], r_cols], writes=[r_Cst])
        while pending_fin is not None:
            try:
                next(pending_fin)
            except StopIteration:
                pending_fin = None
        b.barrier()
        b.es = old


def mlstm_consts():
    i = np.arange(128)
    caus = np.where(i[:, None] <= i[None, :], 0.0, NEG).astype(np.float32)
    sel4 = np.zeros((4, 4, 128), np.float32)
    for h in range(4):
        sel4[h, h, :] = 1.0
    return caus, sel4.reshape(4, 512), np.eye(4, dtype=np.float32)


def build_p2bc(l, nslots):
    nc = bass.Bass("TRN2", target_bir_lowering=False)
    featT_all = nc.dram_tensor("featT_all", [2 * NT, 128, NFT, 128], BF16, kind="ExternalInput").ap()
    tokA_all = nc.dram_tensor("tokA_all", [2 * TOK, 1792], BF16, kind="ExternalInput").ap()
    gT_all = nc.dram_tensor("gT_all", [2 * NT, 8, 128], F32, kind="ExternalInput").ap()
    featT_own = nc.dram_tensor("featT_own", [NT, 128, NFT, 128], BF16, kind="ExternalInput").ap()
    tokA_own = nc.dram_tensor("tokA_own", [TOK, 1792], BF16, kind="ExternalInput").ap()
    decT_d = nc.dram_tensor("decT", [128, 4, 128], F32, kind="ExternalInput").ap()
    qdecT_d = nc.dram_tensor("qdecT", [128, 2, 128], F32, kind="ExternalInput").ap()
    kdec_d = nc.dram_tensor("kdec", [128, 4], F32, kind="ExternalInput").ap()
    jf_d = nc.dram_tensor("jf", [128, 1], F32, kind="ExternalInput").ap()
    convw_d = nc.dram_tensor("convw", [DEPTH, 128, 4, 4], F32, kind="ExternalInput").ap()
    convb_d = nc.dram_tensor("convb", [DEPTH, 128, 4], F32, kind="ExternalInput").ap()
    caus_d = nc.dram_tensor("caus", [128, 128], F32, kind="ExternalInput").ap()
    sel4_d = nc.dram_tensor("sel4", [4, 512], F32, kind="ExternalInput").ap()
    eye4_d = nc.dram_tensor("eye4", [4, 4], F32, kind="ExternalInput").ap()
    identf_d = nc.dram_tensor("identf", [128, 128], F32, kind="ExternalInput").ap()
    o_d = nc.dram_tensor("o", [TOK, D], BF16, kind="ExternalOutput").ap()
    with ExitStack() as es, nc.allow_low_precision("bf16 matmul operands, fp32 accumulation"):
        b = B(nc, es)
        r_in, r_out = b.res(), b.res()
        emit_p2b(b, nslots, tokA_all, featT_own, tokA_own, decT_d, qdecT_d, kdec_d, jf_d, o_d, r_in, r_out)
        emit_p2c(b, l, nslots, featT_all, tokA_all, gT_all, tokA_own, convw_d, convb_d, caus_d, sel4_d, eye4_d,
                 jf_d, identf_d, o_d, r_in, r_out)
        b.finish()
    return nc


def build_p2bc_whole(l, nslots):
    nc = bass.Bass("TRN2", target_bir_lowering=False)
    featT_all = nc.dram_tensor("featT_all", [2 * NT, 128, NFT, 128], BF16, kind="ExternalInput").ap()
    tokA_all = nc.dram_tensor("tokA_all", [2 * TOK, 1792], BF16, kind="ExternalInput").ap()
    gT_all = nc.dram_tensor("gT_all", [2 * NT, 8, 128], F32, kind="ExternalInput").ap()
    decT_d = nc.dram_tensor("decT", [128, 4, 128], F32, kind="ExternalInput").ap()
    qdecT_d = nc.dram_tensor("qdecT", [128, 2, 128], F32, kind="ExternalInput").ap()
    kdec_d = nc.dram_tensor("kdec", [128, 4], F32, kind="ExternalInput").ap()
    jf_d = nc.dram_tensor("jf", [128, 1], F32, kind="ExternalInput").ap()
    convw_d = nc.dram_tensor("convw", [DEPTH, 128, 4, 4], F32, kind="ExternalInput").ap()
    convb_d = nc.dram_tensor("convb", [DEPTH, 128, 4], F32, kind="ExternalInput").ap()
    caus_d = nc.dram_tensor("caus", [128, 128], F32, kind="ExternalInput").ap()
    sel4_d = nc.dram_tensor("sel4", [4, 512], F32, kind="ExternalInput").ap()
    eye4_d = nc.dram_tensor("eye4", [4, 4], F32, kind="ExternalInput").ap()
    identf_d = nc.dram_tensor("identf", [128, 128], F32, kind="ExternalInput").ap()
    o_d = nc.dram_tensor("o", [2 * TOK, D], BF16, kind="ExternalOutput").ap()
    with ExitStack() as es, nc.allow_low_precision("bf16 matmul operands, fp32 accumulation"):
        b = B(nc, es)
        r_in, r_out = b.res(), b.res()
        emit_p2b(b, nslots, tokA_all, featT_all, None, decT_d, qdecT_d, kdec_d, jf_d, o_d, r_in, r_out, whole=True)
        emit_p2c(b, l, nslots, featT_all, tokA_all, gT_all, None, convw_d, convb_d, caus_d, sel4_d, eye4_d,
                 jf_d, identf_d, o_d, r_in, r_out, whole=True)
        b.finish()
    return nc


def conv_layouts(conv_w, conv_b):
    cw = np.ascontiguousarray(conv_w.reshape(DEPTH, 4, 4, 128).transpose(0, 3, 2, 1))
    cb = np.ascontiguousarray(conv_b.reshape(DEPTH, 4, 128).transpose(0, 2, 1))
    return cw, cb


def emit_ln_stats(b, z, r_z, junk, r_junk, st, r_st):
    b.op("act", lambda e: e.activation(out=junk[:], in_=z, func=AF.Identity, accum_out=st[:, 2:3]),
         reads=[r_z], writes=[r_junk, r_st])
    b.op("act", lambda e: e.activation(out=junk[:], in_=z, func=AF.Square, accum_out=st[:, 3:4]),
         reads=[r_z], writes=[r_junk, r_st])
    b.op("dve", lambda e: e.tensor_scalar(out=st[:, 0:1], in0=st[:, 2:3], scalar1=1.0 / D, scalar2=None,
                                          op0=ALU.mult), reads=[r_st], writes=[r_st])
    b.op("dve", lambda e: e.tensor_tensor(out=st[:, 4:5], in0=st[:, 0:1], in1=st[:, 0:1], op=ALU.mult),
         reads=[r_st], writes=[r_st])
    b.op("dve", lambda e: e.scalar_tensor_tensor(out=st[:, 5:6], in0=st[:, 3:4], scalar=1.0 / D, in1=st[:, 4:5],
                                                 op0=ALU.mult, op1=ALU.subtract), reads=[r_st], writes=[r_st])
    b.op("dve", lambda e: e.tensor_scalar(out=st[:, 5:6], in0=st[:, 5:6], scalar1=LN_EPS, scalar2=None,
                                          op0=ALU.add), reads=[r_st], writes=[r_st])
    b.op("act", lambda e: e.activation(out=st[:, 6:7], in_=st[:, 5:6], func=AF.Sqrt), reads=[r_st], writes=[r_st])
    b.op("dve", lambda e: e.reciprocal(out=st[:, 1:2], in_=st[:, 6:7]), reads=[r_st], writes=[r_st])


def emit_p3(b, l, nt, x_d, o_d, wout_d, modrow_d, modcol_d, lnmg_d, lnmb_d, wr_d, br_d, wg_d, wu_d, wd_d,
            lnfg_d, lnfb_d, identf_d, xout_d, r_in, r_out, n_exp=16):
    ntok = nt * 128
    ngrp = nt // 4
    with ExitStack() as es:
        old = b.es
        b.es = es
        xacc = b.sb("p3_xacc", [128, nt, D], F32)
        h2T = b.sb("p3_h2T", [128, 8, ntok], BF16)
        gate = b.sb("p3_gate", [128, nt, 16], F32)
        lng = b.sb("p3_lng", [128, D], F32)
        lnb = b.sb("p3_lnb", [128, D], F32)
        gbc = b.sb("p3_gbc", [128, D], F32)
        junk = b.sb("p3_junk", [128, D], F32)
        st = b.sb("p3_st", [128, 8], F32)
        identf = b.sb("p3_idf", [128, 128], F32)
        identb = b.sb("p3_idb", [128, 128], BF16)
        r_xacc, r_h2T, r_gate, r_ln, r_gbc, r_junk, r_st, r_id = (b.res() for _ in range(8))
        b.dma("sp", identf[:], identf_d, writes=[r_id])
        b.op("dve", lambda e: e.tensor_copy(out=identb[:], in_=identf[:]), reads=[r_id], writes=[r_id])

        with ExitStack() as esa:
            b.es = esa
            wob = b.sb("p3a_wob", [128, 8, D], BF16)
            wst = [b.sb("p3a_wst%d" % i, [128, 8, 256], F32) for i in range(2)]
            mcol = b.sb("p3a_mcol", [128, 48], F32)
            sc1 = b.sb("p3a_sc1", [128, 8], F32)
            wr = b.sb("p3a_wr", [128, 8, 16], F32)
            brow = b.sb("p3a_brow", [128, 16], F32)
            ot = [b.sb("p3a_ot%d" % i, [128, D], BF16) for i in range(2)]
            oT = [b.sb("p3a_oT%d" % i, [128, 8, 128], BF16) for i in range(2)]
            xt = [b.sb("p3a_xt%d" % i, [128, D], F32) for i in range(2)]
            z = b.sb("p3a_z", [128, D], F32)
            x1 = b.sb("p3a_x1", [128, D], F32)
            h2f = b.sb("p3a_h2f", [128, 8, 128], F32)
            rt = b.sb("p3a_rt", [128, 160], F32)
            pOT = b.ps("p3a_pOT", [128, 1024], BF16)
            pM = [b.ps("p3a_pM%d" % i, [128, 512]) for i in range(2)]
            pX = [b.ps("p3a_pX%d" % i, [128, 512]) for i in range(2)]
            pR = b.ps("p3a_pR", [128, 16])
            r_wob, r_mc, r_wr, r_z, r_x1, r_h2f, r_rt, r_pOT, r_pR = (b.res() for _ in range(9))
            r_wst = [b.res(), b.res()]
            r_ot = [b.res(), b.res()]
            r_oT = [b.res(), b.res()]
            r_xt = [b.res(), b.res()]
            r_pM = [b.res(), b.res()]
            r_pX = [b.res(), b.res()]

            b.dma("sp", mcol[:], modcol_d[:, l * 48:(l + 1) * 48], writes=[r_mc])
            b.op("dve", lambda e: e.tensor_scalar(out=sc1[:], in0=mcol[:, 32:40], scalar1=1.0, scalar2=None,
                                                  op0=ALU.add), reads=[r_mc], writes=[r_mc])
            b.dma("sp", wr[:], wr_d.rearrange("(k p) n -> p k n", p=128), writes=[r_wr])
            b.dma("sp", brow[:], br_d.to_broadcast([128, 16]), writes=[r_wr])
            b.dma("sp", lng[:], lnmg_d[l:l + 1, :].to_broadcast([128, D]), writes=[r_ln])
            b.dma("sp", lnb[:], lnmb_d[l:l + 1, :].to_broadcast([128, D]), writes=[r_ln])
            b.dma("sp", gbc[:], modrow_d[l:l + 1, 2048:3072].to_broadcast([128, D]), writes=[r_gbc])
            b.op("dve", lambda e: e.tensor_scalar(out=gbc[:], in0=gbc[:], scalar1=1.0, scalar2=None, op0=ALU.add),
                 reads=[r_gbc], writes=[r_gbc])
            for i4 in range(4):
                i = i4 % 2
                b.dma("sp", wst[i][:], wout_d[l, :, i4 * 256:(i4 + 1) * 256].rearrange("(k p) n -> p k n", p=128),
                      writes=[r_wst[i]])
                b.op("pool", lambda e, i=i, i4=i4: e.tensor_tensor(
                    out=wob[:, :, i4 * 256:(i4 + 1) * 256], in0=wst[i][:],
                    in1=gbc[:, i4 * 256:(i4 + 1) * 256].unsqueeze(1).to_broadcast([128, 8, 256]), op=ALU.mult),
                    reads=[r_wst[i], r_gbc], writes=[r_wob])
            zs = [z, b.sb("p3a_z2", [128, D], F32)]
            x1s = [x1, b.sb("p3a_x12", [128, D], F32)]
            h2fs = [h2f, b.sb("p3a_h2f2", [128, 8, 128], F32)]
            rts = [rt, b.sb("p3a_rt2", [128, 160], F32)]
            sts = [st, b.sb("p3a_st2", [128, 8], F32)]
            junks = [junk, junk]
            r_zs = [r_z, b.res()]
            r_x1s = [r_x1, b.res()]
            r_h2fs = [r_h2f, b.res()]
            r_rts = [r_rt, b.res()]
            r_sts = [r_st, b.res()]
            r_junks = [r_junk, r_junk]

            def stage_A(t):
                s = t % 2
                z_, r_z_ = zs[s], r_zs[s]
                b.dma("sp", ot[s][:], o_d[t * 128:(t + 1) * 128, :], reads=[r_in], writes=[r_ot[s]])
                b.dma("sp", xt[s][:], x_d[t * 128:(t + 1) * 128, :], reads=[r_in], writes=[r_xt[s]])
                for c in range(8):
                    b.op("pe", lambda e, s=s, c=c: e.transpose(
                        out=pOT[:, c * 128:(c + 1) * 128], in_=ot[s][:, c * 128:(c + 1) * 128], identity=identb[:]),
                        reads=[r_ot[s], r_id], writes=[r_pOT], inc=(c == 7))
                b.op("act", lambda e, s=s: e.copy(out=oT[s][:].rearrange("p k n -> p (k n)"), in_=pOT[:]),
                     reads=[r_pOT], writes=[r_oT[s]])
                for nb in range(2):
                    for k in range(8):
                        b.op("pe", lambda e, s=s, k=k, nb=nb: e.matmul(
                            pM[nb][:, :], lhsT=oT[s][:, k, :], rhs=wob[:, k, nb * 512:(nb + 1) * 512],
                            start=(k == 0), stop=(k == 7)),
                            reads=[r_oT[s], r_wob], writes=[r_pM[nb]], inc=(k == 7))
                    b.op("dve", lambda e, s=s, nb=nb: e.scalar_tensor_tensor(
                        out=z_[:, nb * 512:(nb + 1) * 512], in0=xt[s][:, nb * 512:(nb + 1) * 512], scalar=ALPHA,
                        in1=pM[nb][:, :], op0=ALU.mult, op1=ALU.add),
                        reads=[r_xt[s], r_pM[nb]], writes=[r_z_])
                yield

            def stage_B(t):
                s = t % 2
                z_, r_z_ = zs[s], r_zs[s]
                x1_, r_x1_ = x1s[s], r_x1s[s]
                h2f_, r_h2f_ = h2fs[s], r_h2fs[s]
                rt_, r_rt_ = rts[s], r_rts[s]
                st_, r_st_ = sts[s], r_sts[s]
                junk_, r_junk_ = junks[s], r_junks[s]
                emit_ln_stats(b, z_[:], r_z_, junk_, r_junk_, st_, r_st_)
                b.op("dve", lambda e: e.tensor_scalar(out=x1_[:], in0=z_[:], scalar1=st_[:, 0:1], scalar2=st_[:, 1:2],
                                                      op0=ALU.subtract, op1=ALU.mult),
                     reads=[r_z_, r_st_], writes=[r_x1_])
                b.op("pool", lambda e: e.tensor_tensor(out=x1_[:], in0=x1_[:], in1=lng[:], op=ALU.mult),
                     reads=[r_x1_, r_ln], writes=[r_x1_])
                b.op("pool", lambda e: e.tensor_tensor(out=x1_[:], in0=x1_[:], in1=lnb[:], op=ALU.add),
                     reads=[r_x1_, r_ln], writes=[r_x1_])
                for c in range(8):
                    b.op("pe", lambda e, c=c: e.transpose(
                        out=pX[c // 4][:, (c % 4) * 128:(c % 4 + 1) * 128], in_=x1_[:, c * 128:(c + 1) * 128],
                        identity=identf[:]), reads=[r_x1_, r_id], writes=[r_pX[c // 4]], inc=(c % 4 == 3))
                for c in range(8):
                    b.op("act", lambda e, c=c: e.activation(
                        out=h2f_[:, c, :], in_=pX[c // 4][:, (c % 4) * 128:(c % 4 + 1) * 128], func=AF.Identity,
                        scale=sc1[:, c:c + 1], bias=mcol[:, 24 + c:25 + c]),
                        reads=[r_pX[c // 4], r_mc], writes=[r_h2f_])
                b.op("pool", lambda e, t=t: e.tensor_copy(out=h2T[:, :, t * 128:(t + 1) * 128], in_=h2f_[:]),
                     reads=[r_h2f_], writes=[r_h2T])
                b.op("act", lambda e, t=t: e.mul(out=xacc[:, t, :], in_=x1_[:], mul=ALPHA),
                     reads=[r_x1_], writes=[r_xacc])
                yield

            def stage_B2(t):
                s = t % 2
                h2f_, r_h2f_ = h2fs[s], r_h2fs[s]
                rt_, r_rt_ = rts[s], r_rts[s]
                for k in range(8):
                    b.op("pe", lambda e, k=k: e.matmul(pR[:, :], lhsT=h2f_[:, k, :], rhs=wr[:, k, :],
                                                       start=(k == 0), stop=(k == 7)),
                         reads=[r_h2f_, r_wr], writes=[r_pR], inc=(k == 7))
                S_, BS, EQ1, MSK, EQ2, WT_ = 0, 16, 32, 48, 64, 80
                M1, M2, GS, GM, GSEL, TOT, RT = 96, 100, 104, 108, 112, 116, 117

                def v3(o):
                    return rt_[:, o:o + 16].rearrange("p (g e) -> p g e", e=4)

                def bc(o):
                    return rt_[:, o:o + 4].unsqueeze(2).to_broadcast([128, 4, 4])

                def R(fn):
                    b.op("dve", fn, reads=[r_rt_, r_wr], writes=[r_rt_])

                b.op("act", lambda e: e.activation(out=rt_[:, S_:S_ + 16], in_=pR[:, :], func=AF.Sigmoid),
                     reads=[r_pR], writes=[r_rt_])
                R(lambda e: e.tensor_tensor(out=rt_[:, BS:BS + 16], in0=rt_[:, S_:S_ + 16], in1=brow[:], op=ALU.add))
                R(lambda e: e.tensor_reduce(out=rt_[:, M1:M1 + 4], in_=v3(BS), axis=AX.X, op=ALU.max))
                R(lambda e: e.tensor_tensor(out=v3(EQ1), in0=v3(BS), in1=bc(M1), op=ALU.is_equal))
                R(lambda e: e.scalar_tensor_tensor(out=rt_[:, MSK:MSK + 16], in0=rt_[:, EQ1:EQ1 + 16], scalar=-1.0e9,
                                                   in1=rt_[:, BS:BS + 16], op0=ALU.mult, op1=ALU.add))
                R(lambda e: e.tensor_reduce(out=rt_[:, M2:M2 + 4], in_=v3(MSK), axis=AX.X, op=ALU.max))
                R(lambda e: e.tensor_tensor(out=rt_[:, GS:GS + 4], in0=rt_[:, M1:M1 + 4], in1=rt_[:, M2:M2 + 4],
                                            op=ALU.add))
                R(lambda e: e.tensor_reduce(out=rt_[:, GM:GM + 1], in_=rt_[:, GS:GS + 4], axis=AX.X, op=ALU.max))
                R(lambda e: e.tensor_scalar(out=rt_[:, GSEL:GSEL + 4], in0=rt_[:, GS:GS + 4], scalar1=rt_[:, GM:GM + 1],
                                            scalar2=None, op0=ALU.is_equal))
                R(lambda e: e.tensor_tensor(out=v3(EQ2), in0=v3(MSK), in1=bc(M2), op=ALU.is_equal))
                R(lambda e: e.tensor_tensor(out=rt_[:, EQ2:EQ2 + 16], in0=rt_[:, EQ2:EQ2 + 16], in1=rt_[:, EQ1:EQ1 + 16],
                                            op=ALU.add))
                R(lambda e: e.tensor_tensor(out=v3(EQ2), in0=v3(EQ2), in1=bc(GSEL), op=ALU.mult))
                R(lambda e: e.tensor_tensor(out=rt_[:, WT_:WT_ + 16], in0=rt_[:, EQ2:EQ2 + 16], in1=rt_[:, S_:S_ + 16],
                                            op=ALU.mult))
                R(lambda e: e.tensor_reduce(out=rt_[:, TOT:TOT + 1], in_=rt_[:, WT_:WT_ + 16], axis=AX.X, op=ALU.add))
                R(lambda e: e.reciprocal(out=rt_[:, RT:RT + 1], in_=rt_[:, TOT:TOT + 1]))
                b.op("dve", lambda e, t=t: e.tensor_scalar(out=gate[:, t, :], in0=rt_[:, WT_:WT_ + 16],
                                                           scalar1=rt_[:, RT:RT + 1], scalar2=None, op0=ALU.mult),
                     reads=[r_rt_], writes=[r_gate])
                yield

            for _ in stage_A(0):
                pass
            for t in range(nt + 1):
                if t + 1 < nt:
                    for _ in stage_A(t + 1):
                        pass
                if t < nt:
                    for _ in stage_B(t):
                        pass
                if t >= 1:
                    for _ in stage_B2(t - 1):
                        pass
            b.barrier()
            b.es = es

        with ExitStack() as esb:
            b.es = esb
            b.dma("sp", gbc[:], modrow_d[l:l + 1, 5120:6144].to_broadcast([128, D]), writes=[r_gbc])
            b.op("dve", lambda e: e.tensor_scalar(out=gbc[:], in0=gbc[:], scalar1=1.0, scalar2=None, op0=ALU.add),
                 reads=[r_gbc], writes=[r_gbc])
            b.dma("sp", lng[:], lnfg_d[l:l + 1, :].to_broadcast([128, D]), writes=[r_ln])
            b.dma("sp", lnb[:], lnfb_d[l:l + 1, :].to_broadcast([128, D]), writes=[r_ln])
            sgu = [b.sb("p3b_sgu%d" % i, [128, 8, 256], F32) for i in range(2)]
            sd = [b.sb("p3b_sd%d" % i, [128, 2, D], F32) for i in range(2)]
            wg = [b.sb("p3b_wg%d" % i, [128, 8, 256], BF16) for i in range(2)]
            wu = [b.sb("p3b_wu%d" % i, [128, 8, 256], BF16) for i in range(2)]
            wd = [b.sb("p3b_wd%d" % i, [128, 2, D], BF16) for i in range(2)]
            sg = [b.sb("p3b_sg%d" % i, [128, 512], BF16) for i in range(2)]
            hid = [b.sb("p3b_hid%d" % i, [128, 512], BF16) for i in range(4)]
            pg = [b.ps("p3b_pg%d" % i, [128, 512]) for i in range(2)]
            pu = [b.ps("p3b_pu%d" % i, [128, 512]) for i in range(2)]
            py = [b.ps("p3b_py%d" % i, [128, 512]) for i in range(4)]
            r_sgu = [b.res(), b.res()]
            r_sd = [b.res(), b.res()]
            r_wg = [b.res(), b.res()]
            r_wu = [b.res(), b.res()]
            r_wd = [b.res(), b.res()]
            r_sg = [b.res(), b.res()]
            r_hid = [b.res() for _ in range(4)]
            r_pg = [b.res(), b.res()]
            r_pu = [b.res(), b.res()]
            r_py = [b.res() for _ in range(4)]
            sti = 0
            hc = 0
            yc = 0
            pending_down = None
            for ex in range(n_exp):
                w = ex % 2
                si = sti % 2
                sti += 1
                b.dma("sp", sgu[si][:], wg_d[l, ex].rearrange("(k p) f -> p k f", p=128), writes=[r_sgu[si]])
                b.op("pool", lambda e, si=si, w=w: e.tensor_copy(out=wg[w][:], in_=sgu[si][:]),
                     reads=[r_sgu[si]], writes=[r_wg[w]])
                si = sti % 2
                sti += 1
                b.dma("sp", sgu[si][:], wu_d[l, ex].rearrange("(k p) f -> p k f", p=128), writes=[r_sgu[si]])
                b.op("pool", lambda e, si=si, w=w: e.tensor_copy(out=wu[w][:], in_=sgu[si][:]),
                     reads=[r_sgu[si]], writes=[r_wu[w]])
                b.dma("sp", sd[w][:], wd_d[l, ex].rearrange("(k p) n -> p k n", p=128), writes=[r_sd[w]])
                b.op("pool", lambda e, w=w: e.tensor_tensor(
                    out=wd[w][:], in0=sd[w][:], in1=gbc[:].unsqueeze(1).to_broadcast([128, 2, D]), op=ALU.mult),
                    reads=[r_sd[w], r_gbc], writes=[r_wd[w]])
                for tg in range(ngrp):
                    hids = []
                    for fc in range(2):
                        x = fc
                        for k in range(8):
                            b.op("pe", lambda e, w=w, k=k, fc=fc, tg=tg, x=x: e.matmul(
                                pg[x][:, :], lhsT=wg[w][:, k, fc * 128:(fc + 1) * 128],
                                rhs=h2T[:, k, tg * 512:(tg + 1) * 512], start=(k == 0), stop=(k == 7)),
                                reads=[r_wg[w], r_h2T], writes=[r_pg[x]], inc=(k == 7))
                        for k in range(8):
                            b.op("pe", lambda e, w=w, k=k, fc=fc, tg=tg, x=x: e.matmul(
                                pu[x][:, :], lhsT=wu[w][:, k, fc * 128:(fc + 1) * 128],
                                rhs=h2T[:, k, tg * 512:(tg + 1) * 512], start=(k == 0), stop=(k == 7)),
                                reads=[r_wu[w], r_h2T], writes=[r_pu[x]], inc=(k == 7))
                        b.op("act", lambda e, x=x: e.activation(out=sg[x][:], in_=pg[x][:, :], func=AF.Silu),
                             reads=[r_pg[x]], writes=[r_sg[x]])
                        hx = hc % 4
                        hc += 1
                        b.op("dve", lambda e, x=x, hx=hx: e.tensor_tensor(out=hid[hx][:], in0=pu[x][:, :],
                                                                          in1=sg[x][:], op=ALU.mult),
                             reads=[r_pu[x], r_sg[x]], writes=[r_hid[hx]])
                        hids.append(hx)
                    def down_job(tg=tg, w=w, ex=ex, hids=tuple(hids)):
                        nonlocal yc
                        for tt in range(4):
                            t = tg * 4 + tt
                            for nb in range(2):
                                yx = yc % 4
                                yc += 1
                                for fc in range(2):
                                    hx = hids[fc]
                                    b.op("pe", lambda e, hx=hx, tt=tt, w=w, fc=fc, nb=nb, yx=yx: e.matmul(
                                        py[yx][:, :], lhsT=hid[hx][:, tt * 128:(tt + 1) * 128],
                                        rhs=wd[w][:, fc, nb * 512:(nb + 1) * 512], start=(fc == 0), stop=(fc == 1)),
                                        reads=[r_hid[hx], r_wd[w]], writes=[r_py[yx]], inc=(fc == 1))
                                b.op("dve", lambda e, yx=yx, t=t, nb=nb, ex=ex: e.scalar_tensor_tensor(
                                    out=xacc[:, t, nb * 512:(nb + 1) * 512], in0=py[yx][:, :],
                                    scalar=gate[:, t, ex:ex + 1], in1=xacc[:, t, nb * 512:(nb + 1) * 512],
                                    op0=ALU.mult, op1=ALU.add),
                                    reads=[r_py[yx], r_gate, r_xacc], writes=[r_xacc])
                    if pending_down is not None:
                        pending_down()
                    pending_down = down_job
            if pending_down is not None:
                pending_down()
            b.barrier()
            b.es = es

        with ExitStack() as esc:
            b.es = esc
            xo = [b.sb("p3c_xo%d" % i, [128, D], F32) for i in range(2)]
            r_xo = [b.res(), b.res()]
            for t in range(nt):
                s = t % 2
                emit_ln_stats(b, xacc[:, t, :], r_xacc, junk, r_junk, st, r_st)
                b.op("dve", lambda e, t=t, s=s: e.tensor_scalar(
                    out=xo[s][:], in0=xacc[:, t, :], scalar1=st[:, 0:1], scalar2=st[:, 1:2],
                    op0=ALU.subtract, op1=ALU.mult), reads=[r_xacc, r_st], writes=[r_xo[s]])
                b.op("pool", lambda e, s=s: e.tensor_tensor(out=xo[s][:], in0=xo[s][:], in1=lng[:], op=ALU.mult),
                     reads=[r_xo[s], r_ln], writes=[r_xo[s]])
                b.op("pool", lambda e, s=s: e.tensor_tensor(out=xo[s][:], in0=xo[s][:], in1=lnb[:], op=ALU.add),
                     reads=[r_xo[s], r_ln], writes=[r_xo[s]])
                b.dma("pool", xout_d[t * 128:(t + 1) * 128, :], xo[s][:], reads=[r_xo[s]], writes=[r_out])
            b.barrier()
            b.es = es
        b.es = old


def build_p3(l, nt, n_exp=16):
    nc = bass.Bass("TRN2", target_bir_lowering=False)
    x_d = nc.dram_tensor("x", [nt * 128, D], F32, kind="ExternalInput").ap()
    o_d = nc.dram_tensor("o", [nt * 128, D], BF16, kind="ExternalInput").ap()
    wout_d = nc.dram_tensor("w_out", [DEPTH, D, D], F32, kind="ExternalInput").ap()
    modrow_d = nc.dram_tensor("modrow", [DEPTH, 6 * D], F32, kind="ExternalInput").ap()
    modcol_d = nc.dram_tensor("modcol", [128, DEPTH * 48], F32, kind="ExternalInput").ap()
    lnmg_d = nc.dram_tensor("ln_mix_g", [DEPTH, D], F32, kind="ExternalInput").ap()
    lnmb_d = nc.dram_tensor("ln_mix_b", [DEPTH, D], F32, kind="ExternalInput").ap()
    wr_d = nc.dram_tensor("w_router", [D, 16], F32, kind="ExternalInput").ap()
    br_d = nc.dram_tensor("b_router", [1, 16], F32, kind="ExternalInput").ap()
    wg_d = nc.dram_tensor("w_gate", [DEPTH, 16, D, 256], F32, kind="ExternalInput").ap()
    wu_d = nc.dram_tensor("w_up", [DEPTH, 16, D, 256], F32, kind="ExternalInput").ap()
    wd_d = nc.dram_tensor("w_down", [DEPTH, 16, 256, D], F32, kind="ExternalInput").ap()
    lnfg_d = nc.dram_tensor("ln_ffn_g", [DEPTH, D], F32, kind="ExternalInput").ap()
    lnfb_d = nc.dram_tensor("ln_ffn_b", [DEPTH, D], F32, kind="ExternalInput").ap()
    identf_d = nc.dram_tensor("identf", [128, 128], F32, kind="ExternalInput").ap()
    xout_d = nc.dram_tensor("xout", [nt * 128, D], F32, kind="ExternalOutput").ap()
    with ExitStack() as es, nc.allow_low_precision("bf16 matmul operands, fp32 accumulation"):
        b = B(nc, es)
        emit_p3(b, l, nt, x_d, o_d, wout_d, modrow_d, modcol_d, lnmg_d, lnmb_d, wr_d, br_d, wg_d, wu_d, wd_d,
                lnfg_d, lnfb_d, identf_d, xout_d, b.res(), b.res(), n_exp=n_exp)
        b.finish()
    return nc


I32 = mybir.dt.int32


def _decl_p1_inputs(nc):
    d = {}
    d["pos"] = nc.dram_tensor("pos", [128, NT], I32, kind="ExternalInput").ap()
    d["invf"] = nc.dram_tensor("invf", [128, 32], F32, kind="ExternalInput").ap()
    d["w_in"] = nc.dram_tensor("w_in", [1, D, INW], F32, kind="ExternalInput").ap()
    d["i_bias"] = nc.dram_tensor("i_bias", [1, 4], F32, kind="ExternalInput").ap()
    d["f_bias"] = nc.dram_tensor("f_bias", [1, 4], F32, kind="ExternalInput").ap()
    return d


def _decl_p1_outputs(nc):
    d = {}
    d["featT"] = nc.dram_tensor("featT", [NT, 128, NFT, 128], BF16, kind="ExternalOutput").ap()
    d["tokA"] = nc.dram_tensor("tokA", [TOK, 1792], BF16, kind="ExternalOutput").ap()
    d["tokF"] = nc.dram_tensor("tokF", [TOK, 12], F32, kind="ExternalOutput").ap()
    d["gT"] = nc.dram_tensor("gT", [NT, 8, 128], F32, kind="ExternalOutput").ap()
    return d


def _decl_p3_inputs(nc):
    d = {}
    d["o"] = nc.dram_tensor("o", [TOK, D], BF16, kind="ExternalInput").ap()
    d["w_out"] = nc.dram_tensor("w_out", [1, D, D], F32, kind="ExternalInput").ap()
    d["modrow3"] = nc.dram_tensor("modrow3", [1, 6 * D], F32, kind="ExternalInput").ap()
    d["modcol3"] = nc.dram_tensor("modcol3", [128, 48], F32, kind="ExternalInput").ap()
    d["ln_mix_g"] = nc.dram_tensor("ln_mix_g", [1, D], F32, kind="ExternalInput").ap()
    d["ln_mix_b"] = nc.dram_tensor("ln_mix_b", [1, D], F32, kind="ExternalInput").ap()
    d["w_router"] = nc.dram_tensor("w_router", [D, 16], F32, kind="ExternalInput").ap()
    d["b_router"] = nc.dram_tensor("b_router", [1, 16], F32, kind="ExternalInput").ap()
    d["w_gate"] = nc.dram_tensor("w_gate", [1, 16, D, 256], F32, kind="ExternalInput").ap()
    d["w_up"] = nc.dram_tensor("w_up", [1, 16, D, 256], F32, kind="ExternalInput").ap()
    d["w_down"] = nc.dram_tensor("w_down", [1, 16, 256, D], F32, kind="ExternalInput").ap()
    d["ln_ffn_g"] = nc.dram_tensor("ln_ffn_g", [1, D], F32, kind="ExternalInput").ap()
    d["ln_ffn_b"] = nc.dram_tensor("ln_ffn_b", [1, D], F32, kind="ExternalInput").ap()
    return d


def _emit_p3_from(b, i3, x_d, identf_d, xout_d, r_in, r_out):
    emit_p3(b, 0, NT, x_d, i3["o"], i3["w_out"], i3["modrow3"], i3["modcol3"], i3["ln_mix_g"], i3["ln_mix_b"],
            i3["w_router"], i3["b_router"], i3["w_gate"], i3["w_up"], i3["w_down"], i3["ln_ffn_g"],
            i3["ln_ffn_b"], identf_d, xout_d, r_in, r_out)


def build_A():
    nc = bass.Bass("TRN2", target_bir_lowering=False)
    x_d = nc.dram_tensor("x", [TOK, D], F32, kind="ExternalInput").ap()
    identf_d = nc.dram_tensor("identf", [128, 128], F32, kind="ExternalInput").ap()
    ccol_d = nc.dram_tensor("ccol", [128, 8], F32, kind="ExternalInput").ap()
    wada_d = nc.dram_tensor("w_ada", [DEPTH, D, 6 * D], F32, kind="ExternalInput").ap()
    bada_d = nc.dram_tensor("b_ada", [DEPTH, 6 * D], F32, kind="ExternalInput").ap()
    modrow_d = nc.dram_tensor("modrow", [DEPTH, 6 * D], F32, kind="ExternalOutput").ap()
    modcol_d = nc.dram_tensor("modcol", [128, DEPTH * 48], F32, kind="ExternalOutput").ap()
    i1 = _decl_p1_inputs(nc)
    o1 = _decl_p1_outputs(nc)
    with ExitStack() as es, nc.allow_low_precision("bf16 matmul operands, fp32 accumulation"):
        b = B(nc, es)
        cos = b.sb("cos", [128, NT, 32], F32)
        sin = b.sb("sin", [128, NT, 32], F32)
        r_tab, r_mod, r_out = b.res(), b.res(), b.res()
        emit_mod(b, ccol_d, wada_d, bada_d, modrow_d, modcol_d)
        emit_rope_tables(b, i1["pos"], i1["invf"], cos, sin, r_tab, NT)
        emit_p1(b, 0, NT, x_d, i1["w_in"], modcol_d, i1["i_bias"], i1["f_bias"], identf_d, cos, sin, r_tab,
                o1["featT"], o1["tokA"], o1["tokF"], o1["gT"], b.res(), r_out)
        b.finish()
    return nc


def build_B():
    nc = bass.Bass("TRN2", target_bir_lowering=False)
    featT_all = nc.dram_tensor("featT_all", [2 * NT, 128, NFT, 128], BF16, kind="ExternalInput").ap()
    tokA_all = nc.dram_tensor("tokA_all", [2 * TOK, 1792], BF16, kind="ExternalInput").ap()
    gT_all = nc.dram_tensor("gT_all", [2 * NT, 8, 128], F32, kind="ExternalInput").ap()
    featT_own = nc.dram_tensor("featT_own", [NT, 128, NFT, 128], BF16, kind="ExternalInput").ap()
    tokA_own = nc.dram_tensor("tokA_own", [TOK, 1792], BF16, kind="ExternalInput").ap()
    tokF_own = nc.dram_tensor("tokF_own", [TOK, 12], F32, kind="ExternalInput").ap()
    vis_d = nc.dram_tensor("vis", [128, 256], F32, kind="ExternalInput").ap()
    pw_d = nc.dram_tensor("pw", [128, NIT + 1], F32, kind="ExternalInput").ap()
    decT_d = nc.dram_tensor("decT", [128, 4, 128], F32, kind="ExternalInput").ap()
    qdecT_d = nc.dram_tensor("qdecT", [128, 2, 128], F32, kind="ExternalInput").ap()
    kdec_d = nc.dram_tensor("kdec", [128, 4], F32, kind="ExternalInput").ap()
    jf_d = nc.dram_tensor("jf", [128, 1], F32, kind="ExternalInput").ap()
    convw_d = nc.dram_tensor("convw", [1, 128, 4, 4], F32, kind="ExternalInput").ap()
    convb_d = nc.dram_tensor("convb", [1, 128, 4], F32, kind="ExternalInput").ap()
    caus_d = nc.dram_tensor("caus", [128, 128], F32, kind="ExternalInput").ap()
    sel4_d = nc.dram_tensor("sel4", [4, 512], F32, kind="ExternalInput").ap()
    eye4_d = nc.dram_tensor("eye4", [4, 4], F32, kind="ExternalInput").ap()
    identf_d = nc.dram_tensor("identf", [128, 128], F32, kind="ExternalInput").ap()
    o_d = nc.dram_tensor("o", [TOK, D], BF16, kind="ExternalOutput").ap()
    with ExitStack() as es, nc.allow_low_precision("bf16 matmul operands, fp32 accumulation"):
        b = B(nc, es)
        r_in, r_out = b.res(), b.res()
        emit_p2a(b, NT, featT_all, tokA_all, featT_own, tokF_own, vis_d, pw_d, identf_d, o_d, r_in, r_out)
        emit_p2b(b, NT, tokA_all, featT_own, tokA_own, decT_d, qdecT_d, kdec_d, jf_d, o_d, r_in, r_out)
        emit_p2c(b, 0, NT, featT_all, tokA_all, gT_all, tokA_own, convw_d, convb_d, caus_d, sel4_d, eye4_d,
                 jf_d, identf_d, o_d, r_in, r_out)
        b.finish()
    return nc


def build_C(with_p1):
    nc = bass.Bass("TRN2", target_bir_lowering=False)
    x_d = nc.dram_tensor("x", [TOK, D], F32, kind="ExternalInput").ap()
    identf_d = nc.dram_tensor("identf", [128, 128], F32, kind="ExternalInput").ap()
    i3 = _decl_p3_inputs(nc)
    xout_d = nc.dram_tensor("xout", [TOK, D], F32, kind="ExternalOutput").ap()
    if with_p1:
        i1 = _decl_p1_inputs(nc)
        modcol1_d = nc.dram_tensor("modcol1", [128, 48], F32, kind="ExternalInput").ap()
        o1 = _decl_p1_outputs(nc)
    with ExitStack() as es, nc.allow_low_precision("bf16 matmul operands, fp32 accumulation"):
        b = B(nc, es)
        r_in, r_x = b.res(), b.res()
        if with_p1:
            cos = b.sb("cos", [128, NT, 32], F32)
            sin = b.sb("sin", [128, NT, 32], F32)
            r_tab = b.res()
            emit_rope_tables(b, i1["pos"], i1["invf"], cos, sin, r_tab, NT)
        _emit_p3_from(b, i3, x_d, identf_d, xout_d, r_in, r_x)
        if with_p1:
            emit_p1(b, 0, NT, xout_d, i1["w_in"], modcol1_d, i1["i_bias"], i1["f_bias"], identf_d, cos, sin,
                    r_tab, o1["featT"], o1["tokA"], o1["tokF"], o1["gT"], r_x, b.res())
        b.finish()
    return nc


_PROGS = {}


def _prog(name, fn):
    if name not in _PROGS:
        _PROGS[name] = fn()
    return _PROGS[name]


def _own_tiles(a, j):
    sh = a.shape
    return np.ascontiguousarray(a.reshape((32, 128) + sh[1:])[j::2]).reshape((TOK,) + sh[1:])


def kernel(x, c, positions, w_ada, b_ada, w_in, i_bias, f_bias, conv_w, conv_b, w_out, ln_mix_g, ln_mix_b,
           w_router, b_router, w_gate, w_up, w_down, ln_ffn_g, ln_ffn_b):
    f32 = np.float32
    x = np.asarray(x, f32)
    c = np.asarray(c, f32)
    positions = np.asarray(positions, np.int32)
    w_ada, b_ada, w_in = np.asarray(w_ada, f32), np.asarray(b_ada, f32), np.asarray(w_in, f32)
    i_bias, f_bias = np.asarray(i_bias, f32), np.asarray(f_bias, f32)
    w_out = np.asarray(w_out, f32)
    ln_mix_g, ln_mix_b = np.asarray(ln_mix_g, f32), np.asarray(ln_mix_b, f32)
    w_router, b_router = np.asarray(w_router, f32), np.asarray(b_router, f32).reshape(1, 16)
    w_gate, w_up, w_down = np.asarray(w_gate, f32), np.asarray(w_up, f32), np.asarray(w_down, f32)
    ln_ffn_g, ln_ffn_b = np.asarray(ln_ffn_g, f32), np.asarray(ln_ffn_b, f32)
    cw, cb = conv_layouts(np.asarray(conv_w, f32), np.asarray(conv_b, f32))
    ident = np.eye(128, dtype=f32)
    invf = inv_freq_table()
    decT, qdecT, kdec = ret_tables()
    caus, sel4, eye4 = mlstm_consts()
    pw = pw_table()
    cores = list(range(8))

    def p1_in(core, l):
        bi, j = core // 2, core % 2
        pos = np.ascontiguousarray(positions[bi].reshape(32, 128)[j::2].T)
        return {"pos": pos, "invf": invf, "w_in": w_in[l:l + 1], "i_bias": i_bias[l:l + 1],
                "f_bias": f_bias[l:l + 1]}

    def p3_in(core, l, o, modrow, modcol):
        return {"o": o, "w_out": w_out[l:l + 1], "modrow3": np.ascontiguousarray(modrow[l:l + 1]),
                "modcol3": np.ascontiguousarray(modcol[:, l * 48:(l + 1) * 48]),
                "ln_mix_g": ln_mix_g[l:l + 1], "ln_mix_b": ln_mix_b[l:l + 1], "w_router": w_router,
                "b_router": b_router, "w_gate": w_gate[l:l + 1], "w_up": w_up[l:l + 1], "w_down": w_down[l:l + 1],
                "ln_ffn_g": ln_ffn_g[l:l + 1], "ln_ffn_b": ln_ffn_b[l:l + 1]}

    ims = []
    xs = []
    for core in cores:
        bi, j = core // 2, core % 2
        xo = _own_tiles(x[bi], j)
        xs.append(xo)
        im = {"x": xo, "identf": ident, "ccol": np.ascontiguousarray(c[bi].reshape(8, 128).T),
              "w_ada": w_ada, "b_ada": b_ada}
        im.update(p1_in(core, 0))
        ims.append(im)
    res = run_bass_kernel_spmd(_prog("A", build_A), ims, core_ids=cores).results
    modrow = [np.asarray(r["modrow"]) for r in res]
    modcol = [np.asarray(r["modcol"]) for r in res]
    p1o = res
    for l in range(DEPTH):
        ims = []
        for core in cores:
            bi, j = core // 2, core % 2
            a, bb = p1o[2 * bi], p1o[2 * bi + 1]
            ims.append({
                "featT_all": np.concatenate([np.asarray(a["featT"]), np.asarray(bb["featT"])], axis=0),
                "tokA_all": np.concatenate([np.asarray(a["tokA"]), np.asarray(bb["tokA"])], axis=0),
                "gT_all": np.concatenate([np.asarray(a["gT"]), np.asarray(bb["gT"])], axis=0),
                "featT_own": np.asarray(p1o[core]["featT"]), "tokA_own": np.asarray(p1o[core]["tokA"]),
                "tokF_own": np.asarray(p1o[core]["tokF"]),
                "vis": vis_table(j), "pw": pw, "decT": decT, "qdecT": qdecT, "kdec": kdec,
                "jf": np.full((128, 1), float(j), f32), "convw": cw[l:l + 1], "convb": cb[l:l + 1],
                "caus": caus, "sel4": sel4, "eye4": eye4, "identf": ident})
        ob = run_bass_kernel_spmd(_prog("B", build_B), ims, core_ids=cores).results
        last = (l == DEPTH - 1)
        ims = []
        for core in cores:
            im = {"x": xs[core], "identf": ident}
            im.update(p3_in(core, l, np.asarray(ob[core]["o"]), modrow[core], modcol[core]))
            if not last:
                im.update(p1_in(core, l + 1))
                im["modcol1"] = np.ascontiguousarray(modcol[core][:, (l + 1) * 48:(l + 2) * 48])
            ims.append(im)
        if last:
            res = run_bass_kernel_spmd(_prog("E", lambda: build_C(False)), ims, core_ids=cores).results
        else:
            res = run_bass_kernel_spmd(_prog("C", lambda: build_C(True)), ims, core_ids=cores).results
            p1o = res
        xs = [np.asarray(r["xout"]) for r in res]
    out = np.zeros((BATCH, SEQ, D), f32)
    for core in cores:
        bi, j = core // 2, core % 2
        out[bi].reshape(32, 128, D)[j::2] = xs[core].reshape(NT, 128, D)
    return out


def build_fused(depth=DEPTH):
    nc = bass.Bass("TRN2", target_bir_lowering=False)

    def inp(name, shape, dt=F32):
        return nc.dram_tensor(name, list(shape), dt, kind="ExternalInput").ap()

    def scr(name, shape, dt=F32):
        return nc.dram_tensor(name, list(shape), dt).ap()

    x_in = inp("x2", [2 * TOK, D])
    pos_d = inp("pos2", [2, 128, NT], I32)
    invf_d = inp("invf", [128, 32])
    identf_d = inp("identf", [128, 128])
    ccol_d = inp("ccol", [128, 8])
    wada_d = inp("w_ada", [DEPTH, D, 6 * D])
    bada_d = inp("b_ada", [DEPTH, 6 * D])
    win_d = inp("w_in", [DEPTH, D, INW])
    ib_d = inp("i_bias", [DEPTH, 4])
    fb_d = inp("f_bias", [DEPTH, 4])
    convw_d = inp("convw", [DEPTH, 128, 4, 4])
    convb_d = inp("convb", [DEPTH, 128, 4])
    wout_d = inp("w_out", [DEPTH, D, D])
    lnmg_d = inp("ln_mix_g", [DEPTH, D])
    lnmb_d = inp("ln_mix_b", [DEPTH, D])
    wr_d = inp("w_router", [D, 16])
    br_d = inp("b_router", [1, 16])
    wg_d = inp("w_gate", [DEPTH, 16, D, 256])
    wu_d = inp("w_up", [DEPTH, 16, D, 256])
    wd_d = inp("w_down", [DEPTH, 16, 256, D])
    lnfg_d = inp("ln_ffn_g", [DEPTH, D])
    lnfb_d = inp("ln_ffn_b", [DEPTH, D])
    vis_d = inp("vis2", [2, 128, 256])
    jf_d = inp("jf2", [2, 128, 1])
    pw_d = inp("pw", [128, NIT + 1])
    decT_d = inp("decT", [128, 4, 128])
    qdecT_d = inp("qdecT", [128, 2, 128])
    kdec_d = inp("kdec", [128, 4])
    caus_d = inp("caus", [128, 128])
    sel4_d = inp("sel4", [4, 512])
    eye4_d = inp("eye4", [4, 4])
    xfin = nc.dram_tensor("xfin", [2 * TOK, D], F32, kind="ExternalOutput").ap()

    modrow_s = scr("modrow_s", [DEPTH, 6 * D])
    modcol_s = scr("modcol_s", [128, DEPTH * 48])
    featT_s = scr("featT_s", [2 * NT, 128, NFT, 128], BF16)
    tokA_s = scr("tokA_s", [2 * TOK, 1792], BF16)
    tokF_s = scr("tokF_s", [2 * TOK, 12])
    gT_s = scr("gT_s", [2 * NT, 8, 128])
    o_s = scr("o_s", [2 * TOK, D], BF16)
    xs = [scr("xs0", [2 * TOK, D]), scr("xs1", [2 * TOK, D])]

    with ExitStack() as es, nc.allow_low_precision("bf16 matmul operands, fp32 accumulation"):
        b = B(nc, es)
        cos = [b.sb("cos%d" % j, [128, NT, 32], F32) for j in range(2)]
        sin = [b.sb("sin%d" % j, [128, NT, 32], F32) for j in range(2)]
        r_tab = [b.res(), b.res()]
        r_bun, r_o, r_x = b.res(), b.res(), b.res()
        emit_mod(b, ccol_d, wada_d, bada_d, modrow_s, modcol_s)
        for j in range(2):
            emit_rope_tables(b, pos_d[j], invf_d, cos[j], sin[j], r_tab[j], NT)
        for l in range(depth):
            x_l = x_in if l == 0 else xs[l % 2]
            x_n = xfin if l == depth - 1 else xs[(l + 1) % 2]
            for j in range(2):
                sl = slice(j * TOK, (j + 1) * TOK)
                emit_p1(b, l, NT, x_l[sl], win_d, modcol_s, ib_d, fb_d, identf_d, cos[j], sin[j], r_tab[j],
                        featT_s[j * NT:(j + 1) * NT], tokA_s[sl], tokF_s[sl], gT_s[j * NT:(j + 1) * NT],
                        r_x, r_bun)
            for j in range(2):
                sl = slice(j * TOK, (j + 1) * TOK)
                f_own = featT_s[j * NT:(j + 1) * NT]
                emit_p2a(b, NT, featT_s, tokA_s, f_own, tokF_s[sl], vis_d[j], pw_d, identf_d, o_s[sl],
                         r_bun, r_o)
            emit_p2b(b, NT, tokA_s, featT_s, None, decT_d, qdecT_d, kdec_d, jf_d[0], o_s, r_bun, r_o, whole=True)
            emit_p2c(b, l, NT, featT_s, tokA_s, gT_s, None, convw_d, convb_d, caus_d, sel4_d, eye4_d,
                     jf_d[0], identf_d, o_s, r_bun, r_o, whole=True)
            for j in range(2):
                sl = slice(j * TOK, (j + 1) * TOK)
                emit_p3(b, l, NT, x_l[sl], o_s[sl], wout_d, modrow_s, modcol_s, lnmg_d, lnmb_d, wr_d, br_d,
                        wg_d, wu_d, wd_d, lnfg_d, lnfb_d, identf_d, x_n[sl], r_o, r_x)
            if l != depth - 1:
                b.new_epoch()
        b.finish()
    return nc


def kernel_fused(x, c, positions, w_ada, b_ada, w_in, i_bias, f_bias, conv_w, conv_b, w_out, ln_mix_g, ln_mix_b,
                 w_router, b_router, w_gate, w_up, w_down, ln_ffn_g, ln_ffn_b):
    f32 = np.float32
    x = np.asarray(x, f32)
    c = np.asarray(c, f32)
    positions = np.asarray(positions, np.int32)
    cw, cb = conv_layouts(np.asarray(conv_w, f32), np.asarray(conv_b, f32))
    decT, qdecT, kdec = ret_tables()
    caus, sel4, eye4 = mlstm_consts()
    shared = {
        "invf": inv_freq_table(), "identf": np.eye(128, dtype=f32),
        "w_ada": np.asarray(w_ada, f32), "b_ada": np.asarray(b_ada, f32), "w_in": np.asarray(w_in, f32),
        "i_bias": np.asarray(i_bias, f32), "f_bias": np.asarray(f_bias, f32), "convw": cw, "convb": cb,
        "w_out": np.asarray(w_out, f32), "ln_mix_g": np.asarray(ln_mix_g, f32),
        "ln_mix_b": np.asarray(ln_mix_b, f32), "w_router": np.asarray(w_router, f32),
        "b_router": np.asarray(b_router, f32).reshape(1, 16), "w_gate": np.asarray(w_gate, f32),
        "w_up": np.asarray(w_up, f32), "w_down": np.asarray(w_down, f32),
        "ln_ffn_g": np.asarray(ln_ffn_g, f32), "ln_ffn_b": np.asarray(ln_ffn_b, f32),
        "vis2": np.stack([vis_table(0), vis_table(1)]),
        "jf2": np.stack([np.zeros((128, 1), f32), np.ones((128, 1), f32)]),
        "pw": pw_table(), "decT": decT, "qdecT": qdecT, "kdec": kdec, "caus": caus, "sel4": sel4, "eye4": eye4,
    }
    cores = list(range(8))
    ims = []
    for core in cores:
        bi = core % BATCH
        xb = x[bi].reshape(32, 128, D)
        pb = positions[bi].reshape(32, 128)
        im = dict(shared)
        im["x2"] = np.ascontiguousarray(np.concatenate([xb[0::2], xb[1::2]], axis=0)).reshape(2 * TOK, D)
        im["pos2"] = np.ascontiguousarray(np.stack([pb[0::2].T, pb[1::2].T]))
        im["ccol"] = np.ascontiguousarray(c[bi].reshape(8, 128).T)
        ims.append(im)
    res = run_bass_kernel_spmd(_prog("F", build_fused), ims, core_ids=cores).results
    out = np.zeros((BATCH, SEQ, D), f32)
    for bi in range(BATCH):
        xf = np.asarray(res[bi]["xfin"]).reshape(2, NT, 128, D)
        ob = out[bi].reshape(32, 128, D)
        ob[0::2] = xf[0]
        ob[1::2] = xf[1]
    return out


kernel_unfused = kernel
kernel = kernel_fused
```

```python
import numpy as np
from contextlib import ExitStack

import concourse.bass as bass
import concourse.mybir as mybir
from concourse.bass_utils import run_bass_kernel_spmd

F32 = mybir.dt.float32
BF16 = mybir.dt.bfloat16
ALU = mybir.AluOpType
AF = mybir.ActivationFunctionType
AX = mybir.AxisListType

D = 1024
SEQ = 4096
BATCH = 4
DEPTH = 4
NT = 16
TOK = NT * 128
INW = 3916
LN_EPS = 1e-5
ALPHA = (2.0 * DEPTH) ** 0.25
NEG = -1.0e30

ENGS = ("pe", "act", "dve", "pool", "sp")
NRING = 8


class Res:
    __slots__ = ("name", "w", "r")

    def __init__(self, name=""):
        self.name = name
        self.w = None
        self.r = []


class B:
    def __init__(self, nc, es):
        self.nc = nc
        self.es = es
        self.root_es = es
        self.epoch = 0
        self.q = {e: [] for e in ENGS}
        self.sem = {e: es.enter_context(nc.semaphore("s_" + e)) for e in ENGS}
        self.cnt = {e: 0 for e in ENGS}
        self.seen = {e: {} for e in ENGS}
        self.dq = ("sp", "pool")
        self.ring = {e: [es.enter_context(nc.semaphore("d_%s%d" % (e, i))) for i in range(NRING)]
                     for e in self.dq}
        self.rcnt = {e: [0] * NRING for e in self.dq}
        self.rnext = {e: 0 for e in self.dq}
        self.nres = 0

    def res(self, name=""):
        self.nres += 1
        return Res(name or ("r%d" % self.nres))

    def sb(self, name, shape, dt):
        self.nres += 1
        return self.es.enter_context(self.nc.sbuf_tensor("%s_%d" % (name, self.nres), list(shape), dt))

    def ps(self, name, shape, dt=F32):
        self.nres += 1
        return self.es.enter_context(self.nc.psum_tensor("%s_%d" % (name, self.nres), list(shape), dt))

    def _deps(self, eng, reads, writes):
        need = {}

        def add(ev):
            if ev is None:
                return
            s, v = ev
            k = id(s)
            if k not in need or need[k][1] < v:
                need[k] = (s, v)

        for r in reads:
            add(r.w)
        for w in writes:
            add(w.w)
            for ev in w.r:
                add(ev)
        out = []
        seen = self.seen[eng]
        own = id(self.sem[eng])
        for k, (s, v) in need.items():
            if eng == "pe" and k == own:
                continue
            if seen.get(k, 0) >= v:
                continue
            seen[k] = v
            out.append((s, v))
        return out

    def op(self, eng, fn, reads=(), writes=(), inc=True):
        waits = self._deps(eng, reads, writes)
        if inc:
            self.cnt[eng] += 1
            ev = (self.sem[eng], self.cnt[eng])
        else:
            ev = (self.sem[eng], self.cnt[eng] + 1)
        for r in reads:
            r.r.append(ev)
            if len(r.r) > 64:
                r.r = r.r[-64:] if False else self._compact(r.r)
        for w in writes:
            w.w = ev
            w.r = []
        self.q[eng].append((waits, fn, self.sem[eng] if inc else None, 1))

    @staticmethod
    def _compact(evs):
        best = {}
        for s, v in evs:
            k = id(s)
            if k not in best or best[k][1] < v:
                best[k] = (s, v)
        return list(best.values())

    def dma(self, q, out_ap, in_ap, reads=(), writes=()):
        waits = self._deps(q, reads, writes)
        i = self.rnext[q]
        self.rnext[q] = (i + 1) % NRING
        s = self.ring[q][i]
        if self.rcnt[q][i] > 0:
            v = 16 * self.rcnt[q][i]
            if self.seen[q].get(id(s), 0) < v:
                self.seen[q][id(s)] = v
                waits.append((s, v))
        self.rcnt[q][i] += 1
        ev = (s, 16 * self.rcnt[q][i])
        for r in reads:
            r.r.append(ev)
        for w in writes:
            w.w = ev
            w.r = []

        def fn(e, out_ap=out_ap, in_ap=in_ap):
            return e.dma_start(out=out_ap, in_=in_ap)

        self.q[q].append((waits, fn, s, 16))

    def coll(self, kind, groups, in_ap, out_ap, reads=(), writes=()):
        q = "pool"
        waits = self._deps(q, reads, writes)
        i = self.rnext[q]
        self.rnext[q] = (i + 1) % NRING
        s = self.ring[q][i]
        if self.rcnt[q][i] > 0:
            v = 16 * self.rcnt[q][i]
            if self.seen[q].get(id(s), 0) < v:
                self.seen[q][id(s)] = v
                waits.append((s, v))
        self.rcnt[q][i] += 1
        ev = (s, 16 * self.rcnt[q][i])
        for r in reads:
            r.r.append(ev)
        for w in writes:
            w.w = ev
            w.r = []

        def fn(e):
            return e.collective_compute(kind, ALU.bypass, replica_groups=groups, ins=[in_ap], outs=[out_ap])

        self.q[q].append((waits, fn, s, 16))

    def new_epoch(self):
        self.barrier()
        nc, es = self.nc, self.root_es
        self.epoch += 1
        k = self.epoch
        self.sem = {e: es.enter_context(nc.semaphore("s%d_%s" % (k, e))) for e in ENGS}
        self.cnt = {e: 0 for e in ENGS}
        self.seen = {e: {} for e in ENGS}
        self.ring = {e: [es.enter_context(nc.semaphore("d%d_%s%d" % (k, e, i))) for i in range(NRING)]
                     for e in self.dq}
        self.rcnt = {e: [0] * NRING for e in self.dq}
        self.rnext = {e: 0 for e in self.dq}

    def barrier(self, label=None):
        if not hasattr(self, "marks"):
            self.marks = []
        self.marks.append((self.epoch, dict(self.cnt)))
        evs = []
        for q in self.dq:
            for i in range(NRING):
                if self.rcnt[q][i] > 0:
                    evs.append((self.ring[q][i], 16 * self.rcnt[q][i]))
        for e in ENGS:
            if e != "sp" and self.cnt[e] > 0:
                evs.append((self.sem[e], self.cnt[e]))
        for e in ENGS:
            waits = []
            for s_, v in evs:
                if id(s_) == id(self.sem[e]) and e == "pe":
                    continue
                if self.seen[e].get(id(s_), 0) >= v:
                    continue
                self.seen[e][id(s_)] = v
                waits.append((s_, v))
            if waits:
                self.q[e].append((waits, None, None, 0))

    def finish(self):
        nc = self.nc
        fin = []
        for q in self.dq:
            for i in range(NRING):
                if self.rcnt[q][i] > 0:
                    fin.append((self.ring[q][i], 16 * self.rcnt[q][i]))
        for e in ENGS:
            if e != "sp" and self.cnt[e] > 0:
                fin.append((self.sem[e], self.cnt[e]))
        qs = self.q

        def run(e, lst, extra=()):
            for waits, fn, s, n in lst:
                for ws, wv in waits:
                    e.wait_ge(ws, wv)
                if fn is None:
                    continue
                ins = fn(e)
                if s is not None:
                    ins.then_inc(s, n)
            for ws, wv in extra:
                e.wait_ge(ws, wv)

        with nc.Block() as blk:
            @blk.sync
            def _(e):
                run(e, qs["sp"], fin)

            @blk.tensor
            def _(e):
                run(e, qs["pe"])

            @blk.scalar
            def _(e):
                run(e, qs["act"])

            @blk.vector
            def _(e):
                run(e, qs["dve"])

            @blk.gpsimd
            def _(e):
                run(e, qs["pool"])


def emit_mod(b, ccol_d, wada_d, bada_d, modrow_d, modcol_d):
    nc = b.nc
    with ExitStack() as es:
        old = b.es
        b.es = es
        ccol = b.sb("m_ccol", [128, 8], F32)
        cact = b.sb("m_cact", [128, 8], F32)
        one = b.sb("m_one", [1, 1], F32)
        wbuf = [b.sb("m_w%d" % i, [128, 8, 512], F32) for i in range(2)]
        brow = b.sb("m_brow", [1, 6144], F32)
        mrow = b.sb("m_mrow", [1, 6144], F32)
        mcol = b.sb("m_mcol", [128, DEPTH * 48], F32)
        pr = [b.ps("m_pr%d" % i, [1, 512]) for i in range(2)]
        pc = b.ps("m_pc", [128, 48])
        r_ccol, r_cact, r_one, r_brow, r_mrow, r_mcol, r_pc = (b.res() for _ in range(7))
        r_w = [b.res(), b.res()]
        r_pr = [b.res(), b.res()]
        r_out = b.res()

        b.dma("sp", ccol[:], ccol_d, writes=[r_ccol])
        b.op("act", lambda e: e.activation(out=cact[:], in_=ccol[:], func=AF.Silu),
             reads=[r_ccol], writes=[r_cact])
        b.op("dve", lambda e: e.memset(one[:], 1.0), writes=[r_one])
        it = 0
        for l in range(DEPTH):
            b.dma("sp", brow[:], bada_d[l:l + 1, :], writes=[r_brow])
            for nb in range(12):
                s = it % 2
                it += 1
                src = wada_d[l, :, nb * 512:(nb + 1) * 512].rearrange("(k p) n -> p k n", p=128)
                b.dma("sp", wbuf[s][:], src, writes=[r_w[s]])
                for k in range(8):
                    b.op("pe", lambda e, s=s, k=k: e.matmul(
                        pr[s][:], lhsT=cact[:, k:k + 1], rhs=wbuf[s][:, k, :],
                        start=(k == 0), stop=(k == 7)),
                        reads=[r_cact, r_w[s]], writes=[r_pr[s]], inc=(k == 7))
                b.op("dve", lambda e, s=s, nb=nb: e.tensor_tensor(
                    out=mrow[:, nb * 512:(nb + 1) * 512], in0=pr[s][:],
                    in1=brow[:, nb * 512:(nb + 1) * 512], op=ALU.add),
                    reads=[r_pr[s], r_brow], writes=[r_mrow])
            b.dma("pool", modrow_d[l:l + 1, :], mrow[:], reads=[r_mrow], writes=[r_out])
            for c in range(48):
                b.op("pe", lambda e, c=c: e.matmul(
                    pc[:, c:c + 1], lhsT=mrow[:, c * 128:(c + 1) * 128], rhs=one[:, :],
                    start=True, stop=True),
                    reads=[r_mrow, r_one], writes=[r_pc], inc=(c == 47))
            b.op("act", lambda e, l=l: e.copy(out=mcol[:, l * 48:(l + 1) * 48], in_=pc[:]),
                 reads=[r_pc], writes=[r_mcol])
        b.dma("pool", modcol_d, mcol[:], reads=[r_mcol], writes=[r_out])
        b.barrier()
        b.es = old


def build_mod():
    nc = bass.Bass("TRN2", target_bir_lowering=False)
    ccol_d = nc.dram_tensor("ccol", [128, 8], F32, kind="ExternalInput").ap()
    wada_d = nc.dram_tensor("w_ada", [DEPTH, D, 6 * D], F32, kind="ExternalInput").ap()
    bada_d = nc.dram_tensor("b_ada", [DEPTH, 6 * D], F32, kind="ExternalInput").ap()
    modrow_d = nc.dram_tensor("modrow", [DEPTH, 6 * D], F32, kind="ExternalOutput").ap()
    modcol_d = nc.dram_tensor("modcol", [128, DEPTH * 48], F32, kind="ExternalOutput").ap()
    with ExitStack() as es:
        b = B(nc, es)
        emit_mod(b, ccol_d, wada_d, bada_d, modrow_d, modcol_d)
        b.finish()
    return nc


def run_mod(c, w_ada, b_ada):
    nc = build_mod()
    in_maps = []
    for core in range(8):
        bi = core // 2
        in_maps.append({"ccol": np.ascontiguousarray(c[bi].reshape(8, 128).T),
                        "w_ada": w_ada, "b_ada": b_ada})
    res = run_bass_kernel_spmd(nc, in_maps, core_ids=list(range(8)))
    return [r["modrow"] for r in res.results], [r["modcol"] for r in res.results]


NFT = 19
P1_BLK = [(0, 512), (512, 512), (1024, 512), (1536, 332), (1868, 512), (2380, 512),
          (2892, 512), (3404, 512)]


def emit_rope_tables(b, pos_d, invf_d, cos, sin, r_tab, nt):
    with ExitStack() as es:
        old = b.es
        b.es = es
        posi = b.sb("rt_posi", [128, nt], mybir.dt.int32)
        posf = b.sb("rt_posf", [128, nt], F32)
        invf = b.sb("rt_invf", [128, 32], F32)
        ang = b.sb("rt_ang", [128, nt, 32], F32)
        u = b.sb("rt_u", [128, nt, 32], F32)
        r_pi, r_pf, r_if, r_ang, r_u = (b.res() for _ in range(5))
        b.dma("sp", posi[:], pos_d, writes=[r_pi])
        b.dma("sp", invf[:], invf_d, writes=[r_if])
        b.op("dve", lambda e: e.tensor_copy(out=posf[:], in_=posi[:]), reads=[r_pi], writes=[r_pf])
        for t in range(nt):
            b.op("dve", lambda e, t=t: e.tensor_scalar(
                out=ang[:, t, :], in0=invf[:], scalar1=posf[:, t:t + 1], scalar2=None,
                op0=ALU.mult), reads=[r_if, r_pf], writes=[r_ang])
        two_pi = float(np.float32(2.0 * np.pi))
        pi = float(np.float32(np.pi))
        ki = b.sb("rt_ki", [128, nt, 32], mybir.dt.int32)
        kf = b.sb("rt_kf", [128, nt, 32], F32)
        r_ki, r_kf = b.res(), b.res()

        def reduced_sin(dst, shift):
            b.op("dve", lambda e: e.tensor_scalar(
                out=u[:], in0=ang[:], scalar1=shift, scalar2=None, op0=ALU.add),
                reads=[r_ang], writes=[r_u])
            b.op("dve", lambda e: e.tensor_scalar(
                out=kf[:], in0=u[:], scalar1=float(1.0 / (2.0 * np.pi)), scalar2=None, op0=ALU.mult),
                reads=[r_u], writes=[r_kf])
            b.op("dve", lambda e: e.tensor_copy(out=ki[:], in_=kf[:]), reads=[r_kf], writes=[r_ki])
            b.op("dve", lambda e: e.tensor_copy(out=kf[:], in_=ki[:]), reads=[r_ki], writes=[r_kf])
            b.op("dve", lambda e: e.scalar_tensor_tensor(
                out=u[:], in0=kf[:], scalar=-two_pi, in1=u[:], op0=ALU.mult, op1=ALU.add),
                reads=[r_kf, r_u], writes=[r_u])
            b.op("dve", lambda e: e.tensor_scalar(
                out=kf[:], in0=u[:], scalar1=pi, scalar2=two_pi, op0=ALU.is_gt, op1=ALU.mult),
                reads=[r_u], writes=[r_kf])
            b.op("dve", lambda e: e.tensor_tensor(out=u[:], in0=u[:], in1=kf[:], op=ALU.subtract),
                 reads=[r_u, r_kf], writes=[r_u])
            b.op("dve", lambda e: e.tensor_scalar(
                out=kf[:], in0=u[:], scalar1=-pi, scalar2=two_pi, op0=ALU.is_lt, op1=ALU.mult),
                reads=[r_u], writes=[r_kf])
            b.op("dve", lambda e: e.tensor_tensor(out=u[:], in0=u[:], in1=kf[:], op=ALU.add),
                 reads=[r_u, r_kf], writes=[r_u])
            b.op("dve", lambda e: e.tensor_scalar(
                out=u[:], in0=u[:], scalar1=pi, scalar2=-pi, op0=ALU.min, op1=ALU.max),
                reads=[r_u], writes=[r_u])
            b.op("act", lambda e: e.activation(out=dst[:], in_=u[:], func=AF.Sin),
                 reads=[r_u], writes=[r_tab])

        reduced_sin(sin, 0.0)
        reduced_sin(cos, float(np.float32(np.pi / 2)))
        b.barrier()
        b.es = old


def emit_p1(b, l, nt, x_d, win_d, modcol_d, ibias_d, fbias_d, identf_d,
            cos, sin, r_tab, featT_d, tokA_d, tokF_d, gT_d, r_xd, r_out):
    nc = b.nc
    with ExitStack() as es:
        old = b.es
        b.es = es
        wsb = b.sb("p1_w", [128, 8, INW], BF16)
        wst = [b.sb("p1_wst%d" % i, [128, 8, 512], F32) for i in range(2)]
        identf = b.sb("p1_idf", [128, 128], F32)
        identb = b.sb("p1_idb", [128, 128], BF16)
        mcol = b.sb("p1_mcol", [128, 48], F32)
        sc1 = b.sb("p1_sc1", [128, 8], F32)
        bias8 = b.sb("p1_bias8", [128, 8], F32)
        xt = [b.sb("p1_x%d" % i, [128, D], F32) for i in range(2)]
        hT = [b.sb("p1_hT%d" % i, [128, 8, 128], BF16) for i in range(2)]
        rin = [b.sb("p1_rin%d" % i, [128, 512], BF16) for i in range(5)]
        tmpa = b.sb("p1_tmpa", [128, 256], F32)
        tmpb = b.sb("p1_tmpb", [128, 256], F32)
        stA = [b.sb("p1_stA%d" % i, [128, 1792], BF16) for i in range(2)]
        stF = [b.sb("p1_stF%d" % i, [128, 12], F32) for i in range(2)]
        stT = [b.sb("p1_stT%d" % i, [128, NFT, 128], BF16) for i in range(2)]
        stG = [b.sb("p1_stG%d" % i, [8, 128], F32) for i in range(2)]
        r_stG = [b.res(), b.res()]
        ptr = [b.ps("p1_ptr%d" % i, [128, 512]) for i in range(2)]
        pin = [b.ps("p1_pin%d" % i, [128, 512]) for i in range(4)]
        pto = [b.ps("p1_pto%d" % i, [128, 1024], BF16) for i in range(2)]

        r_w, r_idf, r_idb, r_mcol, r_sc1, r_b8, r_ta, r_tb = (b.res() for _ in range(8))
        r_wst = [b.res(), b.res()]
        r_x = [b.res(), b.res()]
        r_hT = [b.res(), b.res()]
        r_rin = [b.res() for _ in range(5)]
        r_stA = [b.res(), b.res()]
        r_stF = [b.res(), b.res()]
        r_stT = [b.res(), b.res()]
        r_ptr = [b.res(), b.res()]
        r_pin = [b.res() for _ in range(4)]
        r_pto = [b.res(), b.res()]

        b.dma("sp", identf[:], identf_d, writes=[r_idf])
        b.op("dve", lambda e: e.tensor_copy(out=identb[:], in_=identf[:]), reads=[r_idf], writes=[r_idb])
        b.dma("sp", mcol[:], modcol_d[:, l * 48:(l + 1) * 48], writes=[r_mcol])
        b.op("dve", lambda e: e.tensor_scalar(out=sc1[:], in0=mcol[:, 8:16], scalar1=1.0, scalar2=None,
                                              op0=ALU.add), reads=[r_mcol], writes=[r_sc1])
        b.dma("sp", bias8[:, 0:4], ibias_d[l:l + 1, :].to_broadcast([128, 4]), writes=[r_b8])
        b.dma("sp", bias8[:, 4:8], fbias_d[l:l + 1, :].to_broadcast([128, 4]), writes=[r_b8])
        pieces = []
        for (s0, d0, n) in ((0, 0, 1860), (3908, 1860, 8), (1860, 1868, 2048)):
            o = 0
            while o < n:
                m = min(512, n - o)
                pieces.append((s0 + o, d0 + o, m))
                o += m
        for i, (s0, d0, m) in enumerate(pieces):
            s = i % 2
            src = win_d[l, :, s0:s0 + m].rearrange("(k p) n -> p k n", p=128)
            b.dma("sp", wst[s][:, :, 0:m], src, writes=[r_wst[s]])
            eng = "pool" if i % 2 == 0 else "act"
            if eng == "pool":
                b.op("pool", lambda e, s=s, d0=d0, m=m: e.tensor_copy(
                    out=wsb[:, :, d0:d0 + m], in_=wst[s][:, :, 0:m]), reads=[r_wst[s]], writes=[r_w])
            else:
                b.op("act", lambda e, s=s, d0=d0, m=m: e.copy(
                    out=wsb[:, :, d0:d0 + m], in_=wst[s][:, :, 0:m]), reads=[r_wst[s]], writes=[r_w])

        def rope(t, src, H, dst, r_src, r_dst, col0=0):
            sv = src.rearrange("p (h two d) -> p h two d", two=2, d=32)
            dv = dst.rearrange("p (h two d) -> p h two d", two=2, d=32)
            if isinstance(cos, (list, tuple)):
                cj, tj = cos[t // NT], t % NT
                sj = sin[t // NT]
                rtab = r_tab[t // NT]
            else:
                cj, sj, tj, rtab = cos, sin, t, r_tab
            cb = cj[:, tj:tj + 1, :].to_broadcast([128, H, 32])
            sb_ = sj[:, tj:tj + 1, :].to_broadcast([128, H, 32])
            ta = tmpa[:, 0:H * 32].rearrange("p (h d) -> p h d", d=32)
            tb = tmpb[:, 0:H * 32].rearrange("p (h d) -> p h d", d=32)
            b.op("dve", lambda e: e.tensor_tensor(out=ta, in0=sv[:, :, 0, :], in1=cb, op=ALU.mult),
                 reads=[r_src, rtab], writes=[r_ta])
            b.op("dve", lambda e: e.tensor_tensor(out=tb, in0=sv[:, :, 1, :], in1=sb_, op=ALU.mult),
                 reads=[r_src, rtab], writes=[r_tb])
            b.op("dve", lambda e: e.tensor_tensor(out=dv[:, :, 0, :], in0=ta, in1=tb, op=ALU.subtract),
                 reads=[r_ta, r_tb], writes=[r_dst])
            b.op("dve", lambda e: e.tensor_tensor(out=ta, in0=sv[:, :, 1, :], in1=cb, op=ALU.mult),
                 reads=[r_src, rtab], writes=[r_ta])
            b.op("dve", lambda e: e.tensor_tensor(out=tb, in0=sv[:, :, 0, :], in1=sb_, op=ALU.mult),
                 reads=[r_src, rtab], writes=[r_tb])
            b.op("dve", lambda e: e.tensor_tensor(out=dv[:, :, 1, :], in0=ta, in1=tb, op=ALU.add),
                 reads=[r_ta, r_tb], writes=[r_dst])

        pin_i = 0
        pto_i = 0
        pending = []
        pgt = pto[1][:, 0:256].bitcast(F32)
        r_pgt = r_pto[1]
        for t in range(nt):
            s = t % 2
            b.dma("sp", xt[s][:], x_d[t * 128:(t + 1) * 128, :], reads=[r_xd], writes=[r_x[s]])
            for c in range(8):
                b.op("pe", lambda e, s=s, c=c: e.transpose(
                    out=ptr[c // 4][:, (c % 4) * 128:(c % 4 + 1) * 128],
                    in_=xt[s][:, c * 128:(c + 1) * 128], identity=identf[:]),
                    reads=[r_x[s], r_idf], writes=[r_ptr[c // 4]], inc=(c % 4 == 3))
            for c in range(8):
                b.op("act", lambda e, s=s, c=c: e.activation(
                    out=hT[s][:, c, :], in_=ptr[c // 4][:, (c % 4) * 128:(c % 4 + 1) * 128],
                    func=AF.Identity, scale=sc1[:, c:c + 1], bias=mcol[:, c:c + 1]),
                    reads=[r_ptr[c // 4], r_sc1, r_mcol], writes=[r_hT[s]])
            A = stA[s]
            Fs = stF[s]
            T = stT[s]
            for bi, (c0, n) in enumerate(P1_BLK):
                pi = pin_i % 4
                pin_i += 1
                P = pin[pi]
                for k in range(8):
                    b.op("pe", lambda e, s=s, k=k, c0=c0, n=n, P=P: e.matmul(
                        P[:, 0:n], lhsT=hT[s][:, k, :], rhs=wsb[:, k, c0:c0 + n],
                        start=(k == 0), stop=(k == 7)),
                        reads=[r_hT[s], r_w], writes=[r_pin[pi]], inc=(k == 7))
                rp = r_pin[pi]
                if bi == 0 or bi == 1:
                    rope(t, P[:, 0:512], 8, rin[bi][:, 0:512], rp, r_rin[bi])
                elif bi == 2:
                    b.op("act", lambda e, P=P, A=A: e.copy(out=A[:, 0:512], in_=P[:, 0:512]),
                         reads=[rp], writes=[r_stA[s]])
                elif bi == 3:
                    rope(t, P[:, 0:320], 5, rin[2][:, 0:320], rp, r_rin[2])
                    b.op("dve", lambda e: e.tensor_copy(out=rin[2][:, 320:384], in_=rin[2][:, 256:320]),
                         reads=[r_rin[2]], writes=[r_rin[2]])
                    b.op("dve", lambda e, P=P, Fs=Fs: e.tensor_copy(out=Fs[:, 0:4], in_=P[:, 320:324]),
                         reads=[rp], writes=[r_stF[s]])
                    b.op("dve", lambda e, P=P, Fs=Fs: e.tensor_tensor(
                        out=Fs[:, 4:12], in0=P[:, 324:332], in1=bias8[:], op=ALU.add),
                        reads=[rp, r_b8], writes=[r_stF[s]])
                elif bi == 4:
                    rope(t, P[:, 0:512], 8, rin[3][:, 0:512], rp, r_rin[3])
                    b.op("pool", lambda e, A=A: e.tensor_copy(out=A[:, 512:768], in_=rin[3][:, 256:512]),
                         reads=[r_rin[3]], writes=[r_stA[s]])
                elif bi == 5:
                    b.op("act", lambda e, P=P, A=A: e.copy(out=A[:, 768:1024], in_=P[:, 0:256]),
                         reads=[rp], writes=[r_stA[s]])
                    b.op("act", lambda e, P=P, A=A: e.activation(out=A[:, 1024:1280], in_=P[:, 256:512],
                                                                 func=AF.Silu),
                         reads=[rp], writes=[r_stA[s]])
                elif bi == 6:
                    b.op("act", lambda e, P=P: e.copy(out=rin[4][:, 0:512], in_=P[:, 0:512]),
                         reads=[rp], writes=[r_rin[4]])
                else:
                    b.op("act", lambda e, P=P, A=A: e.copy(out=A[:, 1280:1536], in_=P[:, 0:256]),
                         reads=[rp], writes=[r_stA[s]])
                    b.op("act", lambda e, P=P, A=A: e.activation(out=A[:, 1536:1792], in_=P[:, 256:512],
                                                                 func=AF.Sigmoid),
                         reads=[rp], writes=[r_stA[s]])
                if bi == 0:
                    srcs = [(rin[0], r_rin[0], i * 128, i) for i in range(4)]
                elif bi == 1:
                    srcs = [(rin[1], r_rin[1], i * 128, 4 + i) for i in range(4)]
                elif bi == 3:
                    srcs = [(rin[2], r_rin[2], 0, 8), (rin[2], r_rin[2], 128, 9), (rin[2], r_rin[2], 256, 10)]
                elif bi == 4:
                    srcs = [(rin[3], r_rin[3], i * 128, 11 + i) for i in range(4)]
                elif bi == 6:
                    srcs = [(rin[4], r_rin[4], i * 128, 15 + i) for i in range(4)]
                else:
                    srcs = []
                if srcs:
                    def tr_job(srcs=srcs, T=T, s=s):
                        nonlocal pto_i
                        po = pto_i % 2
                        pto_i += 1
                        for i, (buf, rb, c, slot) in enumerate(srcs):
                            b.op("pe", lambda e, buf=buf, c=c, i=i, po=po: e.transpose(
                                out=pto[po][:, i * 128:(i + 1) * 128], in_=buf[:, c:c + 128], identity=identb[:]),
                                reads=[rb, r_idb], writes=[r_pto[po]], inc=(i == len(srcs) - 1))
                        s0 = srcs[0][3]
                        n_ = len(srcs)
                        b.op("act", lambda e, po=po, s0=s0, n_=n_, T=T: e.copy(
                            out=T[:, s0:s0 + n_, :], in_=pto[po][:, 0:n_ * 128].rearrange("p (n k) -> p n k", k=128)),
                            reads=[r_pto[po]], writes=[r_stT[s]])
                    pending.append(tr_job)
                while len(pending) > 2:
                    pending.pop(0)()
            def out_job(t=t, s=s, A=A, Fs=Fs, T=T):
                b.dma("pool", tokA_d[t * 128:(t + 1) * 128, :], A[:], reads=[r_stA[s]], writes=[r_out])
                b.dma("pool", tokF_d[t * 128:(t + 1) * 128, :], Fs[:], reads=[r_stF[s]], writes=[r_out])
                b.op("pe", lambda e, Fs=Fs: e.transpose(out=pgt[0:8, 0:128], in_=Fs[:, 4:12], identity=identf[:]),
                     reads=[r_stF[s], r_idf], writes=[r_pgt])
                b.op("act", lambda e, s=s: e.copy(out=stG[s][:], in_=pgt[0:8, 0:128]),
                     reads=[r_pgt], writes=[r_stG[s]])
                b.dma("pool", gT_d[t], stG[s][:], reads=[r_stG[s]], writes=[r_out])
                b.dma("pool", featT_d[t], T[:], reads=[r_stT[s]], writes=[r_out])
            pending.append(out_job)
        while pending:
            pending.pop(0)()
        b.barrier()
        b.es = old


def inv_freq_table():
    half = 32
    inv = (np.float32(10000.0) ** (-(np.arange(half, dtype=np.float32) / np.float32(half)))).astype(np.float32)
    return np.ascontiguousarray(np.broadcast_to(inv[None, :], (128, half))).astype(np.float32)


def build_p1(l, nt):
    nc = bass.Bass("TRN2", target_bir_lowering=False)
    x_d = nc.dram_tensor("x", [nt * 128, D], F32, kind="ExternalInput").ap()
    pos_d = nc.dram_tensor("pos", [128, nt], mybir.dt.int32, kind="ExternalInput").ap()
    invf_d = nc.dram_tensor("invf", [128, 32], F32, kind="ExternalInput").ap()
    identf_d = nc.dram_tensor("identf", [128, 128], F32, kind="ExternalInput").ap()
    win_d = nc.dram_tensor("w_in", [DEPTH, D, INW], F32, kind="ExternalInput").ap()
    modcol_d = nc.dram_tensor("modcol", [128, DEPTH * 48], F32, kind="ExternalInput").ap()
    ib_d = nc.dram_tensor("i_bias", [DEPTH, 4], F32, kind="ExternalInput").ap()
    fb_d = nc.dram_tensor("f_bias", [DEPTH, 4], F32, kind="ExternalInput").ap()
    featT_d = nc.dram_tensor("featT", [nt, 128, NFT, 128], BF16, kind="ExternalOutput").ap()
    tokA_d = nc.dram_tensor("tokA", [nt * 128, 1792], BF16, kind="ExternalOutput").ap()
    tokF_d = nc.dram_tensor("tokF", [nt * 128, 12], F32, kind="ExternalOutput").ap()
    gT_d = nc.dram_tensor("gT", [nt, 8, 128], F32, kind="ExternalOutput").ap()
    with ExitStack() as es, nc.allow_low_precision("bf16 matmul operands, fp32 accumulation"):
        b = B(nc, es)
        cos = b.sb("cos", [128, nt, 32], F32)
        sin = b.sb("sin", [128, nt, 32], F32)
        r_tab = b.res()
        emit_rope_tables(b, pos_d, invf_d, cos, sin, r_tab, nt)
        emit_p1(b, l, nt, x_d, win_d, modcol_d, ib_d, fb_d, identf_d, cos, sin, r_tab,
                featT_d, tokA_d, tokF_d, gT_d, b.res(), b.res())
        b.finish()
    return nc


NIT = 18
TOPK = 256


def gidx(g):
    return (g % 2) * NT + g // 2


def emit_p2a(b, nslots, featT_all, tokA_all, featT_own, tokF_own, vis_d, pw_d, identf_d, o_d, r_in, r_out,
             parities=None):
    nc = b.nc
    NACT = 0
    with ExitStack() as es:
        old = b.es
        b.es = es
        kT = b.sb("a_kT", [128, 4, SEQ], BF16)
        ikT = b.sb("a_ikT", [128, SEQ], BF16)
        Va = b.sb("a_Va", [128, 32, 8, 65], BF16)
        iw = b.sb("a_iw", [128, NT, 4], F32)
        vis = b.sb("a_vis", [128, 256], F32)
        pw = b.sb("a_pw", [128, NIT + 1], F32)
        identf = b.sb("a_idf", [128, 128], F32)
        identb = b.sb("a_idb", [128, 128], BF16)
        Ib = [b.sb("a_I%d" % i, [128, SEQ], F32) for i in range(3)]
        rl = [b.sb("a_rl%d" % i, [128, 512], F32) for i in range(2)]
        Mbs = [b.sb("a_Mb%d" % i, [128, SEQ], BF16) for i in range(3)]
        MT = [b.sb("a_MT%d" % i, [128, 32, 128], BF16) for i in range(3)]
        E = [b.sb("a_E%d" % i, [128, 512], BF16) for i in range(3)]
        qpad = [b.sb("a_qpad%d" % i, [128, 8, 128], BF16) for i in range(2)]
        iqpad = [b.sb("a_iqpad%d" % i, [128, 4, 128], BF16) for i in range(3)]
        r_qpad = [b.res(), b.res()]
        r_iqpad = [b.res(), b.res(), b.res()]
        sms = [b.sb("a_sm%d" % i, [128, 16], F32) for i in range(3)]
        rcp = b.sb("a_rcp", [128, 8], F32)
        deltas = [b.sb("a_delta%d" % i, [128, NIT + 1], F32) for i in range(3)]
        ndeltas = [b.sb("a_ndelta%d" % i, [128, NIT + 1], F32) for i in range(3)]
        osb = [b.sb("a_o%d" % i, [128, 512], BF16) for i in range(2)]
        pI = [b.ps("a_pI%d" % i, [128, 512]) for i in range(2)]
        pS = [b.ps("a_pS%d" % i, [128, 512]) for i in range(3)]
        pO = [b.ps("a_pO%d" % i, [128, 512]) for i in range(2)]
        pMT = b.ps("a_pMT", [128, 1024], BF16)

        (r_kT, r_ikT, r_Va, r_qT, r_iqT, r_iw, r_vis, r_pw, r_idf, r_idb, r_pMT, r_vst, r_rcp) = (
            b.res() for _ in range(13))
        r_I = [b.res(), b.res(), b.res()]
        r_rl = [b.res(), b.res()]
        r_Mb = [b.res(), b.res(), b.res()]
        r_MT = [b.res(), b.res(), b.res()]
        r_E = [b.res() for _ in range(3)]
        r_sm = [b.res(), b.res(), b.res()]
        r_delta = [b.res(), b.res(), b.res()]
        r_osb = [b.res(), b.res()]
        r_pI = [b.res(), b.res()]
        r_pS = [b.res() for _ in range(3)]
        r_pO = [b.res(), b.res()]

        b.dma("sp", identf[:], identf_d, writes=[r_idf])
        b.op("dve", lambda e: e.tensor_copy(out=identb[:], in_=identf[:]), reads=[r_idf], writes=[r_idb])
        b.dma("sp", pw[:], pw_d, writes=[r_pw])
        ngl = 2 * nslots
        for g in range(ngl):
            gi = gidx(g)
            b.dma("sp", kT[:, :, g * 128:(g + 1) * 128], featT_all[gi, :, 4:8, :], reads=[r_in], writes=[r_kT])
            b.dma("sp", ikT[:, g * 128:(g + 1) * 128], featT_all[gi, :, 10, :], reads=[r_in], writes=[r_ikT])
        b.op("pool", lambda e: e.memset(Va[:, :, :, 64:65], 1.0), writes=[r_Va])
        vst = b.sb("a_vst", [128, 8, 512], BF16)
        for g0 in range(0, ngl, 8):
            n = min(8, ngl - g0)
            for i in range(n):
                gi = gidx(g0 + i)
                b.dma("sp", vst[:, i, :], tokA_all[gi * 128:(gi + 1) * 128, 0:512], reads=[r_in], writes=[r_vst])
            b.op("pool", lambda e, g0=g0, n=n: e.tensor_copy(
                out=Va[:, g0:g0 + n, :, 0:64],
                in_=vst[:, 0:n, :].rearrange("p g (h d) -> p g h d", d=64)),
                reads=[r_vst], writes=[r_Va])

        for i in range(2):
            b.op("pool", lambda e, i=i: e.memset(qpad[i][:], 0.0), writes=[r_qpad[i]])
        for i in range(3):
            b.op("pool", lambda e, i=i: e.memset(iqpad[i][:], 0.0), writes=[r_iqpad[i]])
        LO, HI, RNG, CAND, CNT, VV, TT, NCD, SS, SG = range(10)
        MBIG = 30000.0
        cnts = {"i": 0, "s": 0}

        def sel_gen(m):
            L = (2 * m + 2) * 128
            nkb = 2 * m + 2
            s = m % 3
            I = Ib[s]
            sm = sms[s]
            delta = deltas[s]
            ndelta = ndeltas[s]
            Mb = Mbs[s]
            rsm = r_sm[s]
            rdl = r_delta[s]
            for half in range(2):
                hsl = slice(half * 64, half * 64 + 64)
                b.dma("sp", iqpad[s][hsl, half::2, :], featT_own[m, hsl, 8:10, :], reads=[r_in],
                      writes=[r_iqpad[s]])
            for kg in range((L + 511) // 512):
                w = min(512, L - kg * 512)
                for h in range(4):
                    pi = cnts["i"] % 2
                    cnts["i"] += 1
                    hs = slice((h % 2) * 64, (h % 2) * 64 + 64)
                    b.op("pe", lambda e, pi=pi, h=h, kg=kg, w=w: e.matmul(
                        pI[pi][:, 0:w], lhsT=iqpad[s][:, h, :],
                        rhs=ikT[:, kg * 512:kg * 512 + w], start=True, stop=True),
                        reads=[r_iqpad[s], r_ikT], writes=[r_pI[pi]])
                    b.op("act", lambda e, pi=pi, w=w: e.activation(out=rl[pi][:, 0:w], in_=pI[pi][:, 0:w],
                                                                    func=AF.Relu),
                         reads=[r_pI[pi]], writes=[r_rl[pi]])
                    dst = I[:, kg * 512:kg * 512 + w]
                    if h == 0:
                        b.op("dve", lambda e, pi=pi, w=w, dst=dst: e.tensor_scalar(
                            out=dst, in0=rl[pi][:, 0:w], scalar1=iw[:, m, 0:1], scalar2=None, op0=ALU.mult),
                            reads=[r_rl[pi], r_iw], writes=[r_I[s]])
                    else:
                        b.op("dve", lambda e, pi=pi, w=w, dst=dst, h=h: e.scalar_tensor_tensor(
                            out=dst, in0=rl[pi][:, 0:w], scalar=iw[:, m, h:h + 1], in1=dst,
                            op0=ALU.mult, op1=ALU.add),
                            reads=[r_rl[pi], r_iw, r_I[s]], writes=[r_I[s]])
                    yield
            if m == 0:
                b.op("dve", lambda e: e.tensor_tensor(out=I[:, L - 256:L], in0=I[:, L - 256:L],
                                                      in1=vis[:], op=ALU.add),
                     reads=[r_I[s], r_vis], writes=[r_I[s]])
                b.op("dve", lambda e: e.memset(sm[:, TT:TT + 1], 0.5 * NEG), writes=[rsm])
            else:
                b.op("dve", lambda e: e.tensor_reduce(out=sm[:, LO:LO + 1], in_=I[:, 0:L - 256],
                                                      axis=AX.X, op=ALU.min),
                     reads=[r_I[s]], writes=[rsm])
                b.op("dve", lambda e: e.tensor_tensor(out=I[:, L - 256:L], in0=I[:, L - 256:L],
                                                      in1=vis[:], op=ALU.add),
                     reads=[r_I[s], r_vis], writes=[r_I[s]])
                b.op("dve", lambda e: e.tensor_reduce(out=sm[:, HI:HI + 1], in_=I[:, 0:L],
                                                      axis=AX.X, op=ALU.max),
                     reads=[r_I[s]], writes=[rsm])
                yield
                b.op("dve", lambda e: e.tensor_tensor(out=sm[:, RNG:RNG + 1], in0=sm[:, HI:HI + 1],
                                                      in1=sm[:, LO:LO + 1], op=ALU.subtract),
                     reads=[rsm], writes=[rsm])
                b.op("dve", lambda e: e.tensor_scalar(out=delta[:], in0=pw[:], scalar1=sm[:, RNG:RNG + 1],
                                                      scalar2=None, op0=ALU.mult),
                     reads=[rsm, r_pw], writes=[rdl])
                b.op("dve", lambda e: e.tensor_scalar(out=ndelta[:], in0=delta[:], scalar1=-1.0,
                                                      scalar2=None, op0=ALU.mult),
                     reads=[rdl], writes=[rdl])
                b.op("dve", lambda e: e.tensor_tensor(out=sm[:, NCD:NCD + 1], in0=ndelta[:, 0:1],
                                                      in1=sm[:, LO:LO + 1], op=ALU.subtract),
                     reads=[rsm, rdl], writes=[rsm])
                yield
                for n in range(NACT):
                    b.op("act", lambda e: e.activation(
                        out=Mb[:, 0:L], in_=I[:, 0:L], func=AF.Sign, bias=sm[:, NCD:NCD + 1],
                        accum_out=sm[:, SS:SS + 1]),
                        reads=[r_I[s], rsm], writes=[r_Mb[s], rsm])
                    b.op("act", lambda e: e.activation(
                        out=sm[:, SG:SG + 1], in_=sm[:, SS:SS + 1], func=AF.Sign, bias=float(L - (2 * TOPK - 1))),
                        reads=[rsm], writes=[rsm])
                    b.op("act", lambda e, n=n: e.activation(
                        out=sm[:, NCD:NCD + 1], in_=sm[:, SG:SG + 1], func=AF.Identity,
                        scale=ndelta[:, n + 1:n + 2], bias=sm[:, NCD:NCD + 1]),
                        reads=[rsm, rdl], writes=[rsm])
                    yield
                b.op("dve", lambda e: e.tensor_scalar(out=sm[:, CAND:CAND + 1], in0=sm[:, NCD:NCD + 1],
                                                      scalar1=-1.0, scalar2=None, op0=ALU.mult),
                     reads=[rsm], writes=[rsm])
                for n in range(NACT, NIT):
                    b.op("dve", lambda e: e.tensor_scalar(
                        out=Mb[:, 0:L], in0=I[:, 0:L], scalar1=sm[:, CAND:CAND + 1], scalar2=0.0,
                        op0=ALU.is_ge, op1=ALU.add, accum_out=sm[:, CNT:CNT + 1]),
                        reads=[r_I[s], rsm], writes=[r_Mb[s], rsm])
                    b.op("dve", lambda e: e.tensor_scalar(
                        out=sm[:, VV:VV + 1], in0=sm[:, CNT:CNT + 1], scalar1=TOPK - 0.5, scalar2=0.5,
                        op0=ALU.is_ge, op1=ALU.subtract),
                        reads=[rsm], writes=[rsm])
                    b.op("dve", lambda e, n=n: e.scalar_tensor_tensor(
                        out=sm[:, CAND:CAND + 1], in0=sm[:, VV:VV + 1], scalar=delta[:, n:n + 1],
                        in1=sm[:, CAND:CAND + 1], op0=ALU.mult, op1=ALU.add),
                        reads=[rsm, rdl], writes=[rsm])
                    yield
                b.op("dve", lambda e: e.tensor_tensor(out=sm[:, TT:TT + 1], in0=sm[:, CAND:CAND + 1],
                                                      in1=delta[:, NIT:NIT + 1], op=ALU.subtract),
                     reads=[rsm, rdl], writes=[rsm])
            b.op("dve", lambda e: e.tensor_scalar(
                out=Mb[:, 0:L], in0=I[:, 0:L], scalar1=sm[:, TT:TT + 1], scalar2=None, op0=ALU.is_ge),
                reads=[r_I[s], rsm], writes=[r_Mb[s]])
            yield
            MTs = MT[s]
            for k0 in range(0, nkb, 8):
                n = min(8, nkb - k0)
                for i in range(n):
                    b.op("pe", lambda e, i=i, k0=k0: e.transpose(
                        out=pMT[:, i * 128:(i + 1) * 128], in_=Mb[:, (k0 + i) * 128:(k0 + i + 1) * 128],
                        identity=identb[:]),
                        reads=[r_Mb[s], r_idb], writes=[r_pMT], inc=(i == n - 1))
                b.op("act", lambda e, k0=k0, n=n: e.activation(
                    out=MTs[:, k0:k0 + n, :], in_=pMT[:, 0:n * 128].rearrange("p (n k) -> p n k", k=128),
                    func=AF.Identity, scale=MBIG, bias=-MBIG),
                    reads=[r_pMT], writes=[r_MT[s]])
                yield

        def att_gen(m):
            nkb = 2 * m + 2
            s = m % 2
            s3 = m % 3
            MTs = MT[s3]
            O = osb[s]
            for half in range(2):
                hsl = slice(half * 64, half * 64 + 64)
                b.dma("sp", qpad[s][hsl, half::2, :], featT_own[m, hsl, 0:4, :], reads=[r_in],
                      writes=[r_qpad[s]])
            units = [(h, k0, min(4, nkb - k0)) for h in range(8) for k0 in range(0, nkb, 4)]

            def emit_pv(u, si):
                h, k0, n = u
                p = h // 2
                po = pO[h // 4]
                r_po = r_pO[h // 4]
                oc = (h % 4) * 65
                for i in range(n):
                    kb = k0 + i
                    b.op("pe", lambda e, si=si, i=i, kb=kb, h=h, po=po, oc=oc: e.matmul(
                        po[:, oc:oc + 65], lhsT=E[si][:, i * 128:(i + 1) * 128], rhs=Va[:, kb, h, :],
                        start=(kb == 0), stop=(kb == nkb - 1)),
                        reads=[r_E[si], r_Va], writes=[r_po], inc=(i == n - 1))
                if k0 + n == nkb:
                    b.op("dve", lambda e, po=po, oc=oc, h=h: e.reciprocal(out=rcp[:, h:h + 1],
                                                                           in_=po[:, oc + 64:oc + 65]),
                         reads=[r_po], writes=[r_rcp])
                    b.op("dve", lambda e, po=po, oc=oc, h=h: e.tensor_scalar(
                        out=O[:, h * 64:(h + 1) * 64], in0=po[:, oc:oc + 64], scalar1=rcp[:, h:h + 1],
                        scalar2=None, op0=ALU.mult),
                        reads=[r_po, r_rcp], writes=[r_osb[s]])

            prev = None
            for u in units:
                h, k0, n = u
                p = h // 2
                si = cnts["s"] % 3
                cnts["s"] += 1
                b.op("pe", lambda e, si=si, k0=k0, n=n: e.matmul(
                    pS[si][:, 0:n * 128], lhsT=identb[:, :],
                    rhs=MTs[:, k0:k0 + n, :].rearrange("p n k -> p (n k)"), start=True, stop=False),
                    reads=[r_idb, r_MT[s3]], writes=[r_pS[si]], inc=False)
                for i in range(n):
                    kb = k0 + i
                    b.op("pe", lambda e, si=si, i=i, kb=kb, p=p, h=h, n=n: e.matmul(
                        pS[si][:, i * 128:(i + 1) * 128], lhsT=kT[:, p, kb * 128:(kb + 1) * 128],
                        rhs=qpad[s][:, h, :], start=False, stop=(i == n - 1)),
                        reads=[r_kT, r_qpad[s]], writes=[r_pS[si]], inc=(i == n - 1))
                b.op("act", lambda e, si=si, n=n: e.activation(
                    out=E[si][:, 0:n * 128], in_=pS[si][:, 0:n * 128], func=AF.Exp, scale=0.125),
                    reads=[r_pS[si]], writes=[r_E[si]])
                if prev is not None:
                    emit_pv(*prev)
                prev = (u, si)
                yield
            emit_pv(*prev)
            b.dma("pool", o_d[m * 128:(m + 1) * 128, 0:512], O[:], reads=[r_osb[s]], writes=[r_out])

        plist = parities if parities is not None else [(featT_own, tokF_own, vis_d, o_d)]

        def step(gen):
            try:
                next(gen)
                return True
            except StopIteration:
                return False

        for (featT_own, tokF_own, vis_d, o_d) in plist:
            b.dma("sp", vis[:], vis_d, writes=[r_vis])
            b.dma("sp", iw[:], tokF_own[:, 0:4].rearrange("(t p) c -> p t c", p=128), reads=[r_in], writes=[r_iw])
            for _ in sel_gen(0):
                pass
            cur = sel_gen(1) if nslots > 1 else None
            for m in range(nslots):
                A = att_gen(m)
                nxt = sel_gen(m + 2) if m + 2 < nslots else None
                a_live = True
                while a_live:
                    a_live = step(A)
                    if cur is not None and not step(cur):
                        cur = None
                    if nxt is not None and cur is not None:
                        if not step(nxt):
                            nxt = None
                while cur is not None:
                    if not step(cur):
                        cur = None
                cur = nxt
        b.barrier()
        b.es = old


def vis_table(j):
    v = np.zeros((128, 256), np.float32)
    diag = np.zeros((128, 128), np.float32)
    diag[0:64, 64:128] = NEG
    if j == 0:
        v[:, 0:128] = diag
        v[:, 128:256] = NEG
    else:
        v[:, 128:256] = diag
    return v


def pw_table():
    return np.ascontiguousarray(np.broadcast_to(
        (0.5 ** np.arange(1, NIT + 2, dtype=np.float64)).astype(np.float32)[None, :], (128, NIT + 1)))


def build_p2a(nslots):
    nc = bass.Bass("TRN2", target_bir_lowering=False)
    featT_all = nc.dram_tensor("featT_all", [2 * NT, 128, NFT, 128], BF16, kind="ExternalInput").ap()
    tokA_all = nc.dram_tensor("tokA_all", [2 * TOK, 1792], BF16, kind="ExternalInput").ap()
    featT_own = nc.dram_tensor("featT_own", [NT, 128, NFT, 128], BF16, kind="ExternalInput").ap()
    tokF_own = nc.dram_tensor("tokF_own", [TOK, 12], F32, kind="ExternalInput").ap()
    vis_d = nc.dram_tensor("vis", [128, 256], F32, kind="ExternalInput").ap()
    pw_d = nc.dram_tensor("pw", [128, NIT + 1], F32, kind="ExternalInput").ap()
    identf_d = nc.dram_tensor("identf", [128, 128], F32, kind="ExternalInput").ap()
    o_d = nc.dram_tensor("o", [TOK, D], BF16, kind="ExternalOutput").ap()
    with ExitStack() as es, nc.allow_low_precision("bf16 matmul operands, fp32 accumulation"):
        b = B(nc, es)
        emit_p2a(b, nslots, featT_all, tokA_all, featT_own, tokF_own, vis_d, pw_d, identf_d, o_d,
                 b.res(), b.res())
        b.finish()
    return nc


def gen_head_norm(b, src, r_src, gate, r_gate, dst, r_dst, tmp, st, r_tmp, r_st):
    b.op("dve", lambda e: e.tensor_reduce(out=st[:, 0:4], in_=src[:], axis=AX.X, op=ALU.add),
         reads=[r_src], writes=[r_st])
    yield
    b.op("dve", lambda e: e.tensor_scalar(out=st[:, 0:4], in0=st[:, 0:4], scalar1=1.0 / 64, scalar2=None,
                                          op0=ALU.mult), reads=[r_st], writes=[r_st])
    yield
    b.op("dve", lambda e: e.tensor_tensor(out=src[:], in0=src[:],
                                          in1=st[:, 0:4].unsqueeze(2).to_broadcast([128, 4, 64]),
                                          op=ALU.subtract), reads=[r_src, r_st], writes=[r_src])
    yield
    b.op("dve", lambda e: e.tensor_tensor(out=tmp[:], in0=src[:], in1=src[:], op=ALU.mult),
         reads=[r_src], writes=[r_tmp])
    yield
    b.op("dve", lambda e: e.tensor_reduce(out=st[:, 4:8], in_=tmp[:], axis=AX.X, op=ALU.add),
         reads=[r_tmp], writes=[r_st])
    yield
    b.op("dve", lambda e: e.tensor_scalar(out=st[:, 4:8], in0=st[:, 4:8], scalar1=1.0 / 64, scalar2=LN_EPS,
                                          op0=ALU.mult, op1=ALU.add), reads=[r_st], writes=[r_st])
    yield
    b.op("act", lambda e: e.activation(out=st[:, 8:12], in_=st[:, 4:8], func=AF.Sqrt),
         reads=[r_st], writes=[r_st])
    yield
    b.op("dve", lambda e: e.reciprocal(out=st[:, 12:16], in_=st[:, 8:12]), reads=[r_st], writes=[r_st])
    yield
    if gate is None:
        b.op("dve", lambda e: e.tensor_tensor(
            out=dst.rearrange("p (h d) -> p h d", d=64), in0=src[:],
            in1=st[:, 12:16].unsqueeze(2).to_broadcast([128, 4, 64]), op=ALU.mult),
            reads=[r_src, r_st], writes=[r_dst])
        yield
    else:
        b.op("dve", lambda e: e.tensor_tensor(
            out=tmp[:], in0=src[:], in1=st[:, 12:16].unsqueeze(2).to_broadcast([128, 4, 64]), op=ALU.mult),
            reads=[r_src, r_st], writes=[r_tmp])
        yield
        b.op("dve", lambda e: e.tensor_tensor(
            out=dst.rearrange("p (h d) -> p h d", d=64), in0=tmp[:],
            in1=gate.rearrange("p (h d) -> p h d", d=64), op=ALU.mult),
            reads=[r_tmp, r_gate], writes=[r_dst])
        yield


def emit_head_norm(b, src, r_src, gate, r_gate, dst, r_dst, tmp, st, r_tmp, r_st):
    for _ in gen_head_norm(b, src, r_src, gate, r_gate, dst, r_dst, tmp, st, r_tmp, r_st):
        pass


GAMMAS = [1.0 - 2.0 ** (-5.0 - h) for h in range(4)]


def ret_tables():
    i = np.arange(128)
    decT = np.zeros((128, 4, 128), np.float32)
    qdecT = np.zeros((128, 2, 128), np.float32)
    kdec = np.zeros((128, 4), np.float32)
    for h, g in enumerate(GAMMAS):
        diff = i[None, :] - i[:, None]
        decT[:, h, :] = np.where(diff >= 0, g ** np.maximum(diff, 0), 0.0) / 8.0
        qdecT[(h % 2) * 64:(h % 2) * 64 + 64, h // 2, :] = (g ** (i + 1.0))[None, :]
        kdec[:, h] = g ** (127.0 - i) / 8.0
    return decT, qdecT, kdec


def emit_p2b(b, nslots, tokA_all, featT_own, tokA_own, decT_d, qdecT_d, kdec_d, jf_d, o_d, r_in, r_out,
             whole=False):
    with ExitStack() as es:
        old = b.es
        b.es = es
        decT = b.sb("b_decT", [128, 4, 128], F32)
        qdecT = b.sb("b_qdecT", [128, 2, 128], F32)
        kdec = b.sb("b_kdec", [128, 4], F32)
        jf = b.sb("b_jf", [128, 1], F32)
        S = b.sb("b_S", [128, 2, 64], F32)
        SA = b.sb("b_SA", [128, 2, 64], F32)
        Sd = b.sb("b_Sd", [128, 2, 64], F32)
        Sbf = b.sb("b_Sbf", [128, 2, 64], BF16)
        kvin = [b.sb("b_kvin%d" % i, [128, 512], BF16) for i in range(2)]
        vdec = [b.sb("b_vdec%d" % i, [128, 256], BF16) for i in range(2)]
        qk = [b.sb("b_qk%d" % i, [128, 4, 128], BF16) for i in range(2)]
        qd = [b.sb("b_qd%d" % i, [128, 2, 128], BF16) for i in range(2)]
        own = [b.sb("b_own%d" % i, [128, 512], BF16) for i in range(2)]
        scT = [b.sb("b_scT%d" % i, [128, 128], BF16) for i in range(2)]
        ysbs = [b.sb("b_ysb%d" % i, [128, 4, 64], F32) for i in range(2)]
        tmps = [b.sb("b_tmp%d" % i, [128, 4, 64], F32) for i in range(2)]
        sts = [b.sb("b_st%d" % i, [128, 16], F32) for i in range(2)]
        r_ysbs, r_tmps, r_sts = ([b.res(), b.res()] for _ in range(3))
        pending_fin = None
        fin_i = 0
        osb = [b.sb("b_o%d" % i, [128, 256], BF16) for i in range(2)]
        pKV = [b.ps("b_pKV%d" % i, [128, 128]) for i in range(2)]
        pSC = [b.ps("b_pSC%d" % i, [128, 128]) for i in range(2)]
        pY = b.ps("b_pY", [128, 256])
        (r_c, r_S, r_SA, r_Sd, r_Sbf, r_ysb, r_tmp, r_st, r_pY) = (b.res() for _ in range(9))
        r_kvin = [b.res(), b.res()]
        r_vdec = [b.res(), b.res()]
        r_qk = [b.res(), b.res()]
        r_qd = [b.res(), b.res()]
        r_own = [b.res(), b.res()]
        r_scT = [b.res(), b.res()]
        r_osb = [b.res(), b.res()]
        r_pKV = [b.res(), b.res()]
        r_pSC = [b.res(), b.res()]

        b.dma("sp", decT[:], decT_d, writes=[r_c])
        b.dma("sp", qdecT[:], qdecT_d, writes=[r_c])
        b.dma("sp", kdec[:], kdec_d, writes=[r_c])
        b.dma("sp", jf[:], jf_d, writes=[r_c])
        b.op("dve", lambda e: e.memset(S[:], 0.0), writes=[r_S])
        sc_i = 0
        for g in range(2 * nslots):
            m, r = g // 2, g % 2
            s = g % 2
            gi = gidx(g)
            b.dma("sp", kvin[s][:], tokA_all[gi * 128:(gi + 1) * 128, 512:1024], reads=[r_in], writes=[r_kvin[s]])
            if whole:
                so = g % 2
                orow = gi * 128
                b.dma("sp", qk[so][:], featT_own[gi, :, 11:15, :], reads=[r_in], writes=[r_qk[so]])
                b.dma("sp", own[so][:], tokA_all[gi * 128:(gi + 1) * 128, 768:1280], reads=[r_in], writes=[r_own[so]])
                b.op("dve", lambda e: e.tensor_copy(out=Sbf[:], in_=S[:]), reads=[r_S], writes=[r_Sbf])
            elif r == 0:
                b.op("dve", lambda e: e.tensor_copy(out=SA[:], in_=S[:]), reads=[r_S], writes=[r_SA])
                so = m % 2
                b.dma("sp", qk[so][:], featT_own[m, :, 11:15, :], reads=[r_in], writes=[r_qk[so]])
                b.dma("sp", own[so][:], tokA_own[m * 128:(m + 1) * 128, 768:1280], reads=[r_in], writes=[r_own[so]])
            else:
                so = m % 2
                orow = m * 128
                b.op("dve", lambda e: e.tensor_tensor(out=Sd[:], in0=S[:], in1=SA[:], op=ALU.subtract),
                     reads=[r_S, r_SA], writes=[r_Sd])
                b.op("dve", lambda e: e.scalar_tensor_tensor(
                    out=Sbf[:].rearrange("p a e -> p (a e)"), in0=Sd[:].rearrange("p a e -> p (a e)"),
                    scalar=jf[:, 0:1], in1=SA[:].rearrange("p a e -> p (a e)"), op0=ALU.mult, op1=ALU.add),
                    reads=[r_Sd, r_SA, r_c], writes=[r_Sbf])
            if whole or r == 1:
                b.op("dve", lambda e, so=so: e.tensor_tensor(
                    out=qd[so][:], in0=qk[so][:, 0:2, :], in1=qdecT[:], op=ALU.mult),
                    reads=[r_qk[so], r_c], writes=[r_qd[so]])
                for h in range(4):
                    hs = slice((h % 2) * 64, (h % 2) * 64 + 64)
                    p = h // 2
                    si = sc_i % 2
                    sc_i += 1
                    b.op("pe", lambda e, so=so, hs=hs, p=p, si=si: e.matmul(
                        pSC[si][:, :], lhsT=qk[so][hs, 2 + p, :], rhs=qk[so][hs, p, :], start=True, stop=True),
                        reads=[r_qk[so]], writes=[r_pSC[si]])
                    b.op("dve", lambda e, si=si, h=h: e.tensor_tensor(
                        out=scT[si][:], in0=pSC[si][:], in1=decT[:, h, :], op=ALU.mult),
                        reads=[r_pSC[si], r_c], writes=[r_scT[si]])
                    b.op("pe", lambda e, si=si, h=h, so=so: e.matmul(
                        pY[:, h * 64:(h + 1) * 64], lhsT=scT[si][:], rhs=own[so][:, h * 64:(h + 1) * 64],
                        start=True, stop=False),
                        reads=[r_scT[si], r_own[so]], writes=[r_pY], inc=False)
                    b.op("pe", lambda e, h=h, so=so, hs=hs, p=p: e.matmul(
                        pY[:, h * 64:(h + 1) * 64], lhsT=qd[so][hs, p, :], rhs=Sbf[hs, p, :],
                        start=False, stop=True),
                        reads=[r_qd[so], r_Sbf], writes=[r_pY])
                    for _k in range(3):
                        if pending_fin is not None:
                            try:
                                next(pending_fin)
                            except StopIteration:
                                pending_fin = None
                fb = fin_i % 2
                fin_i += 1
                ysb_, tmp_, st_ = ysbs[fb], tmps[fb], sts[fb]
                r_ysb_, r_tmp_, r_st_ = r_ysbs[fb], r_tmps[fb], r_sts[fb]
                b.op("act", lambda e, ysb_=ysb_: e.copy(out=ysb_[:].rearrange("p h d -> p (h d)"), in_=pY[:]),
                     reads=[r_pY], writes=[r_ysb_])

                def fin_gen(ysb_=ysb_, tmp_=tmp_, st_=st_, r_ysb_=r_ysb_, r_tmp_=r_tmp_, r_st_=r_st_, so=so,
                            orow=orow):
                    for _ in gen_head_norm(b, ysb_, r_ysb_, own[so][:, 256:512], r_own[so], osb[so][:], r_osb[so],
                                           tmp_, st_, r_tmp_, r_st_):
                        yield
                    b.dma("pool", o_d[orow:orow + 128, 512:768], osb[so][:], reads=[r_osb[so]], writes=[r_out])

                while pending_fin is not None:
                    try:
                        next(pending_fin)
                    except StopIteration:
                        pending_fin = None
                pending_fin = fin_gen()
            if g == 2 * nslots - 1:
                break
            b.op("dve", lambda e, s=s: e.tensor_tensor(
                out=vdec[s][:].rearrange("p (h d) -> p h d", d=64),
                in0=kvin[s][:, 256:512].rearrange("p (h d) -> p h d", d=64),
                in1=kdec[:].unsqueeze(2).to_broadcast([128, 4, 64]), op=ALU.mult),
                reads=[r_kvin[s], r_c], writes=[r_vdec[s]])
            for p in range(2):
                b.op("pe", lambda e, s=s, p=p: e.matmul(
                    pKV[p][:, :], lhsT=kvin[s][:, p * 128:(p + 1) * 128], rhs=vdec[s][:, p * 128:(p + 1) * 128],
                    start=True, stop=True), reads=[r_kvin[s], r_vdec[s]], writes=[r_pKV[p]])
                for half in range(2):
                    h = 2 * p + half
                    hs = slice(half * 64, half * 64 + 64)
                    b.op("dve", lambda e, p=p, hs=hs, half=half, h=h: e.scalar_tensor_tensor(
                        out=S[hs, p, :], in0=S[hs, p, :], scalar=float(GAMMAS[h] ** 128),
                        in1=pKV[p][hs, half * 64:(half + 1) * 64], op0=ALU.mult, op1=ALU.add),
                        reads=[r_S, r_pKV[p]], writes=[r_S])
        while pending_fin is not None:
            try:
                next(pending_fin)
            except StopIteration:
                pending_fin = None
        b.barrier()
        b.es = old


def emit_p2c(b, l, nslots, featT_all, tokA_all, gT_all, tokA_own, convw_d, convb_d, caus_d, sel4_d, eye4_d,
             jf_d, identf_d, o_d, r_in, r_out, whole=False):
    NG = 2 * nslots
    NTK = NG * 128
    NOWN = NG if whole else nslots
    with ExitStack() as es:
        old = b.es
        b.es = es
        jf = b.sb("c_jf", [128, 1], F32)
        caus = b.sb("c_caus", [128, 128], F32)
        sel4 = b.sb("c_sel4", [4, 4, 128], F32)
        eye4 = b.sb("c_eye4", [4, 4], F32)
        ones4 = b.sb("c_ones4", [4, 128], F32)
        identf = b.sb("c_idf", [128, 128], F32)
        identb = b.sb("c_idb", [128, 128], BF16)
        qkall = b.sb("c_qkall", [128, 4, NTK], BF16)
        qkown = qkall if whole else b.sb("c_qkown", [128, 4, nslots * 128], BF16)
        GTo = b.sb("c_GTo", [4, NOWN, 128], F32)
        acol = b.sb("c_acol", [128, NG, 4], F32)
        ecol = b.sb("c_ecol", [128, NG, 4], F32)
        acolo = b.sb("c_acolo", [128, NOWN, 4], F32)
        ecolo = b.sb("c_ecolo", [128, NOWN, 4], F32)
        GR = b.sb("c_GR", [128, NG + 1, 4], F32)
        GRo = b.sb("c_GRo", [128, NOWN, 4], F32)
        dec = b.sb("c_dec", [128, NG, 4], F32)
        kw = b.sb("c_kw", [128, NG, 4], F32)
        r_c, r_qkall, r_qkown, r_GTo, r_cols, r_GR = (b.res() for _ in range(6))
        if whole:
            r_qkown = r_qkall

        b.dma("sp", jf[:], jf_d, writes=[r_c])
        b.dma("sp", caus[:], caus_d, writes=[r_c])
        b.dma("sp", sel4[:].rearrange("k h n -> k (h n)"), sel4_d, writes=[r_c])
        b.dma("sp", eye4[:], eye4_d, writes=[r_c])
        b.dma("sp", identf[:], identf_d, writes=[r_c])
        b.op("dve", lambda e: e.tensor_copy(out=identb[:], in_=identf[:]), reads=[r_c], writes=[r_c])
        b.op("dve", lambda e: e.memset(ones4[:], 1.0), writes=[r_c])

        with ExitStack() as es1:
            b.es = es1
            gi_ = b.sb("c1_gi", [4, NTK], F32)
            gf_ = b.sb("c1_gf", [4, NTK], F32)
            t1 = b.sb("c1_t1", [4, NTK], F32)
            Bn = b.sb("c1_Bn", [4, NTK], F32)
            aT = b.sb("c1_aT", [4, NTK], F32)
            GT = b.sb("c1_GT", [4, NTK], F32)
            eT = b.sb("c1_eT", [4, NTK], F32)
            onesr = b.sb("c1_ones", [4, NTK], F32)
            gend = b.sb("c1_gend", [4, NG, 4], F32)
            pT = b.ps("c1_pT", [128, 2 * NG * 4])
            pR = b.ps("c1_pR", [128, NG * 4])
            r_g, r_t1, r_Bn, r_aT, r_GT, r_eT, r_on, r_ge, r_pT, r_pR = (b.res() for _ in range(10))
            for g in range(NG):
                gi = gidx(g)
                b.dma("sp", gi_[:, g * 128:(g + 1) * 128], gT_all[gi, 0:4, :], reads=[r_in], writes=[r_g])
                b.dma("sp", gf_[:, g * 128:(g + 1) * 128], gT_all[gi, 4:8, :], reads=[r_in], writes=[r_g])
            b.op("dve", lambda e: e.memset(onesr[:], 1.0), writes=[r_on])
            b.op("act", lambda e: e.activation(out=t1[:], in_=gf_[:], func=AF.Exp, scale=-1.0),
                 reads=[r_g], writes=[r_t1])
            b.op("act", lambda e: e.activation(out=t1[:], in_=t1[:], func=AF.Ln, bias=1.0),
                 reads=[r_t1], writes=[r_t1])
            b.op("dve", lambda e: e.tensor_tensor_scan(out=Bn[:], data0=onesr[:], data1=t1[:], initial=0.0,
                                                       op0=ALU.mult, op1=ALU.add),
                 reads=[r_on, r_t1], writes=[r_Bn])
            b.op("dve", lambda e: e.tensor_tensor(out=aT[:], in0=gi_[:], in1=Bn[:], op=ALU.add),
                 reads=[r_g, r_Bn], writes=[r_aT])
            b.op("dve", lambda e: e.tensor_tensor_scan(out=GT[:], data0=onesr[:], data1=aT[:], initial=0.0,
                                                       op0=ALU.mult, op1=ALU.max),
                 reads=[r_on, r_aT], writes=[r_GT])
            b.op("dve", lambda e: e.tensor_tensor(out=eT[:], in0=Bn[:], in1=GT[:], op=ALU.subtract),
                 reads=[r_Bn, r_GT], writes=[r_eT])
            b.op("act", lambda e: e.activation(out=eT[:], in_=eT[:], func=AF.Exp), reads=[r_eT], writes=[r_eT])
            if whole:
                b.op("dve", lambda e: e.tensor_copy(out=GTo[:].rearrange("k g n -> k (g n)"), in_=GT[:]),
                     reads=[r_GT], writes=[r_GTo])
            else:
                GTv = GT[:].rearrange("k (m r n) -> k m r n", r=2, n=128)
                b.op("dve", lambda e: e.tensor_tensor(out=t1[:, 0:nslots * 128].rearrange("k (m n) -> k m n", n=128),
                                                      in0=GTv[:, :, 1, :], in1=GTv[:, :, 0, :], op=ALU.subtract),
                     reads=[r_GT, r_t1], writes=[r_t1])
                b.op("dve", lambda e: e.scalar_tensor_tensor(
                    out=GTo[:], in0=t1[:, 0:nslots * 128].rearrange("k (m n) -> k m n", n=128), scalar=jf[0:4, 0:1],
                    in1=GTv[:, :, 0, :], op0=ALU.mult, op1=ALU.add),
                    reads=[r_t1, r_GT, r_c], writes=[r_GTo])
            for g in range(NG):
                b.op("pe", lambda e, g=g: e.transpose(out=pT[:, g * 4:(g + 1) * 4], in_=aT[:, g * 128:(g + 1) * 128],
                                                      identity=identf[0:4, 0:4]),
                     reads=[r_aT, r_c], writes=[r_pT], inc=False)
                b.op("pe", lambda e, g=g: e.transpose(out=pT[:, (NG + g) * 4:(NG + g + 1) * 4],
                                                      in_=eT[:, g * 128:(g + 1) * 128], identity=identf[0:4, 0:4]),
                     reads=[r_eT, r_c], writes=[r_pT], inc=(g == NG - 1))
            b.op("act", lambda e: e.copy(out=acol[:].rearrange("p g h -> p (g h)"), in_=pT[:, 0:NG * 4]),
                 reads=[r_pT], writes=[r_cols])
            b.op("act", lambda e: e.copy(out=ecol[:].rearrange("p g h -> p (g h)"), in_=pT[:, NG * 4:2 * NG * 4]),
                 reads=[r_pT], writes=[r_cols])
            b.op("dve", lambda e: e.tensor_tensor(
                out=gend[:], in0=GT[:].rearrange("k (g n) -> k g n", n=128)[:, :, 127:128].to_broadcast([4, NG, 4]),
                in1=eye4[:].unsqueeze(1).to_broadcast([4, NG, 4]), op=ALU.mult),
                reads=[r_GT, r_c], writes=[r_ge])
            b.op("pe", lambda e: e.matmul(pR[:, :], lhsT=ones4[:, :], rhs=gend[:].rearrange("k g h -> k (g h)"),
                                          start=True, stop=True), reads=[r_ge, r_c], writes=[r_pR])
            b.op("dve", lambda e: e.memset(GR[:, 0, :], 0.0), writes=[r_GR])
            b.op("act", lambda e: e.copy(out=GR[:, 1:NG + 1, :].rearrange("p g h -> p (g h)"), in_=pR[:]),
                 reads=[r_pR], writes=[r_GR])
            b.op("dve", lambda e: e.tensor_tensor(out=dec[:], in0=GR[:, 0:NG, :], in1=GR[:, 1:NG + 1, :],
                                                  op=ALU.subtract), reads=[r_GR], writes=[r_cols])
            b.op("act", lambda e: e.activation(out=dec[:], in_=dec[:], func=AF.Exp), reads=[r_cols], writes=[r_cols])
            b.op("dve", lambda e: e.tensor_tensor(out=kw[:], in0=acol[:], in1=GR[:, 1:NG + 1, :], op=ALU.subtract),
                 reads=[r_cols, r_GR], writes=[r_cols])
            b.op("act", lambda e: e.activation(out=kw[:], in_=kw[:], func=AF.Exp), reads=[r_cols], writes=[r_cols])
            b.op("dve", lambda e: e.tensor_scalar(out=kw[:], in0=kw[:], scalar1=0.125, scalar2=None, op0=ALU.mult),
                 reads=[r_cols], writes=[r_cols])

            def blend(dst, src, ncol):
                sv = src.rearrange("p (m r) h -> p m r h", r=2)
                b.op("dve", lambda e: e.tensor_tensor(out=dst, in0=sv[:, :, 1, :], in1=sv[:, :, 0, :],
                                                      op=ALU.subtract), reads=[r_cols, r_GR], writes=[r_cols])
                b.op("dve", lambda e: e.scalar_tensor_tensor(
                    out=dst, in0=dst, scalar=jf[:, 0:1], in1=sv[:, :, 0, :], op0=ALU.mult, op1=ALU.add),
                    reads=[r_cols, r_GR, r_c], writes=[r_cols])
            if whole:
                for dst_, src_ in ((acolo, acol[:]), (ecolo, ecol[:]), (GRo, GR[:, 0:NG, :])):
                    b.op("dve", lambda e, dst_=dst_, src_=src_: e.tensor_copy(out=dst_[:], in_=src_),
                         reads=[r_cols, r_GR], writes=[r_cols])
            else:
                blend(acolo[:], acol[:], 4)
                blend(ecolo[:], ecol[:], 4)
                blend(GRo[:], GR[:, 0:NG, :], 4)
            b.barrier()
            b.es = es

        with ExitStack() as es2:
            b.es = es2
            pre = b.sb("c2_pre", [128, 4, NTK + 3], BF16)
            acc = [b.sb("c2_acc%d" % i, [128, NTK], F32) for i in range(2)]
            cw = b.sb("c2_cw", [128, 4, 4], F32)
            cb = b.sb("c2_cb", [128, 4], F32)
            r_pre, r_cw = b.res(), b.res()
            r_acc = [b.res(), b.res()]
            b.dma("sp", cw[:], convw_d[l], writes=[r_cw])
            b.dma("sp", cb[:], convb_d[l], writes=[r_cw])
            b.op("pool", lambda e: e.memset(pre[:, :, 0:3], 0.0), writes=[r_pre])
            for g in range(NG):
                gi = gidx(g)
                b.dma("sp", pre[:, :, 3 + g * 128:3 + (g + 1) * 128], featT_all[gi, :, 15:19, :],
                      reads=[r_in], writes=[r_pre])
            for c in range(4):
                a = acc[c % 2]
                ra = r_acc[c % 2]
                b.op("dve", lambda e, c=c, a=a: e.tensor_scalar(
                    out=a[:], in0=pre[:, c, 0:NTK], scalar1=cw[:, c, 0:1], scalar2=cb[:, c:c + 1],
                    op0=ALU.mult, op1=ALU.add), reads=[r_pre, r_cw], writes=[ra])
                for j in range(1, 4):
                    b.op("dve", lambda e, c=c, a=a, j=j: e.scalar_tensor_tensor(
                        out=a[:], in0=pre[:, c, j:j + NTK], scalar=cw[:, c, j:j + 1], in1=a[:],
                        op0=ALU.mult, op1=ALU.add), reads=[r_pre, r_cw, ra], writes=[ra])
                b.op("act", lambda e, c=c, a=a: e.activation(out=qkall[:, c, :], in_=a[:], func=AF.Silu),
                     reads=[ra], writes=[r_qkall])
            if not whole:
                for c in range(4):
                    a = acc[c % 2]
                    ra = r_acc[c % 2]
                    v = qkall[:, c, :].rearrange("p (m r n) -> p m r n", r=2, n=128)
                    av = a[:, 0:nslots * 128].rearrange("p (m n) -> p m n", n=128)
                    b.op("dve", lambda e, v=v, av=av: e.tensor_tensor(out=av, in0=v[:, :, 1, :], in1=v[:, :, 0, :],
                                                                      op=ALU.subtract),
                         reads=[r_qkall, ra], writes=[ra])
                    b.op("dve", lambda e, v=v, av=av, c=c: e.scalar_tensor_tensor(
                        out=qkown[:, c, :].rearrange("p (m n) -> p m n", n=128), in0=av, scalar=jf[:, 0:1],
                        in1=v[:, :, 0, :], op0=ALU.mult, op1=ALU.add),
                        reads=[ra, r_qkall, r_c], writes=[r_qkown])
            b.barrier()
            b.es = es

        Cst = b.sb("c_Cst", [128, 2, 65], F32)
        CA = b.sb("c_CA", [128, 2, 65], F32)
        Cd = b.sb("c_Cd", [128, 2, 65], F32)
        Cbf = b.sb("c_Cbf", [128, 2, 65], BF16)
        vin = [b.sb("c_vin%d" % i, [128, 256], BF16) for i in range(2)]
        vk = [b.sb("c_vk%d" % i, [128, 4, 65], BF16) for i in range(2)]
        ktok = [b.sb("c_ktok%d" % i, [128, 256], BF16) for i in range(2)]
        ownv = [b.sb("c_ownv%d" % i, [128, 512], BF16) for i in range(2)]
        vaug = [b.sb("c_vaug%d" % i, [128, 4, 65], BF16) for i in range(2)]
        ngc = [b.sb("c_ngc%d" % i, [128, 128], F32) for i in range(2)]
        WT = [b.sb("c_WT%d" % i, [128, 128], F32) for i in range(2)]
        DT = [b.sb("c_DT%d" % i, [128, 128], BF16) for i in range(2)]
        igq = [b.sb("c_igq%d" % i, [128, 128], F32) for i in range(2)]
        qs = [b.sb("c_qs%d" % i, [128, 128], BF16) for i in range(2)]
        nds = [b.sb("c_nd%d" % i, [128, 4, 65], F32) for i in range(2)]
        hsbs = [b.sb("c_hsb%d" % i, [128, 4, 64], F32) for i in range(2)]
        tmps = [b.sb("c_tmp%d" % i, [128, 4, 64], F32) for i in range(2)]
        sts = [b.sb("c_st%d" % i, [128, 16], F32) for i in range(2)]
        dns = [b.sb("c_dn%d" % i, [128, 8], F32) for i in range(2)]
        r_nds, r_hsbs, r_tmps, r_sts, r_dns = ([b.res(), b.res()] for _ in range(5))
        pending_fin = None
        fin_i = 0
        osb = [b.sb("c_o%d" % i, [128, 256], BF16) for i in range(2)]
        pKt = b.ps("c_pKt", [128, 256], BF16)
        pKV = [b.ps("c_pKV%d" % i, [128, 130]) for i in range(2)]
        pG = [b.ps("c_pG%d" % i, [128, 128]) for i in range(2)]
        pQK = [b.ps("c_pQK%d" % i, [128, 128]) for i in range(2)]
        pN = b.ps("c_pN", [128, 260])
        (r_Cst, r_CA, r_Cd, r_Cbf, r_nd, r_hsb, r_tmp, r_st, r_dn, r_pKt, r_pN) = (b.res() for _ in range(11))
        r_vin = [b.res(), b.res()]
        r_vk = [b.res(), b.res()]
        r_ktok = [b.res(), b.res()]
        r_ownv = [b.res(), b.res()]
        r_vaug = [b.res(), b.res()]
        r_ngc = [b.res(), b.res()]
        r_WT = [b.res(), b.res()]
        r_DT = [b.res(), b.res()]
        r_igq = [b.res(), b.res()]
        r_qs = [b.res(), b.res()]
        r_osb = [b.res(), b.res()]
        r_pKV = [b.res(), b.res()]
        r_pG = [b.res(), b.res()]
        r_pQK = [b.res(), b.res()]

        b.op("dve", lambda e: e.memset(Cst[:], 0.0), writes=[r_Cst])
        hi = 0
        for g in range(NG):
            m, r = g // 2, g % 2
            s = g % 2
            gi = gidx(g)
            b.dma("sp", vin[s][:], tokA_all[gi * 128:(gi + 1) * 128, 1280:1536], reads=[r_in], writes=[r_vin[s]])
            if whole:
                so = g % 2
                mm = g
                orow = gi * 128
                b.dma("sp", ownv[so][:], tokA_all[gi * 128:(gi + 1) * 128, 1280:1792], reads=[r_in],
                      writes=[r_ownv[so]])
                b.op("pool", lambda e, so=so: e.memset(vaug[so][:, :, 64:65], 1.0), writes=[r_vaug[so]])
                b.op("pool", lambda e, so=so: e.tensor_copy(
                    out=vaug[so][:, :, 0:64], in_=ownv[so][:, 0:256].rearrange("p (h d) -> p h d", d=64)),
                    reads=[r_ownv[so]], writes=[r_vaug[so]])
                b.op("dve", lambda e: e.tensor_copy(out=Cbf[:], in_=Cst[:]), reads=[r_Cst], writes=[r_Cbf])
            elif r == 0:
                so = m % 2
                b.op("dve", lambda e: e.tensor_copy(out=CA[:], in_=Cst[:]), reads=[r_Cst], writes=[r_CA])
                b.dma("sp", ownv[so][:], tokA_own[m * 128:(m + 1) * 128, 1280:1792], reads=[r_in],
                      writes=[r_ownv[so]])
                b.op("pool", lambda e, so=so: e.memset(vaug[so][:, :, 64:65], 1.0), writes=[r_vaug[so]])
                b.op("pool", lambda e, so=so: e.tensor_copy(
                    out=vaug[so][:, :, 0:64], in_=ownv[so][:, 0:256].rearrange("p (h d) -> p h d", d=64)),
                    reads=[r_ownv[so]], writes=[r_vaug[so]])
            else:
                so = m % 2
                mm = m
                orow = m * 128
                b.op("dve", lambda e: e.tensor_tensor(out=Cd[:], in0=Cst[:], in1=CA[:], op=ALU.subtract),
                     reads=[r_Cst, r_CA], writes=[r_Cd])
                b.op("dve", lambda e: e.scalar_tensor_tensor(
                    out=Cbf[:].rearrange("p a e -> p (a e)"), in0=Cd[:].rearrange("p a e -> p (a e)"),
                    scalar=jf[:, 0:1], in1=CA[:].rearrange("p a e -> p (a e)"), op0=ALU.mult, op1=ALU.add),
                    reads=[r_Cd, r_CA, r_c], writes=[r_Cbf])
            if whole or r == 1:
                m = mm
                for h in range(4):
                    hs = slice((h % 2) * 64, (h % 2) * 64 + 64)
                    p = h // 2
                    x = hi % 2
                    hi += 1
                    b.op("pe", lambda e, x=x, h=h, m=m: e.matmul(
                        pG[x][:, :], lhsT=sel4[:, h, :], rhs=GTo[:, m, :], start=True, stop=True),
                        reads=[r_c, r_GTo], writes=[r_pG[x]])
                    b.op("dve", lambda e, x=x: e.tensor_tensor(out=ngc[x][:], in0=caus[:], in1=pG[x][:],
                                                               op=ALU.subtract),
                         reads=[r_c, r_pG[x]], writes=[r_ngc[x]])
                    b.op("act", lambda e, x=x, m=m, h=h: e.activation(
                        out=WT[x][:], in_=ngc[x][:], func=AF.Exp, bias=acolo[:, m, h:h + 1]),
                        reads=[r_ngc[x], r_cols], writes=[r_WT[x]])
                    b.op("pe", lambda e, x=x, hs=hs, p=p, m=m: e.matmul(
                        pQK[x][:, :], lhsT=qkown[hs, 2 + p, m * 128:(m + 1) * 128],
                        rhs=qkown[hs, p, m * 128:(m + 1) * 128], start=True, stop=True),
                        reads=[r_qkown], writes=[r_pQK[x]])
                    b.op("dve", lambda e, x=x: e.scalar_tensor_tensor(
                        out=DT[x][:], in0=pQK[x][:], scalar=0.125, in1=WT[x][:], op0=ALU.mult, op1=ALU.mult),
                        reads=[r_pQK[x], r_WT[x]], writes=[r_DT[x]])
                    b.op("act", lambda e, x=x, hs=hs, m=m, h=h: e.activation(
                        out=igq[x][hs, :], in_=pG[x][hs, :], func=AF.Exp, scale=-1.0, bias=GRo[hs, m, h:h + 1]),
                        reads=[r_pG[x], r_cols], writes=[r_igq[x]])
                    b.op("dve", lambda e, x=x, hs=hs, p=p, m=m: e.tensor_tensor(
                        out=qs[x][hs, :], in0=qkown[hs, p, m * 128:(m + 1) * 128], in1=igq[x][hs, :], op=ALU.mult),
                        reads=[r_qkown, r_igq[x]], writes=[r_qs[x]])
                    b.op("pe", lambda e, x=x, h=h, so=so: e.matmul(
                        pN[:, h * 65:(h + 1) * 65], lhsT=DT[x][:], rhs=vaug[so][:, h, :], start=True, stop=False),
                        reads=[r_DT[x], r_vaug[so]], writes=[r_pN], inc=False)
                    b.op("pe", lambda e, x=x, h=h, hs=hs, p=p: e.matmul(
                        pN[:, h * 65:(h + 1) * 65], lhsT=qs[x][hs, :], rhs=Cbf[hs, p, :], start=False, stop=True),
                        reads=[r_qs[x], r_Cbf], writes=[r_pN])
                    for _k in range(5):
                        if pending_fin is not None:
                            try:
                                next(pending_fin)
                            except StopIteration:
                                pending_fin = None
                fb = fin_i % 2
                fin_i += 1
                nd_, hsb_, tmp_, st_, dn_ = nds[fb], hsbs[fb], tmps[fb], sts[fb], dns[fb]
                r_nd_, r_hsb_, r_tmp_, r_st_, r_dn_ = r_nds[fb], r_hsbs[fb], r_tmps[fb], r_sts[fb], r_dns[fb]
                b.op("act", lambda e, nd_=nd_: e.copy(out=nd_[:].rearrange("p h d -> p (h d)"), in_=pN[:]),
                     reads=[r_pN], writes=[r_nd_])

                def fin_gen(nd_=nd_, hsb_=hsb_, tmp_=tmp_, st_=st_, dn_=dn_, r_nd_=r_nd_, r_hsb_=r_hsb_,
                            r_tmp_=r_tmp_, r_st_=r_st_, r_dn_=r_dn_, so=so, m=m, orow=orow):
                    b.op("dve", lambda e: e.tensor_scalar(
                        out=dn_[:, 0:4].unsqueeze(2), in0=nd_[:, :, 64:65], scalar1=-1.0, scalar2=None, op0=ALU.mult),
                        reads=[r_nd_], writes=[r_dn_])
                    yield
                    b.op("dve", lambda e: e.tensor_tensor(
                        out=dn_[:, 0:4].unsqueeze(2), in0=dn_[:, 0:4].unsqueeze(2), in1=nd_[:, :, 64:65], op=ALU.max),
                        reads=[r_nd_, r_dn_], writes=[r_dn_])
                    yield
                    b.op("dve", lambda e: e.tensor_tensor(out=dn_[:, 0:4], in0=dn_[:, 0:4], in1=ecolo[:, m, :],
                                                          op=ALU.max),
                         reads=[r_dn_, r_cols], writes=[r_dn_])
                    yield
                    b.op("dve", lambda e: e.reciprocal(out=dn_[:, 4:8], in_=dn_[:, 0:4]), reads=[r_dn_], writes=[r_dn_])
                    yield
                    b.op("dve", lambda e: e.tensor_tensor(
                        out=tmp_[:], in0=nd_[:, :, 0:64], in1=dn_[:, 4:8].unsqueeze(2).to_broadcast([128, 4, 64]),
                        op=ALU.mult), reads=[r_nd_, r_dn_], writes=[r_tmp_])
                    yield
                    b.op("dve", lambda e: e.tensor_tensor(
                        out=hsb_[:], in0=tmp_[:], in1=ownv[so][:, 256:512].rearrange("p (h d) -> p h d", d=64),
                        op=ALU.mult), reads=[r_tmp_, r_ownv[so]], writes=[r_hsb_])
                    yield
                    for _ in gen_head_norm(b, hsb_, r_hsb_, None, None, osb[so][:], r_osb[so], tmp_, st_,
                                           r_tmp_, r_st_):
                        yield
                    b.dma("pool", o_d[orow:orow + 128, 768:1024], osb[so][:], reads=[r_osb[so]], writes=[r_out])

                while pending_fin is not None:
                    try:
                        next(pending_fin)
                    except StopIteration:
                        pending_fin = None
                pending_fin = fin_gen()
            if g == NG - 1:
                break
            b.op("dve", lambda e, s=s, g=g: e.tensor_tensor(
                out=vk[s][:, :, 0:64], in0=vin[s][:].rearrange("p (h d) -> p h d", d=64),
                in1=kw[:, g, :].unsqueeze(2).to_broadcast([128, 4, 64]), op=ALU.mult),
                reads=[r_vin[s], r_cols], writes=[r_vk[s]])
            b.op("dve", lambda e, s=s, g=g: e.tensor_copy(out=vk[s][:, :, 64:65], in_=kw[:, g, :].unsqueeze(2)),
                 reads=[r_cols], writes=[r_vk[s]])
            for p in range(2):
                b.op("pe", lambda e, p=p, g=g: e.transpose(
                    out=pKt[:, p * 128:(p + 1) * 128], in_=qkall[:, 2 + p, g * 128:(g + 1) * 128],
                    identity=identb[:]), reads=[r_qkall, r_c], writes=[r_pKt], inc=(p == 1))
            b.op("act", lambda e, s=s: e.copy(out=ktok[s][:], in_=pKt[:]), reads=[r_pKt], writes=[r_ktok[s]])
            for p in range(2):
                b.op("pe", lambda e, s=s, p=p: e.matmul(
                    pKV[p][:, :], lhsT=ktok[s][:, p * 128:(p + 1) * 128],
                    rhs=vk[s][:, 2 * p:2 * p + 2, :].rearrange("p a e -> p (a e)"), start=True, stop=True),
                    reads=[r_ktok[s], r_vk[s]], writes=[r_pKV[p]])
                for half in range(2):
                    h = 2 * p + half
                    hs = slice(half * 64, half * 64 + 64)
                    b.op("dve", lambda e, p=p, hs=hs, half=half, h=h, g=g: e.scalar_tensor_tensor(
                        out=Cst[hs, p, :], in0=Cst[hs, p, :], scalar=dec[hs, g, h:h + 1],
                        in1=pKV[p][hs, half * 65:(half + 1) * 65], op0=ALU.mult, op1=ALU.add),
                        reads=[r_Cst, r_pKV[p], r_cols], writes=[r_Cst])
        while pending_fin is not None:
            try:
                next(pending_fin)
            except StopIteration:
                pending_fin = None
        b.barrier()
        b.es = old


def mlstm_consts():
    i = np.arange(128)
    caus = np.where(i[:, None] <= i[None, :], 0.0, NEG).astype(np.float32)
    sel4 = np.zeros((4, 4, 128), np.float32)
    for h in range(4):
        sel4[h, h, :] = 1.0
    return caus, sel4.reshape(4, 512), np.eye(4, dtype=np.float32)


def build_p2bc(l, nslots):
    nc = bass.Bass("TRN2", target_bir_lowering=False)
    featT_all = nc.dram_tensor("featT_all", [2 * NT, 128, NFT, 128], BF16, kind="ExternalInput").ap()
    tokA_all = nc.dram_tensor("tokA_all", [2 * TOK, 1792], BF16, kind="ExternalInput").ap()
    gT_all = nc.dram_tensor("gT_all", [2 * NT, 8, 128], F32, kind="ExternalInput").ap()
    featT_own = nc.dram_tensor("featT_own", [NT, 128, NFT, 128], BF16, kind="ExternalInput").ap()
    tokA_own = nc.dram_tensor("tokA_own", [TOK, 1792], BF16, kind="ExternalInput").ap()
    decT_d = nc.dram_tensor("decT", [128, 4, 128], F32, kind="ExternalInput").ap()
    qdecT_d = nc.dram_tensor("qdecT", [128, 2, 128], F32, kind="ExternalInput").ap()
    kdec_d = nc.dram_tensor("kdec", [128, 4], F32, kind="ExternalInput").ap()
    jf_d = nc.dram_tensor("jf", [128, 1], F32, kind="ExternalInput").ap()
    convw_d = nc.dram_tensor("convw", [DEPTH, 128, 4, 4], F32, kind="ExternalInput").ap()
    convb_d = nc.dram_tensor("convb", [DEPTH, 128, 4], F32, kind="ExternalInput").ap()
    caus_d = nc.dram_tensor("caus", [128, 128], F32, kind="ExternalInput").ap()
    sel4_d = nc.dram_tensor("sel4", [4, 512], F32, kind="ExternalInput").ap()
    eye4_d = nc.dram_tensor("eye4", [4, 4], F32, kind="ExternalInput").ap()
    identf_d = nc.dram_tensor("identf", [128, 128], F32, kind="ExternalInput").ap()
    o_d = nc.dram_tensor("o", [TOK, D], BF16, kind="ExternalOutput").ap()
    with ExitStack() as es, nc.allow_low_precision("bf16 matmul operands, fp32 accumulation"):
        b = B(nc, es)
        r_in, r_out = b.res(), b.res()
        emit_p2b(b, nslots, tokA_all, featT_own, tokA_own, decT_d, qdecT_d, kdec_d, jf_d, o_d, r_in, r_out)
        emit_p2c(b, l, nslots, featT_all, tokA_all, gT_all, tokA_own, convw_d, convb_d, caus_d, sel4_d, eye4_d,
                 jf_d, identf_d, o_d, r_in, r_out)
        b.finish()
    return nc


def build_p2bc_whole(l, nslots):
    nc = bass.Bass("TRN2", target_bir_lowering=False)
    featT_all = nc.dram_tensor("featT_all", [2 * NT, 128, NFT, 128], BF16, kind="ExternalInput").ap()
    tokA_all = nc.dram_tensor("tokA_all", [2 * TOK, 1792], BF16, kind="ExternalInput").ap()
    gT_all = nc.dram_tensor("gT_all", [2 * NT, 8, 128], F32, kind="ExternalInput").ap()
    decT_d = nc.dram_tensor("decT", [128, 4, 128], F32, kind="ExternalInput").ap()
    qdecT_d = nc.dram_tensor("qdecT", [128, 2, 128], F32, kind="ExternalInput").ap()
    kdec_d = nc.dram_tensor("kdec", [128, 4], F32, kind="ExternalInput").ap()
    jf_d = nc.dram_tensor("jf", [128, 1], F32, kind="ExternalInput").ap()
    convw_d = nc.dram_tensor("convw", [DEPTH, 128, 4, 4], F32, kind="ExternalInput").ap()
    convb_d = nc.dram_tensor("convb", [DEPTH, 128, 4], F32, kind="ExternalInput").ap()
    caus_d = nc.dram_tensor("caus", [128, 128], F32, kind="ExternalInput").ap()
    sel4_d = nc.dram_tensor("sel4", [4, 512], F32, kind="ExternalInput").ap()
    eye4_d = nc.dram_tensor("eye4", [4, 4], F32, kind="ExternalInput").ap()
    identf_d = nc.dram_tensor("identf", [128, 128], F32, kind="ExternalInput").ap()
    o_d = nc.dram_tensor("o", [2 * TOK, D], BF16, kind="ExternalOutput").ap()
    with ExitStack() as es, nc.allow_low_precision("bf16 matmul operands, fp32 accumulation"):
        b = B(nc, es)
        r_in, r_out = b.res(), b.res()
        emit_p2b(b, nslots, tokA_all, featT_all, None, decT_d, qdecT_d, kdec_d, jf_d, o_d, r_in, r_out, whole=True)
        emit_p2c(b, l, nslots, featT_all, tokA_all, gT_all, None, convw_d, convb_d, caus_d, sel4_d, eye4_d,
                 jf_d, identf_d, o_d, r_in, r_out, whole=True)
        b.finish()
    return nc


def conv_layouts(conv_w, conv_b):
    cw = np.ascontiguousarray(conv_w.reshape(DEPTH, 4, 4, 128).transpose(0, 3, 2, 1))
    cb = np.ascontiguousarray(conv_b.reshape(DEPTH, 4, 128).transpose(0, 2, 1))
    return cw, cb


def emit_ln_stats(b, z, r_z, junk, r_junk, st, r_st):
    b.op("act", lambda e: e.activation(out=junk[:], in_=z, func=AF.Identity, accum_out=st[:, 2:3]),
         reads=[r_z], writes=[r_junk, r_st])
    b.op("act", lambda e: e.activation(out=junk[:], in_=z, func=AF.Square, accum_out=st[:, 3:4]),
         reads=[r_z], writes=[r_junk, r_st])
    b.op("dve", lambda e: e.tensor_scalar(out=st[:, 0:1], in0=st[:, 2:3], scalar1=1.0 / D, scalar2=None,
                                          op0=ALU.mult), reads=[r_st], writes=[r_st])
    b.op("dve", lambda e: e.tensor_tensor(out=st[:, 4:5], in0=st[:, 0:1], in1=st[:, 0:1], op=ALU.mult),
         reads=[r_st], writes=[r_st])
    b.op("dve", lambda e: e.scalar_tensor_tensor(out=st[:, 5:6], in0=st[:, 3:4], scalar=1.0 / D, in1=st[:, 4:5],
                                                 op0=ALU.mult, op1=ALU.subtract), reads=[r_st], writes=[r_st])
    b.op("dve", lambda e: e.tensor_scalar(out=st[:, 5:6], in0=st[:, 5:6], scalar1=LN_EPS, scalar2=None,
                                          op0=ALU.add), reads=[r_st], writes=[r_st])
    b.op("act", lambda e: e.activation(out=st[:, 6:7], in_=st[:, 5:6], func=AF.Sqrt), reads=[r_st], writes=[r_st])
    b.op("dve", lambda e: e.reciprocal(out=st[:, 1:2], in_=st[:, 6:7]), reads=[r_st], writes=[r_st])


def emit_p3(b, l, nt, x_d, o_d, wout_d, modrow_d, modcol_d, lnmg_d, lnmb_d, wr_d, br_d, wg_d, wu_d, wd_d,
            lnfg_d, lnfb_d, identf_d, xout_d, r_in, r_out, n_exp=16):
    ntok = nt * 128
    ngrp = nt // 4
    with ExitStack() as es:
        old = b.es
        b.es = es
        xacc = b.sb("p3_xacc", [128, nt, D], F32)
        h2T = b.sb("p3_h2T", [128, 8, ntok], BF16)
        gate = b.sb("p3_gate", [128, nt, 16], F32)
        lng = b.sb("p3_lng", [128, D], F32)
        lnb = b.sb("p3_lnb", [128, D], F32)
        gbc = b.sb("p3_gbc", [128, D], F32)
        junk = b.sb("p3_junk", [128, D], F32)
        st = b.sb("p3_st", [128, 8], F32)
        identf = b.sb("p3_idf", [128, 128], F32)
        identb = b.sb("p3_idb", [128, 128], BF16)
        r_xacc, r_h2T, r_gate, r_ln, r_gbc, r_junk, r_st, r_id = (b.res() for _ in range(8))
        b.dma("sp", identf[:], identf_d, writes=[r_id])
        b.op("dve", lambda e: e.tensor_copy(out=identb[:], in_=identf[:]), reads=[r_id], writes=[r_id])

        with ExitStack() as esa:
            b.es = esa
            wob = b.sb("p3a_wob", [128, 8, D], BF16)
            wst = [b.sb("p3a_wst%d" % i, [128, 8, 256], F32) for i in range(2)]
            mcol = b.sb("p3a_mcol", [128, 48], F32)
            sc1 = b.sb("p3a_sc1", [128, 8], F32)
            wr = b.sb("p3a_wr", [128, 8, 16], F32)
            brow = b.sb("p3a_brow", [128, 16], F32)
            ot = [b.sb("p3a_ot%d" % i, [128, D], BF16) for i in range(2)]
            oT = [b.sb("p3a_oT%d" % i, [128, 8, 128], BF16) for i in range(2)]
            xt = [b.sb("p3a_xt%d" % i, [128, D], F32) for i in range(2)]
            z = b.sb("p3a_z", [128, D], F32)
            x1 = b.sb("p3a_x1", [128, D], F32)
            h2f = b.sb("p3a_h2f", [128, 8, 128], F32)
            rt = b.sb("p3a_rt", [128, 160], F32)
            pOT = b.ps("p3a_pOT", [128, 1024], BF16)
            pM = [b.ps("p3a_pM%d" % i, [128, 512]) for i in range(2)]
            pX = [b.ps("p3a_pX%d" % i, [128, 512]) for i in range(2)]
            pR = b.ps("p3a_pR", [128, 16])
            r_wob, r_mc, r_wr, r_z, r_x1, r_h2f, r_rt, r_pOT, r_pR = (b.res() for _ in range(9))
            r_wst = [b.res(), b.res()]
            r_ot = [b.res(), b.res()]
            r_oT = [b.res(), b.res()]
            r_xt = [b.res(), b.res()]
            r_pM = [b.res(), b.res()]
            r_pX = [b.res(), b.res()]

            b.dma("sp", mcol[:], modcol_d[:, l * 48:(l + 1) * 48], writes=[r_mc])
            b.op("dve", lambda e: e.tensor_scalar(out=sc1[:], in0=mcol[:, 32:40], scalar1=1.0, scalar2=None,
                                                  op0=ALU.add), reads=[r_mc], writes=[r_mc])
            b.dma("sp", wr[:], wr_d.rearrange("(k p) n -> p k n", p=128), writes=[r_wr])
            b.dma("sp", brow[:], br_d.to_broadcast([128, 16]), writes=[r_wr])
            b.dma("sp", lng[:], lnmg_d[l:l + 1, :].to_broadcast([128, D]), writes=[r_ln])
            b.dma("sp", lnb[:], lnmb_d[l:l + 1, :].to_broadcast([128, D]), writes=[r_ln])
            b.dma("sp", gbc[:], modrow_d[l:l + 1, 2048:3072].to_broadcast([128, D]), writes=[r_gbc])
            b.op("dve", lambda e: e.tensor_scalar(out=gbc[:], in0=gbc[:], scalar1=1.0, scalar2=None, op0=ALU.add),
                 reads=[r_gbc], writes=[r_gbc])
            for i4 in range(4):
                i = i4 % 2
                b.dma("sp", wst[i][:], wout_d[l, :, i4 * 256:(i4 + 1) * 256].rearrange("(k p) n -> p k n", p=128),
                      writes=[r_wst[i]])
                b.op("pool", lambda e, i=i, i4=i4: e.tensor_tensor(
                    out=wob[:, :, i4 * 256:(i4 + 1) * 256], in0=wst[i][:],
                    in1=gbc[:, i4 * 256:(i4 + 1) * 256].unsqueeze(1).to_broadcast([128, 8, 256]), op=ALU.mult),
                    reads=[r_wst[i], r_gbc], writes=[r_wob])
            zs = [z, b.sb("p3a_z2", [128, D], F32)]
            x1s = [x1, b.sb("p3a_x12", [128, D], F32)]
            h2fs = [h2f, b.sb("p3a_h2f2", [128, 8, 128], F32)]
            rts = [rt, b.sb("p3a_rt2", [128, 160], F32)]
            sts = [st, b.sb("p3a_st2", [128, 8], F32)]
            junks = [junk, junk]
            r_zs = [r_z, b.res()]
            r_x1s = [r_x1, b.res()]
            r_h2fs = [r_h2f, b.res()]
            r_rts = [r_rt, b.res()]
            r_sts = [r_st, b.res()]
            r_junks = [r_junk, r_junk]

            def stage_A(t):
                s = t % 2
                z_, r_z_ = zs[s], r_zs[s]
                b.dma("sp", ot[s][:], o_d[t * 128:(t + 1) * 128, :], reads=[r_in], writes=[r_ot[s]])
                b.dma("sp", xt[s][:], x_d[t * 128:(t + 1) * 128, :], reads=[r_in], writes=[r_xt[s]])
                for c in range(8):
                    b.op("pe", lambda e, s=s, c=c: e.transpose(
                        out=pOT[:, c * 128:(c + 1) * 128], in_=ot[s][:, c * 128:(c + 1) * 128], identity=identb[:]),
                        reads=[r_ot[s], r_id], writes=[r_pOT], inc=(c == 7))
                b.op("act", lambda e, s=s: e.copy(out=oT[s][:].rearrange("p k n -> p (k n)"), in_=pOT[:]),
                     reads=[r_pOT], writes=[r_oT[s]])
                for nb in range(2):
                    for k in range(8):
                        b.op("pe", lambda e, s=s, k=k, nb=nb: e.matmul(
                            pM[nb][:, :], lhsT=oT[s][:, k, :], rhs=wob[:, k, nb * 512:(nb + 1) * 512],
                            start=(k == 0), stop=(k == 7)),
                            reads=[r_oT[s], r_wob], writes=[r_pM[nb]], inc=(k == 7))
                    b.op("dve", lambda e, s=s, nb=nb: e.scalar_tensor_tensor(
                        out=z_[:, nb * 512:(nb + 1) * 512], in0=xt[s][:, nb * 512:(nb + 1) * 512], scalar=ALPHA,
                        in1=pM[nb][:, :], op0=ALU.mult, op1=ALU.add),
                        reads=[r_xt[s], r_pM[nb]], writes=[r_z_])
                yield

            def stage_B(t):
                s = t % 2
                z_, r_z_ = zs[s], r_zs[s]
                x1_, r_x1_ = x1s[s], r_x1s[s]
                h2f_, r_h2f_ = h2fs[s], r_h2fs[s]
                rt_, r_rt_ = rts[s], r_rts[s]
                st_, r_st_ = sts[s], r_sts[s]
                junk_, r_junk_ = junks[s], r_junks[s]
                emit_ln_stats(b, z_[:], r_z_, junk_, r_junk_, st_, r_st_)
                b.op("dve", lambda e: e.tensor_scalar(out=x1_[:], in0=z_[:], scalar1=st_[:, 0:1], scalar2=st_[:, 1:2],
                                                      op0=ALU.subtract, op1=ALU.mult),
                     reads=[r_z_, r_st_], writes=[r_x1_])
                b.op("pool", lambda e: e.tensor_tensor(out=x1_[:], in0=x1_[:], in1=lng[:], op=ALU.mult),
                     reads=[r_x1_, r_ln], writes=[r_x1_])
                b.op("pool", lambda e: e.tensor_tensor(out=x1_[:], in0=x1_[:], in1=lnb[:], op=ALU.add),
                     reads=[r_x1_, r_ln], writes=[r_x1_])
                for c in range(8):
                    b.op("pe", lambda e, c=c: e.transpose(
                        out=pX[c // 4][:, (c % 4) * 128:(c % 4 + 1) * 128], in_=x1_[:, c * 128:(c + 1) * 128],
                        identity=identf[:]), reads=[r_x1_, r_id], writes=[r_pX[c // 4]], inc=(c % 4 == 3))
                for c in range(8):
                    b.op("act", lambda e, c=c: e.activation(
                        out=h2f_[:, c, :], in_=pX[c // 4][:, (c % 4) * 128:(c % 4 + 1) * 128], func=AF.Identity,
                        scale=sc1[:, c:c + 1], bias=mcol[:, 24 + c:25 + c]),
                        reads=[r_pX[c // 4], r_mc], writes=[r_h2f_])
                b.op("pool", lambda e, t=t: e.tensor_copy(out=h2T[:, :, t * 128:(t + 1) * 128], in_=h2f_[:]),
                     reads=[r_h2f_], writes=[r_h2T])
                b.op("act", lambda e, t=t: e.mul(out=xacc[:, t, :], in_=x1_[:], mul=ALPHA),
                     reads=[r_x1_], writes=[r_xacc])
                yield

            def stage_B2(t):
                s = t % 2
                h2f_, r_h2f_ = h2fs[s], r_h2fs[s]
                rt_, r_rt_ = rts[s], r_rts[s]
                for k in range(8):
                    b.op("pe", lambda e, k=k: e.matmul(pR[:, :], lhsT=h2f_[:, k, :], rhs=wr[:, k, :],
                                                       start=(k == 0), stop=(k == 7)),
                         reads=[r_h2f_, r_wr], writes=[r_pR], inc=(k == 7))
                S_, BS, EQ1, MSK, EQ2, WT_ = 0, 16, 32, 48, 64, 80
                M1, M2, GS, GM, GSEL, TOT, RT = 96, 100, 104, 108, 112, 116, 117

                def v3(o):
                    return rt_[:, o:o + 16].rearrange("p (g e) -> p g e", e=4)

                def bc(o):
                    return rt_[:, o:o + 4].unsqueeze(2).to_broadcast([128, 4, 4])

                def R(fn):
                    b.op("dve", fn, reads=[r_rt_, r_wr], writes=[r_rt_])

                b.op("act", lambda e: e.activation(out=rt_[:, S_:S_ + 16], in_=pR[:, :], func=AF.Sigmoid),
                     reads=[r_pR], writes=[r_rt_])
                R(lambda e: e.tensor_tensor(out=rt_[:, BS:BS + 16], in0=rt_[:, S_:S_ + 16], in1=brow[:], op=ALU.add))
                R(lambda e: e.tensor_reduce(out=rt_[:, M1:M1 + 4], in_=v3(BS), axis=AX.X, op=ALU.max))
                R(lambda e: e.tensor_tensor(out=v3(EQ1), in0=v3(BS), in1=bc(M1), op=ALU.is_equal))
                R(lambda e: e.scalar_tensor_tensor(out=rt_[:, MSK:MSK + 16], in0=rt_[:, EQ1:EQ1 + 16], scalar=-1.0e9,
                                                   in1=rt_[:, BS:BS + 16], op0=ALU.mult, op1=ALU.add))
                R(lambda e: e.tensor_reduce(out=rt_[:, M2:M2 + 4], in_=v3(MSK), axis=AX.X, op=ALU.max))
                R(lambda e: e.tensor_tensor(out=rt_[:, GS:GS + 4], in0=rt_[:, M1:M1 + 4], in1=rt_[:, M2:M2 + 4],
                                            op=ALU.add))
                R(lambda e: e.tensor_reduce(out=rt_[:, GM:GM + 1], in_=rt_[:, GS:GS + 4], axis=AX.X, op=ALU.max))
                R(lambda e: e.tensor_scalar(out=rt_[:, GSEL:GSEL + 4], in0=rt_[:, GS:GS + 4], scalar1=rt_[:, GM:GM + 1],
                                            scalar2=None, op0=ALU.is_equal))
                R(lambda e: e.tensor_tensor(out=v3(EQ2), in0=v3(MSK), in1=bc(M2), op=ALU.is_equal))
                R(lambda e: e.tensor_tensor(out=rt_[:, EQ2:EQ2 + 16], in0=rt_[:, EQ2:EQ2 + 16], in1=rt_[:, EQ1:EQ1 + 16],
                                            op=ALU.add))
                R(lambda e: e.tensor_tensor(out=v3(EQ2), in0=v3(EQ2), in1=bc(GSEL), op=ALU.mult))
                R(lambda e: e.tensor_tensor(out=rt_[:, WT_:WT_ + 16], in0=rt_[:, EQ2:EQ2 + 16], in1=rt_[:, S_:S_ + 16],
                                            op=ALU.mult))
                R(lambda e: e.tensor_reduce(out=rt_[:, TOT:TOT + 1], in_=rt_[:, WT_:WT_ + 16], axis=AX.X, op=ALU.add))
                R(lambda e: e.reciprocal(out=rt_[:, RT:RT + 1], in_=rt_[:, TOT:TOT + 1]))
                b.op("dve", lambda e, t=t: e.tensor_scalar(out=gate[:, t, :], in0=rt_[:, WT_:WT_ + 16],
                                                           scalar1=rt_[:, RT:RT + 1], scalar2=None, op0=ALU.mult),
                     reads=[r_rt_], writes=[r_gate])
                yield

            for _ in stage_A(0):
                pass
            for t in range(nt + 1):
                if t + 1 < nt:
                    for _ in stage_A(t + 1):
                        pass
                if t < nt:
                    for _ in stage_B(t):
                        pass
                if t >= 1:
                    for _ in stage_B2(t - 1):
                        pass
            b.barrier()
            b.es = es

        with ExitStack() as esb:
            b.es = esb
            b.dma("sp", gbc[:], modrow_d[l:l + 1, 5120:6144].to_broadcast([128, D]), writes=[r_gbc])
            b.op("dve", lambda e: e.tensor_scalar(out=gbc[:], in0=gbc[:], scalar1=1.0, scalar2=None, op0=ALU.add),
                 reads=[r_gbc], writes=[r_gbc])
            b.dma("sp", lng[:], lnfg_d[l:l + 1, :].to_broadcast([128, D]), writes=[r_ln])
            b.dma("sp", lnb[:], lnfb_d[l:l + 1, :].to_broadcast([128, D]), writes=[r_ln])
            sgu = [b.sb("p3b_sgu%d" % i, [128, 8, 256], F32) for i in range(2)]
            sd = [b.sb("p3b_sd%d" % i, [128, 2, D], F32) for i in range(2)]
            wg = [b.sb("p3b_wg%d" % i, [128, 8, 256], BF16) for i in range(2)]
            wu = [b.sb("p3b_wu%d" % i, [128, 8, 256], BF16) for i in range(2)]
            wd = [b.sb("p3b_wd%d" % i, [128, 2, D], BF16) for i in range(2)]
            sg = [b.sb("p3b_sg%d" % i, [128, 512], BF16) for i in range(2)]
            hid = [b.sb("p3b_hid%d" % i, [128, 512], BF16) for i in range(4)]
            pg = [b.ps("p3b_pg%d" % i, [128, 512]) for i in range(2)]
            pu = [b.ps("p3b_pu%d" % i, [128, 512]) for i in range(2)]
            py = [b.ps("p3b_py%d" % i, [128, 512]) for i in range(4)]
            r_sgu = [b.res(), b.res()]
            r_sd = [b.res(), b.res()]
            r_wg = [b.res(), b.res()]
            r_wu = [b.res(), b.res()]
            r_wd = [b.res(), b.res()]
            r_sg = [b.res(), b.res()]
            r_hid = [b.res() for _ in range(4)]
            r_pg = [b.res(), b.res()]
            r_pu = [b.res(), b.res()]
            r_py = [b.res() for _ in range(4)]
            sti = 0
            hc = 0
            yc = 0
            pending_down = None
            for ex in range(n_exp):
                w = ex % 2
                si = sti % 2
                sti += 1
                b.dma("sp", sgu[si][:], wg_d[l, ex].rearrange("(k p) f -> p k f", p=128), writes=[r_sgu[si]])
                b.op("pool", lambda e, si=si, w=w: e.tensor_copy(out=wg[w][:], in_=sgu[si][:]),
                     reads=[r_sgu[si]], writes=[r_wg[w]])
                si = sti % 2
                sti += 1
                b.dma("sp", sgu[si][:], wu_d[l, ex].rearrange("(k p) f -> p k f", p=128), writes=[r_sgu[si]])
                b.op("pool", lambda e, si=si, w=w: e.tensor_copy(out=wu[w][:], in_=sgu[si][:]),
                     reads=[r_sgu[si]], writes=[r_wu[w]])
                b.dma("sp", sd[w][:], wd_d[l, ex].rearrange("(k p) n -> p k n", p=128), writes=[r_sd[w]])
                b.op("pool", lambda e, w=w: e.tensor_tensor(
                    out=wd[w][:], in0=sd[w][:], in1=gbc[:].unsqueeze(1).to_broadcast([128, 2, D]), op=ALU.mult),
                    reads=[r_sd[w], r_gbc], writes=[r_wd[w]])
                for tg in range(ngrp):
                    hids = []
                    for fc in range(2):
                        x = fc
                        for k in range(8):
                            b.op("pe", lambda e, w=w, k=k, fc=fc, tg=tg, x=x: e.matmul(
                                pg[x][:, :], lhsT=wg[w][:, k, fc * 128:(fc + 1) * 128],
                                rhs=h2T[:, k, tg * 512:(tg + 1) * 512], start=(k == 0), stop=(k == 7)),
                                reads=[r_wg[w], r_h2T], writes=[r_pg[x]], inc=(k == 7))
                        for k in range(8):
                            b.op("pe", lambda e, w=w, k=k, fc=fc, tg=tg, x=x: e.matmul(
                                pu[x][:, :], lhsT=wu[w][:, k, fc * 128:(fc + 1) * 128],
                                rhs=h2T[:, k, tg * 512:(tg + 1) * 512], start=(k == 0), stop=(k == 7)),
                                reads=[r_wu[w], r_h2T], writes=[r_pu[x]], inc=(k == 7))
                        b.op("act", lambda e, x=x: e.activation(out=sg[x][:], in_=pg[x][:, :], func=AF.Silu),
                             reads=[r_pg[x]], writes=[r_sg[x]])
                        hx = hc % 4
                        hc += 1
                        b.op("dve", lambda e, x=x, hx=hx: e.tensor_tensor(out=hid[hx][:], in0=pu[x][:, :],
                                                                          in1=sg[x][:], op=ALU.mult),
                             reads=[r_pu[x], r_sg[x]], writes=[r_hid[hx]])
                        hids.append(hx)
                    def down_job(tg=tg, w=w, ex=ex, hids=tuple(hids)):
                        nonlocal yc
                        for tt in range(4):
                            t = tg * 4 + tt
                            for nb in range(2):
                                yx = yc % 4
                                yc += 1
                                for fc in range(2):
                                    hx = hids[fc]
                                    b.op("pe", lambda e, hx=hx, tt=tt, w=w, fc=fc, nb=nb, yx=yx: e.matmul(
                                        py[yx][:, :], lhsT=hid[hx][:, tt * 128:(tt + 1) * 128],
                                        rhs=wd[w][:, fc, nb * 512:(nb + 1) * 512], start=(fc == 0), stop=(fc == 1)),
                                        reads=[r_hid[hx], r_wd[w]], writes=[r_py[yx]], inc=(fc == 1))
                                b.op("dve", lambda e, yx=yx, t=t, nb=nb, ex=ex: e.scalar_tensor_tensor(
                                    out=xacc[:, t, nb * 512:(nb + 1) * 512], in0=py[yx][:, :],
                                    scalar=gate[:, t, ex:ex + 1], in1=xacc[:, t, nb * 512:(nb + 1) * 512],
                                    op0=ALU.mult, op1=ALU.add),
                                    reads=[r_py[yx], r_gate, r_xacc], writes=[r_xacc])
                    if pending_down is not None:
                        pending_down()
                    pending_down = down_job
            if pending_down is not None:
                pending_down()
            b.barrier()
            b.es = es

        with ExitStack() as esc:
            b.es = esc
            xo = [b.sb("p3c_xo%d" % i, [128, D], F32) for i in range(2)]
            r_xo = [b.res(), b.res()]
            for t in range(nt):
                s = t % 2
                emit_ln_stats(b, xacc[:, t, :], r_xacc, junk, r_junk, st, r_st)
                b.op("dve", lambda e, t=t, s=s: e.tensor_scalar(
                    out=xo[s][:], in0=xacc[:, t, :], scalar1=st[:, 0:1], scalar2=st[:, 1:2],
                    op0=ALU.subtract, op1=ALU.mult), reads=[r_xacc, r_st], writes=[r_xo[s]])
                b.op("pool", lambda e, s=s: e.tensor_tensor(out=xo[s][:], in0=xo[s][:], in1=lng[:], op=ALU.mult),
                     reads=[r_xo[s], r_ln], writes=[r_xo[s]])
                b.op("pool", lambda e, s=s: e.tensor_tensor(out=xo[s][:], in0=xo[s][:], in1=lnb[:], op=ALU.add),
                     reads=[r_xo[s], r_ln], writes=[r_xo[s]])
                b.dma("pool", xout_d[t * 128:(t + 1) * 128, :], xo[s][:], reads=[r_xo[s]], writes=[r_out])
            b.barrier()
            b.es = es
        b.es = old


def build_p3(l, nt, n_exp=16):
    nc = bass.Bass("TRN2", target_bir_lowering=False)
    x_d = nc.dram_tensor("x", [nt * 128, D], F32, kind="ExternalInput").ap()
    o_d = nc.dram_tensor("o", [nt * 128, D], BF16, kind="ExternalInput").ap()
    wout_d = nc.dram_tensor("w_out", [DEPTH, D, D], F32, kind="ExternalInput").ap()
    modrow_d = nc.dram_tensor("modrow", [DEPTH, 6 * D], F32, kind="ExternalInput").ap()
    modcol_d = nc.dram_tensor("modcol", [128, DEPTH * 48], F32, kind="ExternalInput").ap()
    lnmg_d = nc.dram_tensor("ln_mix_g", [DEPTH, D], F32, kind="ExternalInput").ap()
    lnmb_d = nc.dram_tensor("ln_mix_b", [DEPTH, D], F32, kind="ExternalInput").ap()
    wr_d = nc.dram_tensor("w_router", [D, 16], F32, kind="ExternalInput").ap()
    br_d = nc.dram_tensor("b_router", [1, 16], F32, kind="ExternalInput").ap()
    wg_d = nc.dram_tensor("w_gate", [DEPTH, 16, D, 256], F32, kind="ExternalInput").ap()
    wu_d = nc.dram_tensor("w_up", [DEPTH, 16, D, 256], F32, kind="ExternalInput").ap()
    wd_d = nc.dram_tensor("w_down", [DEPTH, 16, 256, D], F32, kind="ExternalInput").ap()
    lnfg_d = nc.dram_tensor("ln_ffn_g", [DEPTH, D], F32, kind="ExternalInput").ap()
    lnfb_d = nc.dram_tensor("ln_ffn_b", [DEPTH, D], F32, kind="ExternalInput").ap()
    identf_d = nc.dram_tensor("identf", [128, 128], F32, kind="ExternalInput").ap()
    xout_d = nc.dram_tensor("xout", [nt * 128, D], F32, kind="ExternalOutput").ap()
    with ExitStack() as es, nc.allow_low_precision("bf16 matmul operands, fp32 accumulation"):
        b = B(nc, es)
        emit_p3(b, l, nt, x_d, o_d, wout_d, modrow_d, modcol_d, lnmg_d, lnmb_d, wr_d, br_d, wg_d, wu_d, wd_d,
                lnfg_d, lnfb_d, identf_d, xout_d, b.res(), b.res(), n_exp=n_exp)
        b.finish()
    return nc


I32 = mybir.dt.int32


def _decl_p1_inputs(nc):
    d = {}
    d["pos"] = nc.dram_tensor("pos", [128, NT], I32, kind="ExternalInput").ap()
    d["invf"] = nc.dram_tensor("invf", [128, 32], F32, kind="ExternalInput").ap()
    d["w_in"] = nc.dram_tensor("w_in", [1, D, INW], F32, kind="ExternalInput").ap()
    d["i_bias"] = nc.dram_tensor("i_bias", [1, 4], F32, kind="ExternalInput").ap()
    d["f_bias"] = nc.dram_tensor("f_bias", [1, 4], F32, kind="ExternalInput").ap()
    return d


def _decl_p1_outputs(nc):
    d = {}
    d["featT"] = nc.dram_tensor("featT", [NT, 128, NFT, 128], BF16, kind="ExternalOutput").ap()
    d["tokA"] = nc.dram_tensor("tokA", [TOK, 1792], BF16, kind="ExternalOutput").ap()
    d["tokF"] = nc.dram_tensor("tokF", [TOK, 12], F32, kind="ExternalOutput").ap()
    d["gT"] = nc.dram_tensor("gT", [NT, 8, 128], F32, kind="ExternalOutput").ap()
    return d


def _decl_p3_inputs(nc):
    d = {}
    d["o"] = nc.dram_tensor("o", [TOK, D], BF16, kind="ExternalInput").ap()
    d["w_out"] = nc.dram_tensor("w_out", [1, D, D], F32, kind="ExternalInput").ap()
    d["modrow3"] = nc.dram_tensor("modrow3", [1, 6 * D], F32, kind="ExternalInput").ap()
    d["modcol3"] = nc.dram_tensor("modcol3", [128, 48], F32, kind="ExternalInput").ap()
    d["ln_mix_g"] = nc.dram_tensor("ln_mix_g", [1, D], F32, kind="ExternalInput").ap()
    d["ln_mix_b"] = nc.dram_tensor("ln_mix_b", [1, D], F32, kind="ExternalInput").ap()
    d["w_router"] = nc.dram_tensor("w_router", [D, 16], F32, kind="ExternalInput").ap()
    d["b_router"] = nc.dram_tensor("b_router", [1, 16], F32, kind="ExternalInput").ap()
    d["w_gate"] = nc.dram_tensor("w_gate", [1, 16, D, 256], F32, kind="ExternalInput").ap()
    d["w_up"] = nc.dram_tensor("w_up", [1, 16, D, 256], F32, kind="ExternalInput").ap()
    d["w_down"] = nc.dram_tensor("w_down", [1, 16, 256, D], F32, kind="ExternalInput").ap()
    d["ln_ffn_g"] = nc.dram_tensor("ln_ffn_g", [1, D], F32, kind="ExternalInput").ap()
    d["ln_ffn_b"] = nc.dram_tensor("ln_ffn_b", [1, D], F32, kind="ExternalInput").ap()
    return d


def _emit_p3_from(b, i3, x_d, identf_d, xout_d, r_in, r_out):
    emit_p3(b, 0, NT, x_d, i3["o"], i3["w_out"], i3["modrow3"], i3["modcol3"], i3["ln_mix_g"], i3["ln_mix_b"],
            i3["w_router"], i3["b_router"], i3["w_gate"], i3["w_up"], i3["w_down"], i3["ln_ffn_g"],
            i3["ln_ffn_b"], identf_d, xout_d, r_in, r_out)


def build_A():
    nc = bass.Bass("TRN2", target_bir_lowering=False)
    x_d = nc.dram_tensor("x", [TOK, D], F32, kind="ExternalInput").ap()
    identf_d = nc.dram_tensor("identf", [128, 128], F32, kind="ExternalInput").ap()
    ccol_d = nc.dram_tensor("ccol", [128, 8], F32, kind="ExternalInput").ap()
    wada_d = nc.dram_tensor("w_ada", [DEPTH, D, 6 * D], F32, kind="ExternalInput").ap()
    bada_d = nc.dram_tensor("b_ada", [DEPTH, 6 * D], F32, kind="ExternalInput").ap()
    modrow_d = nc.dram_tensor("modrow", [DEPTH, 6 * D], F32, kind="ExternalOutput").ap()
    modcol_d = nc.dram_tensor("modcol", [128, DEPTH * 48], F32, kind="ExternalOutput").ap()
    i1 = _decl_p1_inputs(nc)
    o1 = _decl_p1_outputs(nc)
    with ExitStack() as es, nc.allow_low_precision("bf16 matmul operands, fp32 accumulation"):
        b = B(nc, es)
        cos = b.sb("cos", [128, NT, 32], F32)
        sin = b.sb("sin", [128, NT, 32], F32)
        r_tab, r_mod, r_out = b.res(), b.res(), b.res()
        emit_mod(b, ccol_d, wada_d, bada_d, modrow_d, modcol_d)
        emit_rope_tables(b, i1["pos"], i1["invf"], cos, sin, r_tab, NT)
        emit_p1(b, 0, NT, x_d, i1["w_in"], modcol_d, i1["i_bias"], i1["f_bias"], identf_d, cos, sin, r_tab,
                o1["featT"], o1["tokA"], o1["tokF"], o1["gT"], b.res(), r_out)
        b.finish()
    return nc


def build_B():
    nc = bass.Bass("TRN2", target_bir_lowering=False)
    featT_all = nc.dram_tensor("featT_all", [2 * NT, 128, NFT, 128], BF16, kind="ExternalInput").ap()
    tokA_all = nc.dram_tensor("tokA_all", [2 * TOK, 1792], BF16, kind="ExternalInput").ap()
    gT_all = nc.dram_tensor("gT_all", [2 * NT, 8, 128], F32, kind="ExternalInput").ap()
    featT_own = nc.dram_tensor("featT_own", [NT, 128, NFT, 128], BF16, kind="ExternalInput").ap()
    tokA_own = nc.dram_tensor("tokA_own", [TOK, 1792], BF16, kind="ExternalInput").ap()
    tokF_own = nc.dram_tensor("tokF_own", [TOK, 12], F32, kind="ExternalInput").ap()
    vis_d = nc.dram_tensor("vis", [128, 256], F32, kind="ExternalInput").ap()
    pw_d = nc.dram_tensor("pw", [128, NIT + 1], F32, kind="ExternalInput").ap()
    decT_d = nc.dram_tensor("decT", [128, 4, 128], F32, kind="ExternalInput").ap()
    qdecT_d = nc.dram_tensor("qdecT", [128, 2, 128], F32, kind="ExternalInput").ap()
    kdec_d = nc.dram_tensor("kdec", [128, 4], F32, kind="ExternalInput").ap()
    jf_d = nc.dram_tensor("jf", [128, 1], F32, kind="ExternalInput").ap()
    convw_d = nc.dram_tensor("convw", [1, 128, 4, 4], F32, kind="ExternalInput").ap()
    convb_d = nc.dram_tensor("convb", [1, 128, 4], F32, kind="ExternalInput").ap()
    caus_d = nc.dram_tensor("caus", [128, 128], F32, kind="ExternalInput").ap()
    sel4_d = nc.dram_tensor("sel4", [4, 512], F32, kind="ExternalInput").ap()
    eye4_d = nc.dram_tensor("eye4", [4, 4], F32, kind="ExternalInput").ap()
    identf_d = nc.dram_tensor("identf", [128, 128], F32, kind="ExternalInput").ap()
    o_d = nc.dram_tensor("o", [TOK, D], BF16, kind="ExternalOutput").ap()
    with ExitStack() as es, nc.allow_low_precision("bf16 matmul operands, fp32 accumulation"):
        b = B(nc, es)
        r_in, r_out = b.res(), b.res()
        emit_p2a(b, NT, featT_all, tokA_all, featT_own, tokF_own, vis_d, pw_d, identf_d, o_d, r_in, r_out)
        emit_p2b(b, NT, tokA_all, featT_own, tokA_own, decT_d, qdecT_d, kdec_d, jf_d, o_d, r_in, r_out)
        emit_p2c(b, 0, NT, featT_all, tokA_all, gT_all, tokA_own, convw_d, convb_d, caus_d, sel4_d, eye4_d,
                 jf_d, identf_d, o_d, r_in, r_out)
        b.finish()
    return nc


def build_C(with_p1):
    nc = bass.Bass("TRN2", target_bir_lowering=False)
    x_d = nc.dram_tensor("x", [TOK, D], F32, kind="ExternalInput").ap()
    identf_d = nc.dram_tensor("identf", [128, 128], F32, kind="ExternalInput").ap()
    i3 = _decl_p3_inputs(nc)
    xout_d = nc.dram_tensor("xout", [TOK, D], F32, kind="ExternalOutput").ap()
    if with_p1:
        i1 = _decl_p1_inputs(nc)
        modcol1_d = nc.dram_tensor("modcol1", [128, 48], F32, kind="ExternalInput").ap()
        o1 = _decl_p1_outputs(nc)
    with ExitStack() as es, nc.allow_low_precision("bf16 matmul operands, fp32 accumulation"):
        b = B(nc, es)
        r_in, r_x = b.res(), b.res()
        if with_p1:
            cos = b.sb("cos", [128, NT, 32], F32)
            sin = b.sb("sin", [128, NT, 32], F32)
            r_tab = b.res()
            emit_rope_tables(b, i1["pos"], i1["invf"], cos, sin, r_tab, NT)
        _emit_p3_from(b, i3, x_d, identf_d, xout_d, r_in, r_x)
        if with_p1:
            emit_p1(b, 0, NT, xout_d, i1["w_in"], modcol1_d, i1["i_bias"], i1["f_bias"], identf_d, cos, sin,
                    r_tab, o1["featT"], o1["tokA"], o1["tokF"], o1["gT"], r_x, b.res())
        b.finish()
    return nc


_PROGS = {}


def _prog(name, fn):
    if name not in _PROGS:
        _PROGS[name] = fn()
    return _PROGS[name]


def _own_tiles(a, j):
    sh = a.shape
    return np.ascontiguousarray(a.reshape((32, 128) + sh[1:])[j::2]).reshape((TOK,) + sh[1:])


def kernel(x, c, positions, w_ada, b_ada, w_in, i_bias, f_bias, conv_w, conv_b, w_out, ln_mix_g, ln_mix_b,
           w_router, b_router, w_gate, w_up, w_down, ln_ffn_g, ln_ffn_b):
    f32 = np.float32
    x = np.asarray(x, f32)
    c = np.asarray(c, f32)
    positions = np.asarray(positions, np.int32)
    w_ada, b_ada, w_in = np.asarray(w_ada, f32), np.asarray(b_ada, f32), np.asarray(w_in, f32)
    i_bias, f_bias = np.asarray(i_bias, f32), np.asarray(f_bias, f32)
    w_out = np.asarray(w_out, f32)
    ln_mix_g, ln_mix_b = np.asarray(ln_mix_g, f32), np.asarray(ln_mix_b, f32)
    w_router, b_router = np.asarray(w_router, f32), np.asarray(b_router, f32).reshape(1, 16)
    w_gate, w_up, w_down = np.asarray(w_gate, f32), np.asarray(w_up, f32), np.asarray(w_down, f32)
    ln_ffn_g, ln_ffn_b = np.asarray(ln_ffn_g, f32), np.asarray(ln_ffn_b, f32)
    cw, cb = conv_layouts(np.asarray(conv_w, f32), np.asarray(conv_b, f32))
    ident = np.eye(128, dtype=f32)
    invf = inv_freq_table()
    decT, qdecT, kdec = ret_tables()
    caus, sel4, eye4 = mlstm_consts()
    pw = pw_table()
    cores = list(range(8))

    def p1_in(core, l):
        bi, j = core // 2, core % 2
        pos = np.ascontiguousarray(positions[bi].reshape(32, 128)[j::2].T)
        return {"pos": pos, "invf": invf, "w_in": w_in[l:l + 1], "i_bias": i_bias[l:l + 1],
                "f_bias": f_bias[l:l + 1]}

    def p3_in(core, l, o, modrow, modcol):
        return {"o": o, "w_out": w_out[l:l + 1], "modrow3": np.ascontiguousarray(modrow[l:l + 1]),
                "modcol3": np.ascontiguousarray(modcol[:, l * 48:(l + 1) * 48]),
                "ln_mix_g": ln_mix_g[l:l + 1], "ln_mix_b": ln_mix_b[l:l + 1], "w_router": w_router,
                "b_router": b_router, "w_gate": w_gate[l:l + 1], "w_up": w_up[l:l + 1], "w_down": w_down[l:l + 1],
                "ln_ffn_g": ln_ffn_g[l:l + 1], "ln_ffn_b": ln_ffn_b[l:l + 1]}

    ims = []
    xs = []
    for core in cores:
        bi, j = core // 2, core % 2
        xo = _own_tiles(x[bi], j)
        xs.append(xo)
        im = {"x": xo, "identf": ident, "ccol": np.ascontiguousarray(c[bi].reshape(8, 128).T),
              "w_ada": w_ada, "b_ada": b_ada}
        im.update(p1_in(core, 0))
        ims.append(im)
    res = run_bass_kernel_spmd(_prog("A", build_A), ims, core_ids=cores).results
    modrow = [np.asarray(r["modrow"]) for r in res]
    modcol = [np.asarray(r["modcol"]) for r in res]
    p1o = res
    for l in range(DEPTH):
        ims = []
        for core in cores:
            bi, j = core // 2, core % 2
            a, bb = p1o[2 * bi], p1o[2 * bi + 1]
            ims.append({
                "featT_all": np.concatenate([np.asarray(a["featT"]), np.asarray(bb["featT"])], axis=0),
                "tokA_all": np.concatenate([np.asarray(a["tokA"]), np.asarray(bb["tokA"])], axis=0),
                "gT_all": np.concatenate([np.asarray(a["gT"]), np.asarray(bb["gT"])], axis=0),
                "featT_own": np.asarray(p1o[core]["featT"]), "tokA_own": np.asarray(p1o[core]["tokA"]),
                "tokF_own": np.asarray(p1o[core]["tokF"]),
                "vis": vis_table(j), "pw": pw, "decT": decT, "qdecT": qdecT, "kdec": kdec,
                "jf": np.full((128, 1), float(j), f32), "convw": cw[l:l + 1], "convb": cb[l:l + 1],
                "caus": caus, "sel4": sel4, "eye4": eye4, "identf": ident})
        ob = run_bass_kernel_spmd(_prog("B", build_B), ims, core_ids=cores).results
        last = (l == DEPTH - 1)
        ims = []
        for core in cores:
            im = {"x": xs[core], "identf": ident}
            im.update(p3_in(core, l, np.asarray(ob[core]["o"]), modrow[core], modcol[core]))
            if not last:
                im.update(p1_in(core, l + 1))
                im["modcol1"] = np.ascontiguousarray(modcol[core][:, (l + 1) * 48:(l + 2) * 48])
            ims.append(im)
        if last:
            res = run_bass_kernel_spmd(_prog("E", lambda: build_C(False)), ims, core_ids=cores).results
        else:
            res = run_bass_kernel_spmd(_prog("C", lambda: build_C(True)), ims, core_ids=cores).results
            p1o = res
        xs = [np.asarray(r["xout"]) for r in res]
    out = np.zeros((BATCH, SEQ, D), f32)
    for core in cores:
        bi, j = core // 2, core % 2
        out[bi].reshape(32, 128, D)[j::2] = xs[core].reshape(NT, 128, D)
    return out


def build_fused(depth=DEPTH):
    nc = bass.Bass("TRN2", target_bir_lowering=False)

    def inp(name, shape, dt=F32):
        return nc.dram_tensor(name, list(shape), dt, kind="ExternalInput").ap()

    def scr(name, shape, dt=F32):
        return nc.dram_tensor(name, list(shape), dt).ap()

    x_in = inp("x2", [2 * TOK, D])
    pos_d = inp("pos2", [2, 128, NT], I32)
    invf_d = inp("invf", [128, 32])
    identf_d = inp("identf", [128, 128])
    ccol_d = inp("ccol", [128, 8])
    wada_d = inp("w_ada", [DEPTH, D, 6 * D])
    bada_d = inp("b_ada", [DEPTH, 6 * D])
    win_d = inp("w_in", [DEPTH, D, INW])
    ib_d = inp("i_bias", [DEPTH, 4])
    fb_d = inp("f_bias", [DEPTH, 4])
    convw_d = inp("convw", [DEPTH, 128, 4, 4])
    convb_d = inp("convb", [DEPTH, 128, 4])
    wout_d = inp("w_out", [DEPTH, D, D])
    lnmg_d = inp("ln_mix_g", [DEPTH, D])
    lnmb_d = inp("ln_mix_b", [DEPTH, D])
    wr_d = inp("w_router", [D, 16])
    br_d = inp("b_router", [1, 16])
    wg_d = inp("w_gate", [DEPTH, 16, D, 256])
    wu_d = inp("w_up", [DEPTH, 16, D, 256])
    wd_d = inp("w_down", [DEPTH, 16, 256, D])
    lnfg_d = inp("ln_ffn_g", [DEPTH, D])
    lnfb_d = inp("ln_ffn_b", [DEPTH, D])
    vis_d = inp("vis2", [2, 128, 256])
    jf_d = inp("jf2", [2, 128, 1])
    pw_d = inp("pw", [128, NIT + 1])
    decT_d = inp("decT", [128, 4, 128])
    qdecT_d = inp("qdecT", [128, 2, 128])
    kdec_d = inp("kdec", [128, 4])
    caus_d = inp("caus", [128, 128])
    sel4_d = inp("sel4", [4, 512])
    eye4_d = inp("eye4", [4, 4])
    xfin = nc.dram_tensor("xfin", [2 * TOK, D], F32, kind="ExternalOutput").ap()

    modrow_s = scr("modrow_s", [DEPTH, 6 * D])
    modcol_s = scr("modcol_s", [128, DEPTH * 48])
    featT_s = scr("featT_s", [2 * NT, 128, NFT, 128], BF16)
    tokA_s = scr("tokA_s", [2 * TOK, 1792], BF16)
    tokF_s = scr("tokF_s", [2 * TOK, 12])
    gT_s = scr("gT_s", [2 * NT, 8, 128])
    o_s = scr("o_s", [2 * TOK, D], BF16)
    xs = [scr("xs0", [2 * TOK, D]), scr("xs1", [2 * TOK, D])]

    with ExitStack() as es, nc.allow_low_precision("bf16 matmul operands, fp32 accumulation"):
        b = B(nc, es)
        cos = [b.sb("cos%d" % j, [128, NT, 32], F32) for j in range(2)]
        sin = [b.sb("sin%d" % j, [128, NT, 32], F32) for j in range(2)]
        r_tab = [b.res(), b.res()]
        r_bun, r_o, r_x = b.res(), b.res(), b.res()
        emit_mod(b, ccol_d, wada_d, bada_d, modrow_s, modcol_s)
        for j in range(2):
            emit_rope_tables(b, pos_d[j], invf_d, cos[j], sin[j], r_tab[j], NT)
        for l in range(depth):
            x_l = x_in if l == 0 else xs[l % 2]
            x_n = xfin if l == depth - 1 else xs[(l + 1) % 2]
            emit_p1(b, l, 2 * NT, x_l, win_d, modcol_s, ib_d, fb_d, identf_d, cos, sin, r_tab,
                    featT_s, tokA_s, tokF_s, gT_s, r_x, r_bun)
            emit_p2a(b, NT, featT_s, tokA_s, None, None, None, pw_d, identf_d, None, r_bun, r_o,
                     parities=[(featT_s[j * NT:(j + 1) * NT], tokF_s[j * TOK:(j + 1) * TOK], vis_d[j],
                                o_s[j * TOK:(j + 1) * TOK]) for j in range(2)])
            emit_p2b(b, NT, tokA_s, featT_s, None, decT_d, qdecT_d, kdec_d, jf_d[0], o_s, r_bun, r_o, whole=True)
            emit_p2c(b, l, NT, featT_s, tokA_s, gT_s, None, convw_d, convb_d, caus_d, sel4_d, eye4_d,
                     jf_d[0], identf_d, o_s, r_bun, r_o, whole=True)
            for j in range(2):
                sl = slice(j * TOK, (j + 1) * TOK)
                emit_p3(b, l, NT, x_l[sl], o_s[sl], wout_d, modrow_s, modcol_s, lnmg_d, lnmb_d, wr_d, br_d,
                        wg_d, wu_d, wd_d, lnfg_d, lnfb_d, identf_d, x_n[sl], r_o, r_x)
            if l != depth - 1:
                b.new_epoch()
        b.finish()
    return nc


def kernel_fused(x, c, positions, w_ada, b_ada, w_in, i_bias, f_bias, conv_w, conv_b, w_out, ln_mix_g, ln_mix_b,
                 w_router, b_router, w_gate, w_up, w_down, ln_ffn_g, ln_ffn_b):
    f32 = np.float32
    x = np.asarray(x, f32)
    c = np.asarray(c, f32)
    positions = np.asarray(positions, np.int32)
    cw, cb = conv_layouts(np.asarray(conv_w, f32), np.asarray(conv_b, f32))
    decT, qdecT, kdec = ret_tables()
    caus, sel4, eye4 = mlstm_consts()
    shared = {
        "invf": inv_freq_table(), "identf": np.eye(128, dtype=f32),
        "w_ada": np.asarray(w_ada, f32), "b_ada": np.asarray(b_ada, f32), "w_in": np.asarray(w_in, f32),
        "i_bias": np.asarray(i_bias, f32), "f_bias": np.asarray(f_bias, f32), "convw": cw, "convb": cb,
        "w_out": np.asarray(w_out, f32), "ln_mix_g": np.asarray(ln_mix_g, f32),
        "ln_mix_b": np.asarray(ln_mix_b, f32), "w_router": np.asarray(w_router, f32),
        "b_router": np.asarray(b_router, f32).reshape(1, 16), "w_gate": np.asarray(w_gate, f32),
        "w_up": np.asarray(w_up, f32), "w_down": np.asarray(w_down, f32),
        "ln_ffn_g": np.asarray(ln_ffn_g, f32), "ln_ffn_b": np.asarray(ln_ffn_b, f32),
        "vis2": np.stack([vis_table(0), vis_table(1)]),
        "jf2": np.stack([np.zeros((128, 1), f32), np.ones((128, 1), f32)]),
        "pw": pw_table(), "decT": decT, "qdecT": qdecT, "kdec": kdec, "caus": caus, "sel4": sel4, "eye4": eye4,
    }
    cores = list(range(8))
    ims = []
    for core in cores:
        bi = core % BATCH
        xb = x[bi].reshape(32, 128, D)
        pb = positions[bi].reshape(32, 128)
        im = dict(shared)
        im["x2"] = np.ascontiguousarray(np.concatenate([xb[0::2], xb[1::2]], axis=0)).reshape(2 * TOK, D)
        im["pos2"] = np.ascontiguousarray(np.stack([pb[0::2].T, pb[1::2].T]))
        im["ccol"] = np.ascontiguousarray(c[bi].reshape(8, 128).T)
        ims.append(im)
    res = run_bass_kernel_spmd(_prog("F", build_fused), ims, core_ids=cores).results
    out = np.zeros((BATCH, SEQ, D), f32)
    for bi in range(BATCH):
        xf = np.asarray(res[bi]["xfin"]).reshape(2, NT, 128, D)
        ob = out[bi].reshape(32, 128, D)
        ob[0::2] = xf[0]
        ob[1::2] = xf[1]
    return out


kernel_unfused = kernel
kernel = kernel_fused
```

```python
import numpy as np
from contextlib import ExitStack

import concourse.bass as bass
import concourse.mybir as mybir
from concourse.bass_utils import run_bass_kernel_spmd

F32 = mybir.dt.float32
BF16 = mybir.dt.bfloat16
ALU = mybir.AluOpType
AF = mybir.ActivationFunctionType
AX = mybir.AxisListType

D = 1024
SEQ = 4096
BATCH = 4
DEPTH = 4
NT = 16
TOK = NT * 128
INW = 3916
LN_EPS = 1e-5
ALPHA = (2.0 * DEPTH) ** 0.25
NEG = -1.0e30

ENGS = ("pe", "act", "dve", "pool", "sp")
NRING = 8


class Res:
    __slots__ = ("name", "w", "r")

    def __init__(self, name=""):
        self.name = name
        self.w = None
        self.r = []


class B:
    def __init__(self, nc, es):
        self.nc = nc
        self.es = es
        self.root_es = es
        self.epoch = 0
        self.q = {e: [] for e in ENGS}
        self.sem = {e: es.enter_context(nc.semaphore("s_" + e)) for e in ENGS}
        self.cnt = {e: 0 for e in ENGS}
        self.seen = {e: {} for e in ENGS}
        self.dq = ("sp", "pool")
        self.ring = {e: [es.enter_context(nc.semaphore("d_%s%d" % (e, i))) for i in range(NRING)]
                     for e in self.dq}
        self.rcnt = {e: [0] * NRING for e in self.dq}
        self.rnext = {e: 0 for e in self.dq}
        self.nres = 0

    def res(self, name=""):
        self.nres += 1
        return Res(name or ("r%d" % self.nres))

    def sb(self, name, shape, dt):
        self.nres += 1
        return self.es.enter_context(self.nc.sbuf_tensor("%s_%d" % (name, self.nres), list(shape), dt))

    def ps(self, name, shape, dt=F32):
        self.nres += 1
        return self.es.enter_context(self.nc.psum_tensor("%s_%d" % (name, self.nres), list(shape), dt))

    def _deps(self, eng, reads, writes):
        need = {}

        def add(ev):
            if ev is None:
                return
            s, v = ev
            k = id(s)
            if k not in need or need[k][1] < v:
                need[k] = (s, v)

        for r in reads:
            add(r.w)
        for w in writes:
            add(w.w)
            for ev in w.r:
                add(ev)
        out = []
        seen = self.seen[eng]
        own = id(self.sem[eng])
        for k, (s, v) in need.items():
            if eng == "pe" and k == own:
                continue
            if seen.get(k, 0) >= v:
                continue
            seen[k] = v
            out.append((s, v))
        return out

    def op(self, eng, fn, reads=(), writes=(), inc=True):
        waits = self._deps(eng, reads, writes)
        if inc:
            self.cnt[eng] += 1
            ev = (self.sem[eng], self.cnt[eng])
        else:
            ev = (self.sem[eng], self.cnt[eng] + 1)
        for r in reads:
            r.r.append(ev)
            if len(r.r) > 64:
                r.r = r.r[-64:] if False else self._compact(r.r)
        for w in writes:
            w.w = ev
            w.r = []
        self.q[eng].append((waits, fn, self.sem[eng] if inc else None, 1))

    @staticmethod
    def _compact(evs):
        best = {}
        for s, v in evs:
            k = id(s)
            if k not in best or best[k][1] < v:
                best[k] = (s, v)
        return list(best.values())

    def dma(self, q, out_ap, in_ap, reads=(), writes=()):
        waits = self._deps(q, reads, writes)
        i = self.rnext[q]
        self.rnext[q] = (i + 1) % NRING
        s = self.ring[q][i]
        if self.rcnt[q][i] > 0:
            v = 16 * self.rcnt[q][i]
            if self.seen[q].get(id(s), 0) < v:
                self.seen[q][id(s)] = v
                waits.append((s, v))
        self.rcnt[q][i] += 1
        ev = (s, 16 * self.rcnt[q][i])
        for r in reads:
            r.r.append(ev)
        for w in writes:
            w.w = ev
            w.r = []

        def fn(e, out_ap=out_ap, in_ap=in_ap):
            return e.dma_start(out=out_ap, in_=in_ap)

        self.q[q].append((waits, fn, s, 16))

    def coll(self, kind, groups, in_ap, out_ap, reads=(), writes=()):
        q = "pool"
        waits = self._deps(q, reads, writes)
        i = self.rnext[q]
        self.rnext[q] = (i + 1) % NRING
        s = self.ring[q][i]
        if self.rcnt[q][i] > 0:
            v = 16 * self.rcnt[q][i]
            if self.seen[q].get(id(s), 0) < v:
                self.seen[q][id(s)] = v
                waits.append((s, v))
        self.rcnt[q][i] += 1
        ev = (s, 16 * self.rcnt[q][i])
        for r in reads:
            r.r.append(ev)
        for w in writes:
            w.w = ev
            w.r = []

        def fn(e):
            return e.collective_compute(kind, ALU.bypass, replica_groups=groups, ins=[in_ap], outs=[out_ap])

        self.q[q].append((waits, fn, s, 16))

    def new_epoch(self):
        self.barrier()
        nc, es = self.nc, self.root_es
        self.epoch += 1
        k = self.epoch
        self.sem = {e: es.enter_context(nc.semaphore("s%d_%s" % (k, e))) for e in ENGS}
        self.cnt = {e: 0 for e in ENGS}
        self.seen = {e: {} for e in ENGS}
        self.ring = {e: [es.enter_context(nc.semaphore("d%d_%s%d" % (k, e, i))) for i in range(NRING)]
                     for e in self.dq}
        self.rcnt = {e: [0] * NRING for e in self.dq}
        self.rnext = {e: 0 for e in self.dq}

    def barrier(self, label=None):
        if not hasattr(self, "marks"):
            self.marks = []
        self.marks.append((self.epoch, dict(self.cnt)))
        evs = []
        for q in self.dq:
            for i in range(NRING):
                if self.rcnt[q][i] > 0:
                    evs.append((self.ring[q][i], 16 * self.rcnt[q][i]))
        for e in ENGS:
            if e != "sp" and self.cnt[e] > 0:
                evs.append((self.sem[e], self.cnt[e]))
        for e in ENGS:
            waits = []
            for s_, v in evs:
                if id(s_) == id(self.sem[e]) and e == "pe":
                    continue
                if self.seen[e].get(id(s_), 0) >= v:
                    continue
                self.seen[e][id(s_)] = v
                waits.append((s_, v))
            if waits:
                self.q[e].append((waits, None, None, 0))

    def finish(self):
        nc = self.nc
        fin = []
        for q in self.dq:
            for i in range(NRING):
                if self.rcnt[q][i] > 0:
                    fin.append((self.ring[q][i], 16 * self.rcnt[q][i]))
        for e in ENGS:
            if e != "sp" and self.cnt[e] > 0:
                fin.append((self.sem[e], self.cnt[e]))
        qs = self.q

        def run(e, lst, extra=()):
            for waits, fn, s, n in lst:
                for ws, wv in waits:
                    e.wait_ge(ws, wv)
                if fn is None:
                    continue
                ins = fn(e)
                if s is not None:
                    ins.then_inc(s, n)
            for ws, wv in extra:
                e.wait_ge(ws, wv)

        with nc.Block() as blk:
            @blk.sync
            def _(e):
                run(e, qs["sp"], fin)

            @blk.tensor
            def _(e):
                run(e, qs["pe"])

            @blk.scalar
            def _(e):
                run(e, qs["act"])

            @blk.vector
            def _(e):
                run(e, qs["dve"])

            @blk.gpsimd
            def _(e):
                run(e, qs["pool"])


def emit_mod(b, ccol_d, wada_d, bada_d, modrow_d, modcol_d):
    nc = b.nc
    with ExitStack() as es:
        old = b.es
        b.es = es
        ccol = b.sb("m_ccol", [128, 8], F32)
        cact = b.sb("m_cact", [128, 8], F32)
        one = b.sb("m_one", [1, 1], F32)
        wbuf = [b.sb("m_w%d" % i, [128, 8, 512], F32) for i in range(2)]
        brow = b.sb("m_brow", [1, 6144], F32)
        mrow = b.sb("m_mrow", [1, 6144], F32)
        mcol = b.sb("m_mcol", [128, DEPTH * 48], F32)
        pr = [b.ps("m_pr%d" % i, [1, 512]) for i in range(2)]
        pc = b.ps("m_pc", [128, 48])
        r_ccol, r_cact, r_one, r_brow, r_mrow, r_mcol, r_pc = (b.res() for _ in range(7))
        r_w = [b.res(), b.res()]
        r_pr = [b.res(), b.res()]
        r_out = b.res()

        b.dma("sp", ccol[:], ccol_d, writes=[r_ccol])
        b.op("act", lambda e: e.activation(out=cact[:], in_=ccol[:], func=AF.Silu),
             reads=[r_ccol], writes=[r_cact])
        b.op("dve", lambda e: e.memset(one[:], 1.0), writes=[r_one])
        it = 0
        for l in range(DEPTH):
            b.dma("sp", brow[:], bada_d[l:l + 1, :], writes=[r_brow])
            for nb in range(12):
                s = it % 2
                it += 1
                src = wada_d[l, :, nb * 512:(nb + 1) * 512].rearrange("(k p) n -> p k n", p=128)
                b.dma("sp", wbuf[s][:], src, writes=[r_w[s]])
                for k in range(8):
                    b.op("pe", lambda e, s=s, k=k: e.matmul(
                        pr[s][:], lhsT=cact[:, k:k + 1], rhs=wbuf[s][:, k, :],
                        start=(k == 0), stop=(k == 7)),
                        reads=[r_cact, r_w[s]], writes=[r_pr[s]], inc=(k == 7))
                b.op("dve", lambda e, s=s, nb=nb: e.tensor_tensor(
                    out=mrow[:, nb * 512:(nb + 1) * 512], in0=pr[s][:],
                    in1=brow[:, nb * 512:(nb + 1) * 512], op=ALU.add),
                    reads=[r_pr[s], r_brow], writes=[r_mrow])
            b.dma("pool", modrow_d[l:l + 1, :], mrow[:], reads=[r_mrow], writes=[r_out])
            for c in range(48):
                b.op("pe", lambda e, c=c: e.matmul(
                    pc[:, c:c + 1], lhsT=mrow[:, c * 128:(c + 1) * 128], rhs=one[:, :],
                    start=True, stop=True),
                    reads=[r_mrow, r_one], writes=[r_pc], inc=(c == 47))
            b.op("act", lambda e, l=l: e.copy(out=mcol[:, l * 48:(l + 1) * 48], in_=pc[:]),
                 reads=[r_pc], writes=[r_mcol])
        b.dma("pool", modcol_d, mcol[:], reads=[r_mcol], writes=[r_out])
        b.barrier()
        b.es = old


def build_mod():
    nc = bass.Bass("TRN2", target_bir_lowering=False)
    ccol_d = nc.dram_tensor("ccol", [128, 8], F32, kind="ExternalInput").ap()
    wada_d = nc.dram_tensor("w_ada", [DEPTH, D, 6 * D], F32, kind="ExternalInput").ap()
    bada_d = nc.dram_tensor("b_ada", [DEPTH, 6 * D], F32, kind="ExternalInput").ap()
    modrow_d = nc.dram_tensor("modrow", [DEPTH, 6 * D], F32, kind="ExternalOutput").ap()
    modcol_d = nc.dram_tensor("modcol", [128, DEPTH * 48], F32, kind="ExternalOutput").ap()
    with ExitStack() as es:
        b = B(nc, es)
        emit_mod(b, ccol_d, wada_d, bada_d, modrow_d, modcol_d)
        b.finish()
    return nc


def run_mod(c, w_ada, b_ada):
    nc = build_mod()
    in_maps = []
    for core in range(8):
        bi = core // 2
        in_maps.append({"ccol": np.ascontiguousarray(c[bi].reshape(8, 128).T),
                        "w_ada": w_ada, "b_ada": b_ada})
    res = run_bass_kernel_spmd(nc, in_maps, core_ids=list(range(8)))
    return [r["modrow"] for r in res.results], [r["modcol"] for r in res.results]


NFT = 19
P1_BLK = [(0, 512), (512, 512), (1024, 512), (1536, 332), (1868, 512), (2380, 512),
          (2892, 512), (3404, 512)]


def emit_rope_tables(b, pos_d, invf_d, cos, sin, r_tab, nt):
    with ExitStack() as es:
        old = b.es
        b.es = es
        posi = b.sb("rt_posi", [128, nt], mybir.dt.int32)
        posf = b.sb("rt_posf", [128, nt], F32)
        invf = b.sb("rt_invf", [128, 32], F32)
        ang = b.sb("rt_ang", [128, nt, 32], F32)
        u = b.sb("rt_u", [128, nt, 32], F32)
        r_pi, r_pf, r_if, r_ang, r_u = (b.res() for _ in range(5))
        b.dma("sp", posi[:], pos_d, writes=[r_pi])
        b.dma("sp", invf[:], invf_d, writes=[r_if])
        b.op("dve", lambda e: e.tensor_copy(out=posf[:], in_=posi[:]), reads=[r_pi], writes=[r_pf])
        for t in range(nt):
            b.op("dve", lambda e, t=t: e.tensor_scalar(
                out=ang[:, t, :], in0=invf[:], scalar1=posf[:, t:t + 1], scalar2=None,
                op0=ALU.mult), reads=[r_if, r_pf], writes=[r_ang])
        two_pi = float(np.float32(2.0 * np.pi))
        pi = float(np.float32(np.pi))
        ki = b.sb("rt_ki", [128, nt, 32], mybir.dt.int32)
        kf = b.sb("rt_kf", [128, nt, 32], F32)
        r_ki, r_kf = b.res(), b.res()

        def reduced_sin(dst, shift):
            b.op("dve", lambda e: e.tensor_scalar(
                out=u[:], in0=ang[:], scalar1=shift, scalar2=None, op0=ALU.add),
                reads=[r_ang], writes=[r_u])
            b.op("dve", lambda e: e.tensor_scalar(
                out=kf[:], in0=u[:], scalar1=float(1.0 / (2.0 * np.pi)), scalar2=None, op0=ALU.mult),
                reads=[r_u], writes=[r_kf])
            b.op("dve", lambda e: e.tensor_copy(out=ki[:], in_=kf[:]), reads=[r_kf], writes=[r_ki])
            b.op("dve", lambda e: e.tensor_copy(out=kf[:], in_=ki[:]), reads=[r_ki], writes=[r_kf])
            b.op("dve", lambda e: e.scalar_tensor_tensor(
                out=u[:], in0=kf[:], scalar=-two_pi, in1=u[:], op0=ALU.mult, op1=ALU.add),
                reads=[r_kf, r_u], writes=[r_u])
            b.op("dve", lambda e: e.tensor_scalar(
                out=kf[:], in0=u[:], scalar1=pi, scalar2=two_pi, op0=ALU.is_gt, op1=ALU.mult),
                reads=[r_u], writes=[r_kf])
            b.op("dve", lambda e: e.tensor_tensor(out=u[:], in0=u[:], in1=kf[:], op=ALU.subtract),
                 reads=[r_u, r_kf], writes=[r_u])
            b.op("dve", lambda e: e.tensor_scalar(
                out=kf[:], in0=u[:], scalar1=-pi, scalar2=two_pi, op0=ALU.is_lt, op1=ALU.mult),
                reads=[r_u], writes=[r_kf])
            b.op("dve", lambda e: e.tensor_tensor(out=u[:], in0=u[:], in1=kf[:], op=ALU.add),
                 reads=[r_u, r_kf], writes=[r_u])
            b.op("dve", lambda e: e.tensor_scalar(
                out=u[:], in0=u[:], scalar1=pi, scalar2=-pi, op0=ALU.min, op1=ALU.max),
                reads=[r_u], writes=[r_u])
            b.op("act", lambda e: e.activation(out=dst[:], in_=u[:], func=AF.Sin),
                 reads=[r_u], writes=[r_tab])

        reduced_sin(sin, 0.0)
        reduced_sin(cos, float(np.float32(np.pi / 2)))
        b.barrier()
        b.es = old


def emit_p1(b, l, nt, x_d, win_d, modcol_d, ibias_d, fbias_d, identf_d,
            cos, sin, r_tab, featT_d, tokA_d, tokF_d, gT_d, r_xd, r_out):
    nc = b.nc
    with ExitStack() as es:
        old = b.es
        b.es = es
        wsb = b.sb("p1_w", [128, 8, INW], BF16)
        wst = [b.sb("p1_wst%d" % i, [128, 8, 512], F32) for i in range(2)]
        identf = b.sb("p1_idf", [128, 128], F32)
        identb = b.sb("p1_idb", [128, 128], BF16)
        mcol = b.sb("p1_mcol", [128, 48], F32)
        sc1 = b.sb("p1_sc1", [128, 8], F32)
        bias8 = b.sb("p1_bias8", [128, 8], F32)
        xt = [b.sb("p1_x%d" % i, [128, D], F32) for i in range(2)]
        hT = [b.sb("p1_hT%d" % i, [128, 8, 128], BF16) for i in range(2)]
        rin = [b.sb("p1_rin%d" % i, [128, 512], BF16) for i in range(5)]
        tmpa = b.sb("p1_tmpa", [128, 256], F32)
        tmpb = b.sb("p1_tmpb", [128, 256], F32)
        stA = [b.sb("p1_stA%d" % i, [128, 1792], BF16) for i in range(2)]
        stF = [b.sb("p1_stF%d" % i, [128, 12], F32) for i in range(2)]
        stT = [b.sb("p1_stT%d" % i, [128, NFT, 128], BF16) for i in range(2)]
        stG = [b.sb("p1_stG%d" % i, [8, 128], F32) for i in range(2)]
        r_stG = [b.res(), b.res()]
        ptr = [b.ps("p1_ptr%d" % i, [128, 512]) for i in range(2)]
        pin = [b.ps("p1_pin%d" % i, [128, 512]) for i in range(4)]
        pto = [b.ps("p1_pto%d" % i, [128, 1024], BF16) for i in range(2)]

        r_w, r_idf, r_idb, r_mcol, r_sc1, r_b8, r_ta, r_tb = (b.res() for _ in range(8))
        r_wst = [b.res(), b.res()]
        r_x = [b.res(), b.res()]
        r_hT = [b.res(), b.res()]
        r_rin = [b.res() for _ in range(5)]
        r_stA = [b.res(), b.res()]
        r_stF = [b.res(), b.res()]
        r_stT = [b.res(), b.res()]
        r_ptr = [b.res(), b.res()]
        r_pin = [b.res() for _ in range(4)]
        r_pto = [b.res(), b.res()]

        b.dma("sp", identf[:], identf_d, writes=[r_idf])
        b.op("dve", lambda e: e.tensor_copy(out=identb[:], in_=identf[:]), reads=[r_idf], writes=[r_idb])
        b.dma("sp", mcol[:], modcol_d[:, l * 48:(l + 1) * 48], writes=[r_mcol])
        b.op("dve", lambda e: e.tensor_scalar(out=sc1[:], in0=mcol[:, 8:16], scalar1=1.0, scalar2=None,
                                              op0=ALU.add), reads=[r_mcol], writes=[r_sc1])
        b.dma("sp", bias8[:, 0:4], ibias_d[l:l + 1, :].to_broadcast([128, 4]), writes=[r_b8])
        b.dma("sp", bias8[:, 4:8], fbias_d[l:l + 1, :].to_broadcast([128, 4]), writes=[r_b8])
        pieces = []
        for (s0, d0, n) in ((0, 0, 1860), (3908, 1860, 8), (1860, 1868, 2048)):
            o = 0
            while o < n:
                m = min(512, n - o)
                pieces.append((s0 + o, d0 + o, m))
                o += m
        for i, (s0, d0, m) in enumerate(pieces):
            s = i % 2
            src = win_d[l, :, s0:s0 + m].rearrange("(k p) n -> p k n", p=128)
            b.dma("sp", wst[s][:, :, 0:m], src, writes=[r_wst[s]])
            eng = "pool" if i % 2 == 0 else "act"
            if eng == "pool":
                b.op("pool", lambda e, s=s, d0=d0, m=m: e.tensor_copy(
                    out=wsb[:, :, d0:d0 + m], in_=wst[s][:, :, 0:m]), reads=[r_wst[s]], writes=[r_w])
            else:
                b.op("act", lambda e, s=s, d0=d0, m=m: e.copy(
                    out=wsb[:, :, d0:d0 + m], in_=wst[s][:, :, 0:m]), reads=[r_wst[s]], writes=[r_w])

        def rope(t, src, H, dst, r_src, r_dst, col0=0):
            sv = src.rearrange("p (h two d) -> p h two d", two=2, d=32)
            dv = dst.rearrange("p (h two d) -> p h two d", two=2, d=32)
            if isinstance(cos, (list, tuple)):
                cj, tj = cos[t // NT], t % NT
                sj = sin[t // NT]
                rtab = r_tab[t // NT]
            else:
                cj, sj, tj, rtab = cos, sin, t, r_tab
            cb = cj[:, tj:tj + 1, :].to_broadcast([128, H, 32])
            sb_ = sj[:, tj:tj + 1, :].to_broadcast([128, H, 32])
            ta = tmpa[:, 0:H * 32].rearrange("p (h d) -> p h d", d=32)
            tb = tmpb[:, 0:H * 32].rearrange("p (h d) -> p h d", d=32)
            b.op("dve", lambda e: e.tensor_tensor(out=ta, in0=sv[:, :, 0, :], in1=cb, op=ALU.mult),
                 reads=[r_src, rtab], writes=[r_ta])
            b.op("dve", lambda e: e.tensor_tensor(out=tb, in0=sv[:, :, 1, :], in1=sb_, op=ALU.mult),
                 reads=[r_src, rtab], writes=[r_tb])
            b.op("dve", lambda e: e.tensor_tensor(out=dv[:, :, 0, :], in0=ta, in1=tb, op=ALU.subtract),
                 reads=[r_ta, r_tb], writes=[r_dst])
            b.op("dve", lambda e: e.tensor_tensor(out=ta, in0=sv[:, :, 1, :], in1=cb, op=ALU.mult),
                 reads=[r_src, rtab], writes=[r_ta])
            b.op("dve", lambda e: e.tensor_tensor(out=tb, in0=sv[:, :, 0, :], in1=sb_, op=ALU.mult),
                 reads=[r_src, rtab], writes=[r_tb])
            b.op("dve", lambda e: e.tensor_tensor(out=dv[:, :, 1, :], in0=ta, in1=tb, op=ALU.add),
                 reads=[r_ta, r_tb], writes=[r_dst])

        pin_i = 0
        pto_i = 0
        pending = []
        pgt = pto[1][:, 0:256].bitcast(F32)
        r_pgt = r_pto[1]
        for t in range(nt):
            s = t % 2
            b.dma("sp", xt[s][:], x_d[t * 128:(t + 1) * 128, :], reads=[r_xd], writes=[r_x[s]])
            for c in range(8):
                b.op("pe", lambda e, s=s, c=c: e.transpose(
                    out=ptr[c // 4][:, (c % 4) * 128:(c % 4 + 1) * 128],
                    in_=xt[s][:, c * 128:(c + 1) * 128], identity=identf[:]),
                    reads=[r_x[s], r_idf], writes=[r_ptr[c // 4]], inc=(c % 4 == 3))
            for c in range(8):
                b.op("act", lambda e, s=s, c=c: e.activation(
                    out=hT[s][:, c, :], in_=ptr[c // 4][:, (c % 4) * 128:(c % 4 + 1) * 128],
                    func=AF.Identity, scale=sc1[:, c:c + 1], bias=mcol[:, c:c + 1]),
                    reads=[r_ptr[c // 4], r_sc1, r_mcol], writes=[r_hT[s]])
            A = stA[s]
            Fs = stF[s]
            T = stT[s]
            for bi, (c0, n) in enumerate(P1_BLK):
                pi = pin_i % 4
                pin_i += 1
                P = pin[pi]
                for k in range(8):
                    b.op("pe", lambda e, s=s, k=k, c0=c0, n=n, P=P: e.matmul(
                        P[:, 0:n], lhsT=hT[s][:, k, :], rhs=wsb[:, k, c0:c0 + n],
                        start=(k == 0), stop=(k == 7)),
                        reads=[r_hT[s], r_w], writes=[r_pin[pi]], inc=(k == 7))
                rp = r_pin[pi]
                if bi == 0 or bi == 1:
                    rope(t, P[:, 0:512], 8, rin[bi][:, 0:512], rp, r_rin[bi])
                elif bi == 2:
                    b.op("act", lambda e, P=P, A=A: e.copy(out=A[:, 0:512], in_=P[:, 0:512]),
                         reads=[rp], writes=[r_stA[s]])
                elif bi == 3:
                    rope(t, P[:, 0:320], 5, rin[2][:, 0:320], rp, r_rin[2])
                    b.op("dve", lambda e: e.tensor_copy(out=rin[2][:, 320:384], in_=rin[2][:, 256:320]),
                         reads=[r_rin[2]], writes=[r_rin[2]])
                    b.op("dve", lambda e, P=P, Fs=Fs: e.tensor_copy(out=Fs[:, 0:4], in_=P[:, 320:324]),
                         reads=[rp], writes=[r_stF[s]])
                    b.op("dve", lambda e, P=P, Fs=Fs: e.tensor_tensor(
                        out=Fs[:, 4:12], in0=P[:, 324:332], in1=bias8[:], op=ALU.add),
                        reads=[rp, r_b8], writes=[r_stF[s]])
                elif bi == 4:
                    rope(t, P[:, 0:512], 8, rin[3][:, 0:512], rp, r_rin[3])
                    b.op("pool", lambda e, A=A: e.tensor_copy(out=A[:, 512:768], in_=rin[3][:, 256:512]),
                         reads=[r_rin[3]], writes=[r_stA[s]])
                elif bi == 5:
                    b.op("act", lambda e, P=P, A=A: e.copy(out=A[:, 768:1024], in_=P[:, 0:256]),
                         reads=[rp], writes=[r_stA[s]])
                    b.op("act", lambda e, P=P, A=A: e.activation(out=A[:, 1024:1280], in_=P[:, 256:512],
                                                                 func=AF.Silu),
                         reads=[rp], writes=[r_stA[s]])
                elif bi == 6:
                    b.op("act", lambda e, P=P: e.copy(out=rin[4][:, 0:512], in_=P[:, 0:512]),
                         reads=[rp], writes=[r_rin[4]])
                else:
                    b.op("act", lambda e, P=P, A=A: e.copy(out=A[:, 1280:1536], in_=P[:, 0:256]),
                         reads=[rp], writes=[r_stA[s]])
                    b.op("act", lambda e, P=P, A=A: e.activation(out=A[:, 1536:1792], in_=P[:, 256:512],
                                                                 func=AF.Sigmoid),
                         reads=[rp], writes=[r_stA[s]])
                if bi == 0:
                    srcs = [(rin[0], r_rin[0], i * 128, i) for i in range(4)]
                elif bi == 1:
                    srcs = [(rin[1], r_rin[1], i * 128, 4 + i) for i in range(4)]
                elif bi == 3:
                    srcs = [(rin[2], r_rin[2], 0, 8), (rin[2], r_rin[2], 128, 9), (rin[2], r_rin[2], 256, 10)]
                elif bi == 4:
                    srcs = [(rin[3], r_rin[3], i * 128, 11 + i) for i in range(4)]
                elif bi == 6:
                    srcs = [(rin[4], r_rin[4], i * 128, 15 + i) for i in range(4)]
                else:
                    srcs = []
                if srcs:
                    def tr_job(srcs=srcs, T=T, s=s):
                        nonlocal pto_i
                        po = pto_i % 2
                        pto_i += 1
                        for i, (buf, rb, c, slot) in enumerate(srcs):
                            b.op("pe", lambda e, buf=buf, c=c, i=i, po=po: e.transpose(
                                out=pto[po][:, i * 128:(i + 1) * 128], in_=buf[:, c:c + 128], identity=identb[:]),
                                reads=[rb, r_idb], writes=[r_pto[po]], inc=(i == len(srcs) - 1))
                        s0 = srcs[0][3]
                        n_ = len(srcs)
                        b.op("act", lambda e, po=po, s0=s0, n_=n_, T=T: e.copy(
                            out=T[:, s0:s0 + n_, :], in_=pto[po][:, 0:n_ * 128].rearrange("p (n k) -> p n k", k=128)),
                            reads=[r_pto[po]], writes=[r_stT[s]])
                    pending.append(tr_job)
                while len(pending) > 2:
                    pending.pop(0)()
            def out_job(t=t, s=s, A=A, Fs=Fs, T=T):
                b.dma("pool", tokA_d[t * 128:(t + 1) * 128, :], A[:], reads=[r_stA[s]], writes=[r_out])
                b.dma("pool", tokF_d[t * 128:(t + 1) * 128, :], Fs[:], reads=[r_stF[s]], writes=[r_out])
                b.op("pe", lambda e, Fs=Fs: e.transpose(out=pgt[0:8, 0:128], in_=Fs[:, 4:12], identity=identf[:]),
                     reads=[r_stF[s], r_idf], writes=[r_pgt])
                b.op("act", lambda e, s=s: e.copy(out=stG[s][:], in_=pgt[0:8, 0:128]),
                     reads=[r_pgt], writes=[r_stG[s]])
                b.dma("pool", gT_d[t], stG[s][:], reads=[r_stG[s]], writes=[r_out])
                b.dma("pool", featT_d[t], T[:], reads=[r_stT[s]], writes=[r_out])
            pending.append(out_job)
        while pending:
            pending.pop(0)()
        b.barrier()
        b.es = old


def inv_freq_table():
    half = 32
    inv = (np.float32(10000.0) ** (-(np.arange(half, dtype=np.float32) / np.float32(half)))).astype(np.float32)
    return np.ascontiguousarray(np.broadcast_to(inv[None, :], (128, half))).astype(np.float32)


def build_p1(l, nt):
    nc = bass.Bass("TRN2", target_bir_lowering=False)
    x_d = nc.dram_tensor("x", [nt * 128, D], F32, kind="ExternalInput").ap()
    pos_d = nc.dram_tensor("pos", [128, nt], mybir.dt.int32, kind="ExternalInput").ap()
    invf_d = nc.dram_tensor("invf", [128, 32], F32, kind="ExternalInput").ap()
    identf_d = nc.dram_tensor("identf", [128, 128], F32, kind="ExternalInput").ap()
    win_d = nc.dram_tensor("w_in", [DEPTH, D, INW], F32, kind="ExternalInput").ap()
    modcol_d = nc.dram_tensor("modcol", [128, DEPTH * 48], F32, kind="ExternalInput").ap()
    ib_d = nc.dram_tensor("i_bias", [DEPTH, 4], F32, kind="ExternalInput").ap()
    fb_d = nc.dram_tensor("f_bias", [DEPTH, 4], F32, kind="ExternalInput").ap()
    featT_d = nc.dram_tensor("featT", [nt, 128, NFT, 128], BF16, kind="ExternalOutput").ap()
    tokA_d = nc.dram_tensor("tokA", [nt * 128, 1792], BF16, kind="ExternalOutput").ap()
    tokF_d = nc.dram_tensor("tokF", [nt * 128, 12], F32, kind="ExternalOutput").ap()
    gT_d = nc.dram_tensor("gT", [nt, 8, 128], F32, kind="ExternalOutput").ap()
    with ExitStack() as es, nc.allow_low_precision("bf16 matmul operands, fp32 accumulation"):
        b = B(nc, es)
        cos = b.sb("cos", [128, nt, 32], F32)
        sin = b.sb("sin", [128, nt, 32], F32)
        r_tab = b.res()
        emit_rope_tables(b, pos_d, invf_d, cos, sin, r_tab, nt)
        emit_p1(b, l, nt, x_d, win_d, modcol_d, ib_d, fb_d, identf_d, cos, sin, r_tab,
                featT_d, tokA_d, tokF_d, gT_d, b.res(), b.res())
        b.finish()
    return nc


NIT = 18
TOPK = 256


def gidx(g):
    return (g % 2) * NT + g // 2


def emit_p2a(b, nslots, featT_all, tokA_all, featT_own, tokF_own, vis_d, pw_d, identf_d, o_d, r_in, r_out,
             parities=None):
    nc = b.nc
    NACT = 0
    with ExitStack() as es:
        old = b.es
        b.es = es
        kT = b.sb("a_kT", [128, 4, SEQ], BF16)
        ikT = b.sb("a_ikT", [128, SEQ], BF16)
        Va = b.sb("a_Va", [128, 32, 8, 65], BF16)
        iw = b.sb("a_iw", [128, NT, 4], F32)
        vis = b.sb("a_vis", [128, 256], F32)
        pw = b.sb("a_pw", [128, NIT + 1], F32)
        identf = b.sb("a_idf", [128, 128], F32)
        identb = b.sb("a_idb", [128, 128], BF16)
        Ib = [b.sb("a_I%d" % i, [128, SEQ], F32) for i in range(3)]
        rl = [b.sb("a_rl%d" % i, [128, 512], F32) for i in range(2)]
        Mbs = [b.sb("a_Mb%d" % i, [128, SEQ], BF16) for i in range(3)]
        MT = [b.sb("a_MT%d" % i, [128, 32, 128], BF16) for i in range(3)]
        E = [b.sb("a_E%d" % i, [128, 512], BF16) for i in range(3)]
        qpad = [b.sb("a_qpad%d" % i, [128, 8, 128], BF16) for i in range(2)]
        iqpad = [b.sb("a_iqpad%d" % i, [128, 4, 128], BF16) for i in range(3)]
        r_qpad = [b.res(), b.res()]
        r_iqpad = [b.res(), b.res(), b.res()]
        sms = [b.sb("a_sm%d" % i, [128, 16], F32) for i in range(3)]
        rcp = b.sb("a_rcp", [128, 8], F32)
        deltas = [b.sb("a_delta%d" % i, [128, NIT + 1], F32) for i in range(3)]
        ndeltas = [b.sb("a_ndelta%d" % i, [128, NIT + 1], F32) for i in range(3)]
        osb = [b.sb("a_o%d" % i, [128, 512], BF16) for i in range(2)]
        pI = [b.ps("a_pI%d" % i, [128, 512]) for i in range(2)]
        pS = [b.ps("a_pS%d" % i, [128, 512]) for i in range(3)]
        pO = [b.ps("a_pO%d" % i, [128, 512]) for i in range(2)]
        pMT = b.ps("a_pMT", [128, 1024], BF16)

        (r_kT, r_ikT, r_Va, r_qT, r_iqT, r_iw, r_vis, r_pw, r_idf, r_idb, r_pMT, r_vst, r_rcp) = (
            b.res() for _ in range(13))
        r_I = [b.res(), b.res(), b.res()]
        r_rl = [b.res(), b.res()]
        r_Mb = [b.res(), b.res(), b.res()]
        r_MT = [b.res(), b.res(), b.res()]
        r_E = [b.res() for _ in range(3)]
        r_sm = [b.res(), b.res(), b.res()]
        r_delta = [b.res(), b.res(), b.res()]
        r_osb = [b.res(), b.res()]
        r_pI = [b.res(), b.res()]
        r_pS = [b.res() for _ in range(3)]
        r_pO = [b.res(), b.res()]

        b.dma("sp", identf[:], identf_d, writes=[r_idf])
        b.op("dve", lambda e: e.tensor_copy(out=identb[:], in_=identf[:]), reads=[r_idf], writes=[r_idb])
        b.dma("sp", pw[:], pw_d, writes=[r_pw])
        ngl = 2 * nslots
        for g in range(ngl):
            gi = gidx(g)
            b.dma("sp", kT[:, :, g * 128:(g + 1) * 128], featT_all[gi, :, 4:8, :], reads=[r_in], writes=[r_kT])
            b.dma("sp", ikT[:, g * 128:(g + 1) * 128], featT_all[gi, :, 10, :], reads=[r_in], writes=[r_ikT])
        b.op("pool", lambda e: e.memset(Va[:, :, :, 64:65], 1.0), writes=[r_Va])
        vst = b.sb("a_vst", [128, 8, 512], BF16)
        for g0 in range(0, ngl, 8):
            n = min(8, ngl - g0)
            for i in range(n):
                gi = gidx(g0 + i)
                b.dma("sp", vst[:, i, :], tokA_all[gi * 128:(gi + 1) * 128, 0:512], reads=[r_in], writes=[r_vst])
            b.op("pool", lambda e, g0=g0, n=n: e.tensor_copy(
                out=Va[:, g0:g0 + n, :, 0:64],
                in_=vst[:, 0:n, :].rearrange("p g (h d) -> p g h d", d=64)),
                reads=[r_vst], writes=[r_Va])

        for i in range(2):
            b.op("pool", lambda e, i=i: e.memset(qpad[i][:], 0.0), writes=[r_qpad[i]])
        for i in range(3):
            b.op("pool", lambda e, i=i: e.memset(iqpad[i][:], 0.0), writes=[r_iqpad[i]])
        LO, HI, RNG, CAND, CNT, VV, TT, NCD, SS, SG = range(10)
        MBIG = 30000.0
        cnts = {"i": 0, "s": 0}

        def sel_gen(m):
            L = (2 * m + 2) * 128
            nkb = 2 * m + 2
            s = m % 3
            I = Ib[s]
            sm = sms[s]
            delta = deltas[s]
            ndelta = ndeltas[s]
            Mb = Mbs[s]
            rsm = r_sm[s]
            rdl = r_delta[s]
            for half in range(2):
                hsl = slice(half * 64, half * 64 + 64)
                b.dma("sp", iqpad[s][hsl, half::2, :], featT_own[m, hsl, 8:10, :], reads=[r_in],
                      writes=[r_iqpad[s]])
            for kg in range((L + 511) // 512):
                w = min(512, L - kg * 512)
                for h in range(4):
                    pi = cnts["i"] % 2
                    cnts["i"] += 1
                    hs = slice((h % 2) * 64, (h % 2) * 64 + 64)
                    b.op("pe", lambda e, pi=pi, h=h, kg=kg, w=w: e.matmul(
                        pI[pi][:, 0:w], lhsT=iqpad[s][:, h, :],
                        rhs=ikT[:, kg * 512:kg * 512 + w], start=True, stop=True),
                        reads=[r_iqpad[s], r_ikT], writes=[r_pI[pi]])
                    b.op("act", lambda e, pi=pi, w=w: e.activation(out=rl[pi][:, 0:w], in_=pI[pi][:, 0:w],
                                                                    func=AF.Relu),
                         reads=[r_pI[pi]], writes=[r_rl[pi]])
                    dst = I[:, kg * 512:kg * 512 + w]
                    if h == 0:
                        b.op("dve", lambda e, pi=pi, w=w, dst=dst: e.tensor_scalar(
                            out=dst, in0=rl[pi][:, 0:w], scalar1=iw[:, m, 0:1], scalar2=None, op0=ALU.mult),
                            reads=[r_rl[pi], r_iw], writes=[r_I[s]])
                    else:
                        b.op("dve", lambda e, pi=pi, w=w, dst=dst, h=h: e.scalar_tensor_tensor(
                            out=dst, in0=rl[pi][:, 0:w], scalar=iw[:, m, h:h + 1], in1=dst,
                            op0=ALU.mult, op1=ALU.add),
                            reads=[r_rl[pi], r_iw, r_I[s]], writes=[r_I[s]])
                    yield
            if m == 0:
                b.op("dve", lambda e: e.tensor_tensor(out=I[:, L - 256:L], in0=I[:, L - 256:L],
                                                      in1=vis[:], op=ALU.add),
                     reads=[r_I[s], r_vis], writes=[r_I[s]])
                b.op("dve", lambda e: e.memset(sm[:, TT:TT + 1], 0.5 * NEG), writes=[rsm])
            else:
                b.op("dve", lambda e: e.tensor_reduce(out=sm[:, LO:LO + 1], in_=I[:, 0:L - 256],
                                                      axis=AX.X, op=ALU.min),
                     reads=[r_I[s]], writes=[rsm])
                b.op("dve", lambda e: e.tensor_tensor(out=I[:, L - 256:L], in0=I[:, L - 256:L],
                                                      in1=vis[:], op=ALU.add),
                     reads=[r_I[s], r_vis], writes=[r_I[s]])
                b.op("dve", lambda e: e.tensor_reduce(out=sm[:, HI:HI + 1], in_=I[:, 0:L],
                                                      axis=AX.X, op=ALU.max),
                     reads=[r_I[s]], writes=[rsm])
                yield
                b.op("dve", lambda e: e.tensor_tensor(out=sm[:, RNG:RNG + 1], in0=sm[:, HI:HI + 1],
                                                      in1=sm[:, LO:LO + 1], op=ALU.subtract),
                     reads=[rsm], writes=[rsm])
                b.op("dve", lambda e: e.tensor_scalar(out=delta[:], in0=pw[:], scalar1=sm[:, RNG:RNG + 1],
                                                      scalar2=None, op0=ALU.mult),
                     reads=[rsm, r_pw], writes=[rdl])
                b.op("dve", lambda e: e.tensor_scalar(out=ndelta[:], in0=delta[:], scalar1=-1.0,
                                                      scalar2=None, op0=ALU.mult),
                     reads=[rdl], writes=[rdl])
                b.op("dve", lambda e: e.tensor_tensor(out=sm[:, NCD:NCD + 1], in0=ndelta[:, 0:1],
                                                      in1=sm[:, LO:LO + 1], op=ALU.subtract),
                     reads=[rsm, rdl], writes=[rsm])
                yield
                for n in range(NACT):
                    b.op("act", lambda e: e.activation(
                        out=Mb[:, 0:L], in_=I[:, 0:L], func=AF.Sign, bias=sm[:, NCD:NCD + 1],
                        accum_out=sm[:, SS:SS + 1]),
                        reads=[r_I[s], rsm], writes=[r_Mb[s], rsm])
                    b.op("act", lambda e: e.activation(
                        out=sm[:, SG:SG + 1], in_=sm[:, SS:SS + 1], func=AF.Sign, bias=float(L - (2 * TOPK - 1))),
                        reads=[rsm], writes=[rsm])
                    b.op("act", lambda e, n=n: e.activation(
                        out=sm[:, NCD:NCD + 1], in_=sm[:, SG:SG + 1], func=AF.Identity,
                        scale=ndelta[:, n + 1:n + 2], bias=sm[:, NCD:NCD + 1]),
                        reads=[rsm, rdl], writes=[rsm])
                    yield
                b.op("dve", lambda e: e.tensor_scalar(out=sm[:, CAND:CAND + 1], in0=sm[:, NCD:NCD + 1],
                                                      scalar1=-1.0, scalar2=None, op0=ALU.mult),
                     reads=[rsm], writes=[rsm])
                for n in range(NACT, NIT):
                    b.op("dve", lambda e: e.tensor_scalar(
                        out=Mb[:, 0:L], in0=I[:, 0:L], scalar1=sm[:, CAND:CAND + 1], scalar2=0.0,
                        op0=ALU.is_ge, op1=ALU.add, accum_out=sm[:, CNT:CNT + 1]),
                        reads=[r_I[s], rsm], writes=[r_Mb[s], rsm])
                    b.op("dve", lambda e: e.tensor_scalar(
                        out=sm[:, VV:VV + 1], in0=sm[:, CNT:CNT + 1], scalar1=TOPK - 0.5, scalar2=0.5,
                        op0=ALU.is_ge, op1=ALU.subtract),
                        reads=[rsm], writes=[rsm])
                    b.op("dve", lambda e, n=n: e.scalar_tensor_tensor(
                        out=sm[:, CAND:CAND + 1], in0=sm[:, VV:VV + 1], scalar=delta[:, n:n + 1],
                        in1=sm[:, CAND:CAND + 1], op0=ALU.mult, op1=ALU.add),
                        reads=[rsm, rdl], writes=[rsm])
                    yield
                b.op("dve", lambda e: e.tensor_tensor(out=sm[:, TT:TT + 1], in0=sm[:, CAND:CAND + 1],
                                                      in1=delta[:, NIT:NIT + 1], op=ALU.subtract),
                     reads=[rsm, rdl], writes=[rsm])
            b.op("dve", lambda e: e.tensor_scalar(
                out=Mb[:, 0:L], in0=I[:, 0:L], scalar1=sm[:, TT:TT + 1], scalar2=None, op0=ALU.is_ge),
                reads=[r_I[s], rsm], writes=[r_Mb[s]])
            yield
            MTs = MT[s]
            for k0 in range(0, nkb, 8):
                n = min(8, nkb - k0)
                for i in range(n):
                    b.op("pe", lambda e, i=i, k0=k0: e.transpose(
                        out=pMT[:, i * 128:(i + 1) * 128], in_=Mb[:, (k0 + i) * 128:(k0 + i + 1) * 128],
                        identity=identb[:]),
                        reads=[r_Mb[s], r_idb], writes=[r_pMT], inc=(i == n - 1))
                b.op("act", lambda e, k0=k0, n=n: e.activation(
                    out=MTs[:, k0:k0 + n, :], in_=pMT[:, 0:n * 128].rearrange("p (n k) -> p n k", k=128),
                    func=AF.Identity, scale=MBIG, bias=-MBIG),
                    reads=[r_pMT], writes=[r_MT[s]])
                yield

        def att_gen(m):
            nkb = 2 * m + 2
            s = m % 2
            s3 = m % 3
            MTs = MT[s3]
            O = osb[s]
            for half in range(2):
                hsl = slice(half * 64, half * 64 + 64)
                b.dma("sp", qpad[s][hsl, half::2, :], featT_own[m, hsl, 0:4, :], reads=[r_in],
                      writes=[r_qpad[s]])
            units = [(h, k0, min(4, nkb - k0)) for h in range(8) for k0 in range(0, nkb, 4)]

            def emit_pv(u, si):
                h, k0, n = u
                p = h // 2
                po = pO[h // 4]
                r_po = r_pO[h // 4]
                oc = (h % 4) * 65
                for i in range(n):
                    kb = k0 + i
                    b.op("pe", lambda e, si=si, i=i, kb=kb, h=h, po=po, oc=oc: e.matmul(
                        po[:, oc:oc + 65], lhsT=E[si][:, i * 128:(i + 1) * 128], rhs=Va[:, kb, h, :],
                        start=(kb == 0), stop=(kb == nkb - 1)),
                        reads=[r_E[si], r_Va], writes=[r_po], inc=(i == n - 1))
                if k0 + n == nkb:
                    b.op("dve", lambda e, po=po, oc=oc, h=h: e.reciprocal(out=rcp[:, h:h + 1],
                                                                           in_=po[:, oc + 64:oc + 65]),
                         reads=[r_po], writes=[r_rcp])
                    b.op("dve", lambda e, po=po, oc=oc, h=h: e.tensor_scalar(
                        out=O[:, h * 64:(h + 1) * 64], in0=po[:, oc:oc + 64], scalar1=rcp[:, h:h + 1],
                        scalar2=None, op0=ALU.mult),
                        reads=[r_po, r_rcp], writes=[r_osb[s]])

            prev = None
            for u in units:
                h, k0, n = u
                p = h // 2
                si = cnts["s"] % 3
                cnts["s"] += 1
                b.op("pe", lambda e, si=si, k0=k0, n=n: e.matmul(
                    pS[si][:, 0:n * 128], lhsT=identb[:, :],
                    rhs=MTs[:, k0:k0 + n, :].rearrange("p n k -> p (n k)"), start=True, stop=False),
                    reads=[r_idb, r_MT[s3]], writes=[r_pS[si]], inc=False)
                for i in range(n):
                    kb = k0 + i
                    b.op("pe", lambda e, si=si, i=i, kb=kb, p=p, h=h, n=n: e.matmul(
                        pS[si][:, i * 128:(i + 1) * 128], lhsT=kT[:, p, kb * 128:(kb + 1) * 128],
                        rhs=qpad[s][:, h, :], start=False, stop=(i == n - 1)),
                        reads=[r_kT, r_qpad[s]], writes=[r_pS[si]], inc=(i == n - 1))
                b.op("act", lambda e, si=si, n=n: e.activation(
                    out=E[si][:, 0:n * 128], in_=pS[si][:, 0:n * 128], func=AF.Exp, scale=0.125),
                    reads=[r_pS[si]], writes=[r_E[si]])
                if prev is not None:
                    emit_pv(*prev)
                prev = (u, si)
                yield
            emit_pv(*prev)
            b.dma("pool", o_d[m * 128:(m + 1) * 128, 0:512], O[:], reads=[r_osb[s]], writes=[r_out])

        plist = parities if parities is not None else [(featT_own, tokF_own, vis_d, o_d)]

        def step(gen):
            try:
                next(gen)
                return True
            except StopIteration:
                return False

        for (featT_own, tokF_own, vis_d, o_d) in plist:
            b.dma("sp", vis[:], vis_d, writes=[r_vis])
            b.dma("sp", iw[:], tokF_own[:, 0:4].rearrange("(t p) c -> p t c", p=128), reads=[r_in], writes=[r_iw])
            for _ in sel_gen(0):
                pass
            cur = sel_gen(1) if nslots > 1 else None
            for m in range(nslots):
                A = att_gen(m)
                nxt = sel_gen(m + 2) if m + 2 < nslots else None
                a_live = True
                while a_live:
                    a_live = step(A)
                    if cur is not None and not step(cur):
                        cur = None
                    if nxt is not None and cur is not None:
                        if not step(nxt):
                            nxt = None
                while cur is not None:
                    if not step(cur):
                        cur = None
                cur = nxt
        b.barrier()
        b.es = old


def vis_table(j):
    v = np.zeros((128, 256), np.float32)
    diag = np.zeros((128, 128), np.float32)
    diag[0:64, 64:128] = NEG
    if j == 0:
        v[:, 0:128] = diag
        v[:, 128:256] = NEG
    else:
        v[:, 128:256] = diag
    return v


def pw_table():
    return np.ascontiguousarray(np.broadcast_to(
        (0.5 ** np.arange(1, NIT + 2, dtype=np.float64)).astype(np.float32)[None, :], (128, NIT + 1)))


def build_p2a(nslots):
    nc = bass.Bass("TRN2", target_bir_lowering=False)
    featT_all = nc.dram_tensor("featT_all", [2 * NT, 128, NFT, 128], BF16, kind="ExternalInput").ap()
    tokA_all = nc.dram_tensor("tokA_all", [2 * TOK, 1792], BF16, kind="ExternalInput").ap()
    featT_own = nc.dram_tensor("featT_own", [NT, 128, NFT, 128], BF16, kind="ExternalInput").ap()
    tokF_own = nc.dram_tensor("tokF_own", [TOK, 12], F32, kind="ExternalInput").ap()
    vis_d = nc.dram_tensor("vis", [128, 256], F32, kind="ExternalInput").ap()
    pw_d = nc.dram_tensor("pw", [128, NIT + 1], F32, kind="ExternalInput").ap()
    identf_d = nc.dram_tensor("identf", [128, 128], F32, kind="ExternalInput").ap()
    o_d = nc.dram_tensor("o", [TOK, D], BF16, kind="ExternalOutput").ap()
    with ExitStack() as es, nc.allow_low_precision("bf16 matmul operands, fp32 accumulation"):
        b = B(nc, es)
        emit_p2a(b, nslots, featT_all, tokA_all, featT_own, tokF_own, vis_d, pw_d, identf_d, o_d,
                 b.res(), b.res())
        b.finish()
    return nc


def gen_head_norm(b, src, r_src, gate, r_gate, dst, r_dst, tmp, st, r_tmp, r_st):
    b.op("dve", lambda e: e.tensor_reduce(out=st[:, 0:4], in_=src[:], axis=AX.X, op=ALU.add),
         reads=[r_src], writes=[r_st])
    yield
    b.op("dve", lambda e: e.tensor_scalar(out=st[:, 0:4], in0=st[:, 0:4], scalar1=1.0 / 64, scalar2=None,
                                          op0=ALU.mult), reads=[r_st], writes=[r_st])
    yield
    b.op("dve", lambda e: e.tensor_tensor(out=src[:], in0=src[:],
                                          in1=st[:, 0:4].unsqueeze(2).to_broadcast([128, 4, 64]),
                                          op=ALU.subtract), reads=[r_src, r_st], writes=[r_src])
    yield
    b.op("dve", lambda e: e.tensor_tensor(out=tmp[:], in0=src[:], in1=src[:], op=ALU.mult),
         reads=[r_src], writes=[r_tmp])
    yield
    b.op("dve", lambda e: e.tensor_reduce(out=st[:, 4:8], in_=tmp[:], axis=AX.X, op=ALU.add),
         reads=[r_tmp], writes=[r_st])
    yield
    b.op("dve", lambda e: e.tensor_scalar(out=st[:, 4:8], in0=st[:, 4:8], scalar1=1.0 / 64, scalar2=LN_EPS,
                                          op0=ALU.mult, op1=ALU.add), reads=[r_st], writes=[r_st])
    yield
    b.op("act", lambda e: e.activation(out=st[:, 8:12], in_=st[:, 4:8], func=AF.Sqrt),
         reads=[r_st], writes=[r_st])
    yield
    b.op("dve", lambda e: e.reciprocal(out=st[:, 12:16], in_=st[:, 8:12]), reads=[r_st], writes=[r_st])
    yield
    if gate is None:
        b.op("dve", lambda e: e.tensor_tensor(
            out=dst.rearrange("p (h d) -> p h d", d=64), in0=src[:],
            in1=st[:, 12:16].unsqueeze(2).to_broadcast([128, 4, 64]), op=ALU.mult),
            reads=[r_src, r_st], writes=[r_dst])
        yield
    else:
        b.op("dve", lambda e: e.tensor_tensor(
            out=tmp[:], in0=src[:], in1=st[:, 12:16].unsqueeze(2).to_broadcast([128, 4, 64]), op=ALU.mult),
            reads=[r_src, r_st], writes=[r_tmp])
        yield
        b.op("dve", lambda e: e.tensor_tensor(
            out=dst.rearrange("p (h d) -> p h d", d=64), in0=tmp[:],
            in1=gate.rearrange("p (h d) -> p h d", d=64), op=ALU.mult),
            reads=[r_tmp, r_gate], writes=[r_dst])
        yield


def emit_head_norm(b, src, r_src, gate, r_gate, dst, r_dst, tmp, st, r_tmp, r_st):
    for _ in gen_head_norm(b, src, r_src, gate, r_gate, dst, r_dst, tmp, st, r_tmp, r_st):
        pass


GAMMAS = [1.0 - 2.0 ** (-5.0 - h) for h in range(4)]


def ret_tables():
    i = np.arange(128)
    decT = np.zeros((128, 4, 128), np.float32)
    qdecT = np.zeros((128, 2, 128), np.float32)
    kdec = np.zeros((128, 4), np.float32)
    for h, g in enumerate(GAMMAS):
        diff = i[None, :] - i[:, None]
        decT[:, h, :] = np.where(diff >= 0, g ** np.maximum(diff, 0), 0.0) / 8.0
        qdecT[(h % 2) * 64:(h % 2) * 64 + 64, h // 2, :] = (g ** (i + 1.0))[None, :]
        kdec[:, h] = g ** (127.0 - i) / 8.0
    return decT, qdecT, kdec


def emit_p2b(b, nslots, tokA_all, featT_own, tokA_own, decT_d, qdecT_d, kdec_d, jf_d, o_d, r_in, r_out,
             whole=False):
    with ExitStack() as es:
        old = b.es
        b.es = es
        decT = b.sb("b_decT", [128, 4, 128], F32)
        qdecT = b.sb("b_qdecT", [128, 2, 128], F32)
        kdec = b.sb("b_kdec", [128, 4], F32)
        jf = b.sb("b_jf", [128, 1], F32)
        S = b.sb("b_S", [128, 2, 64], F32)
        SA = b.sb("b_SA", [128, 2, 64], F32)
        Sd = b.sb("b_Sd", [128, 2, 64], F32)
        Sbf = b.sb("b_Sbf", [128, 2, 64], BF16)
        kvin = [b.sb("b_kvin%d" % i, [128, 512], BF16) for i in range(2)]
        vdec = [b.sb("b_vdec%d" % i, [128, 256], BF16) for i in range(2)]
        qk = [b.sb("b_qk%d" % i, [128, 4, 128], BF16) for i in range(2)]
        qd = [b.sb("b_qd%d" % i, [128, 2, 128], BF16) for i in range(2)]
        own = [b.sb("b_own%d" % i, [128, 512], BF16) for i in range(2)]
        scT = [b.sb("b_scT%d" % i, [128, 128], BF16) for i in range(2)]
        ysbs = [b.sb("b_ysb%d" % i, [128, 4, 64], F32) for i in range(2)]
        tmps = [b.sb("b_tmp%d" % i, [128, 4, 64], F32) for i in range(2)]
        sts = [b.sb("b_st%d" % i, [128, 16], F32) for i in range(2)]
        r_ysbs, r_tmps, r_sts = ([b.res(), b.res()] for _ in range(3))
        pending_fin = None
        fin_i = 0
        osb = [b.sb("b_o%d" % i, [128, 256], BF16) for i in range(2)]
        pKV = [b.ps("b_pKV%d" % i, [128, 128]) for i in range(2)]
        pSC = [b.ps("b_pSC%d" % i, [128, 128]) for i in range(2)]
        pY = b.ps("b_pY", [128, 256])
        (r_c, r_S, r_SA, r_Sd, r_Sbf, r_ysb, r_tmp, r_st, r_pY) = (b.res() for _ in range(9))
        r_kvin = [b.res(), b.res()]
        r_vdec = [b.res(), b.res()]
        r_qk = [b.res(), b.res()]
        r_qd = [b.res(), b.res()]
        r_own = [b.res(), b.res()]
        r_scT = [b.res(), b.res()]
        r_osb = [b.res(), b.res()]
        r_pKV = [b.res(), b.res()]
        r_pSC = [b.res(), b.res()]

        b.dma("sp", decT[:], decT_d, writes=[r_c])
        b.dma("sp", qdecT[:], qdecT_d, writes=[r_c])
        b.dma("sp", kdec[:], kdec_d, writes=[r_c])
        b.dma("sp", jf[:], jf_d, writes=[r_c])
        b.op("dve", lambda e: e.memset(S[:], 0.0), writes=[r_S])
        sc_i = 0
        for g in range(2 * nslots):
            m, r = g // 2, g % 2
            s = g % 2
            gi = gidx(g)
            b.dma("sp", kvin[s][:], tokA_all[gi * 128:(gi + 1) * 128, 512:1024], reads=[r_in], writes=[r_kvin[s]])
            if whole:
                so = g % 2
                orow = gi * 128
                b.dma("sp", qk[so][:], featT_own[gi, :, 11:15, :], reads=[r_in], writes=[r_qk[so]])
                b.dma("sp", own[so][:], tokA_all[gi * 128:(gi + 1) * 128, 768:1280], reads=[r_in], writes=[r_own[so]])
                b.op("dve", lambda e: e.tensor_copy(out=Sbf[:], in_=S[:]), reads=[r_S], writes=[r_Sbf])
            elif r == 0:
                b.op("dve", lambda e: e.tensor_copy(out=SA[:], in_=S[:]), reads=[r_S], writes=[r_SA])
                so = m % 2
                b.dma("sp", qk[so][:], featT_own[m, :, 11:15, :], reads=[r_in], writes=[r_qk[so]])
                b.dma("sp", own[so][:], tokA_own[m * 128:(m + 1) * 128, 768:1280], reads=[r_in], writes=[r_own[so]])
            else:
                so = m % 2
                orow = m * 128
                b.op("dve", lambda e: e.tensor_tensor(out=Sd[:], in0=S[:], in1=SA[:], op=ALU.subtract),
                     reads=[r_S, r_SA], writes=[r_Sd])
                b.op("dve", lambda e: e.scalar_tensor_tensor(
                    out=Sbf[:].rearrange("p a e -> p (a e)"), in0=Sd[:].rearrange("p a e -> p (a e)"),
                    scalar=jf[:, 0:1], in1=SA[:].rearrange("p a e -> p (a e)"), op0=ALU.mult, op1=ALU.add),
                    reads=[r_Sd, r_SA, r_c], writes=[r_Sbf])
            if whole or r == 1:
                b.op("dve", lambda e, so=so: e.tensor_tensor(
                    out=qd[so][:], in0=qk[so][:, 0:2, :], in1=qdecT[:], op=ALU.mult),
                    reads=[r_qk[so], r_c], writes=[r_qd[so]])
                for h in range(4):
                    hs = slice((h % 2) * 64, (h % 2) * 64 + 64)
                    p = h // 2
                    si = sc_i % 2
                    sc_i += 1
                    b.op("pe", lambda e, so=so, hs=hs, p=p, si=si: e.matmul(
                        pSC[si][:, :], lhsT=qk[so][hs, 2 + p, :], rhs=qk[so][hs, p, :], start=True, stop=True),
                        reads=[r_qk[so]], writes=[r_pSC[si]])
                    b.op("dve", lambda e, si=si, h=h: e.tensor_tensor(
                        out=scT[si][:], in0=pSC[si][:], in1=decT[:, h, :], op=ALU.mult),
                        reads=[r_pSC[si], r_c], writes=[r_scT[si]])
                    b.op("pe", lambda e, si=si, h=h, so=so: e.matmul(
                        pY[:, h * 64:(h + 1) * 64], lhsT=scT[si][:], rhs=own[so][:, h * 64:(h + 1) * 64],
                        start=True, stop=False),
                        reads=[r_scT[si], r_own[so]], writes=[r_pY], inc=False)
                    b.op("pe", lambda e, h=h, so=so, hs=hs, p=p: e.matmul(
                        pY[:, h * 64:(h + 1) * 64], lhsT=qd[so][hs, p, :], rhs=Sbf[hs, p, :],
                        start=False, stop=True),
                        reads=[r_qd[so], r_Sbf], writes=[r_pY])
                    for _k in range(3):
                        if pending_fin is not None:
                            try:
                                next(pending_fin)
                            except StopIteration:
                                pending_fin = None
                fb = fin_i % 2
                fin_i += 1
                ysb_, tmp_, st_ = ysbs[fb], tmps[fb], sts[fb]
                r_ysb_, r_tmp_, r_st_ = r_ysbs[fb], r_tmps[fb], r_sts[fb]
                b.op("act", lambda e, ysb_=ysb_: e.copy(out=ysb_[:].rearrange("p h d -> p (h d)"), in_=pY[:]),
                     reads=[r_pY], writes=[r_ysb_])

                def fin_gen(ysb_=ysb_, tmp_=tmp_, st_=st_, r_ysb_=r_ysb_, r_tmp_=r_tmp_, r_st_=r_st_, so=so,
                            orow=orow):
                    for _ in gen_head_norm(b, ysb_, r_ysb_, own[so][:, 256:512], r_own[so], osb[so][:], r_osb[so],
                                           tmp_, st_, r_tmp_, r_st_):
                        yield
                    b.dma("pool", o_d[orow:orow + 128, 512:768], osb[so][:], reads=[r_osb[so]], writes=[r_out])

                while pending_fin is not None:
                    try:
                        next(pending_fin)
                    except StopIteration:
                        pending_fin = None
                pending_fin = fin_gen()
            if g == 2 * nslots - 1:
                break
            b.op("dve", lambda e, s=s: e.tensor_tensor(
                out=vdec[s][:].rearrange("p (h d) -> p h d", d=64),
                in0=kvin[s][:, 256:512].rearrange("p (h d) -> p h d", d=64),
                in1=kdec[:].unsqueeze(2).to_broadcast([128, 4, 64]), op=ALU.mult),
                reads=[r_kvin[s], r_c], writes=[r_vdec[s]])
            for p in range(2):
                b.op("pe", lambda e, s=s, p=p: e.matmul(
                    pKV[p][:, :], lhsT=kvin[s][:, p * 128:(p + 1) * 128], rhs=vdec[s][:, p * 128:(p + 1) * 128],
                    start=True, stop=True), reads=[r_kvin[s], r_vdec[s]], writes=[r_pKV[p]])
                for half in range(2):
                    h = 2 * p + half
                    hs = slice(half * 64, half * 64 + 64)
                    b.op("dve", lambda e, p=p, hs=hs, half=half, h=h: e.scalar_tensor_tensor(
                        out=S[hs, p, :], in0=S[hs, p, :], scalar=float(GAMMAS[h] ** 128),
                        in1=pKV[p][hs, half * 64:(half + 1) * 64], op0=ALU.mult, op1=ALU.add),
                        reads=[r_S, r_pKV[p]], writes=[r_S])
        while pending_fin is not None:
            try:
                next(pending_fin)
            except StopIteration:
                pending_fin = None
        b.barrier()
        b.es = old


def emit_p2c(b, l, nslots, featT_all, tokA_all, gT_all, tokA_own, convw_d, convb_d, caus_d, sel4_d, eye4_d,
             jf_d, identf_d, o_d, r_in, r_out, whole=False):
    NG = 2 * nslots
    NTK = NG * 128
    NOWN = NG if whole else nslots
    with ExitStack() as es:
        old = b.es
        b.es = es
        jf = b.sb("c_jf", [128, 1], F32)
        caus = b.sb("c_caus", [128, 128], F32)
        sel4 = b.sb("c_sel4", [4, 4, 128], F32)
        eye4 = b.sb("c_eye4", [4, 4], F32)
        ones4 = b.sb("c_ones4", [4, 128], F32)
        identf = b.sb("c_idf", [128, 128], F32)
        identb = b.sb("c_idb", [128, 128], BF16)
        qkall = b.sb("c_qkall", [128, 4, NTK], BF16)
        qkown = qkall if whole else b.sb("c_qkown", [128, 4, nslots * 128], BF16)
        GTo = b.sb("c_GTo", [4, NOWN, 128], F32)
        acol = b.sb("c_acol", [128, NG, 4], F32)
        ecol = b.sb("c_ecol", [128, NG, 4], F32)
        acolo = b.sb("c_acolo", [128, NOWN, 4], F32)
        ecolo = b.sb("c_ecolo", [128, NOWN, 4], F32)
        GR = b.sb("c_GR", [128, NG + 1, 4], F32)
        GRo = b.sb("c_GRo", [128, NOWN, 4], F32)
        dec = b.sb("c_dec", [128, NG, 4], F32)
        kw = b.sb("c_kw", [128, NG, 4], F32)
        r_c, r_qkall, r_qkown, r_GTo, r_cols, r_GR = (b.res() for _ in range(6))
        if whole:
            r_qkown = r_qkall

        b.dma("sp", jf[:], jf_d, writes=[r_c])
        b.dma("sp", caus[:], caus_d, writes=[r_c])
        b.dma("sp", sel4[:].rearrange("k h n -> k (h n)"), sel4_d, writes=[r_c])
        b.dma("sp", eye4[:], eye4_d, writes=[r_c])
        b.dma("sp", identf[:], identf_d, writes=[r_c])
        b.op("dve", lambda e: e.tensor_copy(out=identb[:], in_=identf[:]), reads=[r_c], writes=[r_c])
        b.op("dve", lambda e: e.memset(ones4[:], 1.0), writes=[r_c])

        with ExitStack() as es1:
            b.es = es1
            gi_ = b.sb("c1_gi", [4, NTK], F32)
            gf_ = b.sb("c1_gf", [4, NTK], F32)
            t1 = b.sb("c1_t1", [4, NTK], F32)
            Bn = b.sb("c1_Bn", [4, NTK], F32)
            aT = b.sb("c1_aT", [4, NTK], F32)
            GT = b.sb("c1_GT", [4, NTK], F32)
            eT = b.sb("c1_eT", [4, NTK], F32)
            onesr = b.sb("c1_ones", [4, NTK], F32)
            gend = b.sb("c1_gend", [4, NG, 4], F32)
            pT = b.ps("c1_pT", [128, 2 * NG * 4])
            pR = b.ps("c1_pR", [128, NG * 4])
            r_g, r_t1, r_Bn, r_aT, r_GT, r_eT, r_on, r_ge, r_pT, r_pR = (b.res() for _ in range(10))
            for g in range(NG):
                gi = gidx(g)
                b.dma("sp", gi_[:, g * 128:(g + 1) * 128], gT_all[gi, 0:4, :], reads=[r_in], writes=[r_g])
                b.dma("sp", gf_[:, g * 128:(g + 1) * 128], gT_all[gi, 4:8, :], reads=[r_in], writes=[r_g])
            b.op("dve", lambda e: e.memset(onesr[:], 1.0), writes=[r_on])
            b.op("act", lambda e: e.activation(out=t1[:], in_=gf_[:], func=AF.Exp, scale=-1.0),
                 reads=[r_g], writes=[r_t1])
            b.op("act", lambda e: e.activation(out=t1[:], in_=t1[:], func=AF.Ln, bias=1.0),
                 reads=[r_t1], writes=[r_t1])
            b.op("dve", lambda e: e.tensor_tensor_scan(out=Bn[:], data0=onesr[:], data1=t1[:], initial=0.0,
                                                       op0=ALU.mult, op1=ALU.add),
                 reads=[r_on, r_t1], writes=[r_Bn])
            b.op("dve", lambda e: e.tensor_tensor(out=aT[:], in0=gi_[:], in1=Bn[:], op=ALU.add),
                 reads=[r_g, r_Bn], writes=[r_aT])
            b.op("dve", lambda e: e.tensor_tensor_scan(out=GT[:], data0=onesr[:], data1=aT[:], initial=0.0,
                                                       op0=ALU.mult, op1=ALU.max),
                 reads=[r_on, r_aT], writes=[r_GT])
            b.op("dve", lambda e: e.tensor_tensor(out=eT[:], in0=Bn[:], in1=GT[:], op=ALU.subtract),
                 reads=[r_Bn, r_GT], writes=[r_eT])
            b.op("act", lambda e: e.activation(out=eT[:], in_=eT[:], func=AF.Exp), reads=[r_eT], writes=[r_eT])
            if whole:
                b.op("dve", lambda e: e.tensor_copy(out=GTo[:].rearrange("k g n -> k (g n)"), in_=GT[:]),
                     reads=[r_GT], writes=[r_GTo])
            else:
                GTv = GT[:].rearrange("k (m r n) -> k m r n", r=2, n=128)
                b.op("dve", lambda e: e.tensor_tensor(out=t1[:, 0:nslots * 128].rearrange("k (m n) -> k m n", n=128),
                                                      in0=GTv[:, :, 1, :], in1=GTv[:, :, 0, :], op=ALU.subtract),
                     reads=[r_GT, r_t1], writes=[r_t1])
                b.op("dve", lambda e: e.scalar_tensor_tensor(
                    out=GTo[:], in0=t1[:, 0:nslots * 128].rearrange("k (m n) -> k m n", n=128), scalar=jf[0:4, 0:1],
                    in1=GTv[:, :, 0, :], op0=ALU.mult, op1=ALU.add),
                    reads=[r_t1, r_GT, r_c], writes=[r_GTo])
            for g in range(NG):
                b.op("pe", lambda e, g=g: e.transpose(out=pT[:, g * 4:(g + 1) * 4], in_=aT[:, g * 128:(g + 1) * 128],
                                                      identity=identf[0:4, 0:4]),
                     reads=[r_aT, r_c], writes=[r_pT], inc=False)
                b.op("pe", lambda e, g=g: e.transpose(out=pT[:, (NG + g) * 4:(NG + g + 1) * 4],
                                                      in_=eT[:, g * 128:(g + 1) * 128], identity=identf[0:4, 0:4]),
                     reads=[r_eT, r_c], writes=[r_pT], inc=(g == NG - 1))
            b.op("act", lambda e: e.copy(out=acol[:].rearrange("p g h -> p (g h)"), in_=pT[:, 0:NG * 4]),
                 reads=[r_pT], writes=[r_cols])
            b.op("act", lambda e: e.copy(out=ecol[:].rearrange("p g h -> p (g h)"), in_=pT[:, NG * 4:2 * NG * 4]),
                 reads=[r_pT], writes=[r_cols])
            b.op("dve", lambda e: e.tensor_tensor(
                out=gend[:], in0=GT[:].rearrange("k (g n) -> k g n", n=128)[:, :, 127:128].to_broadcast([4, NG, 4]),
                in1=eye4[:].unsqueeze(1).to_broadcast([4, NG, 4]), op=ALU.mult),
                reads=[r_GT, r_c], writes=[r_ge])
            b.op("pe", lambda e: e.matmul(pR[:, :], lhsT=ones4[:, :], rhs=gend[:].rearrange("k g h -> k (g h)"),
                                          start=True, stop=True), reads=[r_ge, r_c], writes=[r_pR])
            b.op("dve", lambda e: e.memset(GR[:, 0, :], 0.0), writes=[r_GR])
            b.op("act", lambda e: e.copy(out=GR[:, 1:NG + 1, :].rearrange("p g h -> p (g h)"), in_=pR[:]),
                 reads=[r_pR], writes=[r_GR])
            b.op("dve", lambda e: e.tensor_tensor(out=dec[:], in0=GR[:, 0:NG, :], in1=GR[:, 1:NG + 1, :],
                                                  op=ALU.subtract), reads=[r_GR], writes=[r_cols])
            b.op("act", lambda e: e.activation(out=dec[:], in_=dec[:], func=AF.Exp), reads=[r_cols], writes=[r_cols])
            b.op("dve", lambda e: e.tensor_tensor(out=kw[:], in0=acol[:], in1=GR[:, 1:NG + 1, :], op=ALU.subtract),
                 reads=[r_cols, r_GR], writes=[r_cols])
            b.op("act", lambda e: e.activation(out=kw[:], in_=kw[:], func=AF.Exp), reads=[r_cols], writes=[r_cols])
            b.op("dve", lambda e: e.tensor_scalar(out=kw[:], in0=kw[:], scalar1=0.125, scalar2=None, op0=ALU.mult),
                 reads=[r_cols], writes=[r_cols])

            def blend(dst, src, ncol):
                sv = src.rearrange("p (m r) h -> p m r h", r=2)
                b.op("dve", lambda e: e.tensor_tensor(out=dst, in0=sv[:, :, 1, :], in1=sv[:, :, 0, :],
                                                      op=ALU.subtract), reads=[r_cols, r_GR], writes=[r_cols])
                b.op("dve", lambda e: e.scalar_tensor_tensor(
                    out=dst, in0=dst, scalar=jf[:, 0:1], in1=sv[:, :, 0, :], op0=ALU.mult, op1=ALU.add),
                    reads=[r_cols, r_GR, r_c], writes=[r_cols])
            if whole:
                for dst_, src_ in ((acolo, acol[:]), (ecolo, ecol[:]), (GRo, GR[:, 0:NG, :])):
                    b.op("dve", lambda e, dst_=dst_, src_=src_: e.tensor_copy(out=dst_[:], in_=src_),
                         reads=[r_cols, r_GR], writes=[r_cols])
            else:
                blend(acolo[:], acol[:], 4)
                blend(ecolo[:], ecol[:], 4)
                blend(GRo[:], GR[:, 0:NG, :], 4)
            b.barrier()
            b.es = es

        with ExitStack() as es2:
            b.es = es2
            pre = b.sb("c2_pre", [128, 4, NTK + 3], BF16)
            acc = [b.sb("c2_acc%d" % i, [128, NTK], F32) for i in range(2)]
            cw = b.sb("c2_cw", [128, 4, 4], F32)
            cb = b.sb("c2_cb", [128, 4], F32)
            r_pre, r_cw = b.res(), b.res()
            r_acc = [b.res(), b.res()]
            b.dma("sp", cw[:], convw_d[l], writes=[r_cw])
            b.dma("sp", cb[:], convb_d[l], writes=[r_cw])
            b.op("pool", lambda e: e.memset(pre[:, :, 0:3], 0.0), writes=[r_pre])
            for g in range(NG):
                gi = gidx(g)
                b.dma("sp", pre[:, :, 3 + g * 128:3 + (g + 1) * 128], featT_all[gi, :, 15:19, :],
                      reads=[r_in], writes=[r_pre])
            for c in range(4):
                a = acc[c % 2]
                ra = r_acc[c % 2]
                b.op("dve", lambda e, c=c, a=a: e.tensor_scalar(
                    out=a[:], in0=pre[:, c, 0:NTK], scalar1=cw[:, c, 0:1], scalar2=cb[:, c:c + 1],
                    op0=ALU.mult, op1=ALU.add), reads=[r_pre, r_cw], writes=[ra])
                for j in range(1, 4):
                    b.op("dve", lambda e, c=c, a=a, j=j: e.scalar_tensor_tensor(
                        out=a[:], in0=pre[:, c, j:j + NTK], scalar=cw[:, c, j:j + 1], in1=a[:],
                        op0=ALU.mult, op1=ALU.add), reads=[r_pre, r_cw, ra], writes=[ra])
                b.op("act", lambda e, c=c, a=a: e.activation(out=qkall[:, c, :], in_=a[:], func=AF.Silu),
                     reads=[ra], writes=[r_qkall])
            if not whole:
                for c in range(4):
                    a = acc[c % 2]
                    ra = r_acc[c % 2]
                    v = qkall[:, c, :].rearrange("p (m r n) -> p m r n", r=2, n=128)
                    av = a[:, 0:nslots * 128].rearrange("p (m n) -> p m n", n=128)
                    b.op("dve", lambda e, v=v, av=av: e.tensor_tensor(out=av, in0=v[:, :, 1, :], in1=v[:, :, 0, :],
                                                                      op=ALU.subtract),
                         reads=[r_qkall, ra], writes=[ra])
                    b.op("dve", lambda e, v=v, av=av, c=c: e.scalar_tensor_tensor(
                        out=qkown[:, c, :].rearrange("p (m n) -> p m n", n=128), in0=av, scalar=jf[:, 0:1],
                        in1=v[:, :, 0, :], op0=ALU.mult, op1=ALU.add),
                        reads=[ra, r_qkall, r_c], writes=[r_qkown])
            b.barrier()
            b.es = es

        Cst = b.sb("c_Cst", [128, 2, 65], F32)
        CA = b.sb("c_CA", [128, 2, 65], F32)
        Cd = b.sb("c_Cd", [128, 2, 65], F32)
        Cbf = b.sb("c_Cbf", [128, 2, 65], BF16)
        vin = [b.sb("c_vin%d" % i, [128, 256], BF16) for i in range(2)]
        vk = [b.sb("c_vk%d" % i, [128, 4, 65], BF16) for i in range(2)]
        ktok = [b.sb("c_ktok%d" % i, [128, 256], BF16) for i in range(2)]
        ownv = [b.sb("c_ownv%d" % i, [128, 512], BF16) for i in range(2)]
        vaug = [b.sb("c_vaug%d" % i, [128, 4, 65], BF16) for i in range(2)]
        ngc = [b.sb("c_ngc%d" % i, [128, 128], F32) for i in range(2)]
        WT = [b.sb("c_WT%d" % i, [128, 128], F32) for i in range(2)]
        DT = [b.sb("c_DT%d" % i, [128, 128], BF16) for i in range(2)]
        igq = [b.sb("c_igq%d" % i, [128, 128], F32) for i in range(2)]
        qs = [b.sb("c_qs%d" % i, [128, 128], BF16) for i in range(2)]
        nds = [b.sb("c_nd%d" % i, [128, 4, 65], F32) for i in range(2)]
        hsbs = [b.sb("c_hsb%d" % i, [128, 4, 64], F32) for i in range(2)]
        tmps = [b.sb("c_tmp%d" % i, [128, 4, 64], F32) for i in range(2)]
        sts = [b.sb("c_st%d" % i, [128, 16], F32) for i in range(2)]
        dns = [b.sb("c_dn%d" % i, [128, 8], F32) for i in range(2)]
        r_nds, r_hsbs, r_tmps, r_sts, r_dns = ([b.res(), b.res()] for _ in range(5))
        pending_fin = None
        fin_i = 0
        osb = [b.sb("c_o%d" % i, [128, 256], BF16) for i in range(2)]
        pKt = b.ps("c_pKt", [128, 256], BF16)
        pKV = [b.ps("c_pKV%d" % i, [128, 130]) for i in range(2)]
        pG = [b.ps("c_pG%d" % i, [128, 128]) for i in range(2)]
        pQK = [b.ps("c_pQK%d" % i, [128, 128]) for i in range(2)]
        pN = b.ps("c_pN", [128, 260])
        (r_Cst, r_CA, r_Cd, r_Cbf, r_nd, r_hsb, r_tmp, r_st, r_dn, r_pKt, r_pN) = (b.res() for _ in range(11))
        r_vin = [b.res(), b.res()]
        r_vk = [b.res(), b.res()]
        r_ktok = [b.res(), b.res()]
        r_ownv = [b.res(), b.res()]
        r_vaug = [b.res(), b.res()]
        r_ngc = [b.res(), b.res()]
        r_WT = [b.res(), b.res()]
        r_DT = [b.res(), b.res()]
        r_igq = [b.res(), b.res()]
        r_qs = [b.res(), b.res()]
        r_osb = [b.res(), b.res()]
        r_pKV = [b.res(), b.res()]
        r_pG = [b.res(), b.res()]
        r_pQK = [b.res(), b.res()]

        b.op("dve", lambda e: e.memset(Cst[:], 0.0), writes=[r_Cst])
        hi = 0
        for g in range(NG):
            m, r = g // 2, g % 2
            s = g % 2
            gi = gidx(g)
            b.dma("sp", vin[s][:], tokA_all[gi * 128:(gi + 1) * 128, 1280:1536], reads=[r_in], writes=[r_vin[s]])
            if whole:
                so = g % 2
                mm = g
                orow = gi * 128
                b.dma("sp", ownv[so][:], tokA_all[gi * 128:(gi + 1) * 128, 1280:1792], reads=[r_in],
                      writes=[r_ownv[so]])
                b.op("pool", lambda e, so=so: e.memset(vaug[so][:, :, 64:65], 1.0), writes=[r_vaug[so]])
                b.op("pool", lambda e, so=so: e.tensor_copy(
                    out=vaug[so][:, :, 0:64], in_=ownv[so][:, 0:256].rearrange("p (h d) -> p h d", d=64)),
                    reads=[r_ownv[so]], writes=[r_vaug[so]])
                b.op("dve", lambda e: e.tensor_copy(out=Cbf[:], in_=Cst[:]), reads=[r_Cst], writes=[r_Cbf])
            elif r == 0:
                so = m % 2
                b.op("dve", lambda e: e.tensor_copy(out=CA[:], in_=Cst[:]), reads=[r_Cst], writes=[r_CA])
                b.dma("sp", ownv[so][:], tokA_own[m * 128:(m + 1) * 128, 1280:1792], reads=[r_in],
                      writes=[r_ownv[so]])
                b.op("pool", lambda e, so=so: e.memset(vaug[so][:, :, 64:65], 1.0), writes=[r_vaug[so]])
                b.op("pool", lambda e, so=so: e.tensor_copy(
                    out=vaug[so][:, :, 0:64], in_=ownv[so][:, 0:256].rearrange("p (h d) -> p h d", d=64)),
                    reads=[r_ownv[so]], writes=[r_vaug[so]])
            else:
                so = m % 2
                mm = m
                orow = m * 128
                b.op("dve", lambda e: e.tensor_tensor(out=Cd[:], in0=Cst[:], in1=CA[:], op=ALU.subtract),
                     reads=[r_Cst, r_CA], writes=[r_Cd])
                b.op("dve", lambda e: e.scalar_tensor_tensor(
                    out=Cbf[:].rearrange("p a e -> p (a e)"), in0=Cd[:].rearrange("p a e -> p (a e)"),
                    scalar=jf[:, 0:1], in1=CA[:].rearrange("p a e -> p (a e)"), op0=ALU.mult, op1=ALU.add),
                    reads=[r_Cd, r_CA, r_c], writes=[r_Cbf])
            if whole or r == 1:
                m = mm
                for h in range(4):
                    hs = slice((h % 2) * 64, (h % 2) * 64 + 64)
                    p = h // 2
                    x = hi % 2
                    hi += 1
                    b.op("pe", lambda e, x=x, h=h, m=m: e.matmul(
                        pG[x][:, :], lhsT=sel4[:, h, :], rhs=GTo[:, m, :], start=True, stop=True),
                        reads=[r_c, r_GTo], writes=[r_pG[x]])
                    b.op("dve", lambda e, x=x: e.tensor_tensor(out=ngc[x][:], in0=caus[:], in1=pG[x][:],
                                                               op=ALU.subtract),
                         reads=[r_c, r_pG[x]], writes=[r_ngc[x]])
                    b.op("act", lambda e, x=x, m=m, h=h: e.activation(
                        out=WT[x][:], in_=ngc[x][:], func=AF.Exp, bias=acolo[:, m, h:h + 1]),
                        reads=[r_ngc[x], r_cols], writes=[r_WT[x]])
                    b.op("pe", lambda e, x=x, hs=hs, p=p, m=m: e.matmul(
                        pQK[x][:, :], lhsT=qkown[hs, 2 + p, m * 128:(m + 1) * 128],
                        rhs=qkown[hs, p, m * 128:(m + 1) * 128], start=True, stop=True),
                        reads=[r_qkown], writes=[r_pQK[x]])
                    b.op("dve", lambda e, x=x: e.scalar_tensor_tensor(
                        out=DT[x][:], in0=pQK[x][:], scalar=0.125, in1=WT[x][:], op0=ALU.mult, op1=ALU.mult),
                        reads=[r_pQK[x], r_WT[x]], writes=[r_DT[x]])
                    b.op("act", lambda e, x=x, hs=hs, m=m, h=h: e.activation(
                        out=igq[x][hs, :], in_=pG[x][hs, :], func=AF.Exp, scale=-1.0, bias=GRo[hs, m, h:h + 1]),
                        reads=[r_pG[x], r_cols], writes=[r_igq[x]])
                    b.op("dve", lambda e, x=x, hs=hs, p=p, m=m: e.tensor_tensor(
                        out=qs[x][hs, :], in0=qkown[hs, p, m * 128:(m + 1) * 128], in1=igq[x][hs, :], op=ALU.mult),
                        reads=[r_qkown, r_igq[x]], writes=[r_qs[x]])
                    b.op("pe", lambda e, x=x, h=h, so=so: e.matmul(
                        pN[:, h * 65:(h + 1) * 65], lhsT=DT[x][:], rhs=vaug[so][:, h, :], start=True, stop=False),
                        reads=[r_DT[x], r_vaug[so]], writes=[r_pN], inc=False)
                    b.op("pe", lambda e, x=x, h=h, hs=hs, p=p: e.matmul(
                        pN[:, h * 65:(h + 1) * 65], lhsT=qs[x][hs, :], rhs=Cbf[hs, p, :], start=False, stop=True),
                        reads=[r_qs[x], r_Cbf], writes=[r_pN])
                    for _k in range(5):
                        if pending_fin is not None:
                            try:
                                next(pending_fin)
                            except StopIteration:
                                pending_fin = None
                fb = fin_i % 2
                fin_i += 1
                nd_, hsb_, tmp_, st_, dn_ = nds[fb], hsbs[fb], tmps[fb], sts[fb], dns[fb]
                r_nd_, r_hsb_, r_tmp_, r_st_, r_dn_ = r_nds[fb], r_hsbs[fb], r_tmps[fb], r_sts[fb], r_dns[fb]
                b.op("act", lambda e, nd_=nd_: e.copy(out=nd_[:].rearrange("p h d -> p (h d)"), in_=pN[:]),
                     reads=[r_pN], writes=[r_nd_])

                def fin_gen(nd_=nd_, hsb_=hsb_, tmp_=tmp_, st_=st_, dn_=dn_, r_nd_=r_nd_, r_hsb_=r_hsb_,
                            r_tmp_=r_tmp_, r_st_=r_st_, r_dn_=r_dn_, so=so, m=m, orow=orow):
                    b.op("dve", lambda e: e.tensor_scalar(
                        out=dn_[:, 0:4].unsqueeze(2), in0=nd_[:, :, 64:65], scalar1=-1.0, scalar2=None, op0=ALU.mult),
                        reads=[r_nd_], writes=[r_dn_])
                    yield
                    b.op("dve", lambda e: e.tensor_tensor(
                        out=dn_[:, 0:4].unsqueeze(2), in0=dn_[:, 0:4].unsqueeze(2), in1=nd_[:, :, 64:65], op=ALU.max),
                        reads=[r_nd_, r_dn_], writes=[r_dn_])
                    yield
                    b.op("dve", lambda e: e.tensor_tensor(out=dn_[:, 0:4], in0=dn_[:, 0:4], in1=ecolo[:, m, :],
                                                          op=ALU.max),
                         reads=[r_dn_, r_cols], writes=[r_dn_])
                    yield
                    b.op("dve", lambda e: e.reciprocal(out=dn_[:, 4:8], in_=dn_[:, 0:4]), reads=[r_dn_], writes=[r_dn_])
                    yield
                    b.op("dve", lambda e: e.tensor_tensor(
                        out=tmp_[:], in0=nd_[:, :, 0:64], in1=dn_[:, 4:8].unsqueeze(2).to_broadcast([128, 4, 64]),
                        op=ALU.mult), reads=[r_nd_, r_dn_], writes=[r_tmp_])
                    yield
                    b.op("dve", lambda e: e.tensor_tensor(
                        out=hsb_[:], in0=tmp_[:], in1=ownv[so][:, 256:512].rearrange("p (h d) -> p h d", d=64),
                        op=ALU.mult), reads=[r_tmp_, r_ownv[so]], writes=[r_hsb_])
                    yield
                    for _ in gen_head_norm(b, hsb_, r_hsb_, None, None, osb[so][:], r_osb[so], tmp_, st_,
                                           r_tmp_, r_st_):
                        yield
                    b.dma("pool", o_d[orow:orow + 128, 768:1024], osb[so][:], reads=[r_osb[so]], writes=[r_out])

                while pending_fin is not None:
                    try:
                        next(pending_fin)
                    except StopIteration:
                        pending_fin = None
                pending_fin = fin_gen()
            if g == NG - 1:
                break
            b.op("dve", lambda e, s=s, g=g: e.tensor_tensor(
                out=vk[s][:, :, 0:64], in0=vin[s][:].rearrange("p (h d) -> p h d", d=64),
                in1=kw[:, g, :].unsqueeze(2).to_broadcast([128, 4, 64]), op=ALU.mult),
                reads=[r_vin[s], r_cols], writes=[r_vk[s]])
            b.op("dve", lambda e, s=s, g=g: e.tensor_copy(out=vk[s][:, :, 64:65], in_=kw[:, g, :].unsqueeze(2)),
                 reads=[r_cols], writes=[r_vk[s]])
            for p in range(2):
                b.op("pe", lambda e, p=p, g=g: e.transpose(
                    out=pKt[:, p * 128:(p + 1) * 128], in_=qkall[:, 2 + p, g * 128:(g + 1) * 128],
                    identity=identb[:]), reads=[r_qkall, r_c], writes=[r_pKt], inc=(p == 1))
            b.op("act", lambda e, s=s: e.copy(out=ktok[s][:], in_=pKt[:]), reads=[r_pKt], writes=[r_ktok[s]])
            for p in range(2):
                b.op("pe", lambda e, s=s, p=p: e.matmul(
                    pKV[p][:, :], lhsT=ktok[s][:, p * 128:(p + 1) * 128],
                    rhs=vk[s][:, 2 * p:2 * p + 2, :].rearrange("p a e -> p (a e)"), start=True, stop=True),
                    reads=[r_ktok[s], r_vk[s]], writes=[r_pKV[p]])
                for half in range(2):
                    h = 2 * p + half
                    hs = slice(half * 64, half * 64 + 64)
                    b.op("dve", lambda e, p=p, hs=hs, half=half, h=h, g=g: e.scalar_tensor_tensor(
                        out=Cst[hs, p, :], in0=Cst[hs, p, :], scalar=dec[hs, g, h:h + 1],
                        in1=pKV[p][hs, half * 65:(half + 1) * 65], op0=ALU.mult, op1=ALU.add),
                        reads=[r_Cst, r_pKV[p], r_cols], writes=[r_Cst])
        while pending_fin is not None:
            try:
                next(pending_fin)
            except StopIteration:
                pending_fin = None
        b.barrier()
        b.es = old


def mlstm_consts():
    i = np.arange(128)
    caus = np.where(i[:, None] <= i[None, :], 0.0, NEG).astype(np.float32)
    sel4 = np.zeros((4, 4, 128), np.float32)
    for h in range(4):
        sel4[h, h, :] = 1.0
    return caus, sel4.reshape(4, 512), np.eye(4, dtype=np.float32)


def build_p2bc(l, nslots):
    nc = bass.Bass("TRN2", target_bir_lowering=False)
    featT_all = nc.dram_tensor("featT_all", [2 * NT, 128, NFT, 128], BF16, kind="ExternalInput").ap()
    tokA_all = nc.dram_tensor("tokA_all", [2 * TOK, 1792], BF16, kind="ExternalInput").ap()
    gT_all = nc.dram_tensor("gT_all", [2 * NT, 8, 128], F32, kind="ExternalInput").ap()
    featT_own = nc.dram_tensor("featT_own", [NT, 128, NFT, 128], BF16, kind="ExternalInput").ap()
    tokA_own = nc.dram_tensor("tokA_own", [TOK, 1792], BF16, kind="ExternalInput").ap()
    decT_d = nc.dram_tensor("decT", [128, 4, 128], F32, kind="ExternalInput").ap()
    qdecT_d = nc.dram_tensor("qdecT", [128, 2, 128], F32, kind="ExternalInput").ap()
    kdec_d = nc.dram_tensor("kdec", [128, 4], F32, kind="ExternalInput").ap()
    jf_d = nc.dram_tensor("jf", [128, 1], F32, kind="ExternalInput").ap()
    convw_d = nc.dram_tensor("convw", [DEPTH, 128, 4, 4], F32, kind="ExternalInput").ap()
    convb_d = nc.dram_tensor("convb", [DEPTH, 128, 4], F32, kind="ExternalInput").ap()
    caus_d = nc.dram_tensor("caus", [128, 128], F32, kind="ExternalInput").ap()
    sel4_d = nc.dram_tensor("sel4", [4, 512], F32, kind="ExternalInput").ap()
    eye4_d = nc.dram_tensor("eye4", [4, 4], F32, kind="ExternalInput").ap()
    identf_d = nc.dram_tensor("identf", [128, 128], F32, kind="ExternalInput").ap()
    o_d = nc.dram_tensor("o", [TOK, D], BF16, kind="ExternalOutput").ap()
    with ExitStack() as es, nc.allow_low_precision("bf16 matmul operands, fp32 accumulation"):
        b = B(nc, es)
        r_in, r_out = b.res(), b.res()
        emit_p2b(b, nslots, tokA_all, featT_own, tokA_own, decT_d, qdecT_d, kdec_d, jf_d, o_d, r_in, r_out)
        emit_p2c(b, l, nslots, featT_all, tokA_all, gT_all, tokA_own, convw_d, convb_d, caus_d, sel4_d, eye4_d,
                 jf_d, identf_d, o_d, r_in, r_out)
        b.finish()
    return nc


def build_p2bc_whole(l, nslots):
    nc = bass.Bass("TRN2", target_bir_lowering=False)
    featT_all = nc.dram_tensor("featT_all", [2 * NT, 128, NFT, 128], BF16, kind="ExternalInput").ap()
    tokA_all = nc.dram_tensor("tokA_all", [2 * TOK, 1792], BF16, kind="ExternalInput").ap()
    gT_all = nc.dram_tensor("gT_all", [2 * NT, 8, 128], F32, kind="ExternalInput").ap()
    decT_d = nc.dram_tensor("decT", [128, 4, 128], F32, kind="ExternalInput").ap()
    qdecT_d = nc.dram_tensor("qdecT", [128, 2, 128], F32, kind="ExternalInput").ap()
    kdec_d = nc.dram_tensor("kdec", [128, 4], F32, kind="ExternalInput").ap()
    jf_d = nc.dram_tensor("jf", [128, 1], F32, kind="ExternalInput").ap()
    convw_d = nc.dram_tensor("convw", [DEPTH, 128, 4, 4], F32, kind="ExternalInput").ap()
    convb_d = nc.dram_tensor("convb", [DEPTH, 128, 4], F32, kind="ExternalInput").ap()
    caus_d = nc.dram_tensor("caus", [128, 128], F32, kind="ExternalInput").ap()
    sel4_d = nc.dram_tensor("sel4", [4, 512], F32, kind="ExternalInput").ap()
    eye4_d = nc.dram_tensor("eye4", [4, 4], F32, kind="ExternalInput").ap()
    identf_d = nc.dram_tensor("identf", [128, 128], F32, kind="ExternalInput").ap()
    o_d = nc.dram_tensor("o", [2 * TOK, D], BF16, kind="ExternalOutput").ap()
    with ExitStack() as es, nc.allow_low_precision("bf16 matmul operands, fp32 accumulation"):
        b = B(nc, es)
        r_in, r_out = b.res(), b.res()
        emit_p2b(b, nslots, tokA_all, featT_all, None, decT_d, qdecT_d, kdec_d, jf_d, o_d, r_in, r_out, whole=True)
        emit_p2c(b, l, nslots, featT_all, tokA_all, gT_all, None, convw_d, convb_d, caus_d, sel4_d, eye4_d,
                 jf_d, identf_d, o_d, r_in, r_out, whole=True)
        b.finish()
    return nc


def conv_layouts(conv_w, conv_b):
    cw = np.ascontiguousarray(conv_w.reshape(DEPTH, 4, 4, 128).transpose(0, 3, 2, 1))
    cb = np.ascontiguousarray(conv_b.reshape(DEPTH, 4, 128).transpose(0, 2, 1))
    return cw, cb


def emit_ln_stats(b, z, r_z, junk, r_junk, st, r_st):
    b.op("act", lambda e: e.activation(out=junk[:], in_=z, func=AF.Identity, accum_out=st[:, 2:3]),
         reads=[r_z], writes=[r_junk, r_st])
    b.op("act", lambda e: e.activation(out=junk[:], in_=z, func=AF.Square, accum_out=st[:, 3:4]),
         reads=[r_z], writes=[r_junk, r_st])
    b.op("dve", lambda e: e.tensor_scalar(out=st[:, 0:1], in0=st[:, 2:3], scalar1=1.0 / D, scalar2=None,
                                          op0=ALU.mult), reads=[r_st], writes=[r_st])
    b.op("dve", lambda e: e.tensor_tensor(out=st[:, 4:5], in0=st[:, 0:1], in1=st[:, 0:1], op=ALU.mult),
         reads=[r_st], writes=[r_st])
    b.op("dve", lambda e: e.scalar_tensor_tensor(out=st[:, 5:6], in0=st[:, 3:4], scalar=1.0 / D, in1=st[:, 4:5],
                                                 op0=ALU.mult, op1=ALU.subtract), reads=[r_st], writes=[r_st])
    b.op("dve", lambda e: e.tensor_scalar(out=st[:, 5:6], in0=st[:, 5:6], scalar1=LN_EPS, scalar2=None,
                                          op0=ALU.add), reads=[r_st], writes=[r_st])
    b.op("act", lambda e: e.activation(out=st[:, 6:7], in_=st[:, 5:6], func=AF.Sqrt), reads=[r_st], writes=[r_st])
    b.op("dve", lambda e: e.reciprocal(out=st[:, 1:2], in_=st[:, 6:7]), reads=[r_st], writes=[r_st])


def emit_p3(b, l, nt, x_d, o_d, wout_d, modrow_d, modcol_d, lnmg_d, lnmb_d, wr_d, br_d, wg_d, wu_d, wd_d,
            lnfg_d, lnfb_d, identf_d, xout_d, r_in, r_out, n_exp=16):
    ntok = nt * 128
    ngrp = nt // 4
    with ExitStack() as es:
        old = b.es
        b.es = es
        xacc = b.sb("p3_xacc", [128, nt, D], F32)
        h2T = b.sb("p3_h2T", [128, 8, ntok], BF16)
        gate = b.sb("p3_gate", [128, nt, 16], F32)
        lng = b.sb("p3_lng", [128, D], F32)
        lnb = b.sb("p3_lnb", [128, D], F32)
        gbc = b.sb("p3_gbc", [128, D], F32)
        junk = b.sb("p3_junk", [128, D], F32)
        st = b.sb("p3_st", [128, 8], F32)
        identf = b.sb("p3_idf", [128, 128], F32)
        identb = b.sb("p3_idb", [128, 128], BF16)
        r_xacc, r_h2T, r_gate, r_ln, r_gbc, r_junk, r_st, r_id = (b.res() for _ in range(8))
        b.dma("sp", identf[:], identf_d, writes=[r_id])
        b.op("dve", lambda e: e.tensor_copy(out=identb[:], in_=identf[:]), reads=[r_id], writes=[r_id])

        with ExitStack() as esa:
            b.es = esa
            wob = b.sb("p3a_wob", [128, 8, D], BF16)
            wst = [b.sb("p3a_wst%d" % i, [128, 8, 256], F32) for i in range(2)]
            mcol = b.sb("p3a_mcol", [128, 48], F32)
            sc1 = b.sb("p3a_sc1", [128, 8], F32)
            wr = b.sb("p3a_wr", [128, 8, 16], F32)
            brow = b.sb("p3a_brow", [128, 16], F32)
            ot = [b.sb("p3a_ot%d" % i, [128, D], BF16) for i in range(2)]
            oT = [b.sb("p3a_oT%d" % i, [128, 8, 128], BF16) for i in range(2)]
            xt = [b.sb("p3a_xt%d" % i, [128, D], F32) for i in range(2)]
            z = b.sb("p3a_z", [128, D], F32)
            x1 = b.sb("p3a_x1", [128, D], F32)
            h2f = b.sb("p3a_h2f", [128, 8, 128], F32)
            rt = b.sb("p3a_rt", [128, 160], F32)
            pOT = b.ps("p3a_pOT", [128, 1024], BF16)
            pM = [b.ps("p3a_pM%d" % i, [128, 512]) for i in range(2)]
            pX = [b.ps("p3a_pX%d" % i, [128, 512]) for i in range(2)]
            pR = b.ps("p3a_pR", [128, 16])
            r_wob, r_mc, r_wr, r_z, r_x1, r_h2f, r_rt, r_pOT, r_pR = (b.res() for _ in range(9))
            r_wst = [b.res(), b.res()]
            r_ot = [b.res(), b.res()]
            r_oT = [b.res(), b.res()]
            r_xt = [b.res(), b.res()]
            r_pM = [b.res(), b.res()]
            r_pX = [b.res(), b.res()]

            b.dma("sp", mcol[:], modcol_d[:, l * 48:(l + 1) * 48], writes=[r_mc])
            b.op("dve", lambda e: e.tensor_scalar(out=sc1[:], in0=mcol[:, 32:40], scalar1=1.0, scalar2=None,
                                                  op0=ALU.add), reads=[r_mc], writes=[r_mc])
            b.dma("sp", wr[:], wr_d.rearrange("(k p) n -> p k n", p=128), writes=[r_wr])
            b.dma("sp", brow[:], br_d.to_broadcast([128, 16]), writes=[r_wr])
            b.dma("sp", lng[:], lnmg_d[l:l + 1, :].to_broadcast([128, D]), writes=[r_ln])
            b.dma("sp", lnb[:], lnmb_d[l:l + 1, :].to_broadcast([128, D]), writes=[r_ln])
            b.dma("sp", gbc[:], modrow_d[l:l + 1, 2048:3072].to_broadcast([128, D]), writes=[r_gbc])
            b.op("dve", lambda e: e.tensor_scalar(out=gbc[:], in0=gbc[:], scalar1=1.0, scalar2=None, op0=ALU.add),
                 reads=[r_gbc], writes=[r_gbc])
            for i4 in range(4):
                i = i4 % 2
                b.dma("sp", wst[i][:], wout_d[l, :, i4 * 256:(i4 + 1) * 256].rearrange("(k p) n -> p k n", p=128),
                      writes=[r_wst[i]])
                b.op("pool", lambda e, i=i, i4=i4: e.tensor_tensor(
                    out=wob[:, :, i4 * 256:(i4 + 1) * 256], in0=wst[i][:],
                    in1=gbc[:, i4 * 256:(i4 + 1) * 256].unsqueeze(1).to_broadcast([128, 8, 256]), op=ALU.mult),
                    reads=[r_wst[i], r_gbc], writes=[r_wob])
            zs = [z, b.sb("p3a_z2", [128, D], F32)]
            x1s = [x1, b.sb("p3a_x12", [128, D], F32)]
            h2fs = [h2f, b.sb("p3a_h2f2", [128, 8, 128], F32)]
            rts = [rt, b.sb("p3a_rt2", [128, 160], F32)]
            sts = [st, b.sb("p3a_st2", [128, 8], F32)]
            junks = [junk, junk]
            r_zs = [r_z, b.res()]
            r_x1s = [r_x1, b.res()]
            r_h2fs = [r_h2f, b.res()]
            r_rts = [r_rt, b.res()]
            r_sts = [r_st, b.res()]
            r_junks = [r_junk, r_junk]

            def stage_A(t):
                s = t % 2
                z_, r_z_ = zs[s], r_zs[s]
                b.dma("sp", ot[s][:], o_d[t * 128:(t + 1) * 128, :], reads=[r_in], writes=[r_ot[s]])
                b.dma("sp", xt[s][:], x_d[t * 128:(t + 1) * 128, :], reads=[r_in], writes=[r_xt[s]])
                for c in range(8):
                    b.op("pe", lambda e, s=s, c=c: e.transpose(
                        out=pOT[:, c * 128:(c + 1) * 128], in_=ot[s][:, c * 128:(c + 1) * 128], identity=identb[:]),
                        reads=[r_ot[s], r_id], writes=[r_pOT], inc=(c == 7))
                b.op("act", lambda e, s=s: e.copy(out=oT[s][:].rearrange("p k n -> p (k n)"), in_=pOT[:]),
                     reads=[r_pOT], writes=[r_oT[s]])
                for nb in range(2):
                    for k in range(8):
                        b.op("pe", lambda e, s=s, k=k, nb=nb: e.matmul(
                            pM[nb][:, :], lhsT=oT[s][:, k, :], rhs=wob[:, k, nb * 512:(nb + 1) * 512],
                            start=(k == 0), stop=(k == 7)),
                            reads=[r_oT[s], r_wob], writes=[r_pM[nb]], inc=(k == 7))
                    b.op("dve", lambda e, s=s, nb=nb: e.scalar_tensor_tensor(
                        out=z_[:, nb * 512:(nb + 1) * 512], in0=xt[s][:, nb * 512:(nb + 1) * 512], scalar=ALPHA,
                        in1=pM[nb][:, :], op0=ALU.mult, op1=ALU.add),
                        reads=[r_xt[s], r_pM[nb]], writes=[r_z_])
                yield

            def stage_B(t):
                s = t % 2
                z_, r_z_ = zs[s], r_zs[s]
                x1_, r_x1_ = x1s[s], r_x1s[s]
                h2f_, r_h2f_ = h2fs[s], r_h2fs[s]
                rt_, r_rt_ = rts[s], r_rts[s]
                st_, r_st_ = sts[s], r_sts[s]
                junk_, r_junk_ = junks[s], r_junks[s]
                emit_ln_stats(b, z_[:], r_z_, junk_, r_junk_, st_, r_st_)
                b.op("dve", lambda e: e.tensor_scalar(out=x1_[:], in0=z_[:], scalar1=st_[:, 0:1], scalar2=st_[:, 1:2],
                                                      op0=ALU.subtract, op1=ALU.mult),
                     reads=[r_z_, r_st_], writes=[r_x1_])
                b.op("pool", lambda e: e.tensor_tensor(out=x1_[:], in0=x1_[:], in1=lng[:], op=ALU.mult),
                     reads=[r_x1_, r_ln], writes=[r_x1_])
                b.op("pool", lambda e: e.tensor_tensor(out=x1_[:], in0=x1_[:], in1=lnb[:], op=ALU.add),
                     reads=[r_x1_, r_ln], writes=[r_x1_])
                for c in range(8):
                    b.op("pe", lambda e, c=c: e.transpose(
                        out=pX[c // 4][:, (c % 4) * 128:(c % 4 + 1) * 128], in_=x1_[:, c * 128:(c + 1) * 128],
                        identity=identf[:]), reads=[r_x1_, r_id], writes=[r_pX[c // 4]], inc=(c % 4 == 3))
                for c in range(8):
                    b.op("act", lambda e, c=c: e.activation(
                        out=h2f_[:, c, :], in_=pX[c // 4][:, (c % 4) * 128:(c % 4 + 1) * 128], func=AF.Identity,
                        scale=sc1[:, c:c + 1], bias=mcol[:, 24 + c:25 + c]),
                        reads=[r_pX[c // 4], r_mc], writes=[r_h2f_])
                b.op("pool", lambda e, t=t: e.tensor_copy(out=h2T[:, :, t * 128:(t + 1) * 128], in_=h2f_[:]),
                     reads=[r_h2f_], writes=[r_h2T])
                b.op("act", lambda e, t=t: e.mul(out=xacc[:, t, :], in_=x1_[:], mul=ALPHA),
                     reads=[r_x1_], writes=[r_xacc])
                yield

            def stage_B2(t):
                s = t % 2
                h2f_, r_h2f_ = h2fs[s], r_h2fs[s]
                rt_, r_rt_ = rts[s], r_rts[s]
                for k in range(8):
                    b.op("pe", lambda e, k=k: e.matmul(pR[:, :], lhsT=h2f_[:, k, :], rhs=wr[:, k, :],
                                                       start=(k == 0), stop=(k == 7)),
                         reads=[r_h2f_, r_wr], writes=[r_pR], inc=(k == 7))
                S_, BS, EQ1, MSK, EQ2, WT_ = 0, 16, 32, 48, 64, 80
                M1, M2, GS, GM, GSEL, TOT, RT = 96, 100, 104, 108, 112, 116, 117

                def v3(o):
                    return rt_[:, o:o + 16].rearrange("p (g e) -> p g e", e=4)

                def bc(o):
                    return rt_[:, o:o + 4].unsqueeze(2).to_broadcast([128, 4, 4])

                def R(fn):
                    b.op("dve", fn, reads=[r_rt_, r_wr], writes=[r_rt_])

                b.op("act", lambda e: e.activation(out=rt_[:, S_:S_ + 16], in_=pR[:, :], func=AF.Sigmoid),
                     reads=[r_pR], writes=[r_rt_])
                R(lambda e: e.tensor_tensor(out=rt_[:, BS:BS + 16], in0=rt_[:, S_:S_ + 16], in1=brow[:], op=ALU.add))
                R(lambda e: e.tensor_reduce(out=rt_[:, M1:M1 + 4], in_=v3(BS), axis=AX.X, op=ALU.max))
                R(lambda e: e.tensor_tensor(out=v3(EQ1), in0=v3(BS), in1=bc(M1), op=ALU.is_equal))
                R(lambda e: e.scalar_tensor_tensor(out=rt_[:, MSK:MSK + 16], in0=rt_[:, EQ1:EQ1 + 16], scalar=-1.0e9,
                                                   in1=rt_[:, BS:BS + 16], op0=ALU.mult, op1=ALU.add))
                R(lambda e: e.tensor_reduce(out=rt_[:, M2:M2 + 4], in_=v3(MSK), axis=AX.X, op=ALU.max))
                R(lambda e: e.tensor_tensor(out=rt_[:, GS:GS + 4], in0=rt_[:, M1:M1 + 4], in1=rt_[:, M2:M2 + 4],
                                            op=ALU.add))
                R(lambda e: e.tensor_reduce(out=rt_[:, GM:GM + 1], in_=rt_[:, GS:GS + 4], axis=AX.X, op=ALU.max))
                R(lambda e: e.tensor_scalar(out=rt_[:, GSEL:GSEL + 4], in0=rt_[:, GS:GS + 4], scalar1=rt_[:, GM:GM + 1],
                                            scalar2=None, op0=ALU.is_equal))
                R(lambda e: e.tensor_tensor(out=v3(EQ2), in0=v3(MSK), in1=bc(M2), op=ALU.is_equal))
                R(lambda e: e.tensor_tensor(out=rt_[:, EQ2:EQ2 + 16], in0=rt_[:, EQ2:EQ2 + 16], in1=rt_[:, EQ1:EQ1 + 16],
                                            op=ALU.add))
                R(lambda e: e.tensor_tensor(out=v3(EQ2), in0=v3(EQ2), in1=bc(GSEL), op=ALU.mult))
                R(lambda e: e.tensor_tensor(out=rt_[:, WT_:WT_ + 16], in0=rt_[:, EQ2:EQ2 + 16], in1=rt_[:, S_:S_ + 16],
                                            op=ALU.mult))
                R(lambda e: e.tensor_reduce(out=rt_[:, TOT:TOT + 1], in_=rt_[:, WT_:WT_ + 16], axis=AX.X, op=ALU.add))
                R(lambda e: e.reciprocal(out=rt_[:, RT:RT + 1], in_=rt_[:, TOT:TOT + 1]))
                b.op("dve", lambda e, t=t: e.tensor_scalar(out=gate[:, t, :], in0=rt_[:, WT_:WT_ + 16],
                                                           scalar1=rt_[:, RT:RT + 1], scalar2=None, op0=ALU.mult),
                     reads=[r_rt_], writes=[r_gate])
                yield

            for _ in stage_A(0):
                pass
            for t in range(nt + 1):
                if t + 1 < nt:
                    for _ in stage_A(t + 1):
                        pass
                if t < nt:
                    for _ in stage_B(t):
                        pass
                if t >= 1:
                    for _ in stage_B2(t - 1):
                        pass
            b.barrier()
            b.es = es

        with ExitStack() as esb:
            b.es = esb
            b.dma("sp", gbc[:], modrow_d[l:l + 1, 5120:6144].to_broadcast([128, D]), writes=[r_gbc])
            b.op("dve", lambda e: e.tensor_scalar(out=gbc[:], in0=gbc[:], scalar1=1.0, scalar2=None, op0=ALU.add),
                 reads=[r_gbc], writes=[r_gbc])
            b.dma("sp", lng[:], lnfg_d[l:l + 1, :].to_broadcast([128, D]), writes=[r_ln])
            b.dma("sp", lnb[:], lnfb_d[l:l + 1, :].to_broadcast([128, D]), writes=[r_ln])
            sgu = [b.sb("p3b_sgu%d" % i, [128, 8, 256], F32) for i in range(2)]
            sd = [b.sb("p3b_sd%d" % i, [128, 2, D], F32) for i in range(2)]
            wg = [b.sb("p3b_wg%d" % i, [128, 8, 256], BF16) for i in range(2)]
            wu = [b.sb("p3b_wu%d" % i, [128, 8, 256], BF16) for i in range(2)]
            wd = [b.sb("p3b_wd%d" % i, [128, 2, D], BF16) for i in range(2)]
            sg = [b.sb("p3b_sg%d" % i, [128, 512], BF16) for i in range(2)]
            hid = [b.sb("p3b_hid%d" % i, [128, 512], BF16) for i in range(4)]
            pg = [b.ps("p3b_pg%d" % i, [128, 512]) for i in range(2)]
            pu = [b.ps("p3b_pu%d" % i, [128, 512]) for i in range(2)]
            py = [b.ps("p3b_py%d" % i, [128, 512]) for i in range(4)]
            r_sgu = [b.res(), b.res()]
            r_sd = [b.res(), b.res()]
            r_wg = [b.res(), b.res()]
            r_wu = [b.res(), b.res()]
            r_wd = [b.res(), b.res()]
            r_sg = [b.res(), b.res()]
            r_hid = [b.res() for _ in range(4)]
            r_pg = [b.res(), b.res()]
            r_pu = [b.res(), b.res()]
            r_py = [b.res() for _ in range(4)]
            sti = 0
            hc = 0
            yc = 0
            pending_down = None
            for ex in range(n_exp):
                w = ex % 2
                si = sti % 2
                sti += 1
                b.dma("sp", sgu[si][:], wg_d[l, ex].rearrange("(k p) f -> p k f", p=128), writes=[r_sgu[si]])
                b.op("pool", lambda e, si=si, w=w: e.tensor_copy(out=wg[w][:], in_=sgu[si][:]),
                     reads=[r_sgu[si]], writes=[r_wg[w]])
                si = sti % 2
                sti += 1
                b.dma("sp", sgu[si][:], wu_d[l, ex].rearrange("(k p) f -> p k f", p=128), writes=[r_sgu[si]])
                b.op("pool", lambda e, si=si, w=w: e.tensor_copy(out=wu[w][:], in_=sgu[si][:]),
                     reads=[r_sgu[si]], writes=[r_wu[w]])
                b.dma("sp", sd[w][:], wd_d[l, ex].rearrange("(k p) n -> p k n", p=128), writes=[r_sd[w]])
                b.op("pool", lambda e, w=w: e.tensor_tensor(
                    out=wd[w][:], in0=sd[w][:], in1=gbc[:].unsqueeze(1).to_broadcast([128, 2, D]), op=ALU.mult),
                    reads=[r_sd[w], r_gbc], writes=[r_wd[w]])
                for tg in range(ngrp):
                    hids = []
                    for fc in range(2):
                        x = fc
                        for k in range(8):
                            b.op("pe", lambda e, w=w, k=k, fc=fc, tg=tg, x=x: e.matmul(
                                pg[x][:, :], lhsT=wg[w][:, k, fc * 128:(fc + 1) * 128],
                                rhs=h2T[:, k, tg * 512:(tg + 1) * 512], start=(k == 0), stop=(k == 7)),
                                reads=[r_wg[w], r_h2T], writes=[r_pg[x]], inc=(k == 7))
                        for k in range(8):
                            b.op("pe", lambda e, w=w, k=k, fc=fc, tg=tg, x=x: e.matmul(
                                pu[x][:, :], lhsT=wu[w][:, k, fc * 128:(fc + 1) * 128],
                                rhs=h2T[:, k, tg * 512:(tg + 1) * 512], start=(k == 0), stop=(k == 7)),
                                reads=[r_wu[w], r_h2T], writes=[r_pu[x]], inc=(k == 7))
                        b.op("act", lambda e, x=x: e.activation(out=sg[x][:], in_=pg[x][:, :], func=AF.Silu),
                             reads=[r_pg[x]], writes=[r_sg[x]])
                        hx = hc % 4
                        hc += 1
                        b.op("dve", lambda e, x=x, hx=hx: e.tensor_tensor(out=hid[hx][:], in0=pu[x][:, :],
                                                                          in1=sg[x][:], op=ALU.mult),
                             reads=[r_pu[x], r_sg[x]], writes=[r_hid[hx]])
                        hids.append(hx)
                    def down_job(tg=tg, w=w, ex=ex, hids=tuple(hids)):
                        nonlocal yc
                        for tt in range(4):
                            t = tg * 4 + tt
                            for nb in range(2):
                                yx = yc % 4
                                yc += 1
                                for fc in range(2):
                                    hx = hids[fc]
                                    b.op("pe", lambda e, hx=hx, tt=tt, w=w, fc=fc, nb=nb, yx=yx: e.matmul(
                                        py[yx][:, :], lhsT=hid[hx][:, tt * 128:(tt + 1) * 128],
                                        rhs=wd[w][:, fc, nb * 512:(nb + 1) * 512], start=(fc == 0), stop=(fc == 1)),
                                        reads=[r_hid[hx], r_wd[w]], writes=[r_py[yx]], inc=(fc == 1))
                                b.op("dve", lambda e, yx=yx, t=t, nb=nb, ex=ex: e.scalar_tensor_tensor(
                                    out=xacc[:, t, nb * 512:(nb + 1) * 512], in0=py[yx][:, :],
                                    scalar=gate[:, t, ex:ex + 1], in1=xacc[:, t, nb * 512:(nb + 1) * 512],
                                    op0=ALU.mult, op1=ALU.add),
                                    reads=[r_py[yx], r_gate, r_xacc], writes=[r_xacc])
                    if pending_down is not None:
                        pending_down()
                    pending_down = down_job
            if pending_down is not None:
                pending_down()
            b.barrier()
            b.es = es

        with ExitStack() as esc:
            b.es = esc
            xo = [b.sb("p3c_xo%d" % i, [128, D], F32) for i in range(2)]
            r_xo = [b.res(), b.res()]
            for t in range(nt):
                s = t % 2
                emit_ln_stats(b, xacc[:, t, :], r_xacc, junk, r_junk, st, r_st)
                b.op("dve", lambda e, t=t, s=s: e.tensor_scalar(
                    out=xo[s][:], in0=xacc[:, t, :], scalar1=st[:, 0:1], scalar2=st[:, 1:2],
                    op0=ALU.subtract, op1=ALU.mult), reads=[r_xacc, r_st], writes=[r_xo[s]])
                b.op("pool", lambda e, s=s: e.tensor_tensor(out=xo[s][:], in0=xo[s][:], in1=lng[:], op=ALU.mult),
                     reads=[r_xo[s], r_ln], writes=[r_xo[s]])
                b.op("pool", lambda e, s=s: e.tensor_tensor(out=xo[s][:], in0=xo[s][:], in1=lnb[:], op=ALU.add),
                     reads=[r_xo[s], r_ln], writes=[r_xo[s]])
                b.dma("pool", xout_d[t * 128:(t + 1) * 128, :], xo[s][:], reads=[r_xo[s]], writes=[r_out])
            b.barrier()
            b.es = es
        b.es = old


def build_p3(l, nt, n_exp=16):
    nc = bass.Bass("TRN2", target_bir_lowering=False)
    x_d = nc.dram_tensor("x", [nt * 128, D], F32, kind="ExternalInput").ap()
    o_d = nc.dram_tensor("o", [nt * 128, D], BF16, kind="ExternalInput").ap()
    wout_d = nc.dram_tensor("w_out", [DEPTH, D, D], F32, kind="ExternalInput").ap()
    modrow_d = nc.dram_tensor("modrow", [DEPTH, 6 * D], F32, kind="ExternalInput").ap()
    modcol_d = nc.dram_tensor("modcol", [128, DEPTH * 48], F32, kind="ExternalInput").ap()
    lnmg_d = nc.dram_tensor("ln_mix_g", [DEPTH, D], F32, kind="ExternalInput").ap()
    lnmb_d = nc.dram_tensor("ln_mix_b", [DEPTH, D], F32, kind="ExternalInput").ap()
    wr_d = nc.dram_tensor("w_router", [D, 16], F32, kind="ExternalInput").ap()
    br_d = nc.dram_tensor("b_router", [1, 16], F32, kind="ExternalInput").ap()
    wg_d = nc.dram_tensor("w_gate", [DEPTH, 16, D, 256], F32, kind="ExternalInput").ap()
    wu_d = nc.dram_tensor("w_up", [DEPTH, 16, D, 256], F32, kind="ExternalInput").ap()
    wd_d = nc.dram_tensor("w_down", [DEPTH, 16, 256, D], F32, kind="ExternalInput").ap()
    lnfg_d = nc.dram_tensor("ln_ffn_g", [DEPTH, D], F32, kind="ExternalInput").ap()
    lnfb_d = nc.dram_tensor("ln_ffn_b", [DEPTH, D], F32, kind="ExternalInput").ap()
    identf_d = nc.dram_tensor("identf", [128, 128], F32, kind="ExternalInput").ap()
    xout_d = nc.dram_tensor("xout", [nt * 128, D], F32, kind="ExternalOutput").ap()
    with ExitStack() as es, nc.allow_low_precision("bf16 matmul operands, fp32 accumulation"):
        b = B(nc, es)
        emit_p3(b, l, nt, x_d, o_d, wout_d, modrow_d, modcol_d, lnmg_d, lnmb_d, wr_d, br_d, wg_d, wu_d, wd_d,
                lnfg_d, lnfb_d, identf_d, xout_d, b.res(), b.res(), n_exp=n_exp)
        b.finish()
    return nc


I32 = mybir.dt.int32


def _decl_p1_inputs(nc):
    d = {}
    d["pos"] = nc.dram_tensor("pos", [128, NT], I32, kind="ExternalInput").ap()
    d["invf"] = nc.dram_tensor("invf", [128, 32], F32, kind="ExternalInput").ap()
    d["w_in"] = nc.dram_tensor("w_in", [1, D, INW], F32, kind="ExternalInput").ap()
    d["i_bias"] = nc.dram_tensor("i_bias", [1, 4], F32, kind="ExternalInput").ap()
    d["f_bias"] = nc.dram_tensor("f_bias", [1, 4], F32, kind="ExternalInput").ap()
    return d


def _decl_p1_outputs(nc):
    d = {}
    d["featT"] = nc.dram_tensor("featT", [NT, 128, NFT, 128], BF16, kind="ExternalOutput").ap()
    d["tokA"] = nc.dram_tensor("tokA", [TOK, 1792], BF16, kind="ExternalOutput").ap()
    d["tokF"] = nc.dram_tensor("tokF", [TOK, 12], F32, kind="ExternalOutput").ap()
    d["gT"] = nc.dram_tensor("gT", [NT, 8, 128], F32, kind="ExternalOutput").ap()
    return d


def _decl_p3_inputs(nc):
    d = {}
    d["o"] = nc.dram_tensor("o", [TOK, D], BF16, kind="ExternalInput").ap()
    d["w_out"] = nc.dram_tensor("w_out", [1, D, D], F32, kind="ExternalInput").ap()
    d["modrow3"] = nc.dram_tensor("modrow3", [1, 6 * D], F32, kind="ExternalInput").ap()
    d["modcol3"] = nc.dram_tensor("modcol3", [128, 48], F32, kind="ExternalInput").ap()
    d["ln_mix_g"] = nc.dram_tensor("ln_mix_g", [1, D], F32, kind="ExternalInput").ap()
    d["ln_mix_b"] = nc.dram_tensor("ln_mix_b", [1, D], F32, kind="ExternalInput").ap()
    d["w_router"] = nc.dram_tensor("w_router", [D, 16], F32, kind="ExternalInput").ap()
    d["b_router"] = nc.dram_tensor("b_router", [1, 16], F32, kind="ExternalInput").ap()
    d["w_gate"] = nc.dram_tensor("w_gate", [1, 16, D, 256], F32, kind="ExternalInput").ap()
    d["w_up"] = nc.dram_tensor("w_up", [1, 16, D, 256], F32, kind="ExternalInput").ap()
    d["w_down"] = nc.dram_tensor("w_down", [1, 16, 256, D], F32, kind="ExternalInput").ap()
    d["ln_ffn_g"] = nc.dram_tensor("ln_ffn_g", [1, D], F32, kind="ExternalInput").ap()
    d["ln_ffn_b"] = nc.dram_tensor("ln_ffn_b", [1, D], F32, kind="ExternalInput").ap()
    return d


def _emit_p3_from(b, i3, x_d, identf_d, xout_d, r_in, r_out):
    emit_p3(b, 0, NT, x_d, i3["o"], i3["w_out"], i3["modrow3"], i3["modcol3"], i3["ln_mix_g"], i3["ln_mix_b"],
            i3["w_router"], i3["b_router"], i3["w_gate"], i3["w_up"], i3["w_down"], i3["ln_ffn_g"],
            i3["ln_ffn_b"], identf_d, xout_d, r_in, r_out)


def build_A():
    nc = bass.Bass("TRN2", target_bir_lowering=False)
    x_d = nc.dram_tensor("x", [TOK, D], F32, kind="ExternalInput").ap()
    identf_d = nc.dram_tensor("identf", [128, 128], F32, kind="ExternalInput").ap()
    ccol_d = nc.dram_tensor("ccol", [128, 8], F32, kind="ExternalInput").ap()
    wada_d = nc.dram_tensor("w_ada", [DEPTH, D, 6 * D], F32, kind="ExternalInput").ap()
    bada_d = nc.dram_tensor("b_ada", [DEPTH, 6 * D], F32, kind="ExternalInput").ap()
    modrow_d = nc.dram_tensor("modrow", [DEPTH, 6 * D], F32, kind="ExternalOutput").ap()
    modcol_d = nc.dram_tensor("modcol", [128, DEPTH * 48], F32, kind="ExternalOutput").ap()
    i1 = _decl_p1_inputs(nc)
    o1 = _decl_p1_outputs(nc)
    with ExitStack() as es, nc.allow_low_precision("bf16 matmul operands, fp32 accumulation"):
        b = B(nc, es)
        cos = b.sb("cos", [128, NT, 32], F32)
        sin = b.sb("sin", [128, NT, 32], F32)
        r_tab, r_mod, r_out = b.res(), b.res(), b.res()
        emit_mod(b, ccol_d, wada_d, bada_d, modrow_d, modcol_d)
        emit_rope_tables(b, i1["pos"], i1["invf"], cos, sin, r_tab, NT)
        emit_p1(b, 0, NT, x_d, i1["w_in"], modcol_d, i1["i_bias"], i1["f_bias"], identf_d, cos, sin, r_tab,
                o1["featT"], o1["tokA"], o1["tokF"], o1["gT"], b.res(), r_out)
        b.finish()
    return nc


def build_B():
    nc = bass.Bass("TRN2", target_bir_lowering=False)
    featT_all = nc.dram_tensor("featT_all", [2 * NT, 128, NFT, 128], BF16, kind="ExternalInput").ap()
    tokA_all = nc.dram_tensor("tokA_all", [2 * TOK, 1792], BF16, kind="ExternalInput").ap()
    gT_all = nc.dram_tensor("gT_all", [2 * NT, 8, 128], F32, kind="ExternalInput").ap()
    featT_own = nc.dram_tensor("featT_own", [NT, 128, NFT, 128], BF16, kind="ExternalInput").ap()
    tokA_own = nc.dram_tensor("tokA_own", [TOK, 1792], BF16, kind="ExternalInput").ap()
    tokF_own = nc.dram_tensor("tokF_own", [TOK, 12], F32, kind="ExternalInput").ap()
    vis_d = nc.dram_tensor("vis", [128, 256], F32, kind="ExternalInput").ap()
    pw_d = nc.dram_tensor("pw", [128, NIT + 1], F32, kind="ExternalInput").ap()
    decT_d = nc.dram_tensor("decT", [128, 4, 128], F32, kind="ExternalInput").ap()
    qdecT_d = nc.dram_tensor("qdecT", [128, 2, 128], F32, kind="ExternalInput").ap()
    kdec_d = nc.dram_tensor("kdec", [128, 4], F32, kind="ExternalInput").ap()
    jf_d = nc.dram_tensor("jf", [128, 1], F32, kind="ExternalInput").ap()
    convw_d = nc.dram_tensor("convw", [1, 128, 4, 4], F32, kind="ExternalInput").ap()
    convb_d = nc.dram_tensor("convb", [1, 128, 4], F32, kind="ExternalInput").ap()
    caus_d = nc.dram_tensor("caus", [128, 128], F32, kind="ExternalInput").ap()
    sel4_d = nc.dram_tensor("sel4", [4, 512], F32, kind="ExternalInput").ap()
    eye4_d = nc.dram_tensor("eye4", [4, 4], F32, kind="ExternalInput").ap()
    identf_d = nc.dram_tensor("identf", [128, 128], F32, kind="ExternalInput").ap()
    o_d = nc.dram_tensor("o", [TOK, D], BF16, kind="ExternalOutput").ap()
    with ExitStack() as es, nc.allow_low_precision("bf16 matmul operands, fp32 accumulation"):
        b = B(nc, es)
        r_in, r_out = b.res(), b.res()
        emit_p2a(b, NT, featT_all, tokA_all, featT_own, tokF_own, vis_d, pw_d, identf_d, o_d, r_in, r_out)
        emit_p2b(b, NT, tokA_all, featT_own, tokA_own, decT_d, qdecT_d, kdec_d, jf_d, o_d, r_in, r_out)
        emit_p2c(b, 0, NT, featT_all, tokA_all, gT_all, tokA_own, convw_d, convb_d, caus_d, sel4_d, eye4_d,
                 jf_d, identf_d, o_d, r_in, r_out)
        b.finish()
    return nc


def build_C(with_p1):
    nc = bass.Bass("TRN2", target_bir_lowering=False)
    x_d = nc.dram_tensor("x", [TOK, D], F32, kind="ExternalInput").ap()
    identf_d = nc.dram_tensor("identf", [128, 128], F32, kind="ExternalInput").ap()
    i3 = _decl_p3_inputs(nc)
    xout_d = nc.dram_tensor("xout", [TOK, D], F32, kind="ExternalOutput").ap()
    if with_p1:
        i1 = _decl_p1_inputs(nc)
        modcol1_d = nc.dram_tensor("modcol1", [128, 48], F32, kind="ExternalInput").ap()
        o1 = _decl_p1_outputs(nc)
    with ExitStack() as es, nc.allow_low_precision("bf16 matmul operands, fp32 accumulation"):
        b = B(nc, es)
        r_in, r_x = b.res(), b.res()
        if with_p1:
            cos = b.sb("cos", [128, NT, 32], F32)
            sin = b.sb("sin", [128, NT, 32], F32)
            r_tab = b.res()
            emit_rope_tables(b, i1["pos"], i1["invf"], cos, sin, r_tab, NT)
        _emit_p3_from(b, i3, x_d, identf_d, xout_d, r_in, r_x)
        if with_p1:
            emit_p1(b, 0, NT, xout_d, i1["w_in"], modcol1_d, i1["i_bias"], i1["f_bias"], identf_d, cos, sin,
                    r_tab, o1["featT"], o1["tokA"], o1["tokF"], o1["gT"], r_x, b.res())
        b.finish()
    return nc


_PROGS = {}


def _prog(name, fn):
    if name not in _PROGS:
        _PROGS[name] = fn()
    return _PROGS[name]


def _own_tiles(a, j):
    sh = a.shape
    return np.ascontiguousarray(a.reshape((32, 128) + sh[1:])[j::2]).reshape((TOK,) + sh[1:])


def kernel(x, c, positions, w_ada, b_ada, w_in, i_bias, f_bias, conv_w, conv_b, w_out, ln_mix_g, ln_mix_b,
           w_router, b_router, w_gate, w_up, w_down, ln_ffn_g, ln_ffn_b):
    f32 = np.float32
    x = np.asarray(x, f32)
    c = np.asarray(c, f32)
    positions = np.asarray(positions, np.int32)
    w_ada, b_ada, w_in = np.asarray(w_ada, f32), np.asarray(b_ada, f32), np.asarray(w_in, f32)
    i_bias, f_bias = np.asarray(i_bias, f32), np.asarray(f_bias, f32)
    w_out = np.asarray(w_out, f32)
    ln_mix_g, ln_mix_b = np.asarray(ln_mix_g, f32), np.asarray(ln_mix_b, f32)
    w_router, b_router = np.asarray(w_router, f32), np.asarray(b_router, f32).reshape(1, 16)
    w_gate, w_up, w_down = np.asarray(w_gate, f32), np.asarray(w_up, f32), np.asarray(w_down, f32)
    ln_ffn_g, ln_ffn_b = np.asarray(ln_ffn_g, f32), np.asarray(ln_ffn_b, f32)
    cw, cb = conv_layouts(np.asarray(conv_w, f32), np.asarray(conv_b, f32))
    ident = np.eye(128, dtype=f32)
    invf = inv_freq_table()
    decT, qdecT, kdec = ret_tables()
    caus, sel4, eye4 = mlstm_consts()
    pw = pw_table()
    cores = list(range(8))

    def p1_in(core, l):
        bi, j = core // 2, core % 2
        pos = np.ascontiguousarray(positions[bi].reshape(32, 128)[j::2].T)
        return {"pos": pos, "invf": invf, "w_in": w_in[l:l + 1], "i_bias": i_bias[l:l + 1],
                "f_bias": f_bias[l:l + 1]}

    def p3_in(core, l, o, modrow, modcol):
        return {"o": o, "w_out": w_out[l:l + 1], "modrow3": np.ascontiguousarray(modrow[l:l + 1]),
                "modcol3": np.ascontiguousarray(modcol[:, l * 48:(l + 1) * 48]),
                "ln_mix_g": ln_mix_g[l:l + 1], "ln_mix_b": ln_mix_b[l:l + 1], "w_router": w_router,
                "b_router": b_router, "w_gate": w_gate[l:l + 1], "w_up": w_up[l:l + 1], "w_down": w_down[l:l + 1],
                "ln_ffn_g": ln_ffn_g[l:l + 1], "ln_ffn_b": ln_ffn_b[l:l + 1]}

    ims = []
    xs = []
    for core in cores:
        bi, j = core // 2, core % 2
        xo = _own_tiles(x[bi], j)
        xs.append(xo)
        im = {"x": xo, "identf": ident, "ccol": np.ascontiguousarray(c[bi].reshape(8, 128).T),
              "w_ada": w_ada, "b_ada": b_ada}
        im.update(p1_in(core, 0))
        ims.append(im)
    res = run_bass_kernel_spmd(_prog("A", build_A), ims, core_ids=cores).results
    modrow = [np.asarray(r["modrow"]) for r in res]
    modcol = [np.asarray(r["modcol"]) for r in res]
    p1o = res
    for l in range(DEPTH):
        ims = []
        for core in cores:
            bi, j = core // 2, core % 2
            a, bb = p1o[2 * bi], p1o[2 * bi + 1]
            ims.append({
                "featT_all": np.concatenate([np.asarray(a["featT"]), np.asarray(bb["featT"])], axis=0),
                "tokA_all": np.concatenate([np.asarray(a["tokA"]), np.asarray(bb["tokA"])], axis=0),
                "gT_all": np.concatenate([np.asarray(a["gT"]), np.asarray(bb["gT"])], axis=0),
                "featT_own": np.asarray(p1o[core]["featT"]), "tokA_own": np.asarray(p1o[core]["tokA"]),
                "tokF_own": np.asarray(p1o[core]["tokF"]),
                "vis": vis_table(j), "pw": pw, "decT": decT, "qdecT": qdecT, "kdec": kdec,
                "jf": np.full((128, 1), float(j), f32), "convw": cw[l:l + 1], "convb": cb[l:l + 1],
                "caus": caus, "sel4": sel4, "eye4": eye4, "identf": ident})
        ob = run_bass_kernel_spmd(_prog("B", build_B), ims, core_ids=cores).results
        last = (l == DEPTH - 1)
        ims = []
        for core in cores:
            im = {"x": xs[core], "identf": ident}
            im.update(p3_in(core, l, np.asarray(ob[core]["o"]), modrow[core], modcol[core]))
            if not last:
                im.update(p1_in(core, l + 1))
                im["modcol1"] = np.ascontiguousarray(modcol[core][:, (l + 1) * 48:(l + 2) * 48])
            ims.append(im)
        if last:
            res = run_bass_kernel_spmd(_prog("E", lambda: build_C(False)), ims, core_ids=cores).results
        else:
            res = run_bass_kernel_spmd(_prog("C", lambda: build_C(True)), ims, core_ids=cores).results
            p1o = res
        xs = [np.asarray(r["xout"]) for r in res]
    out = np.zeros((BATCH, SEQ, D), f32)
    for core in cores:
        bi, j = core // 2, core % 2
        out[bi].reshape(32, 128, D)[j::2] = xs[core].reshape(NT, 128, D)
    return out


def build_fused(depth=DEPTH):
    nc = bass.Bass("TRN2", target_bir_lowering=False)

    def inp(name, shape, dt=F32):
        return nc.dram_tensor(name, list(shape), dt, kind="ExternalInput").ap()

    def scr(name, shape, dt=F32):
        return nc.dram_tensor(name, list(shape), dt).ap()

    x_in = inp("x2", [2 * TOK, D])
    pos_d = inp("pos2", [2, 128, NT], I32)
    invf_d = inp("invf", [128, 32])
    identf_d = inp("identf", [128, 128])
    ccol_d = inp("ccol", [128, 8])
    wada_d = inp("w_ada", [DEPTH, D, 6 * D])
    bada_d = inp("b_ada", [DEPTH, 6 * D])
    win_d = inp("w_in", [DEPTH, D, INW])
    ib_d = inp("i_bias", [DEPTH, 4])
    fb_d = inp("f_bias", [DEPTH, 4])
    convw_d = inp("convw", [DEPTH, 128, 4, 4])
    convb_d = inp("convb", [DEPTH, 128, 4])
    wout_d = inp("w_out", [DEPTH, D, D])
    lnmg_d = inp("ln_mix_g", [DEPTH, D])
    lnmb_d = inp("ln_mix_b", [DEPTH, D])
    wr_d = inp("w_router", [D, 16])
    br_d = inp("b_router", [1, 16])
    wg_d = inp("w_gate", [DEPTH, 16, D, 256])
    wu_d = inp("w_up", [DEPTH, 16, D, 256])
    wd_d = inp("w_down", [DEPTH, 16, 256, D])
    lnfg_d = inp("ln_ffn_g", [DEPTH, D])
    lnfb_d = inp("ln_ffn_b", [DEPTH, D])
    vis_d = inp("vis2", [2, 128, 256])
    jf_d = inp("jf2", [2, 128, 1])
    pw_d = inp("pw", [128, NIT + 1])
    decT_d = inp("decT", [128, 4, 128])
    qdecT_d = inp("qdecT", [128, 2, 128])
    kdec_d = inp("kdec", [128, 4])
    caus_d = inp("caus", [128, 128])
    sel4_d = inp("sel4", [4, 512])
    eye4_d = inp("eye4", [4, 4])
    xfin = nc.dram_tensor("xfin", [2 * TOK, D], F32, kind="ExternalOutput").ap()

    modrow_s = scr("modrow_s", [DEPTH, 6 * D])
    modcol_s = scr("modcol_s", [128, DEPTH * 48])
    featT_s = scr("featT_s", [2 * NT, 128, NFT, 128], BF16)
    tokA_s = scr("tokA_s", [2 * TOK, 1792], BF16)
    tokF_s = scr("tokF_s", [2 * TOK, 12])
    gT_s = scr("gT_s", [2 * NT, 8, 128])
    o_s = scr("o_s", [2 * TOK, D], BF16)
    xs = [scr("xs0", [2 * TOK, D]), scr("xs1", [2 * TOK, D])]

    with ExitStack() as es, nc.allow_low_precision("bf16 matmul operands, fp32 accumulation"):
        b = B(nc, es)
        cos = [b.sb("cos%d" % j, [128, NT, 32], F32) for j in range(2)]
        sin = [b.sb("sin%d" % j, [128, NT, 32], F32) for j in range(2)]
        r_tab = [b.res(), b.res()]
        r_bun, r_o, r_x = b.res(), b.res(), b.res()
        emit_mod(b, ccol_d, wada_d, bada_d, modrow_s, modcol_s)
        for j in range(2):
            emit_rope_tables(b, pos_d[j], invf_d, cos[j], sin[j], r_tab[j], NT)
        for l in range(depth):
            x_l = x_in if l == 0 else xs[l % 2]
            x_n = xfin if l == depth - 1 else xs[(l + 1) % 2]
            emit_p1(b, l, 2 * NT, x_l, win_d, modcol_s, ib_d, fb_d, identf_d, cos, sin, r_tab,
                    featT_s, tokA_s, tokF_s, gT_s, r_x, r_bun)
            emit_p2a(b, NT, featT_s, tokA_s, None, None, None, pw_d, identf_d, None, r_bun, r_o,
                     parities=[(featT_s[j * NT:(j + 1) * NT], tokF_s[j * TOK:(j + 1) * TOK], vis_d[j],
                                o_s[j * TOK:(j + 1) * TOK]) for j in range(2)])
            emit_p2b(b, NT, tokA_s, featT_s, None, decT_d, qdecT_d, kdec_d, jf_d[0], o_s, r_bun, r_o, whole=True)
            emit_p2c(b, l, NT, featT_s, tokA_s, gT_s, None, convw_d, convb_d, caus_d, sel4_d, eye4_d,
                     jf_d[0], identf_d, o_s, r_bun, r_o, whole=True)
            for j in range(2):
                sl = slice(j * TOK, (j + 1) * TOK)
                emit_p3(b, l, NT, x_l[sl], o_s[sl], wout_d, modrow_s, modcol_s, lnmg_d, lnmb_d, wr_d, br_d,
                        wg_d, wu_d, wd_d, lnfg_d, lnfb_d, identf_d, x_n[sl], r_o, r_x)
            if l != depth - 1:
                b.new_epoch()
        b.finish()
    return nc


def kernel_fused(x, c, positions, w_ada, b_ada, w_in, i_bias, f_bias, conv_w, conv_b, w_out, ln_mix_g, ln_mix_b,
                 w_router, b_router, w_gate, w_up, w_down, ln_ffn_g, ln_ffn_b):
    f32 = np.float32
    x = np.asarray(x, f32)
    c = np.asarray(c, f32)
    positions = np.asarray(positions, np.int32)
    cw, cb = conv_layouts(np.asarray(conv_w, f32), np.asarray(conv_b, f32))
    decT, qdecT, kdec = ret_tables()
    caus, sel4, eye4 = mlstm_consts()
    shared = {
        "invf": inv_freq_table(), "identf": np.eye(128, dtype=f32),
        "w_ada": np.asarray(w_ada, f32), "b_ada": np.asarray(b_ada, f32), "w_in": np.asarray(w_in, f32),
        "i_bias": np.asarray(i_bias, f32), "f_bias": np.asarray(f_bias, f32), "convw": cw, "convb": cb,
        "w_out": np.asarray(w_out, f32), "ln_mix_g": np.asarray(ln_mix_g, f32),
        "ln_mix_b": np.asarray(ln_mix_b, f32), "w_router": np.asarray(w_router, f32),
        "b_router": np.asarray(b_router, f32).reshape(1, 16), "w_gate": np.asarray(w_gate, f32),
        "w_up": np.asarray(w_up, f32), "w_down": np.asarray(w_down, f32),
        "ln_ffn_g": np.asarray(ln_ffn_g, f32), "ln_ffn_b": np.asarray(ln_ffn_b, f32),
        "vis2": np.stack([vis_table(0), vis_table(1)]),
        "jf2": np.stack([np.zeros((128, 1), f32), np.ones((128, 1), f32)]),
        "pw": pw_table(), "decT": decT, "qdecT": qdecT, "kdec": kdec, "caus": caus, "sel4": sel4, "eye4": eye4,
    }
    cores = list(range(8))
    work = {0: 0, 1: 1, 4: 2, 5: 3}
    zeros = {k: np.zeros_like(v) for k, v in shared.items()}
    ims = []
    for core in cores:
        if core in work:
            bi = work[core]
            xb = x[bi].reshape(32, 128, D)
            pb = positions[bi].reshape(32, 128)
            im = dict(shared)
            im["x2"] = np.ascontiguousarray(np.concatenate([xb[0::2], xb[1::2]], axis=0)).reshape(2 * TOK, D)
            im["pos2"] = np.ascontiguousarray(np.stack([pb[0::2].T, pb[1::2].T]))
            im["ccol"] = np.ascontiguousarray(c[bi].reshape(8, 128).T)
        else:
            im = dict(zeros)
            im["x2"] = np.zeros((2 * TOK, D), f32)
            im["pos2"] = np.zeros((2, 128, NT), np.int32)
            im["ccol"] = np.zeros((128, 8), f32)
        ims.append(im)
    res = run_bass_kernel_spmd(_prog("F", build_fused), ims, core_ids=cores).results
    out = np.zeros((BATCH, SEQ, D), f32)
    for core, bi in work.items():
        xf = np.asarray(res[core]["xfin"]).reshape(2, NT, 128, D)
        ob = out[bi].reshape(32, 128, D)
        ob[0::2] = xf[0]
        ob[1::2] = xf[1]
    return out


kernel_unfused = kernel
kernel = kernel_fused
```
